# Optimizing a Trainium2 kernel written in Bass

```python
import jax, jax.numpy as jnp
from jax import lax
import numpy as np

D_MODEL = 1024
BATCH = 4
SEQ = 8192
DEPTH = 4

GRID_W = 64
CTX_LEN = 256
N_EVEN = (DEPTH + 1) // 2
N_ODD = DEPTH // 2
RWKV_WIDTH = D_MODEL // 2
HEAD_DIM = 64
RWKV_HEADS = RWKV_WIDTH // HEAD_DIM
R_DECAY = 64
R_LR = 64
R_GATE = 128
GN_EPS = 64e-5
POOL_WIDTH = D_MODEL // 2
POOL_WINDOWS = (2, 4, 8, 16)
POOL_GROUP = POOL_WIDTH // len(POOL_WINDOWS)
IN_WIDTH = 3 * RWKV_WIDTH + POOL_WIDTH
MIX_WIDTH = RWKV_WIDTH + POOL_WIDTH
FOURIER_GROUPS = 4
MOE_GROUPS = 4
EXPERTS_PER_GROUP = 8
N_EXPERTS = MOE_GROUPS * EXPERTS_PER_GROUP
TOP_K = 2
D_EXPERT = 512
MOE_BLOCK = 128
NORM_EPS = 1e-6

kernel_name = "hybrid_rwkv7_pool_fnet_hmoe_dit"


def rms_norm(x, g):
    xf = x.astype(jnp.float32)
    y = xf * lax.rsqrt(jnp.mean(xf * xf, axis=-1, keepdims=True) + NORM_EPS)
    return y.astype(x.dtype) * g


def modulate(x, g, shift, scale):
    return rms_norm(x, g) * (1 + scale) + shift


def grid_shift(x):
    B, T, C = x.shape
    rows = T // GRID_W
    g = x.reshape(B, rows, GRID_W, 4, C // 4)
    zc = jnp.zeros_like(g[:, :, :1, 0])
    zr = jnp.zeros_like(g[:, :1, :, 2])
    left = jnp.concatenate([zc, g[:, :, :-1, 0]], axis=2)
    right = jnp.concatenate([g[:, :, 1:, 1], zc], axis=2)
    up = jnp.concatenate([zr, g[:, :-1, :, 2]], axis=1)
    down = jnp.concatenate([g[:, 1:, :, 3], zr], axis=1)
    return jnp.stack([left, right, up, down], axis=3).reshape(B, T, C)


def seq_shift(x):
    B, T, C = x.shape
    x2 = x.reshape(B, T, 2, C // 2)
    z = jnp.zeros_like(x2[:, :1, 0])
    prev = jnp.concatenate([z, x2[:, :-1, 0]], axis=1)
    nxt = jnp.concatenate([x2[:, 1:, 1], z], axis=1)
    return jnp.stack([prev, nxt], axis=2).reshape(B, T, C)


def to_heads(x):
    return x.reshape(*x.shape[:-1], RWKV_HEADS, HEAD_DIM).astype(jnp.float32)


def wkv_scan(r, w, k, v, a, b, s0, reverse):
    def step(s, inp):
        r_t, w_t, k_t, v_t, a_t, b_t = inp
        sa = jnp.einsum('bhvk,bhk->bhv', s, a_t)
        s = s * w_t[:, :, None, :] + sa[..., None] * b_t[:, :, None, :] + v_t[..., None] * k_t[:, :, None, :]
        return s, jnp.einsum('bhvk,bhk->bhv', s, r_t)
    xs = tuple(jnp.moveaxis(t, 1, 0) for t in (r, w, k, v, a, b))
    s_final, ys = lax.scan(step, s0, xs, reverse=reverse)
    return jnp.moveaxis(ys, 0, 1), s_final


def rwkv_mix(h, p_rkv, shift_fn, init_states, need_out,
             mu_x, mu_p, decay_w0, decay_w1, decay_w2, lr_a0, lr_a1, lr_a2,
             gate_g1, gate_g2, k_k, k_a, r_k, gn_w, gn_b):
    B, T, _ = h.shape
    f32 = jnp.float32
    hx = shift_fn(h) - h
    x_w = h + hx * mu_x[0]
    x_a = h + hx * mu_x[1]
    r, k, v = [p + (shift_fn(p) - p) * mu_p[n] for n, p in enumerate(jnp.split(p_rkv, 3, axis=-1))]
    kk = to_heads(k * k_k)
    kk = kk * lax.rsqrt(jnp.maximum(jnp.sum(kk * kk, axis=-1, keepdims=True), 1e-12))
    r_h, v_h, k_h, ka_h = to_heads(r), to_heads(v), to_heads(k), to_heads(k_a)
    if init_states is None:
        z = jnp.zeros((B, RWKV_HEADS, HEAD_DIM, HEAD_DIM), f32)
        init_states = (z, z)
    ys, k_dirs, states = [], [], []
    for d in range(2):
        z_w = (decay_w0[d] + jnp.tanh(x_w @ decay_w1[d]) @ decay_w2[d]).astype(f32)
        decay = jnp.exp(-jnp.exp(-jax.nn.softplus(-z_w) - 0.5))
        a = to_heads(jax.nn.sigmoid(lr_a0[d] + (x_a @ lr_a1[d]) @ lr_a2[d]))
        k_d = k_h * (1 + (a - 1) * ka_h)
        y_d, s_d = wkv_scan(r_h, to_heads(decay), k_d, v_h, -kk, kk * a, init_states[d], reverse=(d == 1))
        ys.append(y_d)
        k_dirs.append(k_d)
        states.append(s_d)
    if not need_out:
        return None, (states[0], states[1])
    y = ys[0] + ys[1]
    mu = jnp.mean(y, axis=-1, keepdims=True)
    var = jnp.mean(jnp.square(y - mu), axis=-1, keepdims=True)
    yn = ((y - mu) * lax.rsqrt(var + GN_EPS)).reshape(B, T, RWKV_WIDTH) * gn_w + gn_b
    bonus = (jnp.sum(r_h * (k_dirs[0] + k_dirs[1]) * r_k, axis=-1, keepdims=True) * v_h).reshape(B, T, RWKV_WIDTH)
    x_g = h + hx * mu_x[2]
    g = jax.nn.sigmoid(x_g @ gate_g1) @ gate_g2
    return (yn + bonus).astype(h.dtype) * g, (states[0], states[1])


def pool_mix(u, pool_w, pool_scale):
    B, T, _ = u.shape
    ug = u.reshape(B, T, len(POOL_WINDOWS), POOL_GROUP).astype(jnp.float32)
    cs = jnp.concatenate([jnp.zeros_like(ug[:, :1]), jnp.cumsum(ug, axis=1)], axis=1)
    pos = jnp.arange(T)
    pooled = []
    for gi, win in enumerate(POOL_WINDOWS):
        half = win // 2
        hi = jnp.minimum(pos + half, T)
        lo = jnp.maximum(pos - half, 0)
        cg = cs[:, :, gi]
        pooled.append((cg[:, hi] - cg[:, lo]) / (hi - lo).astype(jnp.float32)[None, :, None])
    diff = (jnp.stack(pooled, axis=2) - ug).astype(u.dtype)
    y = jnp.einsum('btgc,gcd->btgd', diff, pool_w).reshape(B, T, POOL_WIDTH)
    return y * pool_scale


def even_mixer(h, shift_fn, init_states, need_out, w_in, w_out, pool_w, pool_scale, rwkv_params):
    proj = h @ w_in if need_out else h @ w_in[:, :3 * RWKV_WIDTH]
    y_a, states = rwkv_mix(h, proj[..., :3 * RWKV_WIDTH], shift_fn, init_states, need_out, *rwkv_params)
    if not need_out:
        return None, states
    y_b = pool_mix(proj[..., 3 * RWKV_WIDTH:], pool_w, pool_scale)
    return jnp.concatenate([y_a, y_b], axis=-1) @ w_out, states


def fourier_mix(h, w_f):
    B, T, D = h.shape
    hg = h.reshape(B, T, FOURIER_GROUPS, D // FOURIER_GROUPS).astype(jnp.float32)
    f = jnp.fft.fftn(hg, axes=(1, 3), norm='ortho').real
    return f.reshape(B, T, D).astype(h.dtype) @ w_f


def hier_moe(rows, router_c, router_c_b, router_f, router_f_b, w1, w3, w2):
    n, d = rows.shape
    f32 = jnp.float32
    p_group = jax.nn.softmax((rows @ router_c).astype(f32) + router_c_b, axis=-1)
    g_val, g_idx = lax.top_k(p_group, 1)
    logits_f = ((rows @ router_f).astype(f32) + router_f_b).reshape(n, MOE_GROUPS, EXPERTS_PER_GROUP)
    sel = jnp.broadcast_to(g_idx[:, :, None], (n, 1, EXPERTS_PER_GROUP))
    p_local = jax.nn.softmax(jnp.take_along_axis(logits_f, sel, axis=1)[:, 0], axis=-1)
    e_val, e_loc = lax.top_k(p_local, TOP_K)
    gate = g_val * e_val / jnp.sum(e_val, axis=-1, keepdims=True)
    expert = g_idx * EXPERTS_PER_GROUP + e_loc
    nk = n * TOP_K
    e_flat = expert.reshape(nk)
    order = jnp.argsort(e_flat)
    e_sorted = e_flat[order]
    tok_sorted = jnp.repeat(jnp.arange(n), TOP_K)[order]
    gate_sorted = gate.reshape(nk)[order]
    counts = jnp.bincount(e_flat, length=N_EXPERTS)
    padded = (counts + MOE_BLOCK - 1) // MOE_BLOCK * MOE_BLOCK
    start = jnp.cumsum(counts) - counts
    pend = jnp.cumsum(padded)
    pstart = pend - padded
    dest = pstart[e_sorted] + (jnp.arange(nk) - start[e_sorted])
    n_blocks = -(-nk // MOE_BLOCK) + N_EXPERTS
    buf = jnp.zeros((n_blocks * MOE_BLOCK, d), rows.dtype).at[dest].set(rows[tok_sorted])
    block_e = jnp.minimum(jnp.searchsorted(pend, jnp.arange(n_blocks) * MOE_BLOCK, side='right'), N_EXPERTS - 1)

    def expert_ffn(args):
        xb, e = args
        return (jax.nn.silu(xb @ w1[e]) * (xb @ w3[e])) @ w2[e]

    y_buf = lax.map(expert_ffn, (buf.reshape(n_blocks, MOE_BLOCK, d), block_e)).reshape(n_blocks * MOE_BLOCK, d)
    y = y_buf[dest] * gate_sorted[:, None].astype(rows.dtype)
    return jnp.zeros((n, d), rows.dtype).at[tok_sorted].add(y)


def setup_inputs(seed: int = 0) -> dict:
    key = jax.random.key(seed)
    ks = iter(jax.random.split(key, 64))
    f32 = jnp.float32
    D, DA = D_MODEL, RWKV_WIDTH

    def nrm(shape, scale):
        return jax.random.normal(next(ks), shape, f32) * scale

    def unif(shape):
        return jax.random.uniform(next(ks), shape, f32)

    decay_base = -6.0 + 5.0 * jnp.arange(DA, dtype=f32) / (DA - 1)
    return {
        "x": nrm((BATCH, SEQ, D), 1.0),
        "c": nrm((BATCH, D), 1.0),
        "ctx": nrm((BATCH, CTX_LEN, D), 1.0),
        "c_ctx": nrm((D,), 1.0),
        "ada_w": nrm((DEPTH, D, 6 * D), 0.5 * D ** -0.5),
        "ada_b": nrm((DEPTH, 6 * D), 0.1),
        "norm_mix": 1.0 + nrm((DEPTH, D), 0.05),
        "norm_ffn": 1.0 + nrm((DEPTH, D), 0.05),
        "w_in": nrm((N_EVEN, D, IN_WIDTH), D ** -0.5),
        "mu_x": unif((N_EVEN, 3, D)),
        "mu_p": unif((N_EVEN, 3, DA)),
        "decay_w0": decay_base + nrm((N_EVEN, 2, DA), 0.1),
        "decay_w1": nrm((N_EVEN, 2, D, R_DECAY), D ** -0.5),
        "decay_w2": nrm((N_EVEN, 2, R_DECAY, DA), 0.5 * R_DECAY ** -0.5),
        "lr_a0": nrm((N_EVEN, 2, DA), 0.1),
        "lr_a1": nrm((N_EVEN, 2, D, R_LR), D ** -0.5),
        "lr_a2": nrm((N_EVEN, 2, R_LR, DA), 0.5 * R_LR ** -0.5),
        "gate_g1": nrm((N_EVEN, D, R_GATE), D ** -0.5),
        "gate_g2": nrm((N_EVEN, R_GATE, DA), R_GATE ** -0.5),
        "k_k": 0.85 + nrm((N_EVEN, DA), 0.05),
        "k_a": 1.0 + nrm((N_EVEN, DA), 0.05),
        "r_k": nrm((N_EVEN, RWKV_HEADS, HEAD_DIM), 0.1),
        "gn_w": 1.0 + nrm((N_EVEN, DA), 0.05),
        "gn_b": nrm((N_EVEN, DA), 0.02),
        "pool_w": nrm((N_EVEN, len(POOL_WINDOWS), POOL_GROUP, POOL_GROUP), POOL_GROUP ** -0.5),
        "pool_scale": 1.0 + nrm((N_EVEN, POOL_WIDTH), 0.1),
        "w_out": nrm((N_EVEN, MIX_WIDTH, D), MIX_WIDTH ** -0.5),
        "w_fourier": nrm((N_ODD, D, D), D ** -0.5),
        "router_c": nrm((DEPTH, D, MOE_GROUPS), D ** -0.5),
        "router_c_b": nrm((DEPTH, MOE_GROUPS), 0.01),
        "router_f": nrm((DEPTH, D, N_EXPERTS), D ** -0.5),
        "router_f_b": nrm((DEPTH, N_EXPERTS), 0.01),
        "moe_w1": nrm((DEPTH, N_EXPERTS, D, D_EXPERT), D ** -0.5),
        "moe_w3": nrm((DEPTH, N_EXPERTS, D, D_EXPERT), D ** -0.5),
        "moe_w2": nrm((DEPTH, N_EXPERTS, D_EXPERT, D), D_EXPERT ** -0.5),
        "final_norm": 1.0 + nrm((D,), 0.05),
    }


def reference(x, c, ctx, c_ctx, ada_w, ada_b, norm_mix, norm_ffn, w_in, mu_x, mu_p,
              decay_w0, decay_w1, decay_w2, lr_a0, lr_a1, lr_a2, gate_g1, gate_g2,
              k_k, k_a, r_k, gn_w, gn_b, pool_w, pool_scale, w_out, w_fourier,
              router_c, router_c_b, router_f, router_f_b, moe_w1, moe_w3, moe_w2, final_norm):
    B = x.shape[0]
    D = D_MODEL
    s_lat = jax.nn.silu(c)
    s_ctx = jax.nn.silu(c_ctx)
    last_read = 2 * ((DEPTH - 1) // 2)
    lat, cx = x, ctx
    for i in range(DEPTH):
        ctx_in = i <= last_read
        ctx_out = i < last_read
        m = (s_lat @ ada_w[i] + ada_b[i]).reshape(B, 6, 1, D)
        if ctx_in:
            mc = (s_ctx @ ada_w[i] + ada_b[i]).reshape(6, 1, 1, D)
        h = modulate(lat, norm_mix[i], m[:, 0], m[:, 1])
        j = i // 2
        if i % 2 == 0:
            rw = (mu_x[j], mu_p[j], decay_w0[j], decay_w1[j], decay_w2[j], lr_a0[j], lr_a1[j], lr_a2[j],
                  gate_g1[j], gate_g2[j], k_k[j], k_a[j], r_k[j], gn_w[j], gn_b[j])
            hc = modulate(cx, norm_mix[i], mc[0], mc[1])
            yc, ctx_states = even_mixer(hc, seq_shift, None, ctx_out, w_in[j], w_out[j], pool_w[j], pool_scale[j], rw)
            yl, _ = even_mixer(h, grid_shift, ctx_states, True, w_in[j], w_out[j], pool_w[j], pool_scale[j], rw)
            lat = lat + m[:, 2] * yl
            if ctx_out:
                cx = cx + mc[2] * yc
        else:
            lat = lat + m[:, 2] * fourier_mix(h, w_fourier[j])
            if ctx_out:
                hc = modulate(cx, norm_mix[i], mc[0], mc[1])
                cx = cx + mc[2] * fourier_mix(hc, w_fourier[j])
        h2 = modulate(lat, norm_ffn[i], m[:, 3], m[:, 4])
        moe_p = (router_c[i], router_c_b[i], router_f[i], router_f_b[i], moe_w1[i], moe_w3[i], moe_w2[i])
        if ctx_out:
            hc2 = modulate(cx, norm_ffn[i], mc[3], mc[4])
            n_lat = h2.shape[0] * h2.shape[1]
            y = hier_moe(jnp.concatenate([h2.reshape(-1, D), hc2.reshape(-1, D)], axis=0), *moe_p)
            lat = lat + m[:, 5] * y[:n_lat].reshape(lat.shape)
            cx = cx + mc[5] * y[n_lat:].reshape(cx.shape)
        else:
            lat = lat + m[:, 5] * hier_moe(h2.reshape(-1, D), *moe_p).reshape(lat.shape)
    return rms_norm(lat, final_norm)
```

```python
import numpy as np
from contextlib import ExitStack
import concourse.bass as bass
import concourse.mybir as mybir
from concourse.bass_utils import run_bass_kernel_spmd

F32 = mybir.dt.float32
BF16 = mybir.dt.bfloat16
I32 = mybir.dt.int32
AF = mybir.ActivationFunctionType
ALU = mybir.AluOpType
AX = mybir.AxisListType

D = 1024
T = 8192
CT = 256
NT = T + CT
DEPTH = 4
NEXP = 32
DE = 512


class Buf:
    def __init__(self, t, name):
        self.t = t
        self.name = name
        self.lw = None
        self.rd = {}

    def __getitem__(self, idx):
        return self.t[idx]


class Parts:
    def __init__(self, t, name):
        self.t = t
        self.name = name
        self.parts = {}

    def p(self, key):
        b = self.parts.get(key)
        if b is None:
            b = Buf(self.t, f"{self.name}.{key}")
            self.parts[key] = b
        return b

    def all(self):
        return list(self.parts.values())

    def __getitem__(self, idx):
        return self.t[idx]


class Ctx:
    KD = 16

    def __init__(self):
        self.nc = bass.Bass("TRN2", target_bir_lowering=False)
        nc = self.nc
        self.es = ExitStack()
        self.eng = {"pe": nc.tensor, "act": nc.scalar, "dve": nc.vector, "pool": nc.gpsimd, "sp": nc.sync}
        self.csem = {e: self.es.enter_context(nc.semaphore("c_" + e)) for e in ("pe", "act", "dve", "pool")}
        self.ccnt = {e: 0 for e in self.csem}
        self.dsem = {q: [self.es.enter_context(nc.semaphore(f"d_{q}{i}")) for i in range(self.KD)]
                     for q in ("sp", "pool", "act")}
        self.dcnt = {q: 0 for q in self.dsem}
        self.known = {e: {} for e in self.eng}
        self.nalloc = 0
        self.psum_banks = []
        self.psum_i = 0

    def sb(self, shape, dtype=F32, name=None):
        self.nalloc += 1
        name = (name or "sb") + f"_{self.nalloc}"
        es = self.scopes[-1] if getattr(self, "scopes", None) else self.es
        t = es.enter_context(self.nc.sbuf_tensor(name, list(shape), dtype))
        return Buf(t, name)

    def push_scope(self):
        if not hasattr(self, "scopes"):
            self.scopes = []
        self.scopes.append(ExitStack())

    def pop_scope(self):
        self.barrier()
        self.scopes.pop().close()

    def barrier(self):
        for e in self.eng:
            for src, sem in self.csem.items():
                if self.ccnt[src] > 0:
                    self._wait(e, (sem, self.ccnt[src], "bar"))
            self._wait_all_dma(e)

    def _wait_all_dma(self, e):
        for q in self.dsem:
            n = self.dcnt[q]
            for r in range(self.KD):
                cnt = (n - r + self.KD - 1) // self.KD if n > r else 0
                if cnt > 0:
                    self._wait(e, (self.dsem[q][r], 16 * cnt, "dma"))

    def dram(self, name, shape, dtype=F32, kind="Internal"):
        return self.nc.dram_tensor(name, list(shape), dtype, kind=kind)

    def init_psum(self, n=8):
        for i in range(n):
            t = self.es.enter_context(self.nc.psum_tensor(f"ps{i}", [128, 512], F32))
            b = Buf(t, f"ps{i}")
            b.excl = True
            self.psum_banks.append(b)

    def ps(self):
        b = self.psum_banks[self.psum_i % len(self.psum_banks)]
        self.psum_i += 1
        return b

    def _wait(self, e, ev):
        if ev is None:
            return
        sem, val, src = ev
        if src == e and e == "pe":
            return
        k = self.known[e]
        key = sem.name
        if k.get(key, 0) >= val:
            return
        self.eng[e].wait_ge(sem, val)
        k[key] = val

    def _deps(self, e, reads, writes):
        for b in reads:
            self._wait(e, b.lw)
            if getattr(b, "excl", False):
                for ke, ev in b.rd.items():
                    if ke != e:
                        self._wait(e, ev)
        for b in writes:
            self._wait(e, b.lw)
            for ev in b.rd.values():
                self._wait(e, ev)

    def _commit(self, ev, key, reads, writes):
        for b in writes:
            b.lw = ev
            b.rd = {}
        for b in reads:
            b.rd[key] = ev

    def op(self, e, fn, reads=(), writes=()):
        self._deps(e, reads, writes)
        ins = fn(self.eng[e])
        self.ccnt[e] += 1
        ins.then_inc(self.csem[e], 1)
        ev = (self.csem[e], self.ccnt[e], e)
        self._commit(ev, e, reads, writes)
        return ins

    def dma(self, q, out, in_, reads=(), writes=(), indirect=None, **kw):
        i = self.dcnt[q]
        self.dcnt[q] += 1
        sem = self.dsem[q][i % self.KD]
        val = 16 * (i // self.KD + 1)
        if i >= self.KD:
            self._wait(q, (sem, val - 16, "dma"))
        self._deps(q, reads, writes)
        if indirect is None:
            ins = self.eng[q].dma_start(out=out, in_=in_, **kw)
        else:
            ins = self.eng[q].indirect_dma_start(out=out, in_=in_, **indirect)
        ins.then_inc(sem, 16)
        ev = (sem, val, "dma")
        self._commit(ev, (q, i % self.KD), reads, writes)
        return ins

    def finish(self):
        self._wait_all_dma("sp")
        self.es.close()


WEIGHT_SPECS = {
    "ada_w": [4, 1024, 6144], "ada_b": [4, 6144], "norm_mix": [4, 1024], "norm_ffn": [4, 1024],
    "w_in": [2, 1024, 2048], "mu_x": [2, 3, 1024], "mu_p": [2, 3, 512],
    "decay_w0": [2, 2, 512], "decay_w1": [2, 2, 1024, 64], "decay_w2": [2, 2, 64, 512],
    "lr_a0": [2, 2, 512], "lr_a1": [2, 2, 1024, 64], "lr_a2": [2, 2, 64, 512],
    "gate_g1": [2, 1024, 128], "gate_g2": [2, 128, 512],
    "k_k": [2, 512], "k_a": [2, 512], "r_k": [2, 8, 64], "gn_w": [2, 512], "gn_b": [2, 512],
    "pool_w": [2, 4, 128, 128], "pool_scale": [2, 512], "w_out": [2, 1024, 1024],
    "w_fourier": [2, 1024, 1024],
    "router_c": [4, 1024, 4], "router_c_b": [4, 4], "router_f": [4, 1024, 32], "router_f_b": [4, 32],
    "moe_w1": [4, 32, 1024, 512], "moe_w3": [4, 32, 1024, 512], "moe_w2": [4, 32, 512, 1024],
    "final_norm": [1024],
}


class Prog:
    def __init__(self, debug=None, skip=()):
        self.K = Ctx()
        K = self.K
        nc = K.nc
        self.debug = debug or {}
        self.inp = {}
        self.inp["x"] = nc.dram_tensor("x", [T, D], F32, kind="ExternalInput")
        self.inp["c"] = nc.dram_tensor("c", [1, D], F32, kind="ExternalInput")
        self.inp["ctx"] = nc.dram_tensor("ctx", [CT, D], F32, kind="ExternalInput")
        self.inp["c_ctx"] = nc.dram_tensor("c_ctx", [1, D], F32, kind="ExternalInput")
        for k, shp in WEIGHT_SPECS.items():
            if k in skip:
                continue
            self.inp[k] = nc.dram_tensor(k, shp, F32, kind="ExternalInput")
        self.out = nc.dram_tensor("out", [T, D], F32, kind="ExternalOutput")
        self.dbg = {}
        for k, (shp, dt) in self.debug.items():
            self.dbg[k] = nc.dram_tensor("dbg_" + k, shp, dt, kind="ExternalOutput")
        K.init_psum(8)
        self.lat = Parts(nc.dram_tensor("lat", [NT, D], F32), "lat")
        self.ident = K.sb([128, 128], F32, "ident")
        self.identb = K.sb([128, 128], BF16, "identb")
        self.ones = K.sb([128, 128], F32, "ones")
        self._consts()
        self.Ml = K.sb([128, 6, D], F32, "Ml")
        self.Mc = K.sb([128, 6, D], F32, "Mc")
        self.srep = K.sb([128, 2, 8, 128], F32, "srep")

    def _consts(self):
        K = self.K
        K.op("pool", lambda e: e.memset(self.ones[:], 1.0), writes=[self.ones])
        K.op("pool", lambda e: e.memset(self.ident[:], 0.0), writes=[self.ident])
        K.op("pool", lambda e: e.affine_select(out=self.ident[:], in_=self.ident[:], pattern=[[-1, 128]],
                                               compare_op=ALU.not_equal, fill=1.0, base=0, channel_multiplier=1),
             reads=[self.ident], writes=[self.ident])
        K.op("dve", lambda e: e.tensor_copy(out=self.identb[:], in_=self.ident[:]), reads=[self.ident],
             writes=[self.identb])

    def prep_s(self):
        K = self.K
        craw = K.sb([128, 2, 8], F32, "craw")
        csil = K.sb([128, 2, 8], F32, "csil")
        for w, nm in enumerate(("c", "c_ctx")):
            src = self.inp[nm].ap().rearrange("o (kt k) -> k (o kt)", k=128)
            K.dma("sp", craw[:, w, :], src, writes=[craw], allow_slow_non_contiguous=True)
        K.op("act", lambda e: e.activation(out=csil[:], in_=craw[:], func=AF.Silu), reads=[craw], writes=[csil])
        K.op("dve", lambda e: e.tensor_copy(out=self.srep[:], in_=csil[:].unsqueeze(3).to_broadcast([128, 2, 8, 128])),
             reads=[csil], writes=[self.srep])

    def modvec(self, i, need_ctx=True):
        K = self.K
        K.push_scope()
        mv = dict(
            W=[K.sb([128, 8, 512], F32, f"adaW{j}") for j in range(2)],
            b=[K.sb([1, 512], F32, f"adab{j}") for j in range(2)],
            g=K.sb([128, 2, D], F32, "normg"),
        )
        aw = self.inp["ada_w"]
        ab = self.inp["ada_b"]
        g = mv["g"]
        K.dma("sp", g[:, 0, :], self.inp["norm_mix"][i:i + 1, :].partition_broadcast(128), writes=[g])
        K.dma("sp", g[:, 1, :], self.inp["norm_ffn"][i:i + 1, :].partition_broadcast(128), writes=[g])
        targets = [(0, self.Ml)] + ([(1, self.Mc)] if need_ctx else [])
        for nb in range(12):
            W = mv["W"][nb % 2]
            bb = mv["b"][nb % 2]
            K.dma("sp", W[:], aw[i, :, nb * 512:(nb + 1) * 512].rearrange("(kt k) n -> k kt n", k=128), writes=[W])
            K.dma("sp", bb[:], ab[i:i + 1, nb * 512:(nb + 1) * 512], writes=[bb])
            for w, M in targets:
                ps = K.ps()
                for kt in range(8):
                    K.op("pe", lambda e, kt=kt, w=w, ps=ps, W=W: e.matmul(ps[:, :], lhsT=self.srep[:, w, kt, :],
                                                                         rhs=W[:, kt, :], start=(kt == 0), stop=False),
                         reads=[self.srep, W], writes=[ps])
                K.op("pe", lambda e, ps=ps, bb=bb: e.matmul(ps[:, :], lhsT=self.ones[0:1, :], rhs=bb[0:1, :],
                                                            start=False, stop=True),
                     reads=[self.ones, bb], writes=[ps])
                s, half = nb // 2, nb % 2
                dst = M[:, s, half * 512:(half + 1) * 512]
                if s in (1, 4):
                    gi = 0 if s == 1 else 1
                    K.op("dve", lambda e, dst=dst, ps=ps, gi=gi, half=half: e.scalar_tensor_tensor(
                        out=dst, in0=ps[:, :], scalar=1.0, in1=g[:, gi, half * 512:(half + 1) * 512],
                        op0=ALU.add, op1=ALU.mult), reads=[ps, g], writes=[M])
                else:
                    K.op("act", lambda e, dst=dst, ps=ps: e.copy(out=dst, in_=ps[:, :]), reads=[ps], writes=[M])
        K.pop_scope()

    def norm_tile(self, xt, ht, M, sub, st):
        K = self.K
        sh = 0 if sub == 0 else 3
        ga = 1 if sub == 0 else 4
        K.op("act", lambda e: e.activation(out=ht[:], in_=xt[:], func=AF.Square, accum_out=st[:, 0:1]),
             reads=[xt], writes=[ht, st])
        K.op("dve", lambda e: e.tensor_scalar(out=st[:, 1:2], in0=st[:, 0:1], scalar1=1.0 / D, scalar2=1e-6,
                                              op0=ALU.mult, op1=ALU.add), reads=[st], writes=[st])
        K.op("act", lambda e: e.sqrt(out=st[:, 3:4], in_=st[:, 1:2]), reads=[st], writes=[st])
        K.op("dve", lambda e: e.reciprocal(out=st[:, 2:3], in_=st[:, 3:4]), reads=[st], writes=[st])
        K.op("dve", lambda e: e.scalar_tensor_tensor(out=ht[:], in0=xt[:], scalar=st[:, 2:3], in1=M[:, ga, :],
                                                     op0=ALU.mult, op1=ALU.mult), reads=[xt, st, M], writes=[ht])
        K.op("pool", lambda e: e.tensor_tensor(out=ht[:], in0=ht[:], in1=M[:, sh, :], op=ALU.add),
             reads=[ht, M], writes=[ht])

    def tr(self, e_unused, dst_ps_ap, src_ap, ps, src_buf, n_in=128):
        K = self.K
        K.op("pe", lambda e: e.transpose(out=dst_ps_ap, in_=src_ap, identity=self.ident[0:n_in, 0:n_in]),
             reads=[src_buf, self.ident], writes=[ps])

    def init_lat(self):
        K = self.K
        for r in range(0, T, 2048):
            K.dma("sp", self.lat[r:r + 2048, :], self.inp["x"][r:r + 2048, :],
                  writes=[self.lat.p(t) for t in range(r // 128, r // 128 + 16)])
        K.dma("sp", self.lat[T:NT, :], self.inp["ctx"][:, :], writes=[self.lat.p(64), self.lat.p(65)])

    def moe_dram(self):
        nc = self.K.nc
        self.NB = (2 * NT + 32 * 128) // 128
        NB = self.NB
        self.mdram = dict(H2=Parts(nc.dram_tensor("H2", [NT, D], F32), "H2"),
                          XS=Buf(nc.dram_tensor("XS", [NB * 128, D], F32), "XS"),
                          YS=Buf(nc.dram_tensor("YS", [NB * 128, D], F32), "YS"))

    def moe_setup(self):
        K = self.K
        nc = K.nc
        if not hasattr(self, "mdram"):
            self.moe_dram()
        K.push_scope()
        m = dict(self.mdram)
        NTL = 66
        NB = self.NB
        m["OHA"] = K.sb([128, NTL, 2, 32], BF16, "OHA")
        m["RK"] = K.sb([128, NTL, 2], F32, "RK")
        m["cs"] = K.sb([128, 8, 32], F32, "moecs")
        m["be"] = K.sb([128, 3, NB], F32, "moebe")
        m["idf"] = K.sb([128, NB, 8], F32, "moeidf")
        m["GATE"] = K.sb([128, NTL, 2], F32, "GATE")
        m["DEST"] = K.sb([128, NTL, 2], I32, "DEST")
        m["carry"] = K.sb([128, 32], F32, "carry")
        m["Wr"] = K.sb([128, 8, 36], F32, "Wr")
        m["br"] = K.sb([1, 36], F32, "br")
        m["UT"] = K.sb([128, 128], F32, "UT")
        m["PIDX"] = K.sb([128, 8], F32, "PIDX")
        m["JV"] = K.sb([128, NB], F32, "JV")
        m["IDX1"] = K.sb([128, NB, 8], I32, "IDX1")
        m["IDX2"] = K.sb([128, NB, 4], I32, "IDX2")
        big = [K.sb([128, D], F32, f"big{j}") for j in range(8)]
        m["xt"] = big[0:2]
        m["ht"] = big[2:4]
        m["yb"] = big[4:6]
        m["y0"] = big[4:6]
        m["y1"] = big[6:8]
        m["st"] = [K.sb([128, 4], F32, f"mst{j}") for j in range(2)]
        m["hT"] = [K.sb([128, 8, 128], F32, f"mhT{j}") for j in range(2)]
        m["xTb"] = [K.sb([128, 8, 128], BF16, f"mxTb{j}") for j in range(2)]
        m["W1"] = [K.sb([128, 8, 512], BF16, f"mW1{j}") for j in range(2)]
        m["W3"] = [K.sb([128, 8, 512], BF16, f"mW3{j}") for j in range(2)]
        m["W2"] = [K.sb([128, 4, 1024], BF16, f"mW2{j}") for j in range(2)]
        m["hTb"] = [K.sb([128, 4, 128], BF16, f"mhTb{j}") for j in range(2)]
        m["sil"] = [K.sb([128, 512], F32, f"msil{j}") for j in range(2)]
        m["sm"] = [K.sb([128, 128], F32, f"msm{j}") for j in range(2)]
        UT = m["UT"]
        K.op("pool", lambda e: e.memset(UT[:], 1.0), writes=[UT])
        K.op("pool", lambda e: e.affine_select(out=UT[:], in_=UT[:], pattern=[[1, 128]], compare_op=ALU.is_gt,
                                               fill=0.0, base=0, channel_multiplier=-1), reads=[UT], writes=[UT])
        pi = K.sb([128, 8], I32, "pidx_i")
        K.op("pool", lambda e: e.iota(pi[:], pattern=[[128, 8]], base=0, channel_multiplier=1), writes=[pi])
        K.op("dve", lambda e: e.tensor_copy(out=m["PIDX"][:], in_=pi[:]), reads=[pi], writes=[m["PIDX"]])
        ji = K.sb([128, NB], I32, "jv_i")
        K.op("pool", lambda e: e.iota(ji[:], pattern=[[128, NB]], base=0, channel_multiplier=0), writes=[ji])
        K.op("dve", lambda e: e.tensor_copy(out=m["JV"][:], in_=ji[:]), reads=[ji], writes=[m["JV"]])
        self.m = m

    def moe(self, i, with_ctx, final=False):
        K = self.K
        m = self.m
        NB = self.NB
        ntile = 66 if with_ctx else 64
        Wr, br = m["Wr"], m["br"]
        K.dma("sp", Wr[:, :, 0:4], self.inp["router_c"][i].rearrange("(kt k) n -> k kt n", k=128), writes=[Wr])
        K.dma("sp", Wr[:, :, 4:36], self.inp["router_f"][i].rearrange("(kt k) n -> k kt n", k=128), writes=[Wr])
        K.dma("sp", br[:, 0:4], self.inp["router_c_b"][i:i + 1, :], writes=[br])
        K.dma("sp", br[:, 4:36], self.inp["router_f_b"][i:i + 1, :], writes=[br])
        carry = m["carry"]
        K.op("dve", lambda e: e.memset(carry[:], 0.0), writes=[carry])
        OHA, RK, GATE, DEST = m["OHA"], m["RK"], m["GATE"], m["DEST"]
        for tt in range(ntile):
            xt, ht, st, hT, sm = (m[k][tt % 2] for k in ("xt", "ht", "st", "hT", "sm"))
            M = self.Ml if tt < 64 else self.Mc
            K.dma("sp", xt[:], self.lat[tt * 128:(tt + 1) * 128, :], reads=[self.lat.p(tt)], writes=[xt])
            self.norm_tile(xt, ht, M, 1, st)
            K.dma("act", m["H2"][tt * 128:(tt + 1) * 128, :], ht[:], reads=[ht], writes=[m["H2"].p(tt)])
            for half in range(2):
                ps = K.ps()
                for q in range(4):
                    kt = half * 4 + q
                    self.tr(None, ps[:, q * 128:(q + 1) * 128], ht[:, kt * 128:(kt + 1) * 128], ps, ht)
                K.op("act" if half else "dve",
                     lambda e, ps=ps, half=half: (e.copy if half else e.tensor_copy)(
                         out=hT[:, half * 4:(half + 1) * 4, :].rearrange("p a b -> p (a b)"), in_=ps[:, :]),
                     reads=[ps], writes=[hT])
            ps = K.ps()
            for kt in range(8):
                K.op("pe", lambda e, kt=kt, ps=ps: e.matmul(ps[:, 0:36], lhsT=hT[:, kt, :], rhs=Wr[:, kt, :],
                                                           start=(kt == 0), stop=False),
                     reads=[hT, Wr], writes=[ps])
            K.op("pe", lambda e, ps=ps: e.matmul(ps[:, 0:36], lhsT=self.ones[0:1, :], rhs=br[0:1, :],
                                                 start=False, stop=True), reads=[self.ones, br], writes=[ps])
            def dv(fn, rd=(), wr=()):
                K.op("dve", fn, reads=[sm] + list(rd), writes=[sm] + list(wr))
            K.op("dve", lambda e, ps=ps: e.tensor_copy(out=sm[:, 0:36], in_=ps[:, 0:36]), reads=[ps], writes=[sm])
            dv(lambda e: e.reduce_max(out=sm[:, 36:37], in_=sm[:, 0:4], axis=AX.X))
            dv(lambda e: e.tensor_scalar(out=sm[:, 37:38], in0=sm[:, 36:37], scalar1=-1.0, scalar2=None, op0=ALU.mult))
            dv(lambda e: e.tensor_scalar(out=sm[:, 40:44], in0=sm[:, 0:4], scalar1=sm[:, 36:37], scalar2=None,
                                         op0=ALU.is_equal))
            K.op("act", lambda e: e.activation(out=sm[:, 44:48], in_=sm[:, 0:4], func=AF.Exp, bias=sm[:, 37:38],
                                               scale=1.0, accum_out=sm[:, 38:39]), reads=[sm], writes=[sm])
            dv(lambda e: e.reciprocal(out=sm[:, 39:40], in_=sm[:, 38:39]))
            dv(lambda e: e.tensor_scalar(out=sm[:, 48:56], in0=sm[:, 4:12], scalar1=sm[:, 40:41], scalar2=None,
                                         op0=ALU.mult))
            for g in range(1, 4):
                dv(lambda e, g=g: e.scalar_tensor_tensor(out=sm[:, 48:56], in0=sm[:, 4 + 8 * g:12 + 8 * g],
                                                         scalar=sm[:, 40 + g:41 + g], in1=sm[:, 48:56],
                                                         op0=ALU.mult, op1=ALU.add))
            dv(lambda e: e.reduce_max(out=sm[:, 56:57], in_=sm[:, 48:56], axis=AX.X))
            dv(lambda e: e.tensor_scalar(out=sm[:, 60:68], in0=sm[:, 48:56], scalar1=sm[:, 56:57], scalar2=None,
                                         op0=ALU.is_equal))
            dv(lambda e: e.scalar_tensor_tensor(out=sm[:, 68:76], in0=sm[:, 60:68], scalar=-1e30, in1=sm[:, 48:56],
                                                op0=ALU.mult, op1=ALU.add))
            dv(lambda e: e.reduce_max(out=sm[:, 57:58], in_=sm[:, 68:76], axis=AX.X))
            dv(lambda e: e.tensor_scalar(out=sm[:, 76:84], in0=sm[:, 68:76], scalar1=sm[:, 57:58], scalar2=None,
                                         op0=ALU.is_equal))
            dv(lambda e: e.tensor_tensor(out=sm[:, 58:59], in0=sm[:, 56:57], in1=sm[:, 57:58], op=ALU.subtract))
            K.op("act", lambda e: e.activation(out=sm[:, 59:60], in_=sm[:, 58:59], func=AF.Sigmoid),
                 reads=[sm], writes=[sm])
            dv(lambda e, tt=tt: e.tensor_tensor(out=GATE[:, tt, 0:1], in0=sm[:, 59:60], in1=sm[:, 39:40], op=ALU.mult),
               wr=[GATE])
            dv(lambda e, tt=tt: e.tensor_tensor(out=GATE[:, tt, 1:2], in0=sm[:, 39:40], in1=GATE[:, tt, 0:1],
                                                op=ALU.subtract), rd=[GATE], wr=[GATE])
            for k, c0 in ((0, 60), (1, 76)):
                dv(lambda e, tt=tt, k=k, c0=c0: e.tensor_tensor(
                    out=OHA[:, tt, k, :].rearrange("p (g l) -> p g l", g=4),
                    in0=sm[:, 40:44].unsqueeze(2).to_broadcast([128, 4, 8]),
                    in1=sm[:, c0:c0 + 8].unsqueeze(1).to_broadcast([128, 4, 8]), op=ALU.mult), wr=[OHA])
            dv(lambda e, tt=tt: e.tensor_tensor(out=sm[:, 84:116], in0=OHA[:, tt, 0, :], in1=OHA[:, tt, 1, :],
                                                op=ALU.add), rd=[OHA])
            psr = K.ps()
            K.op("pe", lambda e, psr=psr: e.matmul(psr[:, 0:32], lhsT=m["UT"][:], rhs=sm[:, 84:116], start=True,
                                                   stop=True), reads=[m["UT"], sm], writes=[psr])
            K.op("pe", lambda e, psr=psr: e.matmul(psr[:, 32:64], lhsT=self.ones[:], rhs=sm[:, 84:116], start=True,
                                                   stop=True), reads=[self.ones, sm], writes=[psr])
            K.op("dve", lambda e, psr=psr: e.tensor_tensor(out=sm[:, 84:116], in0=psr[:, 0:32], in1=carry[:],
                                                           op=ALU.add), reads=[psr, carry, sm], writes=[sm])
            for k in range(2):
                dv(lambda e, tt=tt, k=k: e.tensor_tensor(out=sm[:, 0:32], in0=sm[:, 84:116], in1=OHA[:, tt, k, :],
                                                         op=ALU.mult), rd=[OHA])
                dv(lambda e, tt=tt, k=k: e.reduce_sum(out=RK[:, tt, k:k + 1], in_=sm[:, 0:32], axis=AX.X), wr=[RK])
            K.op("dve", lambda e, psr=psr: e.tensor_tensor(out=carry[:], in0=psr[:, 32:64], in1=carry[:], op=ALU.add),
                 reads=[psr, carry], writes=[carry])
        cs = m["cs"]

        def cv(fn):
            K.op("dve", fn, reads=[cs, carry], writes=[cs])
        cv(lambda e: e.tensor_scalar(out=cs[:, 0, :], in0=carry[:], scalar1=1.0 / 128, scalar2=127.0 / 256,
                                     op0=ALU.mult, op1=ALU.add))
        cv(lambda e: e.tensor_scalar(out=cs[:, 1, :], in0=cs[:, 0, :], scalar1=8388608.0, scalar2=None, op0=ALU.add))
        cv(lambda e: e.tensor_scalar(out=cs[:, 2, :], in0=cs[:, 1, :], scalar1=-8388608.0, scalar2=128.0,
                                     op0=ALU.add, op1=ALU.mult))
        cv(lambda e: e.tensor_copy(out=cs[:, 3, :], in_=cs[:, 2, :]))
        a, b = 3, 4
        for s in (1, 2, 4, 8, 16):
            cv(lambda e, a=a, b=b, s=s: e.tensor_copy(out=cs[:, b, 0:s], in_=cs[:, a, 0:s]))
            cv(lambda e, a=a, b=b, s=s: e.tensor_tensor(out=cs[:, b, s:32], in0=cs[:, a, s:32], in1=cs[:, a, 0:32 - s],
                                                        op=ALU.add))
            a, b = b, a
        pend_i = a
        cv(lambda e: e.tensor_tensor(out=cs[:, 5, :], in0=cs[:, pend_i, :], in1=cs[:, 2, :], op=ALU.subtract))
        be = m["be"]
        K.op("dve", lambda e: e.memset(be[:, 0, :], 0.0), writes=[be])
        for ex in range(32):
            K.op("dve", lambda e, ex=ex: e.scalar_tensor_tensor(out=be[:, 0, :], in0=m["JV"][:],
                                                                scalar=cs[:, pend_i, ex:ex + 1], in1=be[:, 0, :],
                                                                op0=ALU.is_ge, op1=ALU.add),
                 reads=[m["JV"], cs, be], writes=[be])
        K.op("dve", lambda e: e.tensor_scalar(out=be[:, 0, :], in0=be[:, 0, :], scalar1=31.0, scalar2=None, op0=ALU.min),
             reads=[be], writes=[be])
        K.op("dve", lambda e: e.tensor_scalar(out=be[:, 1, :], in0=be[:, 0, :], scalar1=1024.0,
                                              scalar2=float(i * 32 * 1024), op0=ALU.mult, op1=ALU.add),
             reads=[be], writes=[be])
        K.op("dve", lambda e: e.tensor_scalar(out=be[:, 2, :], in0=be[:, 0, :], scalar1=512.0,
                                              scalar2=float(i * 32 * 512), op0=ALU.mult, op1=ALU.add),
             reads=[be], writes=[be])
        idf = m["idf"]
        K.op("dve", lambda e: e.tensor_tensor(out=idf[:], in0=be[:, 1, :].unsqueeze(2).to_broadcast([128, NB, 8]),
                                              in1=m["PIDX"][:].unsqueeze(1).to_broadcast([128, NB, 8]), op=ALU.add),
             reads=[be, m["PIDX"]], writes=[idf])
        K.op("dve", lambda e: e.tensor_copy(out=m["IDX1"][:], in_=idf[:]), reads=[idf], writes=[m["IDX1"]])
        K.op("dve", lambda e: e.tensor_tensor(out=idf[:, :, 0:4], in0=be[:, 2, :].unsqueeze(2).to_broadcast([128, NB, 4]),
                                              in1=m["PIDX"][:, 0:4].unsqueeze(1).to_broadcast([128, NB, 4]),
                                              op=ALU.add), reads=[be, m["PIDX"]], writes=[idf])
        K.op("dve", lambda e: e.tensor_copy(out=m["IDX2"][:], in_=idf[:, :, 0:4]), reads=[idf], writes=[m["IDX2"]])
        for tt in range(ntile):
            ht, sm = m["ht"][tt % 2], m["sm"][tt % 2]
            K.dma("sp", ht[:], m["H2"][tt * 128:(tt + 1) * 128, :], reads=[m["H2"].p(tt)], writes=[ht])
            for k in range(2):
                K.op("dve", lambda e, tt=tt, k=k: e.tensor_tensor(out=sm[:, 32:64], in0=cs[:, 5, :],
                                                                  in1=OHA[:, tt, k, :], op=ALU.mult),
                     reads=[sm, OHA, cs], writes=[sm])
                K.op("dve", lambda e, k=k: e.reduce_sum(out=sm[:, 66 + k:67 + k], in_=sm[:, 32:64], axis=AX.X),
                     reads=[sm], writes=[sm])
            K.op("dve", lambda e, tt=tt: e.tensor_tensor(out=sm[:, 64:66], in0=sm[:, 66:68], in1=RK[:, tt, :],
                                                         op=ALU.add), reads=[sm, RK], writes=[sm])
            K.op("dve", lambda e, tt=tt: e.tensor_copy(out=DEST[:, tt, :], in_=sm[:, 64:66]), reads=[sm], writes=[DEST])
            for k in range(2):
                K.dma("pool", m["XS"][:, :], ht[:], reads=[ht, DEST], writes=[m["XS"]],
                      indirect=dict(out_offset=bass.IndirectOffsetOnAxis(ap=DEST[:, tt, k:k + 1], axis=0),
                                    in_offset=None))
        w1t = self.inp["moe_w1"].ap().rearrange("l e k n -> (l e k) n")
        w3t = self.inp["moe_w3"].ap().rearrange("l e k n -> (l e k) n")
        w2t = self.inp["moe_w2"].ap().rearrange("l e k n -> (l e k) n")
        for j in range(NB):
            W1, W3, W2, xt, xTb, hTb, sil, yb = (m[k][j % 2] for k in ("W1", "W3", "W2", "xt", "xTb", "hTb", "sil", "yb"))
            for kt in range(8):
                for W, tab in ((W1, w1t), (W3, w3t)):
                    K.dma("pool", W[:, kt, :], tab, reads=[m["IDX1"]], writes=[W],
                          indirect=dict(out_offset=None,
                                        in_offset=bass.IndirectOffsetOnAxis(ap=m["IDX1"][:, j, kt:kt + 1], axis=0)))
            for fc in range(4):
                K.dma("pool", W2[:, fc, :], w2t, reads=[m["IDX2"]], writes=[W2],
                      indirect=dict(out_offset=None,
                                    in_offset=bass.IndirectOffsetOnAxis(ap=m["IDX2"][:, j, fc:fc + 1], axis=0)))
            K.dma("sp", xt[:], m["XS"][j * 128:(j + 1) * 128, :], reads=[m["XS"]], writes=[xt])
            for half in range(2):
                ps = K.ps()
                for q in range(4):
                    kt = half * 4 + q
                    self.tr(None, ps[:, q * 128:(q + 1) * 128], xt[:, kt * 128:(kt + 1) * 128], ps, xt)
                K.op("act" if half else "dve",
                     lambda e, ps=ps, half=half, xTb=xTb: (e.copy if half else e.tensor_copy)(
                         out=xTb[:, half * 4:(half + 1) * 4, :].rearrange("p a b -> p (a b)"), in_=ps[:, :]),
                     reads=[ps], writes=[xTb])
            pa, pb = K.ps(), K.ps()
            for W, pp in ((W1, pa), (W3, pb)):
                for fc in range(4):
                    for kt in range(8):
                        K.op("pe", lambda e, W=W, pp=pp, fc=fc, kt=kt, xTb=xTb: e.matmul(
                            pp[:, fc * 128:(fc + 1) * 128], lhsT=W[:, kt, fc * 128:(fc + 1) * 128], rhs=xTb[:, kt, :],
                            start=(kt == 0), stop=(kt == 7)), reads=[W, xTb], writes=[pp])
            K.op("act", lambda e, pa=pa, sil=sil: e.activation(out=sil[:], in_=pa[:, :], func=AF.Silu),
                 reads=[pa], writes=[sil])
            K.op("dve", lambda e, pb=pb, sil=sil, hTb=hTb: e.tensor_tensor(
                out=hTb[:].rearrange("p a b -> p (a b)"), in0=sil[:], in1=pb[:, :], op=ALU.mult),
                reads=[pb, sil], writes=[hTb])
            for half in range(2):
                py = K.ps()
                for fc in range(4):
                    K.op("pe", lambda e, py=py, fc=fc, half=half, hTb=hTb, W2=W2: e.matmul(
                        py[:, :], lhsT=hTb[:, fc, :], rhs=W2[:, fc, half * 512:(half + 1) * 512],
                        start=(fc == 0), stop=(fc == 3)), reads=[hTb, W2], writes=[py])
                K.op("act" if half else "dve",
                     lambda e, py=py, half=half, yb=yb: (e.copy if half else e.tensor_copy)(
                         out=yb[:, half * 512:(half + 1) * 512], in_=py[:, :]), reads=[py], writes=[yb])
            K.dma("sp", m["YS"][j * 128:(j + 1) * 128, :], yb[:], reads=[yb], writes=[m["YS"]])
        for tt in range(ntile):
            y0, y1, xt = m["y0"][tt % 2], m["y1"][tt % 2], m["xt"][tt % 2]
            M = self.Ml if tt < 64 else self.Mc
            for k, y in ((0, y0), (1, y1)):
                K.dma("pool", y[:], m["YS"][:, :], reads=[m["YS"], DEST], writes=[y],
                      indirect=dict(out_offset=None,
                                    in_offset=bass.IndirectOffsetOnAxis(ap=DEST[:, tt, k:k + 1], axis=0)))
            K.dma("sp", xt[:], self.lat[tt * 128:(tt + 1) * 128, :], reads=[self.lat.p(tt)], writes=[xt])
            K.op("dve", lambda e, tt=tt, y0=y0: e.tensor_scalar(out=y0[:], in0=y0[:], scalar1=GATE[:, tt, 0:1],
                                                                scalar2=None, op0=ALU.mult),
                 reads=[y0, GATE], writes=[y0])
            K.op("dve", lambda e, tt=tt, y0=y0, y1=y1: e.scalar_tensor_tensor(
                out=y0[:], in0=y1[:], scalar=GATE[:, tt, 1:2], in1=y0[:], op0=ALU.mult, op1=ALU.add),
                reads=[y0, y1, GATE], writes=[y0])
            K.op("pool", lambda e, y0=y0, M=M: e.tensor_tensor(out=y0[:], in0=y0[:], in1=M[:, 5, :], op=ALU.mult),
                 reads=[y0, M], writes=[y0])
            K.op("dve", lambda e, y0=y0, xt=xt: e.tensor_tensor(out=xt[:], in0=xt[:], in1=y0[:], op=ALU.add),
                 reads=[y0, xt], writes=[xt])
            K.dma("sp", self.lat[tt * 128:(tt + 1) * 128, :], xt[:], reads=[xt], writes=[self.lat.p(tt)])
        K.pop_scope()

    def final_norm(self):
        K = self.K
        K.push_scope()
        m = dict(xt=[K.sb([128, D], F32, f"fx{j}") for j in range(2)], ht=[K.sb([128, D], F32, f"fh{j}") for j in range(2)],
                 st=[K.sb([128, 4], F32, f"fs{j}") for j in range(2)])
        g = K.sb([128, D], F32, "fng")
        K.dma("sp", g[:], self.inp["final_norm"].ap().rearrange("(o d) -> o d", o=1).partition_broadcast(128), writes=[g])
        for tt in range(64):
            xt, ht, st = m["xt"][tt % 2], m["ht"][tt % 2], m["st"][tt % 2]
            K.dma("sp", xt[:], self.lat[tt * 128:(tt + 1) * 128, :], reads=[self.lat.p(tt)], writes=[xt])
            K.op("act", lambda e, xt=xt, ht=ht, st=st: e.activation(out=ht[:], in_=xt[:], func=AF.Square,
                                                                    accum_out=st[:, 0:1]), reads=[xt], writes=[ht, st])
            K.op("dve", lambda e, st=st: e.tensor_scalar(out=st[:, 1:2], in0=st[:, 0:1], scalar1=1.0 / D, scalar2=1e-6,
                                                         op0=ALU.mult, op1=ALU.add), reads=[st], writes=[st])
            K.op("act", lambda e, st=st: e.sqrt(out=st[:, 3:4], in_=st[:, 1:2]), reads=[st], writes=[st])
            K.op("dve", lambda e, st=st: e.reciprocal(out=st[:, 2:3], in_=st[:, 3:4]), reads=[st], writes=[st])
            K.op("dve", lambda e, xt=xt, ht=ht, st=st: e.scalar_tensor_tensor(
                out=ht[:], in0=xt[:], scalar=st[:, 2:3], in1=g[:], op0=ALU.mult, op1=ALU.mult),
                reads=[xt, st, g], writes=[ht])
            K.dma("sp", self.out[tt * 128:(tt + 1) * 128, :], ht[:], reads=[ht])
        K.pop_scope()


def fourier_consts():
    c = {}
    n = np.arange(256)
    ang = 2 * np.pi * np.outer(n, n) / 256
    c["f_cc"] = (np.cos(ang) / 16).astype(np.float32)
    c["f_sc"] = (-np.sin(ang) / 16).astype(np.float32)
    a = np.arange(128)
    ang = 2 * np.pi * np.outer(a, a) / 128
    s = 1.0 / np.sqrt(8192.0)
    c["f_c128"] = (np.cos(ang) * s).astype(np.float32)
    c["f_s128"] = (np.sin(ang) * s).astype(np.float32)
    f1 = np.arange(128)[:, None]
    b = np.arange(64)[None, :]
    th = 2 * np.pi * f1 * b / 8192
    c["f_tw"] = np.stack([np.cos(th), -np.sin(th)], axis=2).astype(np.float32)
    bb = np.arange(64)
    ang = 2 * np.pi * np.outer(bb, bb) / 64
    c["f_cs64"] = np.concatenate([np.cos(ang), np.sin(ang)], axis=0).astype(np.float32)
    t = np.arange(256)
    ang = 2 * np.pi * np.outer(t, t) / 256
    c["f_c256"] = (np.cos(ang) / 16).astype(np.float32)
    c["f_s256"] = (np.sin(ang) / 16).astype(np.float32)
    return c


FOURIER_SPECS = {"f_cc": [256, 256], "f_sc": [256, 256], "f_c128": [128, 128], "f_s128": [128, 128],
                 "f_tw": [128, 64, 2], "f_cs64": [128, 64], "f_c256": [256, 256], "f_s256": [256, 256]}


def _fourier_declare(self):
    nc = self.K.nc
    for k, shp in FOURIER_SPECS.items():
        self.inp[k] = nc.dram_tensor(k, shp, F32, kind="ExternalInput")
    self.GD = Buf(nc.dram_tensor("GD", [128, 128, D], F32), "GD")


def _fourier(self, i, with_ctx, nb=64, nf=128):
    K = self.K
    j = i // 2
    K.push_scope()
    cc = K.sb([128, 2, 2, 256], BF16, "fcc")
    c128 = K.sb([128, 3, 128], BF16, "fc128")
    tw = K.sb([128, 64, 2], F32, "ftw")
    cs64 = K.sb([128, 64], F32, "fcs64")
    wf = K.sb([128, 8, D], BF16, "fwf")
    big = [K.sb([128, D], F32, f"fbig{q}") for q in range(6)]
    xts, hts, gsb = big[0:2], big[2:4], big[4:6]
    sts = [K.sb([128, 4], F32, f"fst{q}") for q in range(2)]
    hTb = [K.sb([128, 8, 128], BF16, f"fhT{q}") for q in range(2)]
    Zb = [K.sb([128, 2, D], BF16, f"fZ{q}") for q in range(2)]
    gi2 = [K.sb([128, D], F32, f"fgi{q}") for q in range(2)]
    tmp = K.sb([128, 256], F32, "ftmp")
    def ld_cast(dst_ap, dst_buf, src_ap, shape, n=[0]):
        stg = big[4 + n[0] % 2]
        n[0] += 1
        rows, cols = shape
        view = stg[0:rows, 0:cols]
        K.dma("sp", view, src_ap, writes=[stg])
        K.op("dve", lambda e: e.tensor_copy(out=dst_ap, in_=view), reads=[stg], writes=[dst_buf])
    for kt in range(2):
        ld_cast(cc[:, kt, 0, :], cc, self.inp["f_cc"][kt * 128:(kt + 1) * 128, :], (128, 256))
        ld_cast(cc[:, kt, 1, :], cc, self.inp["f_sc"][kt * 128:(kt + 1) * 128, :], (128, 256))
    ld_cast(c128[:, 0, :], c128, self.inp["f_c128"][:, :], (128, 128))
    ld_cast(c128[:, 1, :], c128, self.inp["f_s128"][:, :], (128, 128))
    K.op("dve", lambda e: e.tensor_scalar(out=c128[:, 2, :], in0=c128[:, 1, :], scalar1=-1.0, scalar2=None,
                                          op0=ALU.mult), reads=[c128], writes=[c128])
    for kt in range(8):
        ld_cast(wf[:, kt, :], wf, self.inp["w_fourier"][j, kt * 128:(kt + 1) * 128, :], (128, 1024))
    self._ld_cast = ld_cast
    K.dma("sp", tw[:], self.inp["f_tw"][:, :, :], writes=[tw])
    K.dma("sp", cs64[:], self.inp["f_cs64"][:, :], writes=[cs64])
    ntw = K.sb([128, 64], F32, "fntw")
    K.op("dve", lambda e: e.tensor_scalar(out=ntw[:], in0=tw[:, :, 1], scalar1=-1.0, scalar2=None, op0=ALU.mult),
         reads=[tw], writes=[ntw])
    latv = self.lat[0:T, :].rearrange("(a b) d -> b a d", b=64)
    lat_all = [self.lat.p(t) for t in range(64)]

    def chan_dft(xt, ht, st, hT, Z, M):
        self.norm_tile(xt, ht, M, 0, st)
        for half in range(2):
            ps = K.ps()
            for q in range(4):
                kt = half * 4 + q
                self.tr(None, ps[:, q * 128:(q + 1) * 128], ht[:, kt * 128:(kt + 1) * 128], ps, ht)
            K.op("act" if half else "dve",
                 lambda e, ps=ps, half=half: (e.copy if half else e.tensor_copy)(
                     out=hT[:, half * 4:(half + 1) * 4, :].rearrange("p a b -> p (a b)"), in_=ps[:, :]),
                 reads=[ps], writes=[hT])
        for ri in range(2):
            for hh in range(2):
                ps = K.ps()
                for gg in range(2):
                    g = hh * 2 + gg
                    for kt in range(2):
                        K.op("pe", lambda e, ps=ps, gg=gg, g=g, kt=kt, ri=ri: e.matmul(
                            ps[:, gg * 256:(gg + 1) * 256], lhsT=hT[:, g * 2 + kt, :], rhs=cc[:, kt, ri, :],
                            start=(kt == 0), stop=(kt == 1)), reads=[hT, cc], writes=[ps])
                K.op("act" if hh else "dve",
                     lambda e, ps=ps, hh=hh, ri=ri: (e.copy if hh else e.tensor_copy)(
                         out=Z[:, ri, hh * 512:(hh + 1) * 512], in_=ps[:, :]), reads=[ps], writes=[Z])

    for b in range(nb):
        xt, ht, st, hT, Z, gr, gi = (l[b % 2] for l in (xts, hts, sts, hTb, Zb, gsb, gi2))
        K.dma("sp", xt[:], latv[b], reads=lat_all, writes=[xt])
        chan_dft(xt, ht, st, hT, Z, self.Ml)
        for hh in range(2):
            cs_ = slice(hh * 512, (hh + 1) * 512)
            pr, pi_ = K.ps(), K.ps()
            K.op("pe", lambda e: e.matmul(pr[:, :], lhsT=c128[:, 0, :], rhs=Z[:, 0, cs_], start=True, stop=False),
                 reads=[c128, Z], writes=[pr])
            K.op("pe", lambda e: e.matmul(pr[:, :], lhsT=c128[:, 1, :], rhs=Z[:, 1, cs_], start=False, stop=True),
                 reads=[c128, Z], writes=[pr])
            K.op("pe", lambda e: e.matmul(pi_[:, :], lhsT=c128[:, 0, :], rhs=Z[:, 1, cs_], start=True, stop=False),
                 reads=[c128, Z], writes=[pi_])
            K.op("pe", lambda e: e.matmul(pi_[:, :], lhsT=c128[:, 2, :], rhs=Z[:, 0, cs_], start=False, stop=True),
                 reads=[c128, Z], writes=[pi_])
            K.op("dve", lambda e: e.tensor_scalar(out=gr[:, cs_], in0=pr[:, :], scalar1=tw[:, b, 0:1], scalar2=None,
                                                  op0=ALU.mult), reads=[pr, tw], writes=[gr])
            K.op("dve", lambda e: e.scalar_tensor_tensor(out=gr[:, cs_], in0=pi_[:, :], scalar=ntw[:, b:b + 1],
                                                         in1=gr[:, cs_], op0=ALU.mult, op1=ALU.add),
                 reads=[pi_, ntw, gr], writes=[gr])
            K.op("dve", lambda e: e.tensor_scalar(out=gi[:, cs_], in0=pi_[:, :], scalar1=tw[:, b, 0:1], scalar2=None,
                                                  op0=ALU.mult), reads=[pi_, tw], writes=[gi])
            K.op("dve", lambda e: e.scalar_tensor_tensor(out=gi[:, cs_], in0=pr[:, :], scalar=tw[:, b, 1:2],
                                                         in1=gi[:, cs_], op0=ALU.mult, op1=ALU.add),
                 reads=[pr, tw, gi], writes=[gi])
        K.dma("act", self.GD[b, :, :], gr[:], reads=[gr], writes=[self.GD])
        K.dma("act", self.GD[64 + b, :, :], gi[:], reads=[gi], writes=[self.GD])
    latf = self.lat[0:T, :].rearrange("(f2 f1) d -> f1 f2 d", f1=128)
    YT = [K.sb([128, 8, 64], BF16, f"fYT{q}") for q in range(2)]
    for f1 in range(nf):
        gd, yt, xt, ht = gsb[f1 % 2], YT[f1 % 2], xts[f1 % 2], hts[f1 % 2]
        K.dma("sp", gd[:], self.GD[:, f1, :], reads=[self.GD], writes=[gd])
        K.dma("sp", xt[0:64, :], latf[f1], reads=lat_all, writes=[xt])
        ps = K.ps()
        for kt in range(8):
            K.op("pe", lambda e, kt=kt: e.matmul(ps[:, kt * 64:(kt + 1) * 64], lhsT=gd[:, kt * 128:(kt + 1) * 128],
                                                 rhs=cs64[:, :], start=True, stop=True), reads=[gd, cs64], writes=[ps])
        K.op("act", lambda e: e.copy(out=yt[:].rearrange("p a b -> p (a b)"), in_=ps[:, :]), reads=[ps], writes=[yt])
        for hh in range(2):
            py = K.ps()
            for kt in range(8):
                K.op("pe", lambda e, kt=kt: e.matmul(py[0:64, :], lhsT=yt[:, kt, :],
                                                     rhs=wf[:, kt, hh * 512:(hh + 1) * 512], start=(kt == 0),
                                                     stop=(kt == 7)), reads=[yt, wf], writes=[py])
            K.op("dve", lambda e: e.tensor_tensor(out=ht[0:64, hh * 512:(hh + 1) * 512], in0=py[0:64, :],
                                                  in1=self.Ml[0:64, 2, hh * 512:(hh + 1) * 512], op=ALU.mult),
                 reads=[py, self.Ml], writes=[ht])
        K.op("pool", lambda e: e.tensor_tensor(out=xt[0:64, :], in0=xt[0:64, :], in1=ht[0:64, :], op=ALU.add),
             reads=[xt, ht], writes=[xt])
        K.dma("act", latf[f1], xt[0:64, :], reads=[xt], writes=lat_all)
    if with_ctx:
        c256 = K.sb([128, 2, 2, 256], BF16, "fc256")
        for tt in range(2):
            ld_cast(c256[:, 0, tt, :], c256, self.inp["f_c256"][tt * 128:(tt + 1) * 128, :], (128, 256))
            ld_cast(c256[:, 1, tt, :], c256, self.inp["f_s256"][tt * 128:(tt + 1) * 128, :], (128, 256))
        ctxp = [self.lat.p(64), self.lat.p(65)]
        for tt in range(2):
            K.dma("sp", xts[tt][:], self.lat[T + tt * 128:T + (tt + 1) * 128, :], reads=ctxp, writes=[xts[tt]])
            chan_dft(xts[tt], hts[tt], sts[tt], hTb[tt], Zb[tt], self.Mc)
        for ft in range(2):
            yt = K.sb([128, 8, 128], BF16, f"fcy{ft}")
            for half in range(2):
                ps = K.ps()
                for q in range(4):
                    kt = half * 4 + q
                    n = 0
                    for tt in range(2):
                        for cs_i in range(2):
                            K.op("pe", lambda e, kt=kt, q=q, tt=tt, cs_i=cs_i, n=n: e.matmul(
                                ps[:, q * 128:(q + 1) * 128], lhsT=Zb[tt][:, cs_i, kt * 128:(kt + 1) * 128],
                                rhs=c256[:, cs_i, tt, ft * 128:(ft + 1) * 128], start=(n == 0), stop=(n == 3)),
                                reads=[Zb[tt], c256], writes=[ps])
                            n += 1
                K.op("act", lambda e, half=half: e.copy(out=yt[:, half * 4:(half + 1) * 4, :].rearrange("p a b -> p (a b)"),
                                                        in_=ps[:, :]), reads=[ps], writes=[yt])
            xt, ht = xts[ft], hts[ft]
            for hh in range(2):
                py = K.ps()
                for kt in range(8):
                    K.op("pe", lambda e, kt=kt: e.matmul(py[:, :], lhsT=yt[:, kt, :],
                                                         rhs=wf[:, kt, hh * 512:(hh + 1) * 512], start=(kt == 0),
                                                         stop=(kt == 7)), reads=[yt, wf], writes=[py])
                K.op("dve", lambda e: e.tensor_tensor(out=ht[:, hh * 512:(hh + 1) * 512], in0=py[:, :],
                                                      in1=self.Mc[:, 2, hh * 512:(hh + 1) * 512], op=ALU.mult),
                     reads=[py, self.Mc], writes=[ht])
            K.op("pool", lambda e: e.tensor_tensor(out=xt[:], in0=xt[:], in1=ht[:], op=ALU.add),
                 reads=[xt, ht], writes=[xt])
            K.dma("act", self.lat[T + ft * 128:T + (ft + 1) * 128, :], xt[:], reads=[xt], writes=ctxp)
    K.pop_scope()


Prog.fourier_declare = _fourier_declare
Prog.fourier = _fourier


POOL_WINS = (2, 4, 8, 16)


def pool_consts():
    c = {}
    for nm, L in (("f_icnt_lat", T), ("f_icnt_ctx", CT)):
        a = np.zeros((4, L), np.float32)
        pos = np.arange(L)
        for gi, win in enumerate(POOL_WINS):
            half = win // 2
            hi = np.minimum(pos + half, L)
            lo = np.maximum(pos - half, 0)
            a[gi] = 1.0 / (hi - lo)
        c[nm] = a
    return c


def _even_declare(self):
    nc = self.K.nc
    self.inp["f_icnt_lat"] = nc.dram_tensor("f_icnt_lat", [4, T], F32, kind="ExternalInput")
    self.inp["f_icnt_ctx"] = nc.dram_tensor("f_icnt_ctx", [4, CT], F32, kind="ExternalInput")
    e = {}
    e["HT"] = Buf(nc.dram_tensor("HT", [D, NT], F32), "HT")
    e["PT"] = Buf(nc.dram_tensor("PT", [2048, NT], F32), "PT")
    for d in range(2):
        for nm in ("At", "Bt", "Kt", "Rt", "Bh", "Kh"):
            e[f"{nm}{d}"] = Buf(nc.dram_tensor(f"SC_{nm}{d}", [512, NT], F32), f"{nm}{d}")
        e[f"GL{d}"] = Buf(nc.dram_tensor(f"SC_GL{d}", [512, NT // 64], F32), f"GL{d}")
        e[f"YD{d}"] = Buf(nc.dram_tensor(f"SC_YD{d}", [NT, 512], F32), f"YD{d}")
    for nm in ("VV", "GG", "BON", "YB"):
        e[nm] = Buf(nc.dram_tensor(f"SC_{nm}", [512, NT], F32), nm)
    self.ed = e


def _even_e1(self, i, ntile=66):
    K = self.K
    j = i // 2
    e = self.ed
    K.push_scope()
    win = K.sb([128, 8, 2048], BF16, "win")
    stg = [K.sb([128, D], F32, f"e1stg{q}") for q in range(2)]
    n = 0
    for kt in range(8):
        for hh in range(2):
            sg = stg[n % 2]
            n += 1
            K.dma("sp", sg[:], self.inp["w_in"][j, kt * 128:(kt + 1) * 128, hh * 1024:(hh + 1) * 1024], writes=[sg])
            K.op("dve" if n % 2 else "act",
                 lambda en, sg=sg, kt=kt, hh=hh: (en.tensor_copy if n % 2 else en.copy)(
                     out=win[:, kt, hh * 1024:(hh + 1) * 1024], in_=sg[:]), reads=[sg], writes=[win])
    xts = [K.sb([128, D], F32, f"e1x{q}") for q in range(2)]
    hts = [K.sb([128, D], F32, f"e1h{q}") for q in range(2)]
    sts = [K.sb([128, 4], F32, f"e1s{q}") for q in range(2)]
    hTf = [K.sb([128, 8, 128], F32, f"e1hTf{q}") for q in range(2)]
    hTb = [K.sb([128, 8, 128], BF16, f"e1hTb{q}") for q in range(2)]
    pts = [K.sb([128, 16, 128], F32, f"e1pt{q}") for q in range(2)]
    for tt in range(ntile):
        xt, ht, st, hf, hb, pt = (l[tt % 2] for l in (xts, hts, sts, hTf, hTb, pts))
        M = self.Ml if tt < 64 else self.Mc
        K.dma("sp", xt[:], self.lat[tt * 128:(tt + 1) * 128, :], reads=[self.lat.p(tt)], writes=[xt])
        self.norm_tile(xt, ht, M, 0, st)
        for half in range(2):
            ps = K.ps()
            for q in range(4):
                kt = half * 4 + q
                self.tr(None, ps[:, q * 128:(q + 1) * 128], ht[:, kt * 128:(kt + 1) * 128], ps, ht)
            K.op("act", lambda en, ps=ps, half=half: en.copy(
                out=hf[:, half * 4:(half + 1) * 4, :].rearrange("p a b -> p (a b)"), in_=ps[:, :]),
                reads=[ps], writes=[hf])
            K.op("dve", lambda en, half=half: en.tensor_copy(
                out=hb[:, half * 4:(half + 1) * 4, :], in_=hf[:, half * 4:(half + 1) * 4, :]),
                reads=[hf], writes=[hb])
        import os
        if not os.environ.get("SKIPHT"):
            K.dma("act", e["HT"][:, tt * 128:(tt + 1) * 128].rearrange("(kt k) t -> k kt t", k=128), hf[:],
                  reads=[hf], writes=[e["HT"]])
        for ob in range(4):
            ps = K.ps()
            for q in range(4):
                oc = ob * 4 + q
                for kt in range(8):
                    K.op("pe", lambda en, ps=ps, q=q, oc=oc, kt=kt: en.matmul(
                        ps[:, q * 128:(q + 1) * 128], lhsT=win[:, kt, oc * 128:(oc + 1) * 128], rhs=hb[:, kt, :],
                        start=(kt == 0), stop=(kt == 7)), reads=[win, hb], writes=[ps])
            K.op("act" if ob % 2 else "dve", lambda en, ps=ps, ob=ob: (en.copy if ob % 2 else en.tensor_copy)(
                out=pt[:, ob * 4:(ob + 1) * 4, :].rearrange("p a b -> p (a b)"), in_=ps[:, :]),
                reads=[ps], writes=[pt])
        if not os.environ.get("SKIPPT"):
            K.dma("act", e["PT"][:, tt * 128:(tt + 1) * 128].rearrange("(oc k) t -> k oc t", k=128), pt[:],
                  reads=[pt], writes=[e["PT"]])
    K.pop_scope()


def _even_e2(self, i, dbg=None):
    K = self.K
    j = i // 2
    e = self.ed
    K.push_scope()
    WB = 512
    WT = WB + 128
    eng_rr = [0]

    def ve():
        eng_rr[0] += 1
        return "dve" if eng_rr[0] % 2 else "pool"

    stg = K.sb([128, 8, 128], F32, "e2stg")
    W1 = K.sb([128, 3, 8, 128], BF16, "e2W1")
    W2 = K.sb([128, 3, 512], BF16, "e2W2")
    pw = K.sb([128, 4, 128], BF16, "e2pw")
    for v, (nm, nd) in enumerate((("decay_w1", 2), ("lr_a1", 2), ("gate_g1", 1))):
        for d in range(nd):
            src = self.inp[nm][j, d] if nd == 2 else self.inp[nm][j]
            wcol = 64 if nd == 2 else 128
            K.dma("sp", stg[:, :, 0:wcol], src.rearrange("(kt k) r -> k kt r", k=128), writes=[stg])
            K.op("dve", lambda en, v=v, d=d, wcol=wcol: en.tensor_copy(out=W1[:, v, :, d * wcol:(d + 1) * wcol],
                                                                       in_=stg[:, :, 0:wcol]), reads=[stg], writes=[W1])
    stg2 = stg[:].rearrange("p a b -> p (a b)")
    for v, nm in enumerate(("decay_w2", "lr_a2")):
        for d in range(2):
            K.dma("sp", stg2[d * 64:(d + 1) * 64, 0:512], self.inp[nm][j, d], writes=[stg])
        K.op("dve", lambda en, v=v: en.tensor_copy(out=W2[:, v, :], in_=stg2[:, 0:512]), reads=[stg], writes=[W2])
    K.dma("sp", stg2[:, 0:512], self.inp["gate_g2"][j], writes=[stg])
    K.op("dve", lambda en: en.tensor_copy(out=W2[:, 2, :], in_=stg2[:, 0:512]), reads=[stg], writes=[W2])
    for gi in range(4):
        K.dma("sp", stg2[:, gi * 128:(gi + 1) * 128], self.inp["pool_w"][j, gi], writes=[stg])
    K.op("dve", lambda en: en.tensor_copy(out=pw[:].rearrange("p a b -> p (a b)"), in_=stg2[:, 0:512]),
         reads=[stg], writes=[pw])
    MU = K.sb([128, 2, 3, 8], F32, "e2MU")
    MUP = K.sb([128, 2, 3, 4], F32, "e2MUP")
    COL = K.sb([128, 12, 4], F32, "e2COL")
    K.dma("sp", MU[:, 0, :, :], self.inp["mu_x"][j].rearrange("v (kt k) -> k v kt", k=128), writes=[MU],
          allow_slow_non_contiguous=True)
    K.dma("sp", MUP[:, 0, :, :], self.inp["mu_p"][j].rearrange("v (c k) -> k v c", k=128), writes=[MUP],
          allow_slow_non_contiguous=True)
    for d in range(2):
        K.dma("sp", COL[:, d, :], self.inp["decay_w0"][j, d].rearrange("(c k) -> k c", k=128), writes=[COL],
              allow_slow_non_contiguous=True)
        K.dma("sp", COL[:, 2 + d, :], self.inp["lr_a0"][j, d].rearrange("(c k) -> k c", k=128), writes=[COL],
              allow_slow_non_contiguous=True)
    K.dma("sp", COL[:, 4, :], self.inp["k_k"][j].rearrange("(c k) -> k c", k=128), writes=[COL], allow_slow_non_contiguous=True)
    K.dma("sp", COL[:, 5, :], self.inp["k_a"][j].rearrange("(c k) -> k c", k=128), writes=[COL], allow_slow_non_contiguous=True)
    K.dma("sp", COL[:, 6, :], self.inp["r_k"][j].rearrange("(c h2) k -> (h2 k) c", h2=2), writes=[COL],
          allow_slow_non_contiguous=True)
    K.dma("sp", COL[:, 7, :], self.inp["pool_scale"][j].rearrange("(c k) -> k c", k=128), writes=[COL],
          allow_slow_non_contiguous=True)
    for Mx in (MU, MUP):
        K.op("dve", lambda en, Mx=Mx: en.tensor_scalar(out=Mx[:, 1], in0=Mx[:, 0], scalar1=-1.0, scalar2=1.0,
                                                       op0=ALU.mult, op1=ALU.add), reads=[Mx], writes=[Mx])
    bones = K.sb([128, 128], F32, "e2bones")
    K.op("pool", lambda en: en.memset(bones[:], 0.0), writes=[bones])
    K.op("pool", lambda en: en.memset(bones[0:64, 0:64], 1.0), reads=[bones], writes=[bones])
    K.op("pool", lambda en: en.memset(bones[64:128, 64:128], 1.0), reads=[bones], writes=[bones])
    HTt = K.sb([128, 8, WT], F32, "e2HTt")
    xv = K.sb([128, 8, WB], BF16, "e2xv")
    t1 = [K.sb([128, WB], BF16, f"e2t1{v}") for v in range(3)]
    PTt = [K.sb([128, WT], F32, f"e2PTt{n}") for n in range(4)]
    nm_w = ("Rm", "Km", "Vm", "kk", "sq", "rn", "LW0", "LW1", "AD0", "AD1", "Gm", "kd", "b", "lw", "P0", "P1", "E",
            "tmp", "ks", "IC", "o0", "o1", "o2", "o3")
    w = {n: K.sb([128, WB], F32, "e2" + n) for n in nm_w}
    sw = [K.sb([128, WT], F32, f"e2s{q}") for q in range(2)]
    dfb = K.sb([128, WB], BF16, "e2dfb")
    gl = K.sb([128, 8], F32, "e2gl")
    orr = [0]

    def otile():
        orr[0] += 1
        return w[f"o{orr[0] % 4}"]

    GRID_H = [(-1, True)] * 2 + [(1, True)] * 2 + [(-64, False)] * 2 + [(64, False)] * 2
    GRID_P = [(-1, True), (1, True), (-64, False), (64, False)]
    SEQ_H = [(-1, False)] * 4 + [(1, False)] * 4
    SEQ_P = [(-1, False)] * 2 + [(1, False)] * 2
    blocks = [(T, CT, T, CT, SEQ_H, SEQ_P, "f_icnt_ctx")] + \
             [(t0, WB, 0, T, GRID_H, GRID_P, "f_icnt_lat") for t0 in range(0, T, WB)]

    def load_halo(buf, view, src_rows, t0, Wb, s0, sl):
        lo = max(t0 - 64, s0)
        hi = min(t0 + Wb + 64, s0 + sl)
        if lo > t0 - 64:
            K.op("pool", lambda en: en.memset(view(0, 64), 0.0), writes=[buf])
        if hi < t0 + Wb + 64:
            K.op("pool", lambda en: en.memset(view(64 + Wb, 128 + Wb), 0.0), writes=[buf])
        K.dma("sp", view(lo - (t0 - 64), hi - (t0 - 64)), src_rows(lo, hi), reads=[e["HT"], e["PT"]], writes=[buf])

    def mix(out_ap, out_buf, srcf, src_buf, mu_ap, omu_ap, delta, rowmask, Wb):
        en1 = "dve"
        K.op("pool", lambda en: en.tensor_scalar(out=out_ap, in0=srcf(64, 64 + Wb), scalar1=omu_ap, scalar2=None,
                                              op0=ALU.mult), reads=[src_buf], writes=[out_buf])
        if not rowmask:
            o, s_ = out_ap, srcf(64 + delta, 64 + delta + Wb)
        else:
            ov = out_ap.rearrange("p (r c) -> p r c", c=64)
            sv = srcf(64 + delta, 64 + delta + Wb).rearrange("p (r c) -> p r c", c=64)
            if delta == -1:
                o, s_ = ov[:, :, 1:64], sv[:, :, 1:64]
            else:
                o, s_ = ov[:, :, 0:63], sv[:, :, 0:63]
        K.op(en1, lambda en: en.scalar_tensor_tensor(out=o, in0=s_, scalar=mu_ap, in1=o, op0=ALU.mult, op1=ALU.add),
             reads=[src_buf, out_buf], writes=[out_buf])

    def store(dst, c, t0, Wb, tile):
        K.dma("sp", dst[c * 128:(c + 1) * 128, t0:t0 + Wb], tile[:, 0:Wb], reads=[tile], writes=[dst])

    for (t0, Wb, s0, sl, HS, PS, icn) in blocks:
        R = Wb // 64
        load_halo(HTt, lambda a, b: HTt[:, :, a:b],
                  lambda a, b: e["HT"][:, a:b].rearrange("(kt k) t -> k kt t", k=128), t0, Wb, s0, sl)
        for v in range(3):
            for kt in range(8):
                dl, rm = HS[kt]
                mix(xv[:, kt, 0:Wb], xv, lambda a, b, kt=kt: HTt[:, kt, a:b], HTt, MU[:, 0, v, kt:kt + 1],
                    MU[:, 1, v, kt:kt + 1], dl, rm, Wb)
            ps = K.ps()
            for kt in range(8):
                K.op("pe", lambda en, kt=kt, v=v: en.matmul(ps[:, 0:Wb], lhsT=W1[:, v, kt, :], rhs=xv[:, kt, 0:Wb],
                                                            start=(kt == 0), stop=(kt == 7)), reads=[W1, xv], writes=[ps])
            fn = (AF.Tanh, AF.Copy, AF.Sigmoid)[v]
            K.op("act", lambda en, v=v, fn=fn: en.activation(out=t1[v][:, 0:Wb], in_=ps[:, 0:Wb], func=fn),
                 reads=[ps], writes=[t1[v]])
        for c in range(4):
            cs_ = slice(c * 128, (c + 1) * 128)
            for n in range(4):
                load_halo(PTt[n], lambda a, b, n=n: PTt[n][:, a:b],
                          lambda a, b, n=n: e["PT"][n * 512 + c * 128:n * 512 + (c + 1) * 128, a:b], t0, Wb, s0, sl)
            dl, rm = PS[c]
            for n, nm in enumerate(("Rm", "Km", "Vm")):
                mix(w[nm][:, 0:Wb], w[nm], lambda a, b, n=n: PTt[n][:, a:b], PTt[n], MUP[:, 0, n, c:c + 1],
                    MUP[:, 1, n, c:c + 1], dl, rm, Wb)
            store(e["VV"], c, t0, Wb, w["Vm"])
            for d in range(2):
                ps = K.ps()
                K.op("pe", lambda en, d=d: en.matmul(ps[:, 0:Wb], lhsT=W2[d * 64:(d + 1) * 64, 0, cs_],
                                                     rhs=t1[0][d * 64:(d + 1) * 64, 0:Wb], start=True, stop=True),
                     reads=[W2, t1[0]], writes=[ps])
                K.op("act", lambda en, d=d: en.activation(out=w[f"LW{d}"][:, 0:Wb], in_=ps[:, 0:Wb], func=AF.Sigmoid,
                                                          bias=COL[:, d, c:c + 1], scale=1.0),
                     reads=[ps, COL], writes=[w[f"LW{d}"]])
                ps = K.ps()
                K.op("pe", lambda en, d=d: en.matmul(ps[:, 0:Wb], lhsT=W2[d * 64:(d + 1) * 64, 1, cs_],
                                                     rhs=t1[1][d * 64:(d + 1) * 64, 0:Wb], start=True, stop=True),
                     reads=[W2, t1[1]], writes=[ps])
                K.op("act", lambda en, d=d: en.activation(out=w[f"AD{d}"][:, 0:Wb], in_=ps[:, 0:Wb], func=AF.Sigmoid,
                                                          bias=COL[:, 2 + d, c:c + 1], scale=1.0),
                     reads=[ps, COL], writes=[w[f"AD{d}"]])
            ps = K.ps()
            K.op("pe", lambda en: en.matmul(ps[:, 0:Wb], lhsT=W2[:, 2, cs_], rhs=t1[2][:, 0:Wb], start=True, stop=True),
                 reads=[W2, t1[2]], writes=[ps])
            K.op("act", lambda en: en.copy(out=w["Gm"][:, 0:Wb], in_=ps[:, 0:Wb]), reads=[ps], writes=[w["Gm"]])
            store(e["GG"], c, t0, Wb, w["Gm"])
            K.op("dve", lambda en: en.tensor_scalar(out=w["kk"][:, 0:Wb], in0=w["Km"][:, 0:Wb], scalar1=COL[:, 4, c:c + 1],
                                                    scalar2=None, op0=ALU.mult), reads=[w["Km"], COL], writes=[w["kk"]])
            K.op("pool", lambda en: en.tensor_tensor(out=w["sq"][:, 0:Wb], in0=w["kk"][:, 0:Wb], in1=w["kk"][:, 0:Wb],
                                                     op=ALU.mult), reads=[w["kk"]], writes=[w["sq"]])
            ps = K.ps()
            K.op("pe", lambda en: en.matmul(ps[:, 0:Wb], lhsT=bones[:], rhs=w["sq"][:, 0:Wb], start=True, stop=True),
                 reads=[bones, w["sq"]], writes=[ps])
            K.op("dve", lambda en: en.tensor_scalar(out=w["rn"][:, 0:Wb], in0=ps[:, 0:Wb], scalar1=1e-12, scalar2=None,
                                                    op0=ALU.max), reads=[ps], writes=[w["rn"]])
            K.op("act", lambda en: en.sqrt(out=w["rn"][:, 0:Wb], in_=w["rn"][:, 0:Wb]), reads=[w["rn"]], writes=[w["rn"]])
            K.op("dve", lambda en: en.reciprocal(out=w["rn"][:, 0:Wb], in_=w["rn"][:, 0:Wb]), reads=[w["rn"]],
                 writes=[w["rn"]])
            K.op("dve", lambda en: en.tensor_tensor(out=w["kk"][:, 0:Wb], in0=w["kk"][:, 0:Wb], in1=w["rn"][:, 0:Wb],
                                                    op=ALU.mult), reads=[w["kk"], w["rn"]], writes=[w["kk"]])
            for d in range(2):
                AD, LW = w[f"AD{d}"], w[f"LW{d}"]
                K.op("dve", lambda en: en.tensor_scalar(out=w["tmp"][:, 0:Wb], in0=AD[:, 0:Wb], scalar1=-1.0,
                                                        scalar2=COL[:, 5, c:c + 1], op0=ALU.add, op1=ALU.mult),
                     reads=[AD, COL], writes=[w["tmp"]])
                K.op("dve", lambda en: en.scalar_tensor_tensor(out=w["kd"][:, 0:Wb], in0=w["tmp"][:, 0:Wb], scalar=1.0,
                                                               in1=w["Km"][:, 0:Wb], op0=ALU.add, op1=ALU.mult),
                     reads=[w["tmp"], w["Km"]], writes=[w["kd"]])
                if d == 0:
                    K.op("pool", lambda en: en.tensor_copy(out=w["ks"][:, 0:Wb], in_=w["kd"][:, 0:Wb]),
                         reads=[w["kd"]], writes=[w["ks"]])
                else:
                    K.op("pool", lambda en: en.tensor_tensor(out=w["ks"][:, 0:Wb], in0=w["ks"][:, 0:Wb],
                                                             in1=w["kd"][:, 0:Wb], op=ALU.add),
                         reads=[w["kd"], w["ks"]], writes=[w["ks"]])
                K.op("pool", lambda en: en.tensor_tensor(out=w["b"][:, 0:Wb], in0=w["kk"][:, 0:Wb], in1=AD[:, 0:Wb],
                                                         op=ALU.mult), reads=[w["kk"], AD], writes=[w["b"]])
                K.op("dve", lambda en: en.tensor_scalar(out=w["lw"][:, 0:Wb], in0=LW[:, 0:Wb], scalar1=-0.6065306597126334,
                                                        scalar2=None, op0=ALU.mult), reads=[LW], writes=[w["lw"]])
                cur = w["lw"]
                pp = [w["P0"], w["P1"]]
                for si, sft in enumerate((1, 2, 4, 8, 16, 32)):
                    nxt = pp[si % 2]
                    cv_ = cur[:, 0:Wb].rearrange("p (r c) -> p r c", c=64)
                    nv = nxt[:, 0:Wb].rearrange("p (r c) -> p r c", c=64)
                    if d == 0:
                        K.op("dve", lambda en: en.tensor_tensor(out=nv[:, :, sft:64], in0=cv_[:, :, sft:64],
                                                                in1=cv_[:, :, 0:64 - sft], op=ALU.add),
                             reads=[cur], writes=[nxt])
                        K.op("pool", lambda en: en.tensor_copy(out=nv[:, :, 0:sft], in_=cv_[:, :, 0:sft]),
                             reads=[cur, nxt], writes=[nxt])
                    else:
                        K.op("dve", lambda en: en.tensor_tensor(out=nv[:, :, 0:64 - sft], in0=cv_[:, :, 0:64 - sft],
                                                                in1=cv_[:, :, sft:64], op=ALU.add),
                             reads=[cur], writes=[nxt])
                        K.op("pool", lambda en: en.tensor_copy(out=nv[:, :, 64 - sft:64], in_=cv_[:, :, 64 - sft:64]),
                             reads=[cur, nxt], writes=[nxt])
                    cur = nxt
                cum = cur
                cumv = cum[:, 0:Wb].rearrange("p (r c) -> p r c", c=64)
                last = 63 if d == 0 else 0
                cL = cumv[:, :, last]
                K.op("act", lambda en: en.activation(out=gl[:, 0:R], in_=cL, func=AF.Exp), reads=[cum], writes=[gl])
                K.dma("act", e[f"GL{d}"][c * 128:(c + 1) * 128, t0 // 64:t0 // 64 + R], gl[:, 0:R], reads=[gl],
                      writes=[e[f"GL{d}"]])
                K.op("act", lambda en: en.activation(out=w["E"][:, 0:Wb], in_=cum[:, 0:Wb], func=AF.Exp), reads=[cum],
                     writes=[w["E"]])
                o = otile()
                K.op("dve", lambda en: en.tensor_tensor(out=o[:, 0:Wb], in0=w["Rm"][:, 0:Wb], in1=w["E"][:, 0:Wb],
                                                        op=ALU.mult), reads=[w["Rm"], w["E"]], writes=[o])
                store(e[f"Rt{d}"], c, t0, Wb, o)
                K.op("act", lambda en: en.activation(out=w["E"][:, 0:Wb], in_=cum[:, 0:Wb], func=AF.Exp, scale=-1.0),
                     reads=[cum], writes=[w["E"]])
                for src_, dn in (("b", "Bt"), ("kd", "Kt")):
                    o = otile()
                    K.op(ve(), lambda en, o=o, src_=src_: en.tensor_tensor(out=o[:, 0:Wb], in0=w[src_][:, 0:Wb],
                                                                           in1=w["E"][:, 0:Wb], op=ALU.mult),
                         reads=[w[src_], w["E"]], writes=[o])
                    store(e[f"{dn}{d}"], c, t0, Wb, o)
                K.op("pool", lambda en: en.tensor_tensor(out=w["tmp"][:, 0:Wb], in0=cum[:, 0:Wb], in1=w["lw"][:, 0:Wb],
                                                         op=ALU.subtract), reads=[cum, w["lw"]], writes=[w["tmp"]])
                K.op("act", lambda en: en.activation(out=w["E"][:, 0:Wb], in_=w["tmp"][:, 0:Wb], func=AF.Exp),
                     reads=[w["tmp"]], writes=[w["E"]])
                o = otile()
                K.op("dve", lambda en: en.scalar_tensor_tensor(out=o[:, 0:Wb], in0=w["kk"][:, 0:Wb], scalar=-1.0,
                                                               in1=w["E"][:, 0:Wb], op0=ALU.mult, op1=ALU.mult),
                     reads=[w["kk"], w["E"]], writes=[o])
                store(e[f"At{d}"], c, t0, Wb, o)
                tv = w["tmp"][:, 0:Wb].rearrange("p (r c) -> p r c", c=64)
                K.op("dve", lambda en: en.tensor_tensor(out=tv, in0=cumv, in1=cL.unsqueeze(2).to_broadcast([128, R, 64]),
                                                        op=ALU.subtract), reads=[cum], writes=[w["tmp"]])
                K.op("act", lambda en: en.activation(out=w["E"][:, 0:Wb], in_=w["tmp"][:, 0:Wb], func=AF.Exp, scale=-1.0),
                     reads=[w["tmp"]], writes=[w["E"]])
                for src_, dn in (("b", "Bh"), ("kd", "Kh")):
                    o = otile()
                    K.op(ve(), lambda en, o=o, src_=src_: en.tensor_tensor(out=o[:, 0:Wb], in0=w[src_][:, 0:Wb],
                                                                           in1=w["E"][:, 0:Wb], op=ALU.mult),
                         reads=[w[src_], w["E"]], writes=[o])
                    store(e[f"{dn}{d}"], c, t0, Wb, o)
            K.op("dve", lambda en: en.scalar_tensor_tensor(out=w["tmp"][:, 0:Wb], in0=w["ks"][:, 0:Wb],
                                                           scalar=COL[:, 6, c:c + 1], in1=w["Rm"][:, 0:Wb],
                                                           op0=ALU.mult, op1=ALU.mult),
                 reads=[w["ks"], COL, w["Rm"]], writes=[w["tmp"]])
            ps = K.ps()
            K.op("pe", lambda en: en.matmul(ps[:, 0:Wb], lhsT=bones[:], rhs=w["tmp"][:, 0:Wb], start=True, stop=True),
                 reads=[bones, w["tmp"]], writes=[ps])
            o = otile()
            K.op("dve", lambda en: en.tensor_tensor(out=o[:, 0:Wb], in0=ps[:, 0:Wb], in1=w["Vm"][:, 0:Wb], op=ALU.mult),
                 reads=[ps, w["Vm"]], writes=[o])
            store(e["BON"], c, t0, Wb, o)
            u = PTt[3]
            Wt = Wb + 128
            K.dma("sp", w["IC"][:, 0:Wb], self.inp[icn][c:c + 1, t0 - s0:t0 - s0 + Wb].partition_broadcast(128),
                  writes=[w["IC"]])
            s_a, s_b = sw
            K.op("dve", lambda en: en.tensor_tensor(out=s_a[:, 1:Wt], in0=u[:, 0:Wt - 1], in1=u[:, 1:Wt], op=ALU.add),
                 reads=[u], writes=[s_a])
            cur_s, oth = s_a, s_b
            lo_, hi_ = 1, Wt
            for hs in (1, 2, 4):
                if POOL_WINS[c] < 4 * hs:
                    break
                nlo, nhi = lo_ + hs, hi_ - hs
                K.op("dve", lambda en, cur_s=cur_s, oth=oth, nlo=nlo, nhi=nhi, hs=hs: en.tensor_tensor(
                    out=oth[:, nlo:nhi], in0=cur_s[:, nlo - hs:nhi - hs], in1=cur_s[:, nlo + hs:nhi + hs], op=ALU.add),
                    reads=[cur_s], writes=[oth])
                cur_s, oth = oth, cur_s
                lo_, hi_ = nlo, nhi
            K.op("dve", lambda en: en.tensor_tensor(out=w["tmp"][:, 0:Wb], in0=cur_s[:, 64:64 + Wb], in1=w["IC"][:, 0:Wb],
                                                    op=ALU.mult), reads=[cur_s, w["IC"]], writes=[w["tmp"]])
            K.op("pool", lambda en: en.tensor_tensor(out=dfb[:, 0:Wb], in0=w["tmp"][:, 0:Wb], in1=u[:, 64:64 + Wb],
                                                     op=ALU.subtract), reads=[w["tmp"], u], writes=[dfb])
            ps = K.ps()
            K.op("pe", lambda en: en.matmul(ps[:, 0:Wb], lhsT=pw[:, c, :], rhs=dfb[:, 0:Wb], start=True, stop=True),
                 reads=[pw, dfb], writes=[ps])
            o = otile()
            K.op("dve", lambda en: en.tensor_scalar(out=o[:, 0:Wb], in0=ps[:, 0:Wb], scalar1=COL[:, 7, c:c + 1],
                                                    scalar2=None, op0=ALU.mult), reads=[ps, COL], writes=[o])
            store(e["YB"], c, t0, Wb, o)
    K.pop_scope()


Prog.even_e2 = _even_e2
def _even_e3(self, i, SDT=F32, nsteps=66):
    K = self.K
    e = self.ed
    K.push_scope()
    TEN = ("At", "Bt", "Kt", "Rt", "Bh", "Kh", "VV")

    def ring(nm, shape, n, dt=F32):
        bufs = [K.sb(shape, dt, f"e3{nm}{q}") for q in range(n)]
        cnt = [0]

        def nxt():
            cnt[0] += 1
            return bufs[cnt[0] % n]
        return nxt

    def mk_mask(nm, cmp_op, sgn=1):
        mbuf = K.sb([128, 128], F32, "e3m" + nm)
        K.op("pool", lambda en: en.memset(mbuf[:], 1.0), writes=[mbuf])
        K.op("pool", lambda en: en.affine_select(out=mbuf[:], in_=mbuf[:], pattern=[[sgn, 128]], compare_op=cmp_op,
                                                 fill=0.0, base=0, channel_multiplier=-sgn), reads=[mbuf], writes=[mbuf])
        K.op("pool", lambda en: en.memset(mbuf[0:64, 64:128], 0.0), reads=[mbuf], writes=[mbuf])
        K.op("pool", lambda en: en.memset(mbuf[64:128, 0:64], 0.0), reads=[mbuf], writes=[mbuf])
        return mbuf
    UPs = mk_mask("ups", ALU.is_gt)
    UPi = mk_mask("upi", ALU.is_ge)
    LOs = mk_mask("los", ALU.is_gt, -1)
    LOi = mk_mask("loi", ALU.is_ge, -1)
    BDM = mk_mask("bdm", ALU.is_ge)
    K.op("pool", lambda en: en.memset(BDM[0:64, 0:64], 1.0), reads=[BDM], writes=[BDM])
    K.op("pool", lambda en: en.memset(BDM[64:128, 64:128], 1.0), reads=[BDM], writes=[BDM])
    M4 = []
    MI = []
    for d in range(2):
        strictT, strict, inclT = (UPs, LOs, UPi) if d == 0 else (LOs, UPs, LOi)
        m4 = K.sb([128, 512], F32, f"e3m4{d}")
        for q, src in enumerate((strictT, strict, strictT, inclT)):
            K.op("pool", lambda en, q=q, src=src: en.tensor_copy(out=m4[:, q * 128:(q + 1) * 128], in_=src[:]),
                 reads=[src], writes=[m4])
        M4.append(m4)
        MI.append(inclT)
    identS = self.ident
    S = [[K.sb([128, 128], F32, f"e3S{d}{c}") for c in range(4)] for d in range(2)]
    for d in range(2):
        for c in range(4):
            K.op("pool", lambda en, d=d, c=c: en.memset(S[d][c][:], 0.0), writes=[S[d][c]])
    Ssd = S
    if SDT != F32:
        Ssd = [[K.sb([128, 128], SDT, f"e3Sb{d}{c}") for c in range(4)] for d in range(2)]
        for d in range(2):
            for c in range(4):
                K.op("pool", lambda en, d=d, c=c: en.memset(Ssd[d][c][:], 0.0), writes=[Ssd[d][c]])
    LD = [[{nm: K.sb([128, 4, 128], F32, f"e3ld{d}{b}{nm}") for nm in TEN} for b in range(2)] for d in range(2)]
    GLt = [[K.sb([128, 4, 2], F32, f"e3gl{d}{b}") for b in range(2)] for d in range(2)]
    r_bd = ring("bd", [128, 7, 128], 4, SDT)
    r_tp = ring("tp", [128, 384], 3, SDT)
    r_m = ring("m", [128, 640], 3, SDT)
    r_q = ring("q", [128, 256], 4, SDT)
    r_w = ring("w", [128, 128], 4, SDT)
    r_x = ring("x", [128, 128], 4, SDT)
    r_u = ring("u", [128, 128], 4, SDT)
    r_y = ring("y", [128, 4, 64], 4, F32)
    rr = [0]

    def ve3():
        rr[0] += 1
        return ("dve", "pool")[rr[0] % 2]

    def tok_base(d, sidx):
        if d == 0:
            return T + 128 * sidx if sidx < 2 else 128 * (sidx - 2)
        return T + 128 * (1 - sidx) if sidx < 2 else 128 * (63 - (sidx - 2))

    def load(d, sidx):
        b = sidx % 2
        tb = tok_base(d, sidx)
        for nm in TEN:
            src = e[nm if nm == "VV" else f"{nm}{d}"]
            K.dma("sp", LD[d][b][nm][:], src[:, tb:tb + 128].rearrange("(c p) t -> p c t", p=128), reads=[src],
                  writes=[LD[d][b][nm]])
        K.dma("sp", GLt[d][b][:], e[f"GL{d}"][:, tb // 64:tb // 64 + 2].rearrange("(c p) t -> p c t", p=128),
              reads=[e[f"GL{d}"]], writes=[GLt[d][b]])

    def mm(ps_ap, ps, lhsT, lb, rhs, rb, start=True, stop=True):
        K.op("pe", lambda en: en.matmul(ps_ap, lhsT=lhsT, rhs=rhs, start=start, stop=stop), reads=[lb, rb], writes=[ps])

    def chunk(d, sidx, q):
        b = sidx % 2
        tb = tok_base(d, sidx) + 64 * q
        ld = LD[d][b]
        ysb = r_y()
        for c in range(4):
            bd = r_bd()
            for ti, nm in enumerate(TEN):
                src = ld[nm][:, c, q * 64:(q + 1) * 64]
                K.op(ve3(), lambda en, ti=ti, src=src: en.tensor_tensor(
                    out=bd[:, ti, :].rearrange("p (a b) -> p a b", a=2), in0=src.unsqueeze(1).to_broadcast([128, 2, 64]),
                    in1=BDM[:].rearrange("p (a b) -> p a b", a=2), op=ALU.mult), reads=[ld[nm], BDM], writes=[bd])
            A_, B_, K_, R_ = (bd[:, t_, :] for t_ in range(4))
            pst = K.ps()
            for t_ in range(3):
                K.op("pe", lambda en, t_=t_: en.transpose(out=pst[:, t_ * 128:(t_ + 1) * 128] if SDT == F32 else
                                                          pst[:].bitcast(SDT)[:, t_ * 128:(t_ + 1) * 128],
                                                          in_=bd[:, 4 + t_, :],
                                                          identity=(identS if SDT == F32 else self.identb)[:]),
                     reads=[bd, identS, self.identb], writes=[pst])
            tp = r_tp()
            K.op("act", lambda en: en.copy(out=tp[:], in_=pst[:, 0:384] if SDT == F32 else pst[:].bitcast(SDT)[:, 0:384]),
                 reads=[pst], writes=[tp])
            BhT, KhT, VT = (tp[:, t_ * 128:(t_ + 1) * 128] for t_ in range(3))
            p1 = K.ps()
            mm(p1[:, 0:128], p1, B_, bd, A_, bd)
            mm(p1[:, 128:256], p1, A_, bd, B_, bd)
            mm(p1[:, 256:384], p1, K_, bd, A_, bd)
            mm(p1[:, 384:512], p1, B_, bd, R_, bd)
            p2 = K.ps()
            mm(p2[:, 0:128], p2, K_, bd, R_, bd)
            mt = r_m()
            K.op("dve", lambda en: en.tensor_tensor(out=mt[:, 0:512], in0=p1[:, :], in1=M4[d][:], op=ALU.mult),
                 reads=[p1, M4[d]], writes=[mt])
            K.op("dve", lambda en: en.tensor_tensor(out=mt[:, 512:640], in0=p2[:, 0:128], in1=MI[d][:], op=ALU.mult),
                 reads=[p2, MI[d]], writes=[mt])
            Q, QT, MakT, MrbT, MrkT = (mt[:, t_ * 128:(t_ + 1) * 128] for t_ in range(5))
            W = r_w()
            K.op("pool", lambda en: en.tensor_tensor(out=W[:], in0=Q, in1=identS[:], op=ALU.add),
                 reads=[mt, identS], writes=[W])
            qb = mt
            for lvl in range(1, 6):
                pq = K.ps()
                mm(pq[:, 128:256], pq, Q, qb, QT, qb)
                if lvl < 5:
                    mm(pq[:, 0:128], pq, QT, qb, Q, qb)
                nq = r_q()
                if lvl < 5:
                    K.op("act", lambda en: en.copy(out=nq[:], in_=pq[:, 0:256]), reads=[pq], writes=[nq])
                else:
                    K.op("act", lambda en: en.copy(out=nq[:, 128:256], in_=pq[:, 128:256]), reads=[pq], writes=[nq])
                Q, QT, qb = nq[:, 0:128], nq[:, 128:256], nq
                pw_ = K.ps()
                mm(pw_[:, 0:128], pw_, QT, qb, W[:], W)
                W2_ = r_w()
                K.op("dve", lambda en: en.tensor_tensor(out=W2_[:], in0=pw_[:, 0:128], in1=W[:], op=ALU.add),
                     reads=[pw_, W], writes=[W2_])
                W = W2_
            Sb = Ssd[d][c]
            px = K.ps()
            mm(px[:, 0:128], px, A_, bd, Sb[:], Sb, True, False)
            mm(px[:, 0:128], px, MakT, mt, VT, tp, False, True)
            Xs = r_x()
            K.op("act", lambda en: en.copy(out=Xs[:], in_=px[:, 0:128]), reads=[px], writes=[Xs])
            pu = K.ps()
            mm(pu[:, 0:128], pu, W[:], W, Xs[:], Xs)
            Us = r_u()
            K.op("dve", lambda en: en.tensor_copy(out=Us[:], in_=pu[:, 0:128]), reads=[pu], writes=[Us])
            py = K.ps()
            mm(py[:, 0:128], py, R_, bd, Sb[:], Sb, True, False)
            mm(py[:, 0:128], py, MrbT, mt, Us[:], Us, False, False)
            mm(py[:, 0:128], py, MrkT, mt, VT, tp, False, True)
            K.op("act", lambda en: en.copy(out=ysb[0:64, c, :], in_=py[0:64, 0:64]), reads=[py], writes=[ysb])
            K.op("act", lambda en: en.copy(out=ysb[64:128, c, :], in_=py[64:128, 64:128]), reads=[py], writes=[ysb])
            pss = K.ps()
            mm(pss[:, 0:128], pss, BhT, tp, Us[:], Us, True, False)
            mm(pss[:, 0:128], pss, KhT, tp, VT, tp, False, True)
            Sf = S[d][c]
            K.op("dve", lambda en: en.scalar_tensor_tensor(out=Sf[:], in0=Sf[:], scalar=GLt[d][b][:, c, q:q + 1],
                                                           in1=pss[:, 0:128], op0=ALU.mult, op1=ALU.add),
                 reads=[Sf, GLt[d][b], pss], writes=[Sf])
            if SDT != F32:
                K.op("pool", lambda en: en.tensor_copy(out=Sb[:], in_=Sf[:]), reads=[Sf], writes=[Sb])
        dst = e[f"YD{d}"][tb:tb + 64, :].rearrange("t (c h v) -> h t c v", h=2, v=64)
        for h2 in range(2):
            K.dma("sp", dst[h2], ysb[h2 * 64:(h2 + 1) * 64, :, :], reads=[ysb], writes=[e[f"YD{d}"]])

    for d in range(2):
        load(d, 0)
    for sidx in range(nsteps):
        for d in range(2):
            if sidx + 1 < nsteps:
                load(d, sidx + 1)
        for qq in range(2):
            for d in range(2):
                chunk(d, sidx, qq if d == 0 else 1 - qq)
    if self.dbg.get("S") is not None:
        for d in range(2):
            for c in range(4):
                K.dma("sp", self.dbg["S"][d, c], S[d][c][:], reads=[S[d][c]])
    K.pop_scope()


Prog.even_e3 = _even_e3
def _even_e4(self, i, ntile):
    K = self.K
    j = i // 2
    e = self.ed
    K.push_scope()
    wout = K.sb([128, 8, D], BF16, "e4wout")
    stg = [K.sb([128, D], F32, f"e4stg{q}") for q in range(2)]
    for kt in range(8):
        sg = stg[kt % 2]
        K.dma("sp", sg[:], self.inp["w_out"][j, kt * 128:(kt + 1) * 128, :], writes=[sg])
        K.op("dve", lambda en, sg=sg, kt=kt: en.tensor_copy(out=wout[:, kt, :], in_=sg[:]), reads=[sg], writes=[wout])
    GN = K.sb([128, 2, 4], F32, "e4gn")
    K.dma("sp", GN[:, 0, :], self.inp["gn_w"][j].rearrange("(c k) -> k c", k=128), writes=[GN], allow_slow_non_contiguous=True)
    K.dma("sp", GN[:, 1, :], self.inp["gn_b"][j].rearrange("(c k) -> k c", k=128), writes=[GN], allow_slow_non_contiguous=True)
    y0s = [K.sb([128, 512], F32, f"e4y0{q}") for q in range(2)]
    y1s = [K.sb([128, 512], F32, f"e4y1{q}") for q in range(2)]
    sqs = [K.sb([128, 512], F32, f"e4sq{q}") for q in range(2)]
    sts = [K.sb([128, 4, 8], F32, f"e4st{q}") for q in range(2)]
    bons = [K.sb([128, 4, 128], F32, f"e4bon{q}") for q in range(2)]
    ggs = [K.sb([128, 4, 128], F32, f"e4gg{q}") for q in range(2)]
    ybs = [K.sb([128, 4, 128], F32, f"e4yb{q}") for q in range(2)]
    yTs = [K.sb([128, 4, 128], F32, f"e4yT{q}") for q in range(2)]
    cats = [K.sb([128, 8, 128], BF16, f"e4cat{q}") for q in range(2)]
    xts = stg
    ots = [K.sb([128, D], F32, f"e4o{q}") for q in range(2)]
    for tt in range(ntile):
        y0, y1, sq, st, bon, gg, yb, yT, cat, xt, ot = (l[tt % 2] for l in (y0s, y1s, sqs, sts, bons, ggs, ybs, yTs, cats,
                                                                            xts, ots))
        M = self.Ml if tt < 64 else self.Mc
        tk = slice(tt * 128, (tt + 1) * 128)
        K.dma("sp", y0[:], e["YD0"][tk, :], reads=[e["YD0"]], writes=[y0])
        K.dma("sp", y1[:], e["YD1"][tk, :], reads=[e["YD1"]], writes=[y1])
        for buf, nm in ((bon, "BON"), (gg, "GG"), (yb, "YB")):
            K.dma("sp", buf[:], e[nm][:, tk].rearrange("(c p) t -> p c t", p=128), reads=[e[nm]], writes=[buf])
        K.dma("sp", xt[:], self.lat[tk, :], reads=[self.lat.p(tt)], writes=[xt])
        K.op("pool", lambda en: en.tensor_tensor(out=y0[:], in0=y0[:], in1=y1[:], op=ALU.add), reads=[y0, y1], writes=[y0])
        yv = y0[:].rearrange("p (h v) -> p h v", v=64)
        sv = sq[:].rearrange("p (h v) -> p h v", v=64)
        K.op("dve", lambda en: en.reduce_sum(out=st[:, 0, :], in_=yv, axis=AX.X), reads=[y0], writes=[st])
        K.op("dve", lambda en: en.tensor_scalar(out=st[:, 1, :], in0=st[:, 0, :], scalar1=1.0 / 64, scalar2=None,
                                                op0=ALU.mult), reads=[st], writes=[st])
        K.op("dve", lambda en: en.tensor_tensor(out=yv, in0=yv, in1=st[:, 1, :].unsqueeze(2).to_broadcast([128, 8, 64]),
                                                op=ALU.subtract), reads=[y0, st], writes=[y0])
        K.op("pool", lambda en: en.tensor_tensor(out=sq[:], in0=y0[:], in1=y0[:], op=ALU.mult), reads=[y0], writes=[sq])
        K.op("dve", lambda en: en.reduce_sum(out=st[:, 2, :], in_=sv, axis=AX.X), reads=[sq], writes=[st])
        K.op("dve", lambda en: en.tensor_scalar(out=st[:, 2, :], in0=st[:, 2, :], scalar1=1.0 / 64, scalar2=64e-5,
                                                op0=ALU.mult, op1=ALU.add), reads=[st], writes=[st])
        K.op("act", lambda en: en.sqrt(out=st[:, 3, :], in_=st[:, 2, :]), reads=[st], writes=[st])
        K.op("dve", lambda en: en.reciprocal(out=st[:, 3, :], in_=st[:, 3, :]), reads=[st], writes=[st])
        K.op("dve", lambda en: en.tensor_tensor(out=yv, in0=yv, in1=st[:, 3, :].unsqueeze(2).to_broadcast([128, 8, 64]),
                                                op=ALU.mult), reads=[y0, st], writes=[y0])
        ps = K.ps()
        for c in range(4):
            self.tr(None, ps[:, c * 128:(c + 1) * 128], y0[:, c * 128:(c + 1) * 128], ps, y0)
        for c in range(4):
            K.op("dve", lambda en, c=c: en.tensor_scalar(out=yT[:, c, :], in0=ps[:, c * 128:(c + 1) * 128],
                                                         scalar1=GN[:, 0, c:c + 1], scalar2=GN[:, 1, c:c + 1],
                                                         op0=ALU.mult, op1=ALU.add), reads=[ps, GN], writes=[yT])
        K.op("pool", lambda en: en.tensor_tensor(out=yT[:], in0=yT[:], in1=bon[:], op=ALU.add), reads=[yT, bon],
             writes=[yT])
        K.op("pool", lambda en: en.tensor_tensor(out=cat[:, 0:4, :], in0=yT[:], in1=gg[:], op=ALU.mult), reads=[yT, gg],
             writes=[cat])
        K.op("act", lambda en: en.copy(out=cat[:, 4:8, :], in_=yb[:]), reads=[yb], writes=[cat])
        for hh in range(2):
            po = K.ps()
            for kt in range(8):
                K.op("pe", lambda en, kt=kt: en.matmul(po[:, :], lhsT=cat[:, kt, :], rhs=wout[:, kt, hh * 512:(hh + 1) * 512],
                                                       start=(kt == 0), stop=(kt == 7)), reads=[cat, wout], writes=[po])
            K.op("dve", lambda en: en.tensor_tensor(out=ot[:, hh * 512:(hh + 1) * 512], in0=po[:, :],
                                                    in1=M[:, 2, hh * 512:(hh + 1) * 512], op=ALU.mult),
                 reads=[po, M], writes=[ot])
        K.op("pool", lambda en: en.tensor_tensor(out=ot[:], in0=ot[:], in1=xt[:], op=ALU.add), reads=[ot, xt], writes=[ot])
        K.dma("sp", self.lat[tk, :], ot[:], reads=[ot], writes=[self.lat.p(tt)])
    K.pop_scope()


def _even_mixer(self, i):
    self.even_e1(i, ntile=66)
    self.even_e2(i)
    self.even_e3(i)
    self.even_e4(i, ntile=66 if i < 2 else 64)


Prog.even_e4 = _even_e4
Prog.even_mixer = _even_mixer
Prog.even_declare = _even_declare
Prog.even_e1 = _even_e1


def build_program():
    P = Prog()
    P.fourier_declare()
    P.even_declare()
    P.init_lat()
    P.prep_s()
    for i in range(DEPTH):
        P.modvec(i, need_ctx=(i <= 2))
        if i % 2 == 0:
            P.even_mixer(i)
        else:
            P.fourier(i, with_ctx=(i < 2))
        P.moe_setup()
        P.moe(i, with_ctx=(i < 2))
    P.final_norm()
    P.K.finish()
    return P


def kernel(**inputs):
    x = np.asarray(inputs["x"], dtype=np.float32)
    B = x.shape[0]
    P = build_program()
    consts = fourier_consts()
    pconsts = pool_consts()
    in_maps = []
    for b in range(B):
        d = {"x": np.ascontiguousarray(x[b]),
             "c": np.ascontiguousarray(np.asarray(inputs["c"], dtype=np.float32)[b:b + 1]),
             "ctx": np.ascontiguousarray(np.asarray(inputs["ctx"], dtype=np.float32)[b]),
             "c_ctx": np.ascontiguousarray(np.asarray(inputs["c_ctx"], dtype=np.float32)[None, :])}
        for k in WEIGHT_SPECS:
            d[k] = np.ascontiguousarray(np.asarray(inputs[k], dtype=np.float32))
        d.update(consts)
        d.update(pconsts)
        in_maps.append(d)
    res = run_bass_kernel_spmd(P.K.nc, in_maps, core_ids=list(range(B)))
    return np.stack([np.asarray(r["out"]) for r in res.results], axis=0).astype(np.float32)
```

```python
import numpy as np
from contextlib import ExitStack
import concourse.bass as bass
import concourse.mybir as mybir
from concourse.bass_utils import run_bass_kernel_spmd

F32 = mybir.dt.float32
BF16 = mybir.dt.bfloat16
I32 = mybir.dt.int32
AF = mybir.ActivationFunctionType
ALU = mybir.AluOpType
AX = mybir.AxisListType

D = 1024
T = 8192
CT = 256
NT = T + CT
DEPTH = 4
NEXP = 32
DE = 512


class Buf:
    def __init__(self, t, name):
        self.t = t
        self.name = name
        self.lw = None
        self.rd = {}

    def __getitem__(self, idx):
        return self.t[idx]


class Parts:
    def __init__(self, t, name):
        self.t = t
        self.name = name
        self.parts = {}

    def p(self, key):
        b = self.parts.get(key)
        if b is None:
            b = Buf(self.t, f"{self.name}.{key}")
            self.parts[key] = b
        return b

    def all(self):
        return list(self.parts.values())

    def __getitem__(self, idx):
        return self.t[idx]


class Ctx:
    KD = 16
    SAME_ENG_SYNC = True

    def __init__(self):
        self.nc = bass.Bass("TRN2", target_bir_lowering=False)
        nc = self.nc
        self.es = ExitStack()
        self.eng = {"pe": nc.tensor, "act": nc.scalar, "dve": nc.vector, "pool": nc.gpsimd, "sp": nc.sync}
        self.csem = {e: self.es.enter_context(nc.semaphore("c_" + e)) for e in ("pe", "act", "dve", "pool")}
        self.ccnt = {e: 0 for e in self.csem}
        self.dsem = {q: [self.es.enter_context(nc.semaphore(f"d_{q}{i}")) for i in range(self.KD)]
                     for q in ("sp", "pool", "act")}
        self.dcnt = {q: 0 for q in self.dsem}
        self.known = {e: {} for e in self.eng}
        self.nalloc = 0
        self.psum_banks = []
        self.psum_i = 0

    def sb(self, shape, dtype=F32, name=None):
        self.nalloc += 1
        name = (name or "sb") + f"_{self.nalloc}"
        es = self.scopes[-1] if getattr(self, "scopes", None) else self.es
        t = es.enter_context(self.nc.sbuf_tensor(name, list(shape), dtype))
        return Buf(t, name)

    def push_scope(self):
        if not hasattr(self, "scopes"):
            self.scopes = []
        self.scopes.append(ExitStack())

    def pop_scope(self):
        self.barrier()
        self.scopes.pop().close()

    def barrier(self):
        for e in self.eng:
            for src, sem in self.csem.items():
                if self.ccnt[src] > 0:
                    self._wait(e, (sem, self.ccnt[src], "bar"))
            self._wait_all_dma(e)

    def _wait_all_dma(self, e):
        for q in self.dsem:
            n = self.dcnt[q]
            for r in range(self.KD):
                cnt = (n - r + self.KD - 1) // self.KD if n > r else 0
                if cnt > 0:
                    self._wait(e, (self.dsem[q][r], 16 * cnt, "dma"))

    def dram(self, name, shape, dtype=F32, kind="Internal"):
        return self.nc.dram_tensor(name, list(shape), dtype, kind=kind)

    def init_psum(self, n=8):
        for i in range(n):
            t = self.es.enter_context(self.nc.psum_tensor(f"ps{i}", [128, 512], F32))
            b = Buf(t, f"ps{i}")
            b.excl = True
            self.psum_banks.append(b)

    def ps(self):
        b = self.psum_banks[self.psum_i % len(self.psum_banks)]
        self.psum_i += 1
        return b

    def _wait(self, e, ev):
        if ev is None:
            return
        sem, val, src = ev
        if src == e and (e == "pe" or not self.SAME_ENG_SYNC):
            return
        k = self.known[e]
        key = sem.name
        if k.get(key, 0) >= val:
            return
        self.eng[e].wait_ge(sem, val)
        k[key] = val

    def _deps(self, e, reads, writes):
        for b in reads:
            self._wait(e, b.lw)
            if getattr(b, "excl", False):
                for ke, ev in b.rd.items():
                    if ke != e:
                        self._wait(e, ev)
        for b in writes:
            self._wait(e, b.lw)
            for ev in b.rd.values():
                self._wait(e, ev)

    def _commit(self, ev, key, reads, writes):
        for b in writes:
            b.lw = ev
            b.rd = {}
        for b in reads:
            b.rd[key] = ev

    def op(self, e, fn, reads=(), writes=()):
        self._deps(e, reads, writes)
        ins = fn(self.eng[e])
        self.ccnt[e] += 1
        ins.then_inc(self.csem[e], 1)
        ev = (self.csem[e], self.ccnt[e], e)
        self._commit(ev, e, reads, writes)
        return ins

    def dma(self, q, out, in_, reads=(), writes=(), indirect=None, **kw):
        i = self.dcnt[q]
        self.dcnt[q] += 1
        sem = self.dsem[q][i % self.KD]
        val = 16 * (i // self.KD + 1)
        if i >= self.KD:
            self._wait(q, (sem, val - 16, "dma"))
        self._deps(q, reads, writes)
        if indirect is None:
            ins = self.eng[q].dma_start(out=out, in_=in_, **kw)
        else:
            ins = self.eng[q].indirect_dma_start(out=out, in_=in_, **indirect)
        ins.then_inc(sem, 16)
        ev = (sem, val, "dma")
        self._commit(ev, (q, i % self.KD), reads, writes)
        return ins

    def finish(self):
        self._wait_all_dma("sp")
        self.es.close()


WEIGHT_SPECS = {
    "ada_w": [4, 1024, 6144], "ada_b": [4, 6144], "norm_mix": [4, 1024], "norm_ffn": [4, 1024],
    "w_in": [2, 1024, 2048], "mu_x": [2, 3, 1024], "mu_p": [2, 3, 512],
    "decay_w0": [2, 2, 512], "decay_w1": [2, 2, 1024, 64], "decay_w2": [2, 2, 64, 512],
    "lr_a0": [2, 2, 512], "lr_a1": [2, 2, 1024, 64], "lr_a2": [2, 2, 64, 512],
    "gate_g1": [2, 1024, 128], "gate_g2": [2, 128, 512],
    "k_k": [2, 512], "k_a": [2, 512], "r_k": [2, 8, 64], "gn_w": [2, 512], "gn_b": [2, 512],
    "pool_w": [2, 4, 128, 128], "pool_scale": [2, 512], "w_out": [2, 1024, 1024],
    "w_fourier": [2, 1024, 1024],
    "router_c": [4, 1024, 4], "router_c_b": [4, 4], "router_f": [4, 1024, 32], "router_f_b": [4, 32],
    "moe_w1": [4, 32, 1024, 512], "moe_w3": [4, 32, 1024, 512], "moe_w2": [4, 32, 512, 1024],
    "final_norm": [1024],
}


class Prog:
    BLK = 512

    def __init__(self, debug=None, skip=()):
        self.K = Ctx()
        K = self.K
        nc = K.nc
        self.debug = debug or {}
        self.inp = {}
        self.inp["x"] = nc.dram_tensor("x", [T, D], F32, kind="ExternalInput")
        self.inp["c"] = nc.dram_tensor("c", [1, D], F32, kind="ExternalInput")
        self.inp["ctx"] = nc.dram_tensor("ctx", [CT, D], F32, kind="ExternalInput")
        self.inp["c_ctx"] = nc.dram_tensor("c_ctx", [1, D], F32, kind="ExternalInput")
        for k, shp in WEIGHT_SPECS.items():
            if k in skip:
                continue
            self.inp[k] = nc.dram_tensor(k, shp, F32, kind="ExternalInput")
        self.out = nc.dram_tensor("out", [T, D], F32, kind="ExternalOutput")
        self.dbg = {}
        for k, (shp, dt) in self.debug.items():
            self.dbg[k] = nc.dram_tensor("dbg_" + k, shp, dt, kind="ExternalOutput")
        K.init_psum(8)
        self.lat = Parts(nc.dram_tensor("lat", [NT, D], F32), "lat")
        self.ident = K.sb([128, 128], F32, "ident")
        self.identb = K.sb([128, 128], BF16, "identb")
        self.ones = K.sb([128, 128], F32, "ones")
        self._consts()
        self.Ml = K.sb([128, 6, D], F32, "Ml")
        self.Mc = K.sb([128, 6, D], F32, "Mc")
        self.srep = K.sb([128, 2, 8, 128], F32, "srep")

    def _consts(self):
        K = self.K
        K.op("pool", lambda e: e.memset(self.ones[:], 1.0), writes=[self.ones])
        K.op("pool", lambda e: e.memset(self.ident[:], 0.0), writes=[self.ident])
        K.op("pool", lambda e: e.affine_select(out=self.ident[:], in_=self.ident[:], pattern=[[-1, 128]],
                                               compare_op=ALU.not_equal, fill=1.0, base=0, channel_multiplier=1),
             reads=[self.ident], writes=[self.ident])
        K.op("dve", lambda e: e.tensor_copy(out=self.identb[:], in_=self.ident[:]), reads=[self.ident],
             writes=[self.identb])

    def prep_s(self):
        K = self.K
        craw = K.sb([128, 2, 8], F32, "craw")
        csil = K.sb([128, 2, 8], F32, "csil")
        for w, nm in enumerate(("c", "c_ctx")):
            src = self.inp[nm].ap().rearrange("o (kt k) -> k (o kt)", k=128)
            K.dma("sp", craw[:, w, :], src, writes=[craw], allow_slow_non_contiguous=True)
        K.op("act", lambda e: e.activation(out=csil[:], in_=craw[:], func=AF.Silu), reads=[craw], writes=[csil])
        K.op("dve", lambda e: e.tensor_copy(out=self.srep[:], in_=csil[:].unsqueeze(3).to_broadcast([128, 2, 8, 128])),
             reads=[csil], writes=[self.srep])

    def modvec(self, i, need_ctx=True):
        K = self.K
        K.push_scope()
        mv = dict(
            W=[K.sb([128, 8, 512], F32, f"adaW{j}") for j in range(2)],
            b=[K.sb([1, 512], F32, f"adab{j}") for j in range(2)],
            g=K.sb([128, 2, D], F32, "normg"),
        )
        aw = self.inp["ada_w"]
        ab = self.inp["ada_b"]
        g = mv["g"]
        K.dma("sp", g[:, 0, :], self.inp["norm_mix"][i:i + 1, :].partition_broadcast(128), writes=[g])
        K.dma("sp", g[:, 1, :], self.inp["norm_ffn"][i:i + 1, :].partition_broadcast(128), writes=[g])
        targets = [(0, self.Ml)] + ([(1, self.Mc)] if need_ctx else [])
        for nb in range(12):
            W = mv["W"][nb % 2]
            bb = mv["b"][nb % 2]
            K.dma("sp", W[:], aw[i, :, nb * 512:(nb + 1) * 512].rearrange("(kt k) n -> k kt n", k=128), writes=[W])
            K.dma("sp", bb[:], ab[i:i + 1, nb * 512:(nb + 1) * 512], writes=[bb])
            for w, M in targets:
                ps = K.ps()
                for kt in range(8):
                    K.op("pe", lambda e, kt=kt, w=w, ps=ps, W=W: e.matmul(ps[:, :], lhsT=self.srep[:, w, kt, :],
                                                                         rhs=W[:, kt, :], start=(kt == 0), stop=False),
                         reads=[self.srep, W], writes=[ps])
                K.op("pe", lambda e, ps=ps, bb=bb: e.matmul(ps[:, :], lhsT=self.ones[0:1, :], rhs=bb[0:1, :],
                                                            start=False, stop=True),
                     reads=[self.ones, bb], writes=[ps])
                s, half = nb // 2, nb % 2
                dst = M[:, s, half * 512:(half + 1) * 512]
                if s in (1, 4):
                    gi = 0 if s == 1 else 1
                    K.op("dve", lambda e, dst=dst, ps=ps, gi=gi, half=half: e.scalar_tensor_tensor(
                        out=dst, in0=ps[:, :], scalar=1.0, in1=g[:, gi, half * 512:(half + 1) * 512],
                        op0=ALU.add, op1=ALU.mult), reads=[ps, g], writes=[M])
                else:
                    K.op("act", lambda e, dst=dst, ps=ps: e.copy(out=dst, in_=ps[:, :]), reads=[ps], writes=[M])
        K.pop_scope()

    def norm_tile(self, xt, ht, M, sub, st):
        K = self.K
        sh = 0 if sub == 0 else 3
        ga = 1 if sub == 0 else 4
        K.op("act", lambda e: e.activation(out=ht[:], in_=xt[:], func=AF.Square, accum_out=st[:, 0:1]),
             reads=[xt], writes=[ht, st])
        K.op("dve", lambda e: e.tensor_scalar(out=st[:, 1:2], in0=st[:, 0:1], scalar1=1.0 / D, scalar2=1e-6,
                                              op0=ALU.mult, op1=ALU.add), reads=[st], writes=[st])
        K.op("act", lambda e: e.sqrt(out=st[:, 3:4], in_=st[:, 1:2]), reads=[st], writes=[st])
        K.op("dve", lambda e: e.reciprocal(out=st[:, 2:3], in_=st[:, 3:4]), reads=[st], writes=[st])
        K.op("dve", lambda e: e.scalar_tensor_tensor(out=ht[:], in0=xt[:], scalar=st[:, 2:3], in1=M[:, ga, :],
                                                     op0=ALU.mult, op1=ALU.mult), reads=[xt, st, M], writes=[ht])
        K.op("pool", lambda e: e.tensor_tensor(out=ht[:], in0=ht[:], in1=M[:, sh, :], op=ALU.add),
             reads=[ht, M], writes=[ht])

    def tr(self, e_unused, dst_ps_ap, src_ap, ps, src_buf, n_in=128):
        K = self.K
        K.op("pe", lambda e: e.transpose(out=dst_ps_ap, in_=src_ap, identity=self.ident[0:n_in, 0:n_in]),
             reads=[src_buf, self.ident], writes=[ps])

    def init_lat(self):
        K = self.K
        for r in range(0, T, 2048):
            K.dma("sp", self.lat[r:r + 2048, :], self.inp["x"][r:r + 2048, :],
                  writes=[self.lat.p(t) for t in range(r // 128, r // 128 + 16)])
        K.dma("sp", self.lat[T:NT, :], self.inp["ctx"][:, :], writes=[self.lat.p(64), self.lat.p(65)])

    def moe_dram(self):
        nc = self.K.nc
        BLK = self.BLK
        self.NB = (2 * NT + 32 * BLK) // BLK
        NB = self.NB
        self.mdram = dict(H2=Parts(nc.dram_tensor("H2", [NT, D], F32), "H2"),
                          XS=Buf(nc.dram_tensor("XS", [NB * BLK, D], F32), "XS"),
                          YS=Buf(nc.dram_tensor("YS", [NB * BLK, D], F32), "YS"))

    def moe_setup(self):
        K = self.K
        nc = K.nc
        if not hasattr(self, "mdram"):
            self.moe_dram()
        K.push_scope()
        m = dict(self.mdram)
        NTL = 66
        NB = self.NB
        m["OHA"] = K.sb([128, NTL, 2, 32], BF16, "OHA")
        m["RK"] = K.sb([128, NTL, 2], F32, "RK")
        m["cs"] = K.sb([128, 8, 32], F32, "moecs")
        m["be"] = K.sb([128, 3, NB], F32, "moebe")
        m["idf"] = K.sb([128, NB, 8], F32, "moeidf")
        m["GATE"] = K.sb([128, NTL, 2], F32, "GATE")
        m["DEST"] = K.sb([128, NTL, 2], I32, "DEST")
        m["carry"] = K.sb([128, 32], F32, "carry")
        m["Wr"] = K.sb([128, 8, 36], F32, "Wr")
        m["br"] = K.sb([1, 36], F32, "br")
        m["UT"] = K.sb([128, 128], F32, "UT")
        m["PIDX"] = K.sb([128, 8], F32, "PIDX")
        m["JV"] = K.sb([128, NB], F32, "JV")
        m["IDX1"] = K.sb([128, NB, 8], I32, "IDX1")
        m["IDX2"] = K.sb([128, NB, 4], I32, "IDX2")
        big = [K.sb([128, D], F32, f"big{j}") for j in range(8)]
        m["xt"] = big[0:2]
        m["ht"] = big[2:4]
        m["yb"] = big[4:6]
        m["y0"] = big[4:6]
        m["y1"] = big[6:8]
        m["st"] = [K.sb([128, 4], F32, f"mst{j}") for j in range(2)]
        m["hT"] = [K.sb([128, 8, 128], F32, f"mhT{j}") for j in range(2)]
        m["xTb"] = [K.sb([128, 8, 128], BF16, f"mxTb{j}") for j in range(2)]
        m["W1"] = [K.sb([128, 8, 512], BF16, f"mW1{j}") for j in range(2)]
        m["W3"] = [K.sb([128, 8, 512], BF16, f"mW3{j}") for j in range(2)]
        m["W2"] = [K.sb([128, 4, 1024], BF16, f"mW2{j}") for j in range(2)]
        m["hTb"] = [K.sb([128, 4, 128], BF16, f"mhTb{j}") for j in range(2)]
        m["sil"] = [K.sb([128, 512], F32, f"msil{j}") for j in range(2)]
        m["sm"] = [K.sb([128, 128], F32, f"msm{j}") for j in range(2)]
        UT = m["UT"]
        K.op("pool", lambda e: e.memset(UT[:], 1.0), writes=[UT])
        K.op("pool", lambda e: e.affine_select(out=UT[:], in_=UT[:], pattern=[[1, 128]], compare_op=ALU.is_gt,
                                               fill=0.0, base=0, channel_multiplier=-1), reads=[UT], writes=[UT])
        pi = K.sb([128, 8], I32, "pidx_i")
        K.op("pool", lambda e: e.iota(pi[:], pattern=[[128, 8]], base=0, channel_multiplier=1), writes=[pi])
        K.op("dve", lambda e: e.tensor_copy(out=m["PIDX"][:], in_=pi[:]), reads=[pi], writes=[m["PIDX"]])
        ji = K.sb([128, NB], I32, "jv_i")
        K.op("pool", lambda e: e.iota(ji[:], pattern=[[self.BLK, NB]], base=0, channel_multiplier=0), writes=[ji])
        K.op("dve", lambda e: e.tensor_copy(out=m["JV"][:], in_=ji[:]), reads=[ji], writes=[m["JV"]])
        self.m = m

    def moe(self, i, with_ctx, final=False):
        K = self.K
        m = self.m
        NB = self.NB
        ntile = 66 if with_ctx else 64
        Wr, br = m["Wr"], m["br"]
        K.dma("sp", Wr[:, :, 0:4], self.inp["router_c"][i].rearrange("(kt k) n -> k kt n", k=128), writes=[Wr])
        K.dma("sp", Wr[:, :, 4:36], self.inp["router_f"][i].rearrange("(kt k) n -> k kt n", k=128), writes=[Wr])
        K.dma("sp", br[:, 0:4], self.inp["router_c_b"][i:i + 1, :], writes=[br])
        K.dma("sp", br[:, 4:36], self.inp["router_f_b"][i:i + 1, :], writes=[br])
        carry = m["carry"]
        K.op("dve", lambda e: e.memset(carry[:], 0.0), writes=[carry])
        OHA, RK, GATE, DEST = m["OHA"], m["RK"], m["GATE"], m["DEST"]
        for tt in range(ntile):
            xt, ht, st, hT, sm = (m[k][tt % 2] for k in ("xt", "ht", "st", "hT", "sm"))
            M = self.Ml if tt < 64 else self.Mc
            K.dma("sp", xt[:], self.lat[tt * 128:(tt + 1) * 128, :], reads=[self.lat.p(tt)], writes=[xt])
            self.norm_tile(xt, ht, M, 1, st)
            K.dma("act", m["H2"][tt * 128:(tt + 1) * 128, :], ht[:], reads=[ht], writes=[m["H2"].p(tt)])
            for half in range(2):
                ps = K.ps()
                for q in range(4):
                    kt = half * 4 + q
                    self.tr(None, ps[:, q * 128:(q + 1) * 128], ht[:, kt * 128:(kt + 1) * 128], ps, ht)
                K.op("act" if half else "dve",
                     lambda e, ps=ps, half=half: (e.copy if half else e.tensor_copy)(
                         out=hT[:, half * 4:(half + 1) * 4, :].rearrange("p a b -> p (a b)"), in_=ps[:, :]),
                     reads=[ps], writes=[hT])
            ps = K.ps()
            for kt in range(8):
                K.op("pe", lambda e, kt=kt, ps=ps: e.matmul(ps[:, 0:36], lhsT=hT[:, kt, :], rhs=Wr[:, kt, :],
                                                           start=(kt == 0), stop=False),
                     reads=[hT, Wr], writes=[ps])
            K.op("pe", lambda e, ps=ps: e.matmul(ps[:, 0:36], lhsT=self.ones[0:1, :], rhs=br[0:1, :],
                                                 start=False, stop=True), reads=[self.ones, br], writes=[ps])
            def dv(fn, rd=(), wr=()):
                K.op("dve", fn, reads=[sm] + list(rd), writes=[sm] + list(wr))
            K.op("dve", lambda e, ps=ps: e.tensor_copy(out=sm[:, 0:36], in_=ps[:, 0:36]), reads=[ps], writes=[sm])
            dv(lambda e: e.reduce_max(out=sm[:, 36:37], in_=sm[:, 0:4], axis=AX.X))
            dv(lambda e: e.tensor_scalar(out=sm[:, 37:38], in0=sm[:, 36:37], scalar1=-1.0, scalar2=None, op0=ALU.mult))
            dv(lambda e: e.tensor_scalar(out=sm[:, 40:44], in0=sm[:, 0:4], scalar1=sm[:, 36:37], scalar2=None,
                                         op0=ALU.is_equal))
            K.op("act", lambda e: e.activation(out=sm[:, 44:48], in_=sm[:, 0:4], func=AF.Exp, bias=sm[:, 37:38],
                                               scale=1.0, accum_out=sm[:, 38:39]), reads=[sm], writes=[sm])
            dv(lambda e: e.reciprocal(out=sm[:, 39:40], in_=sm[:, 38:39]))
            dv(lambda e: e.tensor_scalar(out=sm[:, 48:56], in0=sm[:, 4:12], scalar1=sm[:, 40:41], scalar2=None,
                                         op0=ALU.mult))
            for g in range(1, 4):
                dv(lambda e, g=g: e.scalar_tensor_tensor(out=sm[:, 48:56], in0=sm[:, 4 + 8 * g:12 + 8 * g],
                                                         scalar=sm[:, 40 + g:41 + g], in1=sm[:, 48:56],
                                                         op0=ALU.mult, op1=ALU.add))
            dv(lambda e: e.reduce_max(out=sm[:, 56:57], in_=sm[:, 48:56], axis=AX.X))
            dv(lambda e: e.tensor_scalar(out=sm[:, 60:68], in0=sm[:, 48:56], scalar1=sm[:, 56:57], scalar2=None,
                                         op0=ALU.is_equal))
            dv(lambda e: e.scalar_tensor_tensor(out=sm[:, 68:76], in0=sm[:, 60:68], scalar=-1e30, in1=sm[:, 48:56],
                                                op0=ALU.mult, op1=ALU.add))
            dv(lambda e: e.reduce_max(out=sm[:, 57:58], in_=sm[:, 68:76], axis=AX.X))
            dv(lambda e: e.tensor_scalar(out=sm[:, 76:84], in0=sm[:, 68:76], scalar1=sm[:, 57:58], scalar2=None,
                                         op0=ALU.is_equal))
            dv(lambda e: e.tensor_tensor(out=sm[:, 58:59], in0=sm[:, 56:57], in1=sm[:, 57:58], op=ALU.subtract))
            K.op("act", lambda e: e.activation(out=sm[:, 59:60], in_=sm[:, 58:59], func=AF.Sigmoid),
                 reads=[sm], writes=[sm])
            dv(lambda e, tt=tt: e.tensor_tensor(out=GATE[:, tt, 0:1], in0=sm[:, 59:60], in1=sm[:, 39:40], op=ALU.mult),
               wr=[GATE])
            dv(lambda e, tt=tt: e.tensor_tensor(out=GATE[:, tt, 1:2], in0=sm[:, 39:40], in1=GATE[:, tt, 0:1],
                                                op=ALU.subtract), rd=[GATE], wr=[GATE])
            for k, c0 in ((0, 60), (1, 76)):
                dv(lambda e, tt=tt, k=k, c0=c0: e.tensor_tensor(
                    out=OHA[:, tt, k, :].rearrange("p (g l) -> p g l", g=4),
                    in0=sm[:, 40:44].unsqueeze(2).to_broadcast([128, 4, 8]),
                    in1=sm[:, c0:c0 + 8].unsqueeze(1).to_broadcast([128, 4, 8]), op=ALU.mult), wr=[OHA])
            dv(lambda e, tt=tt: e.tensor_tensor(out=sm[:, 84:116], in0=OHA[:, tt, 0, :], in1=OHA[:, tt, 1, :],
                                                op=ALU.add), rd=[OHA])
            psr = K.ps()
            K.op("pe", lambda e, psr=psr: e.matmul(psr[:, 0:32], lhsT=m["UT"][:], rhs=sm[:, 84:116], start=True,
                                                   stop=True), reads=[m["UT"], sm], writes=[psr])
            K.op("pe", lambda e, psr=psr: e.matmul(psr[:, 32:64], lhsT=self.ones[:], rhs=sm[:, 84:116], start=True,
                                                   stop=True), reads=[self.ones, sm], writes=[psr])
            K.op("dve", lambda e, psr=psr: e.tensor_tensor(out=sm[:, 84:116], in0=psr[:, 0:32], in1=carry[:],
                                                           op=ALU.add), reads=[psr, carry, sm], writes=[sm])
            for k in range(2):
                dv(lambda e, tt=tt, k=k: e.tensor_tensor(out=sm[:, 0:32], in0=sm[:, 84:116], in1=OHA[:, tt, k, :],
                                                         op=ALU.mult), rd=[OHA])
                dv(lambda e, tt=tt, k=k: e.reduce_sum(out=RK[:, tt, k:k + 1], in_=sm[:, 0:32], axis=AX.X), wr=[RK])
            K.op("dve", lambda e, psr=psr: e.tensor_tensor(out=carry[:], in0=psr[:, 32:64], in1=carry[:], op=ALU.add),
                 reads=[psr, carry], writes=[carry])
        cs = m["cs"]

        def cv(fn):
            K.op("dve", fn, reads=[cs, carry], writes=[cs])
        BLK = self.BLK
        cv(lambda e: e.tensor_scalar(out=cs[:, 0, :], in0=carry[:], scalar1=1.0 / BLK, scalar2=(BLK - 1.0) / (2 * BLK),
                                     op0=ALU.mult, op1=ALU.add))
        cv(lambda e: e.tensor_scalar(out=cs[:, 1, :], in0=cs[:, 0, :], scalar1=8388608.0, scalar2=None, op0=ALU.add))
        cv(lambda e: e.tensor_scalar(out=cs[:, 2, :], in0=cs[:, 1, :], scalar1=-8388608.0, scalar2=float(BLK),
                                     op0=ALU.add, op1=ALU.mult))
        cv(lambda e: e.tensor_copy(out=cs[:, 3, :], in_=cs[:, 2, :]))
        a, b = 3, 4
        for s in (1, 2, 4, 8, 16):
            cv(lambda e, a=a, b=b, s=s: e.tensor_copy(out=cs[:, b, 0:s], in_=cs[:, a, 0:s]))
            cv(lambda e, a=a, b=b, s=s: e.tensor_tensor(out=cs[:, b, s:32], in0=cs[:, a, s:32], in1=cs[:, a, 0:32 - s],
                                                        op=ALU.add))
            a, b = b, a
        pend_i = a
        cv(lambda e: e.tensor_tensor(out=cs[:, 5, :], in0=cs[:, pend_i, :], in1=cs[:, 2, :], op=ALU.subtract))
        be = m["be"]
        K.op("dve", lambda e: e.memset(be[:, 0, :], 0.0), writes=[be])
        for ex in range(32):
            K.op("dve", lambda e, ex=ex: e.scalar_tensor_tensor(out=be[:, 0, :], in0=m["JV"][:],
                                                                scalar=cs[:, pend_i, ex:ex + 1], in1=be[:, 0, :],
                                                                op0=ALU.is_ge, op1=ALU.add),
                 reads=[m["JV"], cs, be], writes=[be])
        K.op("dve", lambda e: e.tensor_scalar(out=be[:, 0, :], in0=be[:, 0, :], scalar1=31.0, scalar2=None, op0=ALU.min),
             reads=[be], writes=[be])
        K.op("dve", lambda e: e.tensor_scalar(out=be[:, 1, :], in0=be[:, 0, :], scalar1=1024.0,
                                              scalar2=float(i * 32 * 1024), op0=ALU.mult, op1=ALU.add),
             reads=[be], writes=[be])
        K.op("dve", lambda e: e.tensor_scalar(out=be[:, 2, :], in0=be[:, 0, :], scalar1=512.0,
                                              scalar2=float(i * 32 * 512), op0=ALU.mult, op1=ALU.add),
             reads=[be], writes=[be])
        idf = m["idf"]
        K.op("dve", lambda e: e.tensor_tensor(out=idf[:], in0=be[:, 1, :].unsqueeze(2).to_broadcast([128, NB, 8]),
                                              in1=m["PIDX"][:].unsqueeze(1).to_broadcast([128, NB, 8]), op=ALU.add),
             reads=[be, m["PIDX"]], writes=[idf])
        K.op("dve", lambda e: e.tensor_copy(out=m["IDX1"][:], in_=idf[:]), reads=[idf], writes=[m["IDX1"]])
        K.op("dve", lambda e: e.tensor_tensor(out=idf[:, :, 0:4], in0=be[:, 2, :].unsqueeze(2).to_broadcast([128, NB, 4]),
                                              in1=m["PIDX"][:, 0:4].unsqueeze(1).to_broadcast([128, NB, 4]),
                                              op=ALU.add), reads=[be, m["PIDX"]], writes=[idf])
        K.op("dve", lambda e: e.tensor_copy(out=m["IDX2"][:], in_=idf[:, :, 0:4]), reads=[idf], writes=[m["IDX2"]])
        for tt in range(ntile):
            ht, sm = m["ht"][tt % 2], m["sm"][tt % 2]
            K.dma("sp", ht[:], m["H2"][tt * 128:(tt + 1) * 128, :], reads=[m["H2"].p(tt)], writes=[ht])
            for k in range(2):
                K.op("dve", lambda e, tt=tt, k=k: e.tensor_tensor(out=sm[:, 32:64], in0=cs[:, 5, :],
                                                                  in1=OHA[:, tt, k, :], op=ALU.mult),
                     reads=[sm, OHA, cs], writes=[sm])
                K.op("dve", lambda e, k=k: e.reduce_sum(out=sm[:, 66 + k:67 + k], in_=sm[:, 32:64], axis=AX.X),
                     reads=[sm], writes=[sm])
            K.op("dve", lambda e, tt=tt: e.tensor_tensor(out=sm[:, 64:66], in0=sm[:, 66:68], in1=RK[:, tt, :],
                                                         op=ALU.add), reads=[sm, RK], writes=[sm])
            K.op("dve", lambda e, tt=tt: e.tensor_copy(out=DEST[:, tt, :], in_=sm[:, 64:66]), reads=[sm], writes=[DEST])
            for k in range(2):
                K.dma("pool", m["XS"][:, :], ht[:], reads=[ht, DEST], writes=[m["XS"]],
                      indirect=dict(out_offset=bass.IndirectOffsetOnAxis(ap=DEST[:, tt, k:k + 1], axis=0),
                                    in_offset=None))
        w1t = self.inp["moe_w1"].ap().rearrange("l e k n -> (l e k) n")
        w3t = self.inp["moe_w3"].ap().rearrange("l e k n -> (l e k) n")
        w2t = self.inp["moe_w2"].ap().rearrange("l e k n -> (l e k) n")
        for j in range(NB):
            W1, W3, W2, xt, xTb, hTb, sil, yb = (m[k][j % 2] for k in ("W1", "W3", "W2", "xt", "xTb", "hTb", "sil", "yb"))
            for kt in range(8):
                for W, tab in ((W1, w1t), (W3, w3t)):
                    K.dma("pool", W[:, kt, :], tab, reads=[m["IDX1"]], writes=[W],
                          indirect=dict(out_offset=None,
                                        in_offset=bass.IndirectOffsetOnAxis(ap=m["IDX1"][:, j, kt:kt + 1], axis=0)))
            for fc in range(4):
                K.dma("pool", W2[:, fc, :], w2t, reads=[m["IDX2"]], writes=[W2],
                      indirect=dict(out_offset=None,
                                    in_offset=bass.IndirectOffsetOnAxis(ap=m["IDX2"][:, j, fc:fc + 1], axis=0)))
            for sub in range(self.BLK // 128):
                r0 = j * self.BLK + sub * 128
                xt, xTb, hTb, sil, yb = (m[k][(j * 4 + sub) % 2] for k in ("xt", "xTb", "hTb", "sil", "yb"))
                K.dma("sp", xt[:], m["XS"][r0:r0 + 128, :], reads=[m["XS"]], writes=[xt])
                for half in range(2):
                    ps = K.ps()
                    for q in range(4):
                        kt = half * 4 + q
                        self.tr(None, ps[:, q * 128:(q + 1) * 128], xt[:, kt * 128:(kt + 1) * 128], ps, xt)
                    K.op("act" if half else "dve",
                         lambda e, ps=ps, half=half, xTb=xTb: (e.copy if half else e.tensor_copy)(
                             out=xTb[:, half * 4:(half + 1) * 4, :].rearrange("p a b -> p (a b)"), in_=ps[:, :]),
                         reads=[ps], writes=[xTb])
                pa, pb = K.ps(), K.ps()
                for W, pp in ((W1, pa), (W3, pb)):
                    for fc in range(4):
                        for kt in range(8):
                            K.op("pe", lambda e, W=W, pp=pp, fc=fc, kt=kt, xTb=xTb: e.matmul(
                                pp[:, fc * 128:(fc + 1) * 128], lhsT=W[:, kt, fc * 128:(fc + 1) * 128], rhs=xTb[:, kt, :],
                                start=(kt == 0), stop=(kt == 7)), reads=[W, xTb], writes=[pp])
                K.op("act", lambda e, pa=pa, sil=sil: e.activation(out=sil[:], in_=pa[:, :], func=AF.Silu),
                     reads=[pa], writes=[sil])
                K.op("dve", lambda e, pb=pb, sil=sil, hTb=hTb: e.tensor_tensor(
                    out=hTb[:].rearrange("p a b -> p (a b)"), in0=sil[:], in1=pb[:, :], op=ALU.mult),
                    reads=[pb, sil], writes=[hTb])
                for half in range(2):
                    py = K.ps()
                    for fc in range(4):
                        K.op("pe", lambda e, py=py, fc=fc, half=half, hTb=hTb, W2=W2: e.matmul(
                            py[:, :], lhsT=hTb[:, fc, :], rhs=W2[:, fc, half * 512:(half + 1) * 512],
                            start=(fc == 0), stop=(fc == 3)), reads=[hTb, W2], writes=[py])
                    K.op("act" if half else "dve",
                         lambda e, py=py, half=half, yb=yb: (e.copy if half else e.tensor_copy)(
                             out=yb[:, half * 512:(half + 1) * 512], in_=py[:, :]), reads=[py], writes=[yb])
                K.dma("sp", m["YS"][r0:r0 + 128, :], yb[:], reads=[yb], writes=[m["YS"]])
        for tt in range(ntile):
            y0, y1, xt = m["y0"][tt % 2], m["y1"][tt % 2], m["xt"][tt % 2]
            M = self.Ml if tt < 64 else self.Mc
            for k, y in ((0, y0), (1, y1)):
                K.dma("pool", y[:], m["YS"][:, :], reads=[m["YS"], DEST], writes=[y],
                      indirect=dict(out_offset=None,
                                    in_offset=bass.IndirectOffsetOnAxis(ap=DEST[:, tt, k:k + 1], axis=0)))
            K.dma("sp", xt[:], self.lat[tt * 128:(tt + 1) * 128, :], reads=[self.lat.p(tt)], writes=[xt])
            K.op("dve", lambda e, tt=tt, y0=y0: e.tensor_scalar(out=y0[:], in0=y0[:], scalar1=GATE[:, tt, 0:1],
                                                                scalar2=None, op0=ALU.mult),
                 reads=[y0, GATE], writes=[y0])
            K.op("dve", lambda e, tt=tt, y0=y0, y1=y1: e.scalar_tensor_tensor(
                out=y0[:], in0=y1[:], scalar=GATE[:, tt, 1:2], in1=y0[:], op0=ALU.mult, op1=ALU.add),
                reads=[y0, y1, GATE], writes=[y0])
            K.op("pool", lambda e, y0=y0, M=M: e.tensor_tensor(out=y0[:], in0=y0[:], in1=M[:, 5, :], op=ALU.mult),
                 reads=[y0, M], writes=[y0])
            K.op("dve", lambda e, y0=y0, xt=xt: e.tensor_tensor(out=xt[:], in0=xt[:], in1=y0[:], op=ALU.add),
                 reads=[y0, xt], writes=[xt])
            K.dma("sp", self.lat[tt * 128:(tt + 1) * 128, :], xt[:], reads=[xt], writes=[self.lat.p(tt)])
        K.pop_scope()

    def final_norm(self):
        K = self.K
        K.push_scope()
        m = dict(xt=[K.sb([128, D], F32, f"fx{j}") for j in range(2)], ht=[K.sb([128, D], F32, f"fh{j}") for j in range(2)],
                 st=[K.sb([128, 4], F32, f"fs{j}") for j in range(2)])
        g = K.sb([128, D], F32, "fng")
        K.dma("sp", g[:], self.inp["final_norm"].ap().rearrange("(o d) -> o d", o=1).partition_broadcast(128), writes=[g])
        for tt in range(64):
            xt, ht, st = m["xt"][tt % 2], m["ht"][tt % 2], m["st"][tt % 2]
            K.dma("sp", xt[:], self.lat[tt * 128:(tt + 1) * 128, :], reads=[self.lat.p(tt)], writes=[xt])
            K.op("act", lambda e, xt=xt, ht=ht, st=st: e.activation(out=ht[:], in_=xt[:], func=AF.Square,
                                                                    accum_out=st[:, 0:1]), reads=[xt], writes=[ht, st])
            K.op("dve", lambda e, st=st: e.tensor_scalar(out=st[:, 1:2], in0=st[:, 0:1], scalar1=1.0 / D, scalar2=1e-6,
                                                         op0=ALU.mult, op1=ALU.add), reads=[st], writes=[st])
            K.op("act", lambda e, st=st: e.sqrt(out=st[:, 3:4], in_=st[:, 1:2]), reads=[st], writes=[st])
            K.op("dve", lambda e, st=st: e.reciprocal(out=st[:, 2:3], in_=st[:, 3:4]), reads=[st], writes=[st])
            K.op("dve", lambda e, xt=xt, ht=ht, st=st: e.scalar_tensor_tensor(
                out=ht[:], in0=xt[:], scalar=st[:, 2:3], in1=g[:], op0=ALU.mult, op1=ALU.mult),
                reads=[xt, st, g], writes=[ht])
            K.dma("sp", self.out[tt * 128:(tt + 1) * 128, :], ht[:], reads=[ht])
        K.pop_scope()


def fourier_consts():
    c = {}
    n = np.arange(256)
    ang = 2 * np.pi * np.outer(n, n) / 256
    c["f_cc"] = (np.cos(ang) / 16).astype(np.float32)
    c["f_sc"] = (-np.sin(ang) / 16).astype(np.float32)
    a = np.arange(128)
    ang = 2 * np.pi * np.outer(a, a) / 128
    s = 1.0 / np.sqrt(8192.0)
    c["f_c128"] = (np.cos(ang) * s).astype(np.float32)
    c["f_s128"] = (np.sin(ang) * s).astype(np.float32)
    f1 = np.arange(128)[:, None]
    b = np.arange(64)[None, :]
    th = 2 * np.pi * f1 * b / 8192
    c["f_tw"] = np.stack([np.cos(th), -np.sin(th)], axis=2).astype(np.float32)
    bb = np.arange(64)
    ang = 2 * np.pi * np.outer(bb, bb) / 64
    c["f_cs64"] = np.concatenate([np.cos(ang), np.sin(ang)], axis=0).astype(np.float32)
    t = np.arange(256)
    ang = 2 * np.pi * np.outer(t, t) / 256
    c["f_c256"] = (np.cos(ang) / 16).astype(np.float32)
    c["f_s256"] = (np.sin(ang) / 16).astype(np.float32)
    return c


FOURIER_SPECS = {"f_cc": [256, 256], "f_sc": [256, 256], "f_c128": [128, 128], "f_s128": [128, 128],
                 "f_tw": [128, 64, 2], "f_cs64": [128, 64], "f_c256": [256, 256], "f_s256": [256, 256]}


def _fourier_declare(self):
    nc = self.K.nc
    for k, shp in FOURIER_SPECS.items():
        self.inp[k] = nc.dram_tensor(k, shp, F32, kind="ExternalInput")
    self.GD = Buf(nc.dram_tensor("GD", [128, 128, D], F32), "GD")


def _fourier(self, i, with_ctx, nb=64, nf=128):
    K = self.K
    j = i // 2
    K.push_scope()
    cc = K.sb([128, 2, 2, 256], BF16, "fcc")
    c128 = K.sb([128, 3, 128], BF16, "fc128")
    tw = K.sb([128, 64, 2], F32, "ftw")
    cs64 = K.sb([128, 64], F32, "fcs64")
    wf = K.sb([128, 8, D], BF16, "fwf")
    big = [K.sb([128, D], F32, f"fbig{q}") for q in range(6)]
    xts, hts, gsb = big[0:2], big[2:4], big[4:6]
    sts = [K.sb([128, 4], F32, f"fst{q}") for q in range(2)]
    hTb = [K.sb([128, 8, 128], BF16, f"fhT{q}") for q in range(2)]
    Zb = [K.sb([128, 2, D], BF16, f"fZ{q}") for q in range(2)]
    gi2 = [K.sb([128, D], F32, f"fgi{q}") for q in range(2)]
    tmp = K.sb([128, 256], F32, "ftmp")
    def ld_cast(dst_ap, dst_buf, src_ap, shape, n=[0]):
        stg = big[4 + n[0] % 2]
        n[0] += 1
        rows, cols = shape
        view = stg[0:rows, 0:cols]
        K.dma("sp", view, src_ap, writes=[stg])
        K.op("dve", lambda e: e.tensor_copy(out=dst_ap, in_=view), reads=[stg], writes=[dst_buf])
    for kt in range(2):
        ld_cast(cc[:, kt, 0, :], cc, self.inp["f_cc"][kt * 128:(kt + 1) * 128, :], (128, 256))
        ld_cast(cc[:, kt, 1, :], cc, self.inp["f_sc"][kt * 128:(kt + 1) * 128, :], (128, 256))
    ld_cast(c128[:, 0, :], c128, self.inp["f_c128"][:, :], (128, 128))
    ld_cast(c128[:, 1, :], c128, self.inp["f_s128"][:, :], (128, 128))
    K.op("dve", lambda e: e.tensor_scalar(out=c128[:, 2, :], in0=c128[:, 1, :], scalar1=-1.0, scalar2=None,
                                          op0=ALU.mult), reads=[c128], writes=[c128])
    for kt in range(8):
        ld_cast(wf[:, kt, :], wf, self.inp["w_fourier"][j, kt * 128:(kt + 1) * 128, :], (128, 1024))
    self._ld_cast = ld_cast
    K.dma("sp", tw[:], self.inp["f_tw"][:, :, :], writes=[tw])
    K.dma("sp", cs64[:], self.inp["f_cs64"][:, :], writes=[cs64])
    ntw = K.sb([128, 64], F32, "fntw")
    K.op("dve", lambda e: e.tensor_scalar(out=ntw[:], in0=tw[:, :, 1], scalar1=-1.0, scalar2=None, op0=ALU.mult),
         reads=[tw], writes=[ntw])
    latv = self.lat[0:T, :].rearrange("(a b) d -> b a d", b=64)
    lat_all = [self.lat.p(t) for t in range(64)]

    def chan_dft(xt, ht, st, hT, Z, M):
        self.norm_tile(xt, ht, M, 0, st)
        for half in range(2):
            ps = K.ps()
            for q in range(4):
                kt = half * 4 + q
                self.tr(None, ps[:, q * 128:(q + 1) * 128], ht[:, kt * 128:(kt + 1) * 128], ps, ht)
            K.op("act" if half else "dve",
                 lambda e, ps=ps, half=half: (e.copy if half else e.tensor_copy)(
                     out=hT[:, half * 4:(half + 1) * 4, :].rearrange("p a b -> p (a b)"), in_=ps[:, :]),
                 reads=[ps], writes=[hT])
        for ri in range(2):
            for hh in range(2):
                ps = K.ps()
                for gg in range(2):
                    g = hh * 2 + gg
                    for kt in range(2):
                        K.op("pe", lambda e, ps=ps, gg=gg, g=g, kt=kt, ri=ri: e.matmul(
                            ps[:, gg * 256:(gg + 1) * 256], lhsT=hT[:, g * 2 + kt, :], rhs=cc[:, kt, ri, :],
                            start=(kt == 0), stop=(kt == 1)), reads=[hT, cc], writes=[ps])
                K.op("act" if hh else "dve",
                     lambda e, ps=ps, hh=hh, ri=ri: (e.copy if hh else e.tensor_copy)(
                         out=Z[:, ri, hh * 512:(hh + 1) * 512], in_=ps[:, :]), reads=[ps], writes=[Z])

    for b in range(nb):
        xt, ht, st, hT, Z, gr, gi = (l[b % 2] for l in (xts, hts, sts, hTb, Zb, gsb, gi2))
        K.dma("sp", xt[:], latv[b], reads=lat_all, writes=[xt])
        chan_dft(xt, ht, st, hT, Z, self.Ml)
        for hh in range(2):
            cs_ = slice(hh * 512, (hh + 1) * 512)
            pr, pi_ = K.ps(), K.ps()
            K.op("pe", lambda e: e.matmul(pr[:, :], lhsT=c128[:, 0, :], rhs=Z[:, 0, cs_], start=True, stop=False),
                 reads=[c128, Z], writes=[pr])
            K.op("pe", lambda e: e.matmul(pr[:, :], lhsT=c128[:, 1, :], rhs=Z[:, 1, cs_], start=False, stop=True),
                 reads=[c128, Z], writes=[pr])
            K.op("pe", lambda e: e.matmul(pi_[:, :], lhsT=c128[:, 0, :], rhs=Z[:, 1, cs_], start=True, stop=False),
                 reads=[c128, Z], writes=[pi_])
            K.op("pe", lambda e: e.matmul(pi_[:, :], lhsT=c128[:, 2, :], rhs=Z[:, 0, cs_], start=False, stop=True),
                 reads=[c128, Z], writes=[pi_])
            K.op("dve", lambda e: e.tensor_scalar(out=gr[:, cs_], in0=pr[:, :], scalar1=tw[:, b, 0:1], scalar2=None,
                                                  op0=ALU.mult), reads=[pr, tw], writes=[gr])
            K.op("dve", lambda e: e.scalar_tensor_tensor(out=gr[:, cs_], in0=pi_[:, :], scalar=ntw[:, b:b + 1],
                                                         in1=gr[:, cs_], op0=ALU.mult, op1=ALU.add),
                 reads=[pi_, ntw, gr], writes=[gr])
            K.op("dve", lambda e: e.tensor_scalar(out=gi[:, cs_], in0=pi_[:, :], scalar1=tw[:, b, 0:1], scalar2=None,
                                                  op0=ALU.mult), reads=[pi_, tw], writes=[gi])
            K.op("dve", lambda e: e.scalar_tensor_tensor(out=gi[:, cs_], in0=pr[:, :], scalar=tw[:, b, 1:2],
                                                         in1=gi[:, cs_], op0=ALU.mult, op1=ALU.add),
                 reads=[pr, tw, gi], writes=[gi])
        K.dma("act", self.GD[b, :, :], gr[:], reads=[gr], writes=[self.GD])
        K.dma("act", self.GD[64 + b, :, :], gi[:], reads=[gi], writes=[self.GD])
    latf = self.lat[0:T, :].rearrange("(f2 f1) d -> f1 f2 d", f1=128)
    YT = [K.sb([128, 8, 64], BF16, f"fYT{q}") for q in range(2)]
    for f1 in range(nf):
        gd, yt, xt, ht = gsb[f1 % 2], YT[f1 % 2], xts[f1 % 2], hts[f1 % 2]
        K.dma("sp", gd[:], self.GD[:, f1, :], reads=[self.GD], writes=[gd])
        K.dma("sp", xt[0:64, :], latf[f1], reads=lat_all, writes=[xt])
        ps = K.ps()
        for kt in range(8):
            K.op("pe", lambda e, kt=kt: e.matmul(ps[:, kt * 64:(kt + 1) * 64], lhsT=gd[:, kt * 128:(kt + 1) * 128],
                                                 rhs=cs64[:, :], start=True, stop=True), reads=[gd, cs64], writes=[ps])
        K.op("act", lambda e: e.copy(out=yt[:].rearrange("p a b -> p (a b)"), in_=ps[:, :]), reads=[ps], writes=[yt])
        for hh in range(2):
            py = K.ps()
            for kt in range(8):
                K.op("pe", lambda e, kt=kt: e.matmul(py[0:64, :], lhsT=yt[:, kt, :],
                                                     rhs=wf[:, kt, hh * 512:(hh + 1) * 512], start=(kt == 0),
                                                     stop=(kt == 7)), reads=[yt, wf], writes=[py])
            K.op("dve", lambda e: e.tensor_tensor(out=ht[0:64, hh * 512:(hh + 1) * 512], in0=py[0:64, :],
                                                  in1=self.Ml[0:64, 2, hh * 512:(hh + 1) * 512], op=ALU.mult),
                 reads=[py, self.Ml], writes=[ht])
        K.op("pool", lambda e: e.tensor_tensor(out=xt[0:64, :], in0=xt[0:64, :], in1=ht[0:64, :], op=ALU.add),
             reads=[xt, ht], writes=[xt])
        K.dma("act", latf[f1], xt[0:64, :], reads=[xt], writes=lat_all)
    if with_ctx:
        c256 = K.sb([128, 2, 2, 256], BF16, "fc256")
        for tt in range(2):
            ld_cast(c256[:, 0, tt, :], c256, self.inp["f_c256"][tt * 128:(tt + 1) * 128, :], (128, 256))
            ld_cast(c256[:, 1, tt, :], c256, self.inp["f_s256"][tt * 128:(tt + 1) * 128, :], (128, 256))
        ctxp = [self.lat.p(64), self.lat.p(65)]
        for tt in range(2):
            K.dma("sp", xts[tt][:], self.lat[T + tt * 128:T + (tt + 1) * 128, :], reads=ctxp, writes=[xts[tt]])
            chan_dft(xts[tt], hts[tt], sts[tt], hTb[tt], Zb[tt], self.Mc)
        for ft in range(2):
            yt = K.sb([128, 8, 128], BF16, f"fcy{ft}")
            for half in range(2):
                ps = K.ps()
                for q in range(4):
                    kt = half * 4 + q
                    n = 0
                    for tt in range(2):
                        for cs_i in range(2):
                            K.op("pe", lambda e, kt=kt, q=q, tt=tt, cs_i=cs_i, n=n: e.matmul(
                                ps[:, q * 128:(q + 1) * 128], lhsT=Zb[tt][:, cs_i, kt * 128:(kt + 1) * 128],
                                rhs=c256[:, cs_i, tt, ft * 128:(ft + 1) * 128], start=(n == 0), stop=(n == 3)),
                                reads=[Zb[tt], c256], writes=[ps])
                            n += 1
                K.op("act", lambda e, half=half: e.copy(out=yt[:, half * 4:(half + 1) * 4, :].rearrange("p a b -> p (a b)"),
                                                        in_=ps[:, :]), reads=[ps], writes=[yt])
            xt, ht = xts[ft], hts[ft]
            for hh in range(2):
                py = K.ps()
                for kt in range(8):
                    K.op("pe", lambda e, kt=kt: e.matmul(py[:, :], lhsT=yt[:, kt, :],
                                                         rhs=wf[:, kt, hh * 512:(hh + 1) * 512], start=(kt == 0),
                                                         stop=(kt == 7)), reads=[yt, wf], writes=[py])
                K.op("dve", lambda e: e.tensor_tensor(out=ht[:, hh * 512:(hh + 1) * 512], in0=py[:, :],
                                                      in1=self.Mc[:, 2, hh * 512:(hh + 1) * 512], op=ALU.mult),
                     reads=[py, self.Mc], writes=[ht])
            K.op("pool", lambda e: e.tensor_tensor(out=xt[:], in0=xt[:], in1=ht[:], op=ALU.add),
                 reads=[xt, ht], writes=[xt])
            K.dma("act", self.lat[T + ft * 128:T + (ft + 1) * 128, :], xt[:], reads=[xt], writes=ctxp)
    K.pop_scope()


Prog.fourier_declare = _fourier_declare
Prog.fourier = _fourier


POOL_WINS = (2, 4, 8, 16)


def pool_consts():
    c = {}
    for nm, L in (("f_icnt_lat", T), ("f_icnt_ctx", CT)):
        a = np.zeros((4, L), np.float32)
        pos = np.arange(L)
        for gi, win in enumerate(POOL_WINS):
            half = win // 2
            hi = np.minimum(pos + half, L)
            lo = np.maximum(pos - half, 0)
            a[gi] = 1.0 / (hi - lo)
        c[nm] = a
    return c


def _even_declare(self):
    nc = self.K.nc
    self.inp["f_icnt_lat"] = nc.dram_tensor("f_icnt_lat", [4, T], F32, kind="ExternalInput")
    self.inp["f_icnt_ctx"] = nc.dram_tensor("f_icnt_ctx", [4, CT], F32, kind="ExternalInput")
    e = {}
    e["HT"] = Buf(nc.dram_tensor("HT", [D, NT], F32), "HT")
    e["PT"] = Buf(nc.dram_tensor("PT", [2048, NT], F32), "PT")
    for d in range(2):
        for nm in ("At", "Bt", "Kt", "Rt", "Bh", "Kh"):
            e[f"{nm}{d}"] = Buf(nc.dram_tensor(f"SC_{nm}{d}", [512, NT], F32), f"{nm}{d}")
        e[f"GL{d}"] = Buf(nc.dram_tensor(f"SC_GL{d}", [512, NT // 64], F32), f"GL{d}")
        e[f"YD{d}"] = Buf(nc.dram_tensor(f"SC_YD{d}", [NT, 512], F32), f"YD{d}")
    for nm in ("VV", "GG", "BON", "YB"):
        e[nm] = Buf(nc.dram_tensor(f"SC_{nm}", [512, NT], F32), nm)
    self.ed = e


def _even_e1(self, i, ntile=66):
    K = self.K
    j = i // 2
    e = self.ed
    K.push_scope()
    win = K.sb([128, 8, 2048], BF16, "win")
    stg = [K.sb([128, D], F32, f"e1stg{q}") for q in range(2)]
    n = 0
    for kt in range(8):
        for hh in range(2):
            sg = stg[n % 2]
            n += 1
            K.dma("sp", sg[:], self.inp["w_in"][j, kt * 128:(kt + 1) * 128, hh * 1024:(hh + 1) * 1024], writes=[sg])
            K.op("dve" if n % 2 else "act",
                 lambda en, sg=sg, kt=kt, hh=hh: (en.tensor_copy if n % 2 else en.copy)(
                     out=win[:, kt, hh * 1024:(hh + 1) * 1024], in_=sg[:]), reads=[sg], writes=[win])
    xts = [K.sb([128, D], F32, f"e1x{q}") for q in range(2)]
    hts = [K.sb([128, D], F32, f"e1h{q}") for q in range(2)]
    sts = [K.sb([128, 4], F32, f"e1s{q}") for q in range(2)]
    hTf = [K.sb([128, 8, 128], F32, f"e1hTf{q}") for q in range(2)]
    hTb = [K.sb([128, 8, 128], BF16, f"e1hTb{q}") for q in range(2)]
    pts = [K.sb([128, 16, 128], F32, f"e1pt{q}") for q in range(2)]
    for tt in range(ntile):
        xt, ht, st, hf, hb, pt = (l[tt % 2] for l in (xts, hts, sts, hTf, hTb, pts))
        M = self.Ml if tt < 64 else self.Mc
        K.dma("sp", xt[:], self.lat[tt * 128:(tt + 1) * 128, :], reads=[self.lat.p(tt)], writes=[xt])
        self.norm_tile(xt, ht, M, 0, st)
        for half in range(2):
            ps = K.ps()
            for q in range(4):
                kt = half * 4 + q
                self.tr(None, ps[:, q * 128:(q + 1) * 128], ht[:, kt * 128:(kt + 1) * 128], ps, ht)
            K.op("act", lambda en, ps=ps, half=half: en.copy(
                out=hf[:, half * 4:(half + 1) * 4, :].rearrange("p a b -> p (a b)"), in_=ps[:, :]),
                reads=[ps], writes=[hf])
            K.op("dve", lambda en, half=half: en.tensor_copy(
                out=hb[:, half * 4:(half + 1) * 4, :], in_=hf[:, half * 4:(half + 1) * 4, :]),
                reads=[hf], writes=[hb])
        import os
        if not os.environ.get("SKIPHT"):
            K.dma("act", e["HT"][:, tt * 128:(tt + 1) * 128].rearrange("(kt k) t -> k kt t", k=128), hf[:],
                  reads=[hf], writes=[e["HT"]])
        for ob in range(4):
            ps = K.ps()
            for q in range(4):
                oc = ob * 4 + q
                for kt in range(8):
                    K.op("pe", lambda en, ps=ps, q=q, oc=oc, kt=kt: en.matmul(
                        ps[:, q * 128:(q + 1) * 128], lhsT=win[:, kt, oc * 128:(oc + 1) * 128], rhs=hb[:, kt, :],
                        start=(kt == 0), stop=(kt == 7)), reads=[win, hb], writes=[ps])
            K.op("act" if ob % 2 else "dve", lambda en, ps=ps, ob=ob: (en.copy if ob % 2 else en.tensor_copy)(
                out=pt[:, ob * 4:(ob + 1) * 4, :].rearrange("p a b -> p (a b)"), in_=ps[:, :]),
                reads=[ps], writes=[pt])
        if not os.environ.get("SKIPPT"):
            K.dma("act", e["PT"][:, tt * 128:(tt + 1) * 128].rearrange("(oc k) t -> k oc t", k=128), pt[:],
                  reads=[pt], writes=[e["PT"]])
    K.pop_scope()


def _even_e2(self, i, dbg=None):
    K = self.K
    j = i // 2
    e = self.ed
    K.push_scope()
    WB = 512
    WT = WB + 128
    eng_rr = [0]

    def ve():
        eng_rr[0] += 1
        return "dve" if eng_rr[0] % 2 else "pool"

    stg = K.sb([128, 8, 128], F32, "e2stg")
    W1 = K.sb([128, 3, 8, 128], BF16, "e2W1")
    W2 = K.sb([128, 3, 512], BF16, "e2W2")
    pw = K.sb([128, 4, 128], BF16, "e2pw")
    for v, (nm, nd) in enumerate((("decay_w1", 2), ("lr_a1", 2), ("gate_g1", 1))):
        for d in range(nd):
            src = self.inp[nm][j, d] if nd == 2 else self.inp[nm][j]
            wcol = 64 if nd == 2 else 128
            K.dma("sp", stg[:, :, 0:wcol], src.rearrange("(kt k) r -> k kt r", k=128), writes=[stg])
            K.op("dve", lambda en, v=v, d=d, wcol=wcol: en.tensor_copy(out=W1[:, v, :, d * wcol:(d + 1) * wcol],
                                                                       in_=stg[:, :, 0:wcol]), reads=[stg], writes=[W1])
    stg2 = stg[:].rearrange("p a b -> p (a b)")
    for v, nm in enumerate(("decay_w2", "lr_a2")):
        for d in range(2):
            K.dma("sp", stg2[d * 64:(d + 1) * 64, 0:512], self.inp[nm][j, d], writes=[stg])
        K.op("dve", lambda en, v=v: en.tensor_copy(out=W2[:, v, :], in_=stg2[:, 0:512]), reads=[stg], writes=[W2])
    K.dma("sp", stg2[:, 0:512], self.inp["gate_g2"][j], writes=[stg])
    K.op("dve", lambda en: en.tensor_copy(out=W2[:, 2, :], in_=stg2[:, 0:512]), reads=[stg], writes=[W2])
    for gi in range(4):
        K.dma("sp", stg2[:, gi * 128:(gi + 1) * 128], self.inp["pool_w"][j, gi], writes=[stg])
    K.op("dve", lambda en: en.tensor_copy(out=pw[:].rearrange("p a b -> p (a b)"), in_=stg2[:, 0:512]),
         reads=[stg], writes=[pw])
    MU = K.sb([128, 2, 3, 8], F32, "e2MU")
    MUP = K.sb([128, 2, 3, 4], F32, "e2MUP")
    COL = K.sb([128, 12, 4], F32, "e2COL")
    K.dma("sp", MU[:, 0, :, :], self.inp["mu_x"][j].rearrange("v (kt k) -> k v kt", k=128), writes=[MU],
          allow_slow_non_contiguous=True)
    K.dma("sp", MUP[:, 0, :, :], self.inp["mu_p"][j].rearrange("v (c k) -> k v c", k=128), writes=[MUP],
          allow_slow_non_contiguous=True)
    for d in range(2):
        K.dma("sp", COL[:, d, :], self.inp["decay_w0"][j, d].rearrange("(c k) -> k c", k=128), writes=[COL],
              allow_slow_non_contiguous=True)
        K.dma("sp", COL[:, 2 + d, :], self.inp["lr_a0"][j, d].rearrange("(c k) -> k c", k=128), writes=[COL],
              allow_slow_non_contiguous=True)
    K.dma("sp", COL[:, 4, :], self.inp["k_k"][j].rearrange("(c k) -> k c", k=128), writes=[COL], allow_slow_non_contiguous=True)
    K.dma("sp", COL[:, 5, :], self.inp["k_a"][j].rearrange("(c k) -> k c", k=128), writes=[COL], allow_slow_non_contiguous=True)
    K.dma("sp", COL[:, 6, :], self.inp["r_k"][j].rearrange("(c h2) k -> (h2 k) c", h2=2), writes=[COL],
          allow_slow_non_contiguous=True)
    K.dma("sp", COL[:, 7, :], self.inp["pool_scale"][j].rearrange("(c k) -> k c", k=128), writes=[COL],
          allow_slow_non_contiguous=True)
    for Mx in (MU, MUP):
        K.op("dve", lambda en, Mx=Mx: en.tensor_scalar(out=Mx[:, 1], in0=Mx[:, 0], scalar1=-1.0, scalar2=1.0,
                                                       op0=ALU.mult, op1=ALU.add), reads=[Mx], writes=[Mx])
    bones = K.sb([128, 128], F32, "e2bones")
    K.op("pool", lambda en: en.memset(bones[:], 0.0), writes=[bones])
    K.op("pool", lambda en: en.memset(bones[0:64, 0:64], 1.0), reads=[bones], writes=[bones])
    K.op("pool", lambda en: en.memset(bones[64:128, 64:128], 1.0), reads=[bones], writes=[bones])
    HTt = K.sb([128, 8, WT], F32, "e2HTt")
    xv = K.sb([128, 8, WB], BF16, "e2xv")
    t1 = [K.sb([128, WB], BF16, f"e2t1{v}") for v in range(3)]
    PTt = [K.sb([128, WT], F32, f"e2PTt{n}") for n in range(4)]
    nm_w = ("Rm", "Km", "Vm", "kk", "sq", "rn", "LW0", "LW1", "AD0", "AD1", "Gm", "kd", "b", "lw", "P0", "P1", "E",
            "tmp", "ks", "IC", "o0", "o1", "o2", "o3")
    w = {n: K.sb([128, WB], F32, "e2" + n) for n in nm_w}
    sw = [K.sb([128, WT], F32, f"e2s{q}") for q in range(2)]
    dfb = K.sb([128, WB], BF16, "e2dfb")
    gl = K.sb([128, 8], F32, "e2gl")
    orr = [0]

    def otile():
        orr[0] += 1
        return w[f"o{orr[0] % 4}"]

    GRID_H = [(-1, True)] * 2 + [(1, True)] * 2 + [(-64, False)] * 2 + [(64, False)] * 2
    GRID_P = [(-1, True), (1, True), (-64, False), (64, False)]
    SEQ_H = [(-1, False)] * 4 + [(1, False)] * 4
    SEQ_P = [(-1, False)] * 2 + [(1, False)] * 2
    blocks = [(T, CT, T, CT, SEQ_H, SEQ_P, "f_icnt_ctx")] + \
             [(t0, WB, 0, T, GRID_H, GRID_P, "f_icnt_lat") for t0 in range(0, T, WB)]

    def load_halo(buf, view, src_rows, t0, Wb, s0, sl):
        lo = max(t0 - 64, s0)
        hi = min(t0 + Wb + 64, s0 + sl)
        if lo > t0 - 64:
            K.op("pool", lambda en: en.memset(view(0, 64), 0.0), writes=[buf])
        if hi < t0 + Wb + 64:
            K.op("pool", lambda en: en.memset(view(64 + Wb, 128 + Wb), 0.0), writes=[buf])
        K.dma("sp", view(lo - (t0 - 64), hi - (t0 - 64)), src_rows(lo, hi), reads=[e["HT"], e["PT"]], writes=[buf])

    def mix(out_ap, out_buf, srcf, src_buf, mu_ap, omu_ap, delta, rowmask, Wb):
        en1 = "dve"
        K.op("pool", lambda en: en.tensor_scalar(out=out_ap, in0=srcf(64, 64 + Wb), scalar1=omu_ap, scalar2=None,
                                              op0=ALU.mult), reads=[src_buf], writes=[out_buf])
        if not rowmask:
            o, s_ = out_ap, srcf(64 + delta, 64 + delta + Wb)
        else:
            ov = out_ap.rearrange("p (r c) -> p r c", c=64)
            sv = srcf(64 + delta, 64 + delta + Wb).rearrange("p (r c) -> p r c", c=64)
            if delta == -1:
                o, s_ = ov[:, :, 1:64], sv[:, :, 1:64]
            else:
                o, s_ = ov[:, :, 0:63], sv[:, :, 0:63]
        K.op(en1, lambda en: en.scalar_tensor_tensor(out=o, in0=s_, scalar=mu_ap, in1=o, op0=ALU.mult, op1=ALU.add),
             reads=[src_buf, out_buf], writes=[out_buf])

    def store(dst, c, t0, Wb, tile):
        K.dma("sp", dst[c * 128:(c + 1) * 128, t0:t0 + Wb], tile[:, 0:Wb], reads=[tile], writes=[dst])

    for (t0, Wb, s0, sl, HS, PS, icn) in blocks:
        R = Wb // 64
        load_halo(HTt, lambda a, b: HTt[:, :, a:b],
                  lambda a, b: e["HT"][:, a:b].rearrange("(kt k) t -> k kt t", k=128), t0, Wb, s0, sl)
        for v in range(3):
            for kt in range(8):
                dl, rm = HS[kt]
                mix(xv[:, kt, 0:Wb], xv, lambda a, b, kt=kt: HTt[:, kt, a:b], HTt, MU[:, 0, v, kt:kt + 1],
                    MU[:, 1, v, kt:kt + 1], dl, rm, Wb)
            ps = K.ps()
            for kt in range(8):
                K.op("pe", lambda en, kt=kt, v=v: en.matmul(ps[:, 0:Wb], lhsT=W1[:, v, kt, :], rhs=xv[:, kt, 0:Wb],
                                                            start=(kt == 0), stop=(kt == 7)), reads=[W1, xv], writes=[ps])
            fn = (AF.Tanh, AF.Copy, AF.Sigmoid)[v]
            K.op("act", lambda en, v=v, fn=fn: en.activation(out=t1[v][:, 0:Wb], in_=ps[:, 0:Wb], func=fn),
                 reads=[ps], writes=[t1[v]])
        for c in range(4):
            cs_ = slice(c * 128, (c + 1) * 128)
            for n in range(4):
                load_halo(PTt[n], lambda a, b, n=n: PTt[n][:, a:b],
                          lambda a, b, n=n: e["PT"][n * 512 + c * 128:n * 512 + (c + 1) * 128, a:b], t0, Wb, s0, sl)
            dl, rm = PS[c]
            for n, nm in enumerate(("Rm", "Km", "Vm")):
                mix(w[nm][:, 0:Wb], w[nm], lambda a, b, n=n: PTt[n][:, a:b], PTt[n], MUP[:, 0, n, c:c + 1],
                    MUP[:, 1, n, c:c + 1], dl, rm, Wb)
            store(e["VV"], c, t0, Wb, w["Vm"])
            for d in range(2):
                ps = K.ps()
                K.op("pe", lambda en, d=d: en.matmul(ps[:, 0:Wb], lhsT=W2[d * 64:(d + 1) * 64, 0, cs_],
                                                     rhs=t1[0][d * 64:(d + 1) * 64, 0:Wb], start=True, stop=True),
                     reads=[W2, t1[0]], writes=[ps])
                K.op("act", lambda en, d=d: en.activation(out=w[f"LW{d}"][:, 0:Wb], in_=ps[:, 0:Wb], func=AF.Sigmoid,
                                                          bias=COL[:, d, c:c + 1], scale=1.0),
                     reads=[ps, COL], writes=[w[f"LW{d}"]])
                ps = K.ps()
                K.op("pe", lambda en, d=d: en.matmul(ps[:, 0:Wb], lhsT=W2[d * 64:(d + 1) * 64, 1, cs_],
                                                     rhs=t1[1][d * 64:(d + 1) * 64, 0:Wb], start=True, stop=True),
                     reads=[W2, t1[1]], writes=[ps])
                K.op("act", lambda en, d=d: en.activation(out=w[f"AD{d}"][:, 0:Wb], in_=ps[:, 0:Wb], func=AF.Sigmoid,
                                                          bias=COL[:, 2 + d, c:c + 1], scale=1.0),
                     reads=[ps, COL], writes=[w[f"AD{d}"]])
            ps = K.ps()
            K.op("pe", lambda en: en.matmul(ps[:, 0:Wb], lhsT=W2[:, 2, cs_], rhs=t1[2][:, 0:Wb], start=True, stop=True),
                 reads=[W2, t1[2]], writes=[ps])
            K.op("act", lambda en: en.copy(out=w["Gm"][:, 0:Wb], in_=ps[:, 0:Wb]), reads=[ps], writes=[w["Gm"]])
            store(e["GG"], c, t0, Wb, w["Gm"])
            K.op("dve", lambda en: en.tensor_scalar(out=w["kk"][:, 0:Wb], in0=w["Km"][:, 0:Wb], scalar1=COL[:, 4, c:c + 1],
                                                    scalar2=None, op0=ALU.mult), reads=[w["Km"], COL], writes=[w["kk"]])
            K.op("pool", lambda en: en.tensor_tensor(out=w["sq"][:, 0:Wb], in0=w["kk"][:, 0:Wb], in1=w["kk"][:, 0:Wb],
                                                     op=ALU.mult), reads=[w["kk"]], writes=[w["sq"]])
            ps = K.ps()
            K.op("pe", lambda en: en.matmul(ps[:, 0:Wb], lhsT=bones[:], rhs=w["sq"][:, 0:Wb], start=True, stop=True),
                 reads=[bones, w["sq"]], writes=[ps])
            K.op("dve", lambda en: en.tensor_scalar(out=w["rn"][:, 0:Wb], in0=ps[:, 0:Wb], scalar1=1e-12, scalar2=None,
                                                    op0=ALU.max), reads=[ps], writes=[w["rn"]])
            K.op("act", lambda en: en.sqrt(out=w["rn"][:, 0:Wb], in_=w["rn"][:, 0:Wb]), reads=[w["rn"]], writes=[w["rn"]])
            K.op("dve", lambda en: en.reciprocal(out=w["rn"][:, 0:Wb], in_=w["rn"][:, 0:Wb]), reads=[w["rn"]],
                 writes=[w["rn"]])
            K.op("dve", lambda en: en.tensor_tensor(out=w["kk"][:, 0:Wb], in0=w["kk"][:, 0:Wb], in1=w["rn"][:, 0:Wb],
                                                    op=ALU.mult), reads=[w["kk"], w["rn"]], writes=[w["kk"]])
            for d in range(2):
                AD, LW = w[f"AD{d}"], w[f"LW{d}"]
                K.op("dve", lambda en: en.tensor_scalar(out=w["tmp"][:, 0:Wb], in0=AD[:, 0:Wb], scalar1=-1.0,
                                                        scalar2=COL[:, 5, c:c + 1], op0=ALU.add, op1=ALU.mult),
                     reads=[AD, COL], writes=[w["tmp"]])
                K.op("dve", lambda en: en.scalar_tensor_tensor(out=w["kd"][:, 0:Wb], in0=w["tmp"][:, 0:Wb], scalar=1.0,
                                                               in1=w["Km"][:, 0:Wb], op0=ALU.add, op1=ALU.mult),
                     reads=[w["tmp"], w["Km"]], writes=[w["kd"]])
                if d == 0:
                    K.op("pool", lambda en: en.tensor_copy(out=w["ks"][:, 0:Wb], in_=w["kd"][:, 0:Wb]),
                         reads=[w["kd"]], writes=[w["ks"]])
                else:
                    K.op("pool", lambda en: en.tensor_tensor(out=w["ks"][:, 0:Wb], in0=w["ks"][:, 0:Wb],
                                                             in1=w["kd"][:, 0:Wb], op=ALU.add),
                         reads=[w["kd"], w["ks"]], writes=[w["ks"]])
                K.op("pool", lambda en: en.tensor_tensor(out=w["b"][:, 0:Wb], in0=w["kk"][:, 0:Wb], in1=AD[:, 0:Wb],
                                                         op=ALU.mult), reads=[w["kk"], AD], writes=[w["b"]])
                K.op("dve", lambda en: en.tensor_scalar(out=w["lw"][:, 0:Wb], in0=LW[:, 0:Wb], scalar1=-0.6065306597126334,
                                                        scalar2=None, op0=ALU.mult), reads=[LW], writes=[w["lw"]])
                cur = w["lw"]
                pp = [w["P0"], w["P1"]]
                for si, sft in enumerate((1, 2, 4, 8, 16, 32)):
                    nxt = pp[si % 2]
                    cv_ = cur[:, 0:Wb].rearrange("p (r c) -> p r c", c=64)
                    nv = nxt[:, 0:Wb].rearrange("p (r c) -> p r c", c=64)
                    if d == 0:
                        K.op("dve", lambda en: en.tensor_tensor(out=nv[:, :, sft:64], in0=cv_[:, :, sft:64],
                                                                in1=cv_[:, :, 0:64 - sft], op=ALU.add),
                             reads=[cur], writes=[nxt])
                        K.op("pool", lambda en: en.tensor_copy(out=nv[:, :, 0:sft], in_=cv_[:, :, 0:sft]),
                             reads=[cur, nxt], writes=[nxt])
                    else:
                        K.op("dve", lambda en: en.tensor_tensor(out=nv[:, :, 0:64 - sft], in0=cv_[:, :, 0:64 - sft],
                                                                in1=cv_[:, :, sft:64], op=ALU.add),
                             reads=[cur], writes=[nxt])
                        K.op("pool", lambda en: en.tensor_copy(out=nv[:, :, 64 - sft:64], in_=cv_[:, :, 64 - sft:64]),
                             reads=[cur, nxt], writes=[nxt])
                    cur = nxt
                cum = cur
                cumv = cum[:, 0:Wb].rearrange("p (r c) -> p r c", c=64)
                last = 63 if d == 0 else 0
                cL = cumv[:, :, last]
                K.op("act", lambda en: en.activation(out=gl[:, 0:R], in_=cL, func=AF.Exp), reads=[cum], writes=[gl])
                K.dma("act", e[f"GL{d}"][c * 128:(c + 1) * 128, t0 // 64:t0 // 64 + R], gl[:, 0:R], reads=[gl],
                      writes=[e[f"GL{d}"]])
                K.op("act", lambda en: en.activation(out=w["E"][:, 0:Wb], in_=cum[:, 0:Wb], func=AF.Exp), reads=[cum],
                     writes=[w["E"]])
                o = otile()
                K.op("dve", lambda en: en.tensor_tensor(out=o[:, 0:Wb], in0=w["Rm"][:, 0:Wb], in1=w["E"][:, 0:Wb],
                                                        op=ALU.mult), reads=[w["Rm"], w["E"]], writes=[o])
                store(e[f"Rt{d}"], c, t0, Wb, o)
                K.op("act", lambda en: en.activation(out=w["E"][:, 0:Wb], in_=cum[:, 0:Wb], func=AF.Exp, scale=-1.0),
                     reads=[cum], writes=[w["E"]])
                for src_, dn in (("b", "Bt"), ("kd", "Kt")):
                    o = otile()
                    K.op(ve(), lambda en, o=o, src_=src_: en.tensor_tensor(out=o[:, 0:Wb], in0=w[src_][:, 0:Wb],
                                                                           in1=w["E"][:, 0:Wb], op=ALU.mult),
                         reads=[w[src_], w["E"]], writes=[o])
                    store(e[f"{dn}{d}"], c, t0, Wb, o)
                K.op("pool", lambda en: en.tensor_tensor(out=w["tmp"][:, 0:Wb], in0=cum[:, 0:Wb], in1=w["lw"][:, 0:Wb],
                                                         op=ALU.subtract), reads=[cum, w["lw"]], writes=[w["tmp"]])
                K.op("act", lambda en: en.activation(out=w["E"][:, 0:Wb], in_=w["tmp"][:, 0:Wb], func=AF.Exp),
                     reads=[w["tmp"]], writes=[w["E"]])
                o = otile()
                K.op("dve", lambda en: en.scalar_tensor_tensor(out=o[:, 0:Wb], in0=w["kk"][:, 0:Wb], scalar=-1.0,
                                                               in1=w["E"][:, 0:Wb], op0=ALU.mult, op1=ALU.mult),
                     reads=[w["kk"], w["E"]], writes=[o])
                store(e[f"At{d}"], c, t0, Wb, o)
                tv = w["tmp"][:, 0:Wb].rearrange("p (r c) -> p r c", c=64)
                K.op("dve", lambda en: en.tensor_tensor(out=tv, in0=cumv, in1=cL.unsqueeze(2).to_broadcast([128, R, 64]),
                                                        op=ALU.subtract), reads=[cum], writes=[w["tmp"]])
                K.op("act", lambda en: en.activation(out=w["E"][:, 0:Wb], in_=w["tmp"][:, 0:Wb], func=AF.Exp, scale=-1.0),
                     reads=[w["tmp"]], writes=[w["E"]])
                for src_, dn in (("b", "Bh"), ("kd", "Kh")):
                    o = otile()
                    K.op(ve(), lambda en, o=o, src_=src_: en.tensor_tensor(out=o[:, 0:Wb], in0=w[src_][:, 0:Wb],
                                                                           in1=w["E"][:, 0:Wb], op=ALU.mult),
                         reads=[w[src_], w["E"]], writes=[o])
                    store(e[f"{dn}{d}"], c, t0, Wb, o)
            K.op("dve", lambda en: en.scalar_tensor_tensor(out=w["tmp"][:, 0:Wb], in0=w["ks"][:, 0:Wb],
                                                           scalar=COL[:, 6, c:c + 1], in1=w["Rm"][:, 0:Wb],
                                                           op0=ALU.mult, op1=ALU.mult),
                 reads=[w["ks"], COL, w["Rm"]], writes=[w["tmp"]])
            ps = K.ps()
            K.op("pe", lambda en: en.matmul(ps[:, 0:Wb], lhsT=bones[:], rhs=w["tmp"][:, 0:Wb], start=True, stop=True),
                 reads=[bones, w["tmp"]], writes=[ps])
            o = otile()
            K.op("dve", lambda en: en.tensor_tensor(out=o[:, 0:Wb], in0=ps[:, 0:Wb], in1=w["Vm"][:, 0:Wb], op=ALU.mult),
                 reads=[ps, w["Vm"]], writes=[o])
            store(e["BON"], c, t0, Wb, o)
            u = PTt[3]
            Wt = Wb + 128
            K.dma("sp", w["IC"][:, 0:Wb], self.inp[icn][c:c + 1, t0 - s0:t0 - s0 + Wb].partition_broadcast(128),
                  writes=[w["IC"]])
            s_a, s_b = sw
            K.op("dve", lambda en: en.tensor_tensor(out=s_a[:, 1:Wt], in0=u[:, 0:Wt - 1], in1=u[:, 1:Wt], op=ALU.add),
                 reads=[u], writes=[s_a])
            cur_s, oth = s_a, s_b
            lo_, hi_ = 1, Wt
            for hs in (1, 2, 4):
                if POOL_WINS[c] < 4 * hs:
                    break
                nlo, nhi = lo_ + hs, hi_ - hs
                K.op("dve", lambda en, cur_s=cur_s, oth=oth, nlo=nlo, nhi=nhi, hs=hs: en.tensor_tensor(
                    out=oth[:, nlo:nhi], in0=cur_s[:, nlo - hs:nhi - hs], in1=cur_s[:, nlo + hs:nhi + hs], op=ALU.add),
                    reads=[cur_s], writes=[oth])
                cur_s, oth = oth, cur_s
                lo_, hi_ = nlo, nhi
            K.op("dve", lambda en: en.tensor_tensor(out=w["tmp"][:, 0:Wb], in0=cur_s[:, 64:64 + Wb], in1=w["IC"][:, 0:Wb],
                                                    op=ALU.mult), reads=[cur_s, w["IC"]], writes=[w["tmp"]])
            K.op("pool", lambda en: en.tensor_tensor(out=dfb[:, 0:Wb], in0=w["tmp"][:, 0:Wb], in1=u[:, 64:64 + Wb],
                                                     op=ALU.subtract), reads=[w["tmp"], u], writes=[dfb])
            ps = K.ps()
            K.op("pe", lambda en: en.matmul(ps[:, 0:Wb], lhsT=pw[:, c, :], rhs=dfb[:, 0:Wb], start=True, stop=True),
                 reads=[pw, dfb], writes=[ps])
            o = otile()
            K.op("dve", lambda en: en.tensor_scalar(out=o[:, 0:Wb], in0=ps[:, 0:Wb], scalar1=COL[:, 7, c:c + 1],
                                                    scalar2=None, op0=ALU.mult), reads=[ps, COL], writes=[o])
            store(e["YB"], c, t0, Wb, o)
    K.pop_scope()


Prog.even_e2 = _even_e2
def _even_e3(self, i, SDT=F32, nsteps=66):
    K = self.K
    e = self.ed
    K.push_scope()
    TEN = ("At", "Bt", "Kt", "Rt", "Bh", "Kh", "VV")

    def ring(nm, shape, n, dt=F32):
        bufs = [K.sb(shape, dt, f"e3{nm}{q}") for q in range(n)]
        cnt = [0]

        def nxt():
            cnt[0] += 1
            return bufs[cnt[0] % n]
        return nxt

    def mk_mask(nm, cmp_op, sgn=1):
        mbuf = K.sb([128, 128], F32, "e3m" + nm)
        K.op("pool", lambda en: en.memset(mbuf[:], 1.0), writes=[mbuf])
        K.op("pool", lambda en: en.affine_select(out=mbuf[:], in_=mbuf[:], pattern=[[sgn, 128]], compare_op=cmp_op,
                                                 fill=0.0, base=0, channel_multiplier=-sgn), reads=[mbuf], writes=[mbuf])
        K.op("pool", lambda en: en.memset(mbuf[0:64, 64:128], 0.0), reads=[mbuf], writes=[mbuf])
        K.op("pool", lambda en: en.memset(mbuf[64:128, 0:64], 0.0), reads=[mbuf], writes=[mbuf])
        return mbuf
    UPs = mk_mask("ups", ALU.is_gt)
    UPi = mk_mask("upi", ALU.is_ge)
    LOs = mk_mask("los", ALU.is_gt, -1)
    LOi = mk_mask("loi", ALU.is_ge, -1)
    BDM = mk_mask("bdm", ALU.is_ge)
    K.op("pool", lambda en: en.memset(BDM[0:64, 0:64], 1.0), reads=[BDM], writes=[BDM])
    K.op("pool", lambda en: en.memset(BDM[64:128, 64:128], 1.0), reads=[BDM], writes=[BDM])
    M4 = []
    MI = []
    for d in range(2):
        strictT, strict, inclT = (UPs, LOs, UPi) if d == 0 else (LOs, UPs, LOi)
        m4 = K.sb([128, 512], F32, f"e3m4{d}")
        for q, src in enumerate((strictT, strict, strictT, inclT)):
            K.op("pool", lambda en, q=q, src=src: en.tensor_copy(out=m4[:, q * 128:(q + 1) * 128], in_=src[:]),
                 reads=[src], writes=[m4])
        M4.append(m4)
        MI.append(inclT)
    identS = self.ident
    import os
    if os.environ.get("E3PAD"):
        _pad = K.sb([128, 128], F32, "e3pad")
    S = [[K.sb([128, 128], F32, f"e3S{d}{c}") for c in range(4)] for d in range(2)]
    for d in range(2):
        for c in range(4):
            K.op("pool", lambda en, d=d, c=c: en.memset(S[d][c][:], 0.0), writes=[S[d][c]])
    Ssd = S
    if SDT != F32:
        Ssd = [[K.sb([128, 128], SDT, f"e3Sb{d}{c}") for c in range(4)] for d in range(2)]
        for d in range(2):
            for c in range(4):
                K.op("pool", lambda en, d=d, c=c: en.memset(Ssd[d][c][:], 0.0), writes=[Ssd[d][c]])
    LD = [[{nm: K.sb([128, 4, 64], F32, f"e3ld{d}{b}{nm}") for nm in TEN} for b in range(2)] for d in range(2)]
    GLall = [K.sb([128, 4, NT // 64], F32, f"e3glall{d}") for d in range(2)]
    for d in range(2):
        K.dma("sp", GLall[d][:], e[f"GL{d}"][:, :].rearrange("(c p) t -> p c t", p=128), reads=[e[f"GL{d}"]],
              writes=[GLall[d]])
    r_bd = ring("bd", [128, 7, 128], 9, SDT)
    r_tp = ring("tp", [128, 384], 9, SDT)
    r_m = ring("m", [128, 640], 9, SDT)
    r_q = ring("q", [128, 256], 16, SDT)
    r_w = ring("w", [128, 128], 16, SDT)
    r_x = ring("x", [128, 128], 9, SDT)
    r_u = ring("u", [128, 128], 9, SDT)
    r_y = ring("y", [128, 4, 64], 4, F32)
    rr = [0]
    NCH = 2 * nsteps

    def ve3():
        rr[0] += 1
        return ("dve", "pool")[rr[0] % 2]

    def tok_base(d, n):
        if d == 0:
            return T + 64 * n if n < 4 else 64 * (n - 4)
        return T + 64 * (3 - n) if n < 4 else 64 * (127 - (n - 4))

    def load(d, n):
        b = n % 2
        tb = tok_base(d, n)
        for nm in TEN:
            src = e[nm if nm == "VV" else f"{nm}{d}"]
            K.dma("sp", LD[d][b][nm][:], src[:, tb:tb + 64].rearrange("(c p) t -> p c t", p=128), reads=[src],
                  writes=[LD[d][b][nm]])

    def mm(ps_ap, ps, lhsT, lb, rhs, rb, start=True, stop=True):
        K.op("pe", lambda en: en.matmul(ps_ap, lhsT=lhsT, rhs=rhs, start=start, stop=stop), reads=[lb, rb], writes=[ps])

    def unit(d, n, c, ysb):
        b = n % 2
        ld = LD[d][b]
        bd = r_bd()
        for ti, nm in enumerate(TEN):
            src = ld[nm][:, c, :]
            K.op(ve3(), lambda en, ti=ti, src=src: en.tensor_tensor(
                out=bd[:, ti, :].rearrange("p (a b) -> p a b", a=2), in0=src.unsqueeze(1).to_broadcast([128, 2, 64]),
                in1=BDM[:].rearrange("p (a b) -> p a b", a=2), op=ALU.mult), reads=[ld[nm], BDM], writes=[bd])
        yield
        A_, B_, K_, R_ = (bd[:, t_, :] for t_ in range(4))
        pst = K.ps()
        for t_ in range(3):
            K.op("pe", lambda en, t_=t_: en.transpose(out=pst[:, t_ * 128:(t_ + 1) * 128], in_=bd[:, 4 + t_, :],
                                                      identity=identS[:]), reads=[bd, identS], writes=[pst])
        tp = r_tp()
        K.op("act", lambda en: en.copy(out=tp[:], in_=pst[:, 0:384]), reads=[pst], writes=[tp])
        BhT, KhT, VT = (tp[:, t_ * 128:(t_ + 1) * 128] for t_ in range(3))
        p1 = K.ps()
        mm(p1[:, 0:128], p1, B_, bd, A_, bd)
        mm(p1[:, 128:256], p1, A_, bd, B_, bd)
        mm(p1[:, 256:384], p1, K_, bd, A_, bd)
        mm(p1[:, 384:512], p1, B_, bd, R_, bd)
        p2 = K.ps()
        mm(p2[:, 0:128], p2, K_, bd, R_, bd)
        mt = r_m()
        K.op("dve", lambda en: en.tensor_tensor(out=mt[:, 0:512], in0=p1[:, :], in1=M4[d][:], op=ALU.mult),
             reads=[p1, M4[d]], writes=[mt])
        K.op("dve", lambda en: en.tensor_tensor(out=mt[:, 512:640], in0=p2[:, 0:128], in1=MI[d][:], op=ALU.mult),
             reads=[p2, MI[d]], writes=[mt])
        Q, QT, MakT, MrbT, MrkT = (mt[:, t_ * 128:(t_ + 1) * 128] for t_ in range(5))
        W = r_w()
        K.op("pool", lambda en: en.tensor_tensor(out=W[:], in0=Q, in1=identS[:], op=ALU.add),
             reads=[mt, identS], writes=[W])
        yield
        qb = mt
        for lvl in range(1, 6):
            pq = K.ps()
            mm(pq[:, 128:256], pq, Q, qb, QT, qb)
            if lvl < 5:
                mm(pq[:, 0:128], pq, QT, qb, Q, qb)
            nq = r_q()
            if lvl < 5:
                K.op("act", lambda en: en.copy(out=nq[:], in_=pq[:, 0:256]), reads=[pq], writes=[nq])
            else:
                K.op("act", lambda en: en.copy(out=nq[:, 128:256], in_=pq[:, 128:256]), reads=[pq], writes=[nq])
            Q, QT, qb = nq[:, 0:128], nq[:, 128:256], nq
            yield
            pw_ = K.ps()
            mm(pw_[:, 0:128], pw_, QT, qb, W[:], W)
            W2_ = r_w()
            K.op("dve", lambda en: en.tensor_tensor(out=W2_[:], in0=pw_[:, 0:128], in1=W[:], op=ALU.add),
                 reads=[pw_, W], writes=[W2_])
            W = W2_
            yield
        Sb = S[d][c]
        px = K.ps()
        mm(px[:, 0:128], px, A_, bd, Sb[:], Sb, True, False)
        mm(px[:, 0:128], px, MakT, mt, VT, tp, False, True)
        Xs = r_x()
        K.op("act", lambda en: en.copy(out=Xs[:], in_=px[:, 0:128]), reads=[px], writes=[Xs])
        yield
        pu = K.ps()
        mm(pu[:, 0:128], pu, W[:], W, Xs[:], Xs)
        Us = r_u()
        K.op("dve", lambda en: en.tensor_copy(out=Us[:], in_=pu[:, 0:128]), reads=[pu], writes=[Us])
        if self.dbg.get("e3dbg") is not None and d == 0 and c == 0 and n in (0, 1):
            dd = self.dbg["e3dbg"]
            K.dma("sp", dd[n, 0, :, 0:128], Xs[:], reads=[Xs])
            K.dma("sp", dd[n, 1, :, 0:128], Us[:], reads=[Us])
            K.dma("sp", dd[n, 2, :, 0:128], W[:], reads=[W])
            K.dma("sp", dd[n, 3, :, 0:640], mt[:], reads=[mt])
            K.dma("sp", dd[n, 4, :, 0:128], Sb[:], reads=[Sb])
            K.dma("sp", dd[n, 5, :, 0:384], tp[:], reads=[tp])
            for t_ in range(4):
                K.dma("sp", dd[n, 6, :, t_ * 128:(t_ + 1) * 128], bd[:, t_, :], reads=[bd])
        yield
        py = K.ps()
        mm(py[:, 0:128], py, R_, bd, Sb[:], Sb, True, False)
        mm(py[:, 0:128], py, MrbT, mt, Us[:], Us, False, False)
        mm(py[:, 0:128], py, MrkT, mt, VT, tp, False, True)
        pss = K.ps()
        mm(pss[:, 0:128], pss, BhT, tp, Us[:], Us, True, False)
        mm(pss[:, 0:128], pss, KhT, tp, VT, tp, False, True)
        K.op("act", lambda en: en.copy(out=ysb[0:64, c, :], in_=py[0:64, 0:64]), reads=[py], writes=[ysb])
        K.op("act", lambda en: en.copy(out=ysb[64:128, c, :], in_=py[64:128, 64:128]), reads=[py], writes=[ysb])
        gcol = tok_base(d, n) // 64
        K.op("dve", lambda en: en.scalar_tensor_tensor(out=Sb[:], in0=Sb[:], scalar=GLall[d][:, c, gcol:gcol + 1],
                                                       in1=pss[:, 0:128], op0=ALU.mult, op1=ALU.add),
             reads=[Sb, GLall[d], pss], writes=[Sb])

    for d in range(2):
        load(d, 0)
    for n in range(NCH):
        for d in range(2):
            if n + 1 < NCH:
                load(d, n + 1)
        ysbs = [r_y(), r_y()]
        gens = [unit(d, n, c, ysbs[d]) for c in range(4) for d in range(2)]
        while gens:
            alive = []
            for g in gens:
                try:
                    next(g)
                    alive.append(g)
                except StopIteration:
                    pass
            gens = alive
        for d in range(2):
            tb = tok_base(d, n)
            dst = e[f"YD{d}"][tb:tb + 64, :].rearrange("t (c h v) -> h t c v", h=2, v=64)
            for h2 in range(2):
                K.dma("sp", dst[h2], ysbs[d][h2 * 64:(h2 + 1) * 64, :, :], reads=[ysbs[d]], writes=[e[f"YD{d}"]])
    if self.dbg.get("S") is not None:
        for d in range(2):
            for c in range(4):
                K.dma("sp", self.dbg["S"][d, c], S[d][c][:], reads=[S[d][c]])
    K.pop_scope()


Prog.even_e3 = _even_e3
def _even_e4(self, i, ntile):
    K = self.K
    j = i // 2
    e = self.ed
    K.push_scope()
    wout = K.sb([128, 8, D], BF16, "e4wout")
    stg = [K.sb([128, D], F32, f"e4stg{q}") for q in range(2)]
    for kt in range(8):
        sg = stg[kt % 2]
        K.dma("sp", sg[:], self.inp["w_out"][j, kt * 128:(kt + 1) * 128, :], writes=[sg])
        K.op("dve", lambda en, sg=sg, kt=kt: en.tensor_copy(out=wout[:, kt, :], in_=sg[:]), reads=[sg], writes=[wout])
    GN = K.sb([128, 2, 4], F32, "e4gn")
    K.dma("sp", GN[:, 0, :], self.inp["gn_w"][j].rearrange("(c k) -> k c", k=128), writes=[GN], allow_slow_non_contiguous=True)
    K.dma("sp", GN[:, 1, :], self.inp["gn_b"][j].rearrange("(c k) -> k c", k=128), writes=[GN], allow_slow_non_contiguous=True)
    y0s = [K.sb([128, 512], F32, f"e4y0{q}") for q in range(2)]
    y1s = [K.sb([128, 512], F32, f"e4y1{q}") for q in range(2)]
    sqs = [K.sb([128, 512], F32, f"e4sq{q}") for q in range(2)]
    sts = [K.sb([128, 4, 8], F32, f"e4st{q}") for q in range(2)]
    bons = [K.sb([128, 4, 128], F32, f"e4bon{q}") for q in range(2)]
    ggs = [K.sb([128, 4, 128], F32, f"e4gg{q}") for q in range(2)]
    ybs = [K.sb([128, 4, 128], F32, f"e4yb{q}") for q in range(2)]
    yTs = [K.sb([128, 4, 128], F32, f"e4yT{q}") for q in range(2)]
    cats = [K.sb([128, 8, 128], BF16, f"e4cat{q}") for q in range(2)]
    xts = stg
    ots = [K.sb([128, D], F32, f"e4o{q}") for q in range(2)]
    for tt in range(ntile):
        y0, y1, sq, st, bon, gg, yb, yT, cat, xt, ot = (l[tt % 2] for l in (y0s, y1s, sqs, sts, bons, ggs, ybs, yTs, cats,
                                                                            xts, ots))
        M = self.Ml if tt < 64 else self.Mc
        tk = slice(tt * 128, (tt + 1) * 128)
        K.dma("sp", y0[:], e["YD0"][tk, :], reads=[e["YD0"]], writes=[y0])
        K.dma("sp", y1[:], e["YD1"][tk, :], reads=[e["YD1"]], writes=[y1])
        for buf, nm in ((bon, "BON"), (gg, "GG"), (yb, "YB")):
            K.dma("sp", buf[:], e[nm][:, tk].rearrange("(c p) t -> p c t", p=128), reads=[e[nm]], writes=[buf])
        K.dma("sp", xt[:], self.lat[tk, :], reads=[self.lat.p(tt)], writes=[xt])
        K.op("pool", lambda en: en.tensor_tensor(out=y0[:], in0=y0[:], in1=y1[:], op=ALU.add), reads=[y0, y1], writes=[y0])
        yv = y0[:].rearrange("p (h v) -> p h v", v=64)
        sv = sq[:].rearrange("p (h v) -> p h v", v=64)
        K.op("dve", lambda en: en.reduce_sum(out=st[:, 0, :], in_=yv, axis=AX.X), reads=[y0], writes=[st])
        K.op("dve", lambda en: en.tensor_scalar(out=st[:, 1, :], in0=st[:, 0, :], scalar1=1.0 / 64, scalar2=None,
                                                op0=ALU.mult), reads=[st], writes=[st])
        K.op("dve", lambda en: en.tensor_tensor(out=yv, in0=yv, in1=st[:, 1, :].unsqueeze(2).to_broadcast([128, 8, 64]),
                                                op=ALU.subtract), reads=[y0, st], writes=[y0])
        K.op("pool", lambda en: en.tensor_tensor(out=sq[:], in0=y0[:], in1=y0[:], op=ALU.mult), reads=[y0], writes=[sq])
        K.op("dve", lambda en: en.reduce_sum(out=st[:, 2, :], in_=sv, axis=AX.X), reads=[sq], writes=[st])
        K.op("dve", lambda en: en.tensor_scalar(out=st[:, 2, :], in0=st[:, 2, :], scalar1=1.0 / 64, scalar2=64e-5,
                                                op0=ALU.mult, op1=ALU.add), reads=[st], writes=[st])
        K.op("act", lambda en: en.sqrt(out=st[:, 3, :], in_=st[:, 2, :]), reads=[st], writes=[st])
        K.op("dve", lambda en: en.reciprocal(out=st[:, 3, :], in_=st[:, 3, :]), reads=[st], writes=[st])
        K.op("dve", lambda en: en.tensor_tensor(out=yv, in0=yv, in1=st[:, 3, :].unsqueeze(2).to_broadcast([128, 8, 64]),
                                                op=ALU.mult), reads=[y0, st], writes=[y0])
        ps = K.ps()
        for c in range(4):
            self.tr(None, ps[:, c * 128:(c + 1) * 128], y0[:, c * 128:(c + 1) * 128], ps, y0)
        for c in range(4):
            K.op("dve", lambda en, c=c: en.tensor_scalar(out=yT[:, c, :], in0=ps[:, c * 128:(c + 1) * 128],
                                                         scalar1=GN[:, 0, c:c + 1], scalar2=GN[:, 1, c:c + 1],
                                                         op0=ALU.mult, op1=ALU.add), reads=[ps, GN], writes=[yT])
        K.op("pool", lambda en: en.tensor_tensor(out=yT[:], in0=yT[:], in1=bon[:], op=ALU.add), reads=[yT, bon],
             writes=[yT])
        K.op("pool", lambda en: en.tensor_tensor(out=cat[:, 0:4, :], in0=yT[:], in1=gg[:], op=ALU.mult), reads=[yT, gg],
             writes=[cat])
        K.op("act", lambda en: en.copy(out=cat[:, 4:8, :], in_=yb[:]), reads=[yb], writes=[cat])
        for hh in range(2):
            po = K.ps()
            for kt in range(8):
                K.op("pe", lambda en, kt=kt: en.matmul(po[:, :], lhsT=cat[:, kt, :], rhs=wout[:, kt, hh * 512:(hh + 1) * 512],
                                                       start=(kt == 0), stop=(kt == 7)), reads=[cat, wout], writes=[po])
            K.op("dve", lambda en: en.tensor_tensor(out=ot[:, hh * 512:(hh + 1) * 512], in0=po[:, :],
                                                    in1=M[:, 2, hh * 512:(hh + 1) * 512], op=ALU.mult),
                 reads=[po, M], writes=[ot])
        K.op("pool", lambda en: en.tensor_tensor(out=ot[:], in0=ot[:], in1=xt[:], op=ALU.add), reads=[ot, xt], writes=[ot])
        K.dma("sp", self.lat[tk, :], ot[:], reads=[ot], writes=[self.lat.p(tt)])
    K.pop_scope()


def _even_mixer(self, i):
    self.even_e1(i, ntile=66)
    self.even_e2(i)
    self.even_e3(i)
    self.even_e4(i, ntile=66 if i < 2 else 64)


Prog.even_e4 = _even_e4
Prog.even_mixer = _even_mixer
Prog.even_declare = _even_declare
Prog.even_e1 = _even_e1


def build_program():
    P = Prog()
    P.fourier_declare()
    P.even_declare()
    P.init_lat()
    P.prep_s()
    for i in range(DEPTH):
        P.modvec(i, need_ctx=(i <= 2))
        if i % 2 == 0:
            P.even_mixer(i)
        else:
            P.fourier(i, with_ctx=(i < 2))
        P.moe_setup()
        P.moe(i, with_ctx=(i < 2))
    P.final_norm()
    P.K.finish()
    return P


def kernel(**inputs):
    x = np.asarray(inputs["x"], dtype=np.float32)
    B = x.shape[0]
    P = build_program()
    consts = fourier_consts()
    pconsts = pool_consts()
    in_maps = []
    for b in range(B):
        d = {"x": np.ascontiguousarray(x[b]),
             "c": np.ascontiguousarray(np.asarray(inputs["c"], dtype=np.float32)[b:b + 1]),
             "ctx": np.ascontiguousarray(np.asarray(inputs["ctx"], dtype=np.float32)[b]),
             "c_ctx": np.ascontiguousarray(np.asarray(inputs["c_ctx"], dtype=np.float32)[None, :])}
        for k in WEIGHT_SPECS:
            d[k] = np.ascontiguousarray(np.asarray(inputs[k], dtype=np.float32))
        d.update(consts)
        d.update(pconsts)
        in_maps.append(d)
    res = run_bass_kernel_spmd(P.K.nc, in_maps, core_ids=list(range(B)))
    return np.stack([np.asarray(r["out"]) for r in res.results], axis=0).astype(np.float32)
```

```python
import numpy as np
from contextlib import ExitStack
import concourse.bass as bass
import concourse.mybir as mybir
from concourse.bass_utils import run_bass_kernel_spmd

F32 = mybir.dt.float32
BF16 = mybir.dt.bfloat16
I32 = mybir.dt.int32
AF = mybir.ActivationFunctionType
ALU = mybir.AluOpType
AX = mybir.AxisListType

D = 1024
T = 8192
CT = 256
NT = T + CT
DEPTH = 4
NEXP = 32
DE = 512


class Buf:
    def __init__(self, t, name):
        self.t = t
        self.name = name
        self.lw = None
        self.rd = {}

    def __getitem__(self, idx):
        return self.t[idx]


class Parts:
    def __init__(self, t, name):
        self.t = t
        self.name = name
        self.parts = {}

    def p(self, key):
        b = self.parts.get(key)
        if b is None:
            b = Buf(self.t, f"{self.name}.{key}")
            self.parts[key] = b
        return b

    def all(self):
        return list(self.parts.values())

    def __getitem__(self, idx):
        return self.t[idx]


class Ctx:
    KD = 16
    SAME_ENG_SYNC = True

    def __init__(self):
        self.nc = bass.Bass("TRN2", target_bir_lowering=False)
        nc = self.nc
        self.es = ExitStack()
        self.eng = {"pe": nc.tensor, "act": nc.scalar, "dve": nc.vector, "pool": nc.gpsimd, "sp": nc.sync}
        self.csem = {e: self.es.enter_context(nc.semaphore("c_" + e)) for e in ("pe", "act", "dve", "pool")}
        self.ccnt = {e: 0 for e in self.csem}
        self.dsem = {q: [self.es.enter_context(nc.semaphore(f"d_{q}{i}")) for i in range(self.KD)]
                     for q in ("sp", "pool", "act")}
        self.dcnt = {q: 0 for q in self.dsem}
        self.known = {e: {} for e in self.eng}
        self.nalloc = 0
        self.psum_banks = []
        self.psum_i = 0

    def sb(self, shape, dtype=F32, name=None):
        self.nalloc += 1
        name = (name or "sb") + f"_{self.nalloc}"
        es = self.scopes[-1] if getattr(self, "scopes", None) else self.es
        t = es.enter_context(self.nc.sbuf_tensor(name, list(shape), dtype))
        return Buf(t, name)

    def push_scope(self):
        if not hasattr(self, "scopes"):
            self.scopes = []
        self.scopes.append(ExitStack())

    def pop_scope(self):
        self.barrier()
        self.scopes.pop().close()

    def barrier(self):
        for e in self.eng:
            for src, sem in self.csem.items():
                if self.ccnt[src] > 0:
                    self._wait(e, (sem, self.ccnt[src], "bar"))
            self._wait_all_dma(e)

    def _wait_all_dma(self, e):
        for q in self.dsem:
            n = self.dcnt[q]
            for r in range(self.KD):
                cnt = (n - r + self.KD - 1) // self.KD if n > r else 0
                if cnt > 0:
                    self._wait(e, (self.dsem[q][r], 16 * cnt, "dma"))

    def dram(self, name, shape, dtype=F32, kind="Internal"):
        return self.nc.dram_tensor(name, list(shape), dtype, kind=kind)

    def init_psum(self, n=8):
        for i in range(n):
            t = self.es.enter_context(self.nc.psum_tensor(f"ps{i}", [128, 512], F32))
            b = Buf(t, f"ps{i}")
            b.excl = True
            self.psum_banks.append(b)

    def ps(self):
        b = self.psum_banks[self.psum_i % len(self.psum_banks)]
        self.psum_i += 1
        return b

    def _wait(self, e, ev):
        if ev is None:
            return
        sem, val, src = ev
        if src == e and (e == "pe" or not self.SAME_ENG_SYNC):
            return
        k = self.known[e]
        key = sem.name
        if k.get(key, 0) >= val:
            return
        self.eng[e].wait_ge(sem, val)
        k[key] = val

    def _deps(self, e, reads, writes):
        for b in reads:
            self._wait(e, b.lw)
            if getattr(b, "excl", False):
                for ke, ev in b.rd.items():
                    if ke != e:
                        self._wait(e, ev)
        for b in writes:
            self._wait(e, b.lw)
            for ev in b.rd.values():
                self._wait(e, ev)

    def _commit(self, ev, key, reads, writes):
        for b in writes:
            b.lw = ev
            b.rd = {}
        for b in reads:
            b.rd[key] = ev

    def op(self, e, fn, reads=(), writes=()):
        self._deps(e, reads, writes)
        ins = fn(self.eng[e])
        self.ccnt[e] += 1
        ins.then_inc(self.csem[e], 1)
        ev = (self.csem[e], self.ccnt[e], e)
        self._commit(ev, e, reads, writes)
        return ins

    def dma(self, q, out, in_, reads=(), writes=(), indirect=None, **kw):
        i = self.dcnt[q]
        self.dcnt[q] += 1
        sem = self.dsem[q][i % self.KD]
        val = 16 * (i // self.KD + 1)
        if i >= self.KD:
            self._wait(q, (sem, val - 16, "dma"))
        self._deps(q, reads, writes)
        if indirect is None:
            ins = self.eng[q].dma_start(out=out, in_=in_, **kw)
        else:
            ins = self.eng[q].indirect_dma_start(out=out, in_=in_, **indirect)
        ins.then_inc(sem, 16)
        ev = (sem, val, "dma")
        self._commit(ev, (q, i % self.KD), reads, writes)
        return ins

    def finish(self):
        self._wait_all_dma("sp")
        self.es.close()


WEIGHT_SPECS = {
    "ada_w": [4, 1024, 6144], "ada_b": [4, 6144], "norm_mix": [4, 1024], "norm_ffn": [4, 1024],
    "w_in": [2, 1024, 2048], "mu_x": [2, 3, 1024], "mu_p": [2, 3, 512],
    "decay_w0": [2, 2, 512], "decay_w1": [2, 2, 1024, 64], "decay_w2": [2, 2, 64, 512],
    "lr_a0": [2, 2, 512], "lr_a1": [2, 2, 1024, 64], "lr_a2": [2, 2, 64, 512],
    "gate_g1": [2, 1024, 128], "gate_g2": [2, 128, 512],
    "k_k": [2, 512], "k_a": [2, 512], "r_k": [2, 8, 64], "gn_w": [2, 512], "gn_b": [2, 512],
    "pool_w": [2, 4, 128, 128], "pool_scale": [2, 512], "w_out": [2, 1024, 1024],
    "w_fourier": [2, 1024, 1024],
    "router_c": [4, 1024, 4], "router_c_b": [4, 4], "router_f": [4, 1024, 32], "router_f_b": [4, 32],
    "moe_w1": [4, 32, 1024, 512], "moe_w3": [4, 32, 1024, 512], "moe_w2": [4, 32, 512, 1024],
    "final_norm": [1024],
}


class Prog:
    BLK = 384
    SCAN_DT = BF16

    def __init__(self, debug=None, skip=()):
        self.K = Ctx()
        K = self.K
        nc = K.nc
        self.debug = debug or {}
        self.inp = {}
        self.inp["x"] = nc.dram_tensor("x", [T, D], F32, kind="ExternalInput")
        self.inp["c"] = nc.dram_tensor("c", [1, D], F32, kind="ExternalInput")
        self.inp["ctx"] = nc.dram_tensor("ctx", [CT, D], F32, kind="ExternalInput")
        self.inp["c_ctx"] = nc.dram_tensor("c_ctx", [1, D], F32, kind="ExternalInput")
        for k, shp in WEIGHT_SPECS.items():
            if k in skip:
                continue
            self.inp[k] = nc.dram_tensor(k, shp, F32, kind="ExternalInput")
        self.out = nc.dram_tensor("out", [T, D], F32, kind="ExternalOutput")
        self.dbg = {}
        for k, (shp, dt) in self.debug.items():
            self.dbg[k] = nc.dram_tensor("dbg_" + k, shp, dt, kind="ExternalOutput")
        K.init_psum(8)
        self.lat = Parts(nc.dram_tensor("lat", [NT, D], F32), "lat")
        self.ident = K.sb([128, 128], F32, "ident")
        self.identb = K.sb([128, 128], BF16, "identb")
        self.ones = K.sb([128, 128], F32, "ones")
        self._consts()
        self.Ml = K.sb([128, 6, D], F32, "Ml")
        self.Mc = K.sb([128, 6, D], F32, "Mc")
        self.srep = K.sb([128, 2, 8, 128], F32, "srep")

    def _consts(self):
        K = self.K
        K.op("pool", lambda e: e.memset(self.ones[:], 1.0), writes=[self.ones])
        K.op("pool", lambda e: e.memset(self.ident[:], 0.0), writes=[self.ident])
        K.op("pool", lambda e: e.affine_select(out=self.ident[:], in_=self.ident[:], pattern=[[-1, 128]],
                                               compare_op=ALU.not_equal, fill=1.0, base=0, channel_multiplier=1),
             reads=[self.ident], writes=[self.ident])
        K.op("dve", lambda e: e.tensor_copy(out=self.identb[:], in_=self.ident[:]), reads=[self.ident],
             writes=[self.identb])

    def prep_s(self):
        K = self.K
        craw = K.sb([128, 2, 8], F32, "craw")
        csil = K.sb([128, 2, 8], F32, "csil")
        for w, nm in enumerate(("c", "c_ctx")):
            src = self.inp[nm].ap().rearrange("o (kt k) -> k (o kt)", k=128)
            K.dma("sp", craw[:, w, :], src, writes=[craw], allow_slow_non_contiguous=True)
        K.op("act", lambda e: e.activation(out=csil[:], in_=craw[:], func=AF.Silu), reads=[craw], writes=[csil])
        K.op("dve", lambda e: e.tensor_copy(out=self.srep[:], in_=csil[:].unsqueeze(3).to_broadcast([128, 2, 8, 128])),
             reads=[csil], writes=[self.srep])

    def modvec(self, i, need_ctx=True):
        K = self.K
        K.push_scope()
        mv = dict(
            W=[K.sb([128, 8, 512], F32, f"adaW{j}") for j in range(2)],
            b=[K.sb([1, 512], F32, f"adab{j}") for j in range(2)],
            g=K.sb([128, 2, D], F32, "normg"),
        )
        aw = self.inp["ada_w"]
        ab = self.inp["ada_b"]
        g = mv["g"]
        K.dma("sp", g[:, 0, :], self.inp["norm_mix"][i:i + 1, :].partition_broadcast(128), writes=[g])
        K.dma("sp", g[:, 1, :], self.inp["norm_ffn"][i:i + 1, :].partition_broadcast(128), writes=[g])
        targets = [(0, self.Ml)] + ([(1, self.Mc)] if need_ctx else [])
        for nb in range(12):
            W = mv["W"][nb % 2]
            bb = mv["b"][nb % 2]
            K.dma("sp", W[:], aw[i, :, nb * 512:(nb + 1) * 512].rearrange("(kt k) n -> k kt n", k=128), writes=[W])
            K.dma("sp", bb[:], ab[i:i + 1, nb * 512:(nb + 1) * 512], writes=[bb])
            for w, M in targets:
                ps = K.ps()
                for kt in range(8):
                    K.op("pe", lambda e, kt=kt, w=w, ps=ps, W=W: e.matmul(ps[:, :], lhsT=self.srep[:, w, kt, :],
                                                                         rhs=W[:, kt, :], start=(kt == 0), stop=False),
                         reads=[self.srep, W], writes=[ps])
                K.op("pe", lambda e, ps=ps, bb=bb: e.matmul(ps[:, :], lhsT=self.ones[0:1, :], rhs=bb[0:1, :],
                                                            start=False, stop=True),
                     reads=[self.ones, bb], writes=[ps])
                s, half = nb // 2, nb % 2
                dst = M[:, s, half * 512:(half + 1) * 512]
                if s in (1, 4):
                    gi = 0 if s == 1 else 1
                    K.op("dve", lambda e, dst=dst, ps=ps, gi=gi, half=half: e.scalar_tensor_tensor(
                        out=dst, in0=ps[:, :], scalar=1.0, in1=g[:, gi, half * 512:(half + 1) * 512],
                        op0=ALU.add, op1=ALU.mult), reads=[ps, g], writes=[M])
                else:
                    K.op("act", lambda e, dst=dst, ps=ps: e.copy(out=dst, in_=ps[:, :]), reads=[ps], writes=[M])
        K.pop_scope()

    def norm_tile(self, xt, ht, M, sub, st):
        K = self.K
        sh = 0 if sub == 0 else 3
        ga = 1 if sub == 0 else 4
        K.op("act", lambda e: e.activation(out=ht[:], in_=xt[:], func=AF.Square, accum_out=st[:, 0:1]),
             reads=[xt], writes=[ht, st])
        K.op("dve", lambda e: e.tensor_scalar(out=st[:, 1:2], in0=st[:, 0:1], scalar1=1.0 / D, scalar2=1e-6,
                                              op0=ALU.mult, op1=ALU.add), reads=[st], writes=[st])
        K.op("act", lambda e: e.sqrt(out=st[:, 3:4], in_=st[:, 1:2]), reads=[st], writes=[st])
        K.op("dve", lambda e: e.reciprocal(out=st[:, 2:3], in_=st[:, 3:4]), reads=[st], writes=[st])
        K.op("dve", lambda e: e.scalar_tensor_tensor(out=ht[:], in0=xt[:], scalar=st[:, 2:3], in1=M[:, ga, :],
                                                     op0=ALU.mult, op1=ALU.mult), reads=[xt, st, M], writes=[ht])
        K.op("pool", lambda e: e.tensor_tensor(out=ht[:], in0=ht[:], in1=M[:, sh, :], op=ALU.add),
             reads=[ht, M], writes=[ht])

    def tr(self, e_unused, dst_ps_ap, src_ap, ps, src_buf, n_in=128):
        K = self.K
        K.op("pe", lambda e: e.transpose(out=dst_ps_ap, in_=src_ap, identity=self.ident[0:n_in, 0:n_in]),
             reads=[src_buf, self.ident], writes=[ps])

    def init_lat(self):
        K = self.K
        for r in range(0, T, 2048):
            K.dma("sp", self.lat[r:r + 2048, :], self.inp["x"][r:r + 2048, :],
                  writes=[self.lat.p(t) for t in range(r // 128, r // 128 + 16)])
        K.dma("sp", self.lat[T:NT, :], self.inp["ctx"][:, :], writes=[self.lat.p(64), self.lat.p(65)])

    def moe_dram(self):
        nc = self.K.nc
        BLK = self.BLK
        self.NB = (2 * NT + 32 * BLK) // BLK
        NB = self.NB
        self.mdram = dict(H2=Parts(nc.dram_tensor("H2", [NT, D], F32), "H2"),
                          XS=Buf(nc.dram_tensor("XS", [NB * BLK, D], F32), "XS"),
                          YS=Buf(nc.dram_tensor("YS", [NB * BLK, D], F32), "YS"))

    def moe_setup(self):
        K = self.K
        nc = K.nc
        if not hasattr(self, "mdram"):
            self.moe_dram()
        K.push_scope()
        m = dict(self.mdram)
        NTL = 66
        NB = self.NB
        m["OHA"] = K.sb([128, NTL, 2, 32], BF16, "OHA")
        m["RK"] = K.sb([128, NTL, 2], F32, "RK")
        m["cs"] = K.sb([128, 8, 32], F32, "moecs")
        m["be"] = K.sb([128, 3, NB], F32, "moebe")
        m["idf"] = K.sb([128, NB, 8], F32, "moeidf")
        m["GATE"] = K.sb([128, NTL, 2], F32, "GATE")
        m["DEST"] = K.sb([128, NTL, 2], I32, "DEST")
        m["carry"] = K.sb([128, 32], F32, "carry")
        m["Wr"] = K.sb([128, 8, 36], F32, "Wr")
        m["br"] = K.sb([1, 36], F32, "br")
        m["UT"] = K.sb([128, 128], F32, "UT")
        m["PIDX"] = K.sb([128, 8], F32, "PIDX")
        m["JV"] = K.sb([128, NB], F32, "JV")
        m["IDX1"] = K.sb([128, NB, 8], I32, "IDX1")
        m["IDX2"] = K.sb([128, NB, 4], I32, "IDX2")
        big = [K.sb([128, D], F32, f"big{j}") for j in range(8)]
        m["xt"] = big[0:2]
        m["ht"] = big[2:4]
        m["yb"] = big[4:6]
        m["y0"] = big[4:6]
        m["y1"] = big[6:8]
        m["st"] = [K.sb([128, 4], F32, f"mst{j}") for j in range(2)]
        m["hT"] = [K.sb([128, 8, 128], F32, f"mhT{j}") for j in range(2)]
        m["xTb"] = [K.sb([128, 8, 128], BF16, f"mxTb{j}") for j in range(2)]
        m["W1"] = [K.sb([128, 8, 512], BF16, f"mW1{j}") for j in range(2)]
        m["W3"] = [K.sb([128, 8, 512], BF16, f"mW3{j}") for j in range(2)]
        m["W2"] = [K.sb([128, 4, 1024], BF16, f"mW2{j}") for j in range(2)]
        m["hTb"] = [K.sb([128, 4, 128], BF16, f"mhTb{j}") for j in range(2)]
        m["sil"] = [K.sb([128, 512], F32, f"msil{j}") for j in range(2)]
        m["sm"] = [K.sb([128, 128], F32, f"msm{j}") for j in range(2)]
        UT = m["UT"]
        K.op("pool", lambda e: e.memset(UT[:], 1.0), writes=[UT])
        K.op("pool", lambda e: e.affine_select(out=UT[:], in_=UT[:], pattern=[[1, 128]], compare_op=ALU.is_gt,
                                               fill=0.0, base=0, channel_multiplier=-1), reads=[UT], writes=[UT])
        pi = K.sb([128, 8], I32, "pidx_i")
        K.op("pool", lambda e: e.iota(pi[:], pattern=[[128, 8]], base=0, channel_multiplier=1), writes=[pi])
        K.op("dve", lambda e: e.tensor_copy(out=m["PIDX"][:], in_=pi[:]), reads=[pi], writes=[m["PIDX"]])
        ji = K.sb([128, NB], I32, "jv_i")
        K.op("pool", lambda e: e.iota(ji[:], pattern=[[self.BLK, NB]], base=0, channel_multiplier=0), writes=[ji])
        K.op("dve", lambda e: e.tensor_copy(out=m["JV"][:], in_=ji[:]), reads=[ji], writes=[m["JV"]])
        self.m = m

    def moe(self, i, with_ctx, final=False):
        K = self.K
        m = self.m
        NB = self.NB
        ntile = 66 if with_ctx else 64
        Wr, br = m["Wr"], m["br"]
        K.dma("sp", Wr[:, :, 0:4], self.inp["router_c"][i].rearrange("(kt k) n -> k kt n", k=128), writes=[Wr])
        K.dma("sp", Wr[:, :, 4:36], self.inp["router_f"][i].rearrange("(kt k) n -> k kt n", k=128), writes=[Wr])
        K.dma("sp", br[:, 0:4], self.inp["router_c_b"][i:i + 1, :], writes=[br])
        K.dma("sp", br[:, 4:36], self.inp["router_f_b"][i:i + 1, :], writes=[br])
        carry = m["carry"]
        K.op("dve", lambda e: e.memset(carry[:], 0.0), writes=[carry])
        OHA, RK, GATE, DEST = m["OHA"], m["RK"], m["GATE"], m["DEST"]
        for tt in range(ntile):
            xt, ht, st, hT, sm = (m[k][tt % 2] for k in ("xt", "ht", "st", "hT", "sm"))
            M = self.Ml if tt < 64 else self.Mc
            K.dma("sp", xt[:], self.lat[tt * 128:(tt + 1) * 128, :], reads=[self.lat.p(tt)], writes=[xt])
            self.norm_tile(xt, ht, M, 1, st)
            K.dma("act", m["H2"][tt * 128:(tt + 1) * 128, :], ht[:], reads=[ht], writes=[m["H2"].p(tt)])
            for half in range(2):
                ps = K.ps()
                for q in range(4):
                    kt = half * 4 + q
                    self.tr(None, ps[:, q * 128:(q + 1) * 128], ht[:, kt * 128:(kt + 1) * 128], ps, ht)
                K.op("act" if half else "dve",
                     lambda e, ps=ps, half=half: (e.copy if half else e.tensor_copy)(
                         out=hT[:, half * 4:(half + 1) * 4, :].rearrange("p a b -> p (a b)"), in_=ps[:, :]),
                     reads=[ps], writes=[hT])
            ps = K.ps()
            for kt in range(8):
                K.op("pe", lambda e, kt=kt, ps=ps: e.matmul(ps[:, 0:36], lhsT=hT[:, kt, :], rhs=Wr[:, kt, :],
                                                           start=(kt == 0), stop=False),
                     reads=[hT, Wr], writes=[ps])
            K.op("pe", lambda e, ps=ps: e.matmul(ps[:, 0:36], lhsT=self.ones[0:1, :], rhs=br[0:1, :],
                                                 start=False, stop=True), reads=[self.ones, br], writes=[ps])
            def dv(fn, rd=(), wr=()):
                K.op("dve", fn, reads=[sm] + list(rd), writes=[sm] + list(wr))
            K.op("dve", lambda e, ps=ps: e.tensor_copy(out=sm[:, 0:36], in_=ps[:, 0:36]), reads=[ps], writes=[sm])
            dv(lambda e: e.reduce_max(out=sm[:, 36:37], in_=sm[:, 0:4], axis=AX.X))
            dv(lambda e: e.tensor_scalar(out=sm[:, 37:38], in0=sm[:, 36:37], scalar1=-1.0, scalar2=None, op0=ALU.mult))
            dv(lambda e: e.tensor_scalar(out=sm[:, 40:44], in0=sm[:, 0:4], scalar1=sm[:, 36:37], scalar2=None,
                                         op0=ALU.is_equal))
            K.op("act", lambda e: e.activation(out=sm[:, 44:48], in_=sm[:, 0:4], func=AF.Exp, bias=sm[:, 37:38],
                                               scale=1.0, accum_out=sm[:, 38:39]), reads=[sm], writes=[sm])
            dv(lambda e: e.reciprocal(out=sm[:, 39:40], in_=sm[:, 38:39]))
            dv(lambda e: e.tensor_scalar(out=sm[:, 48:56], in0=sm[:, 4:12], scalar1=sm[:, 40:41], scalar2=None,
                                         op0=ALU.mult))
            for g in range(1, 4):
                dv(lambda e, g=g: e.scalar_tensor_tensor(out=sm[:, 48:56], in0=sm[:, 4 + 8 * g:12 + 8 * g],
                                                         scalar=sm[:, 40 + g:41 + g], in1=sm[:, 48:56],
                                                         op0=ALU.mult, op1=ALU.add))
            dv(lambda e: e.reduce_max(out=sm[:, 56:57], in_=sm[:, 48:56], axis=AX.X))
            dv(lambda e: e.tensor_scalar(out=sm[:, 60:68], in0=sm[:, 48:56], scalar1=sm[:, 56:57], scalar2=None,
                                         op0=ALU.is_equal))
            dv(lambda e: e.scalar_tensor_tensor(out=sm[:, 68:76], in0=sm[:, 60:68], scalar=-1e30, in1=sm[:, 48:56],
                                                op0=ALU.mult, op1=ALU.add))
            dv(lambda e: e.reduce_max(out=sm[:, 57:58], in_=sm[:, 68:76], axis=AX.X))
            dv(lambda e: e.tensor_scalar(out=sm[:, 76:84], in0=sm[:, 68:76], scalar1=sm[:, 57:58], scalar2=None,
                                         op0=ALU.is_equal))
            dv(lambda e: e.tensor_tensor(out=sm[:, 58:59], in0=sm[:, 56:57], in1=sm[:, 57:58], op=ALU.subtract))
            K.op("act", lambda e: e.activation(out=sm[:, 59:60], in_=sm[:, 58:59], func=AF.Sigmoid),
                 reads=[sm], writes=[sm])
            dv(lambda e, tt=tt: e.tensor_tensor(out=GATE[:, tt, 0:1], in0=sm[:, 59:60], in1=sm[:, 39:40], op=ALU.mult),
               wr=[GATE])
            dv(lambda e, tt=tt: e.tensor_tensor(out=GATE[:, tt, 1:2], in0=sm[:, 39:40], in1=GATE[:, tt, 0:1],
                                                op=ALU.subtract), rd=[GATE], wr=[GATE])
            for k, c0 in ((0, 60), (1, 76)):
                dv(lambda e, tt=tt, k=k, c0=c0: e.tensor_tensor(
                    out=OHA[:, tt, k, :].rearrange("p (g l) -> p g l", g=4),
                    in0=sm[:, 40:44].unsqueeze(2).to_broadcast([128, 4, 8]),
                    in1=sm[:, c0:c0 + 8].unsqueeze(1).to_broadcast([128, 4, 8]), op=ALU.mult), wr=[OHA])
            dv(lambda e, tt=tt: e.tensor_tensor(out=sm[:, 84:116], in0=OHA[:, tt, 0, :], in1=OHA[:, tt, 1, :],
                                                op=ALU.add), rd=[OHA])
            psr = K.ps()
            K.op("pe", lambda e, psr=psr: e.matmul(psr[:, 0:32], lhsT=m["UT"][:], rhs=sm[:, 84:116], start=True,
                                                   stop=True), reads=[m["UT"], sm], writes=[psr])
            K.op("pe", lambda e, psr=psr: e.matmul(psr[:, 32:64], lhsT=self.ones[:], rhs=sm[:, 84:116], start=True,
                                                   stop=True), reads=[self.ones, sm], writes=[psr])
            K.op("dve", lambda e, psr=psr: e.tensor_tensor(out=sm[:, 84:116], in0=psr[:, 0:32], in1=carry[:],
                                                           op=ALU.add), reads=[psr, carry, sm], writes=[sm])
            for k in range(2):
                dv(lambda e, tt=tt, k=k: e.tensor_tensor(out=sm[:, 0:32], in0=sm[:, 84:116], in1=OHA[:, tt, k, :],
                                                         op=ALU.mult), rd=[OHA])
                dv(lambda e, tt=tt, k=k: e.reduce_sum(out=RK[:, tt, k:k + 1], in_=sm[:, 0:32], axis=AX.X), wr=[RK])
            K.op("dve", lambda e, psr=psr: e.tensor_tensor(out=carry[:], in0=psr[:, 32:64], in1=carry[:], op=ALU.add),
                 reads=[psr, carry], writes=[carry])
        cs = m["cs"]

        def cv(fn):
            K.op("dve", fn, reads=[cs, carry], writes=[cs])
        BLK = self.BLK
        cv(lambda e: e.tensor_scalar(out=cs[:, 0, :], in0=carry[:], scalar1=1.0 / BLK, scalar2=(BLK - 1.0) / (2 * BLK),
                                     op0=ALU.mult, op1=ALU.add))
        cv(lambda e: e.tensor_scalar(out=cs[:, 1, :], in0=cs[:, 0, :], scalar1=8388608.0, scalar2=None, op0=ALU.add))
        cv(lambda e: e.tensor_scalar(out=cs[:, 2, :], in0=cs[:, 1, :], scalar1=-8388608.0, scalar2=float(BLK),
                                     op0=ALU.add, op1=ALU.mult))
        cv(lambda e: e.tensor_copy(out=cs[:, 3, :], in_=cs[:, 2, :]))
        a, b = 3, 4
        for s in (1, 2, 4, 8, 16):
            cv(lambda e, a=a, b=b, s=s: e.tensor_copy(out=cs[:, b, 0:s], in_=cs[:, a, 0:s]))
            cv(lambda e, a=a, b=b, s=s: e.tensor_tensor(out=cs[:, b, s:32], in0=cs[:, a, s:32], in1=cs[:, a, 0:32 - s],
                                                        op=ALU.add))
            a, b = b, a
        pend_i = a
        cv(lambda e: e.tensor_tensor(out=cs[:, 5, :], in0=cs[:, pend_i, :], in1=cs[:, 2, :], op=ALU.subtract))
        be = m["be"]
        K.op("dve", lambda e: e.memset(be[:, 0, :], 0.0), writes=[be])
        for ex in range(32):
            K.op("dve", lambda e, ex=ex: e.scalar_tensor_tensor(out=be[:, 0, :], in0=m["JV"][:],
                                                                scalar=cs[:, pend_i, ex:ex + 1], in1=be[:, 0, :],
                                                                op0=ALU.is_ge, op1=ALU.add),
                 reads=[m["JV"], cs, be], writes=[be])
        K.op("dve", lambda e: e.tensor_scalar(out=be[:, 0, :], in0=be[:, 0, :], scalar1=31.0, scalar2=None, op0=ALU.min),
             reads=[be], writes=[be])
        K.op("dve", lambda e: e.tensor_scalar(out=be[:, 1, :], in0=be[:, 0, :], scalar1=1024.0,
                                              scalar2=float(i * 32 * 1024), op0=ALU.mult, op1=ALU.add),
             reads=[be], writes=[be])
        K.op("dve", lambda e: e.tensor_scalar(out=be[:, 2, :], in0=be[:, 0, :], scalar1=512.0,
                                              scalar2=float(i * 32 * 512), op0=ALU.mult, op1=ALU.add),
             reads=[be], writes=[be])
        idf = m["idf"]
        K.op("dve", lambda e: e.tensor_tensor(out=idf[:], in0=be[:, 1, :].unsqueeze(2).to_broadcast([128, NB, 8]),
                                              in1=m["PIDX"][:].unsqueeze(1).to_broadcast([128, NB, 8]), op=ALU.add),
             reads=[be, m["PIDX"]], writes=[idf])
        K.op("dve", lambda e: e.tensor_copy(out=m["IDX1"][:], in_=idf[:]), reads=[idf], writes=[m["IDX1"]])
        K.op("dve", lambda e: e.tensor_tensor(out=idf[:, :, 0:4], in0=be[:, 2, :].unsqueeze(2).to_broadcast([128, NB, 4]),
                                              in1=m["PIDX"][:, 0:4].unsqueeze(1).to_broadcast([128, NB, 4]),
                                              op=ALU.add), reads=[be, m["PIDX"]], writes=[idf])
        K.op("dve", lambda e: e.tensor_copy(out=m["IDX2"][:], in_=idf[:, :, 0:4]), reads=[idf], writes=[m["IDX2"]])
        for tt in range(ntile):
            ht, sm = m["ht"][tt % 2], m["sm"][tt % 2]
            K.dma("sp", ht[:], m["H2"][tt * 128:(tt + 1) * 128, :], reads=[m["H2"].p(tt)], writes=[ht])
            for k in range(2):
                K.op("dve", lambda e, tt=tt, k=k: e.tensor_tensor(out=sm[:, 32:64], in0=cs[:, 5, :],
                                                                  in1=OHA[:, tt, k, :], op=ALU.mult),
                     reads=[sm, OHA, cs], writes=[sm])
                K.op("dve", lambda e, k=k: e.reduce_sum(out=sm[:, 66 + k:67 + k], in_=sm[:, 32:64], axis=AX.X),
                     reads=[sm], writes=[sm])
            K.op("dve", lambda e, tt=tt: e.tensor_tensor(out=sm[:, 64:66], in0=sm[:, 66:68], in1=RK[:, tt, :],
                                                         op=ALU.add), reads=[sm, RK], writes=[sm])
            K.op("dve", lambda e, tt=tt: e.tensor_copy(out=DEST[:, tt, :], in_=sm[:, 64:66]), reads=[sm], writes=[DEST])
            for k in range(2):
                K.dma("pool", m["XS"][:, :], ht[:], reads=[ht, DEST], writes=[m["XS"]],
                      indirect=dict(out_offset=bass.IndirectOffsetOnAxis(ap=DEST[:, tt, k:k + 1], axis=0),
                                    in_offset=None))
        w1t = self.inp["moe_w1"].ap().rearrange("l e k n -> (l e k) n")
        w3t = self.inp["moe_w3"].ap().rearrange("l e k n -> (l e k) n")
        w2t = self.inp["moe_w2"].ap().rearrange("l e k n -> (l e k) n")
        for j in range(NB):
            W1, W3, W2, xt, xTb, hTb, sil, yb = (m[k][j % 2] for k in ("W1", "W3", "W2", "xt", "xTb", "hTb", "sil", "yb"))
            for kt in range(8):
                for W, tab in ((W1, w1t), (W3, w3t)):
                    K.dma("pool", W[:, kt, :], tab, reads=[m["IDX1"]], writes=[W],
                          indirect=dict(out_offset=None,
                                        in_offset=bass.IndirectOffsetOnAxis(ap=m["IDX1"][:, j, kt:kt + 1], axis=0)))
            for fc in range(4):
                K.dma("pool", W2[:, fc, :], w2t, reads=[m["IDX2"]], writes=[W2],
                      indirect=dict(out_offset=None,
                                    in_offset=bass.IndirectOffsetOnAxis(ap=m["IDX2"][:, j, fc:fc + 1], axis=0)))
            for sub in range(self.BLK // 128):
                r0 = j * self.BLK + sub * 128
                xt, xTb, hTb, sil, yb = (m[k][(j * 4 + sub) % 2] for k in ("xt", "xTb", "hTb", "sil", "yb"))
                K.dma("sp", xt[:], m["XS"][r0:r0 + 128, :], reads=[m["XS"]], writes=[xt])
                for half in range(2):
                    ps = K.ps()
                    for q in range(4):
                        kt = half * 4 + q
                        self.tr(None, ps[:, q * 128:(q + 1) * 128], xt[:, kt * 128:(kt + 1) * 128], ps, xt)
                    K.op("act" if half else "dve",
                         lambda e, ps=ps, half=half, xTb=xTb: (e.copy if half else e.tensor_copy)(
                             out=xTb[:, half * 4:(half + 1) * 4, :].rearrange("p a b -> p (a b)"), in_=ps[:, :]),
                         reads=[ps], writes=[xTb])
                pa, pb = K.ps(), K.ps()
                for W, pp in ((W1, pa), (W3, pb)):
                    for fc in range(4):
                        for kt in range(8):
                            K.op("pe", lambda e, W=W, pp=pp, fc=fc, kt=kt, xTb=xTb: e.matmul(
                                pp[:, fc * 128:(fc + 1) * 128], lhsT=W[:, kt, fc * 128:(fc + 1) * 128], rhs=xTb[:, kt, :],
                                start=(kt == 0), stop=(kt == 7)), reads=[W, xTb], writes=[pp])
                K.op("act", lambda e, pa=pa, sil=sil: e.activation(out=sil[:], in_=pa[:, :], func=AF.Silu),
                     reads=[pa], writes=[sil])
                K.op("dve", lambda e, pb=pb, sil=sil, hTb=hTb: e.tensor_tensor(
                    out=hTb[:].rearrange("p a b -> p (a b)"), in0=sil[:], in1=pb[:, :], op=ALU.mult),
                    reads=[pb, sil], writes=[hTb])
                for half in range(2):
                    py = K.ps()
                    for fc in range(4):
                        K.op("pe", lambda e, py=py, fc=fc, half=half, hTb=hTb, W2=W2: e.matmul(
                            py[:, :], lhsT=hTb[:, fc, :], rhs=W2[:, fc, half * 512:(half + 1) * 512],
                            start=(fc == 0), stop=(fc == 3)), reads=[hTb, W2], writes=[py])
                    K.op("act" if half else "dve",
                         lambda e, py=py, half=half, yb=yb: (e.copy if half else e.tensor_copy)(
                             out=yb[:, half * 512:(half + 1) * 512], in_=py[:, :]), reads=[py], writes=[yb])
                K.dma("sp", m["YS"][r0:r0 + 128, :], yb[:], reads=[yb], writes=[m["YS"]])
        for tt in range(ntile):
            y0, y1, xt = m["y0"][tt % 2], m["y1"][tt % 2], m["xt"][tt % 2]
            M = self.Ml if tt < 64 else self.Mc
            for k, y in ((0, y0), (1, y1)):
                K.dma("pool", y[:], m["YS"][:, :], reads=[m["YS"], DEST], writes=[y],
                      indirect=dict(out_offset=None,
                                    in_offset=bass.IndirectOffsetOnAxis(ap=DEST[:, tt, k:k + 1], axis=0)))
            K.dma("sp", xt[:], self.lat[tt * 128:(tt + 1) * 128, :], reads=[self.lat.p(tt)], writes=[xt])
            K.op("dve", lambda e, tt=tt, y0=y0: e.tensor_scalar(out=y0[:], in0=y0[:], scalar1=GATE[:, tt, 0:1],
                                                                scalar2=None, op0=ALU.mult),
                 reads=[y0, GATE], writes=[y0])
            K.op("dve", lambda e, tt=tt, y0=y0, y1=y1: e.scalar_tensor_tensor(
                out=y0[:], in0=y1[:], scalar=GATE[:, tt, 1:2], in1=y0[:], op0=ALU.mult, op1=ALU.add),
                reads=[y0, y1, GATE], writes=[y0])
            K.op("pool", lambda e, y0=y0, M=M: e.tensor_tensor(out=y0[:], in0=y0[:], in1=M[:, 5, :], op=ALU.mult),
                 reads=[y0, M], writes=[y0])
            K.op("dve", lambda e, y0=y0, xt=xt: e.tensor_tensor(out=xt[:], in0=xt[:], in1=y0[:], op=ALU.add),
                 reads=[y0, xt], writes=[xt])
            K.dma("sp", self.lat[tt * 128:(tt + 1) * 128, :], xt[:], reads=[xt], writes=[self.lat.p(tt)])
        K.pop_scope()

    def final_norm(self):
        K = self.K
        K.push_scope()
        m = dict(xt=[K.sb([128, D], F32, f"fx{j}") for j in range(2)], ht=[K.sb([128, D], F32, f"fh{j}") for j in range(2)],
                 st=[K.sb([128, 4], F32, f"fs{j}") for j in range(2)])
        g = K.sb([128, D], F32, "fng")
        K.dma("sp", g[:], self.inp["final_norm"].ap().rearrange("(o d) -> o d", o=1).partition_broadcast(128), writes=[g])
        for tt in range(64):
            xt, ht, st = m["xt"][tt % 2], m["ht"][tt % 2], m["st"][tt % 2]
            K.dma("sp", xt[:], self.lat[tt * 128:(tt + 1) * 128, :], reads=[self.lat.p(tt)], writes=[xt])
            K.op("act", lambda e, xt=xt, ht=ht, st=st: e.activation(out=ht[:], in_=xt[:], func=AF.Square,
                                                                    accum_out=st[:, 0:1]), reads=[xt], writes=[ht, st])
            K.op("dve", lambda e, st=st: e.tensor_scalar(out=st[:, 1:2], in0=st[:, 0:1], scalar1=1.0 / D, scalar2=1e-6,
                                                         op0=ALU.mult, op1=ALU.add), reads=[st], writes=[st])
            K.op("act", lambda e, st=st: e.sqrt(out=st[:, 3:4], in_=st[:, 1:2]), reads=[st], writes=[st])
            K.op("dve", lambda e, st=st: e.reciprocal(out=st[:, 2:3], in_=st[:, 3:4]), reads=[st], writes=[st])
            K.op("dve", lambda e, xt=xt, ht=ht, st=st: e.scalar_tensor_tensor(
                out=ht[:], in0=xt[:], scalar=st[:, 2:3], in1=g[:], op0=ALU.mult, op1=ALU.mult),
                reads=[xt, st, g], writes=[ht])
            K.dma("sp", self.out[tt * 128:(tt + 1) * 128, :], ht[:], reads=[ht])
        K.pop_scope()


def fourier_consts():
    c = {}
    n = np.arange(256)
    ang = 2 * np.pi * np.outer(n, n) / 256
    c["f_cc"] = (np.cos(ang) / 16).astype(np.float32)
    c["f_sc"] = (-np.sin(ang) / 16).astype(np.float32)
    a = np.arange(128)
    ang = 2 * np.pi * np.outer(a, a) / 128
    s = 1.0 / np.sqrt(8192.0)
    c["f_c128"] = (np.cos(ang) * s).astype(np.float32)
    c["f_s128"] = (np.sin(ang) * s).astype(np.float32)
    f1 = np.arange(128)[:, None]
    b = np.arange(64)[None, :]
    th = 2 * np.pi * f1 * b / 8192
    c["f_tw"] = np.stack([np.cos(th), -np.sin(th)], axis=2).astype(np.float32)
    bb = np.arange(64)
    ang = 2 * np.pi * np.outer(bb, bb) / 64
    c["f_cs64"] = np.concatenate([np.cos(ang), np.sin(ang)], axis=0).astype(np.float32)
    t = np.arange(256)
    ang = 2 * np.pi * np.outer(t, t) / 256
    c["f_c256"] = (np.cos(ang) / 16).astype(np.float32)
    c["f_s256"] = (np.sin(ang) / 16).astype(np.float32)
    return c


FOURIER_SPECS = {"f_cc": [256, 256], "f_sc": [256, 256], "f_c128": [128, 128], "f_s128": [128, 128],
                 "f_tw": [128, 64, 2], "f_cs64": [128, 64], "f_c256": [256, 256], "f_s256": [256, 256]}


def _fourier_declare(self):
    nc = self.K.nc
    for k, shp in FOURIER_SPECS.items():
        self.inp[k] = nc.dram_tensor(k, shp, F32, kind="ExternalInput")
    self.GD = Buf(nc.dram_tensor("GD", [128, 128, D], F32), "GD")


def _fourier(self, i, with_ctx, nb=64, nf=128):
    K = self.K
    j = i // 2
    K.push_scope()
    cc = K.sb([128, 2, 2, 256], BF16, "fcc")
    c128 = K.sb([128, 3, 128], BF16, "fc128")
    tw = K.sb([128, 64, 2], F32, "ftw")
    cs64 = K.sb([128, 64], F32, "fcs64")
    wf = K.sb([128, 8, D], BF16, "fwf")
    big = [K.sb([128, D], F32, f"fbig{q}") for q in range(6)]
    xts, hts, gsb = big[0:2], big[2:4], big[4:6]
    sts = [K.sb([128, 4], F32, f"fst{q}") for q in range(2)]
    hTb = [K.sb([128, 8, 128], BF16, f"fhT{q}") for q in range(2)]
    Zb = [K.sb([128, 2, D], BF16, f"fZ{q}") for q in range(2)]
    gi2 = [K.sb([128, D], F32, f"fgi{q}") for q in range(2)]
    tmp = K.sb([128, 256], F32, "ftmp")
    def ld_cast(dst_ap, dst_buf, src_ap, shape, n=[0]):
        stg = big[4 + n[0] % 2]
        n[0] += 1
        rows, cols = shape
        view = stg[0:rows, 0:cols]
        K.dma("sp", view, src_ap, writes=[stg])
        K.op("dve", lambda e: e.tensor_copy(out=dst_ap, in_=view), reads=[stg], writes=[dst_buf])
    for kt in range(2):
        ld_cast(cc[:, kt, 0, :], cc, self.inp["f_cc"][kt * 128:(kt + 1) * 128, :], (128, 256))
        ld_cast(cc[:, kt, 1, :], cc, self.inp["f_sc"][kt * 128:(kt + 1) * 128, :], (128, 256))
    ld_cast(c128[:, 0, :], c128, self.inp["f_c128"][:, :], (128, 128))
    ld_cast(c128[:, 1, :], c128, self.inp["f_s128"][:, :], (128, 128))
    K.op("dve", lambda e: e.tensor_scalar(out=c128[:, 2, :], in0=c128[:, 1, :], scalar1=-1.0, scalar2=None,
                                          op0=ALU.mult), reads=[c128], writes=[c128])
    for kt in range(8):
        ld_cast(wf[:, kt, :], wf, self.inp["w_fourier"][j, kt * 128:(kt + 1) * 128, :], (128, 1024))
    self._ld_cast = ld_cast
    K.dma("sp", tw[:], self.inp["f_tw"][:, :, :], writes=[tw])
    K.dma("sp", cs64[:], self.inp["f_cs64"][:, :], writes=[cs64])
    ntw = K.sb([128, 64], F32, "fntw")
    K.op("dve", lambda e: e.tensor_scalar(out=ntw[:], in0=tw[:, :, 1], scalar1=-1.0, scalar2=None, op0=ALU.mult),
         reads=[tw], writes=[ntw])
    latv = self.lat[0:T, :].rearrange("(a b) d -> b a d", b=64)
    lat_all = [self.lat.p(t) for t in range(64)]

    def chan_dft(xt, ht, st, hT, Z, M):
        self.norm_tile(xt, ht, M, 0, st)
        for half in range(2):
            ps = K.ps()
            for q in range(4):
                kt = half * 4 + q
                self.tr(None, ps[:, q * 128:(q + 1) * 128], ht[:, kt * 128:(kt + 1) * 128], ps, ht)
            K.op("act" if half else "dve",
                 lambda e, ps=ps, half=half: (e.copy if half else e.tensor_copy)(
                     out=hT[:, half * 4:(half + 1) * 4, :].rearrange("p a b -> p (a b)"), in_=ps[:, :]),
                 reads=[ps], writes=[hT])
        for ri in range(2):
            for hh in range(2):
                ps = K.ps()
                for gg in range(2):
                    g = hh * 2 + gg
                    for kt in range(2):
                        K.op("pe", lambda e, ps=ps, gg=gg, g=g, kt=kt, ri=ri: e.matmul(
                            ps[:, gg * 256:(gg + 1) * 256], lhsT=hT[:, g * 2 + kt, :], rhs=cc[:, kt, ri, :],
                            start=(kt == 0), stop=(kt == 1)), reads=[hT, cc], writes=[ps])
                K.op("act" if hh else "dve",
                     lambda e, ps=ps, hh=hh, ri=ri: (e.copy if hh else e.tensor_copy)(
                         out=Z[:, ri, hh * 512:(hh + 1) * 512], in_=ps[:, :]), reads=[ps], writes=[Z])

    for b in range(nb):
        xt, ht, st, hT, Z, gr, gi = (l[b % 2] for l in (xts, hts, sts, hTb, Zb, gsb, gi2))
        K.dma("sp", xt[:], latv[b], reads=lat_all, writes=[xt])
        chan_dft(xt, ht, st, hT, Z, self.Ml)
        for hh in range(2):
            cs_ = slice(hh * 512, (hh + 1) * 512)
            pr, pi_ = K.ps(), K.ps()
            K.op("pe", lambda e: e.matmul(pr[:, :], lhsT=c128[:, 0, :], rhs=Z[:, 0, cs_], start=True, stop=False),
                 reads=[c128, Z], writes=[pr])
            K.op("pe", lambda e: e.matmul(pr[:, :], lhsT=c128[:, 1, :], rhs=Z[:, 1, cs_], start=False, stop=True),
                 reads=[c128, Z], writes=[pr])
            K.op("pe", lambda e: e.matmul(pi_[:, :], lhsT=c128[:, 0, :], rhs=Z[:, 1, cs_], start=True, stop=False),
                 reads=[c128, Z], writes=[pi_])
            K.op("pe", lambda e: e.matmul(pi_[:, :], lhsT=c128[:, 2, :], rhs=Z[:, 0, cs_], start=False, stop=True),
                 reads=[c128, Z], writes=[pi_])
            K.op("dve", lambda e: e.tensor_scalar(out=gr[:, cs_], in0=pr[:, :], scalar1=tw[:, b, 0:1], scalar2=None,
                                                  op0=ALU.mult), reads=[pr, tw], writes=[gr])
            K.op("dve", lambda e: e.scalar_tensor_tensor(out=gr[:, cs_], in0=pi_[:, :], scalar=ntw[:, b:b + 1],
                                                         in1=gr[:, cs_], op0=ALU.mult, op1=ALU.add),
                 reads=[pi_, ntw, gr], writes=[gr])
            K.op("dve", lambda e: e.tensor_scalar(out=gi[:, cs_], in0=pi_[:, :], scalar1=tw[:, b, 0:1], scalar2=None,
                                                  op0=ALU.mult), reads=[pi_, tw], writes=[gi])
            K.op("dve", lambda e: e.scalar_tensor_tensor(out=gi[:, cs_], in0=pr[:, :], scalar=tw[:, b, 1:2],
                                                         in1=gi[:, cs_], op0=ALU.mult, op1=ALU.add),
                 reads=[pr, tw, gi], writes=[gi])
        K.dma("act", self.GD[b, :, :], gr[:], reads=[gr], writes=[self.GD])
        K.dma("act", self.GD[64 + b, :, :], gi[:], reads=[gi], writes=[self.GD])
    latf = self.lat[0:T, :].rearrange("(f2 f1) d -> f1 f2 d", f1=128)
    YT = [K.sb([128, 8, 64], BF16, f"fYT{q}") for q in range(2)]
    for f1 in range(nf):
        gd, yt, xt, ht = gsb[f1 % 2], YT[f1 % 2], xts[f1 % 2], hts[f1 % 2]
        K.dma("sp", gd[:], self.GD[:, f1, :], reads=[self.GD], writes=[gd])
        K.dma("sp", xt[0:64, :], latf[f1], reads=lat_all, writes=[xt])
        ps = K.ps()
        for kt in range(8):
            K.op("pe", lambda e, kt=kt: e.matmul(ps[:, kt * 64:(kt + 1) * 64], lhsT=gd[:, kt * 128:(kt + 1) * 128],
                                                 rhs=cs64[:, :], start=True, stop=True), reads=[gd, cs64], writes=[ps])
        K.op("act", lambda e: e.copy(out=yt[:].rearrange("p a b -> p (a b)"), in_=ps[:, :]), reads=[ps], writes=[yt])
        for hh in range(2):
            py = K.ps()
            for kt in range(8):
                K.op("pe", lambda e, kt=kt: e.matmul(py[0:64, :], lhsT=yt[:, kt, :],
                                                     rhs=wf[:, kt, hh * 512:(hh + 1) * 512], start=(kt == 0),
                                                     stop=(kt == 7)), reads=[yt, wf], writes=[py])
            K.op("dve", lambda e: e.tensor_tensor(out=ht[0:64, hh * 512:(hh + 1) * 512], in0=py[0:64, :],
                                                  in1=self.Ml[0:64, 2, hh * 512:(hh + 1) * 512], op=ALU.mult),
                 reads=[py, self.Ml], writes=[ht])
        K.op("pool", lambda e: e.tensor_tensor(out=xt[0:64, :], in0=xt[0:64, :], in1=ht[0:64, :], op=ALU.add),
             reads=[xt, ht], writes=[xt])
        K.dma("act", latf[f1], xt[0:64, :], reads=[xt], writes=lat_all)
    if with_ctx:
        c256 = K.sb([128, 2, 2, 256], BF16, "fc256")
        for tt in range(2):
            ld_cast(c256[:, 0, tt, :], c256, self.inp["f_c256"][tt * 128:(tt + 1) * 128, :], (128, 256))
            ld_cast(c256[:, 1, tt, :], c256, self.inp["f_s256"][tt * 128:(tt + 1) * 128, :], (128, 256))
        ctxp = [self.lat.p(64), self.lat.p(65)]
        for tt in range(2):
            K.dma("sp", xts[tt][:], self.lat[T + tt * 128:T + (tt + 1) * 128, :], reads=ctxp, writes=[xts[tt]])
            chan_dft(xts[tt], hts[tt], sts[tt], hTb[tt], Zb[tt], self.Mc)
        for ft in range(2):
            yt = K.sb([128, 8, 128], BF16, f"fcy{ft}")
            for half in range(2):
                ps = K.ps()
                for q in range(4):
                    kt = half * 4 + q
                    n = 0
                    for tt in range(2):
                        for cs_i in range(2):
                            K.op("pe", lambda e, kt=kt, q=q, tt=tt, cs_i=cs_i, n=n: e.matmul(
                                ps[:, q * 128:(q + 1) * 128], lhsT=Zb[tt][:, cs_i, kt * 128:(kt + 1) * 128],
                                rhs=c256[:, cs_i, tt, ft * 128:(ft + 1) * 128], start=(n == 0), stop=(n == 3)),
                                reads=[Zb[tt], c256], writes=[ps])
                            n += 1
                K.op("act", lambda e, half=half: e.copy(out=yt[:, half * 4:(half + 1) * 4, :].rearrange("p a b -> p (a b)"),
                                                        in_=ps[:, :]), reads=[ps], writes=[yt])
            xt, ht = xts[ft], hts[ft]
            for hh in range(2):
                py = K.ps()
                for kt in range(8):
                    K.op("pe", lambda e, kt=kt: e.matmul(py[:, :], lhsT=yt[:, kt, :],
                                                         rhs=wf[:, kt, hh * 512:(hh + 1) * 512], start=(kt == 0),
                                                         stop=(kt == 7)), reads=[yt, wf], writes=[py])
                K.op("dve", lambda e: e.tensor_tensor(out=ht[:, hh * 512:(hh + 1) * 512], in0=py[:, :],
                                                      in1=self.Mc[:, 2, hh * 512:(hh + 1) * 512], op=ALU.mult),
                     reads=[py, self.Mc], writes=[ht])
            K.op("pool", lambda e: e.tensor_tensor(out=xt[:], in0=xt[:], in1=ht[:], op=ALU.add),
                 reads=[xt, ht], writes=[xt])
            K.dma("act", self.lat[T + ft * 128:T + (ft + 1) * 128, :], xt[:], reads=[xt], writes=ctxp)
    K.pop_scope()


Prog.fourier_declare = _fourier_declare
Prog.fourier = _fourier


POOL_WINS = (2, 4, 8, 16)


def pool_consts():
    c = {}
    for nm, L in (("f_icnt_lat", T), ("f_icnt_ctx", CT)):
        a = np.zeros((4, L), np.float32)
        pos = np.arange(L)
        for gi, win in enumerate(POOL_WINS):
            half = win // 2
            hi = np.minimum(pos + half, L)
            lo = np.maximum(pos - half, 0)
            a[gi] = 1.0 / (hi - lo)
        c[nm] = a
    return c


def _even_declare(self):
    nc = self.K.nc
    self.inp["f_icnt_lat"] = nc.dram_tensor("f_icnt_lat", [4, T], F32, kind="ExternalInput")
    self.inp["f_icnt_ctx"] = nc.dram_tensor("f_icnt_ctx", [4, CT], F32, kind="ExternalInput")
    e = {}
    e["HT"] = Buf(nc.dram_tensor("HT", [D, NT], F32), "HT")
    e["PT"] = Buf(nc.dram_tensor("PT", [2048, NT], F32), "PT")
    for d in range(2):
        for nm in ("At", "Bt", "Kt", "Rt", "Bh", "Kh"):
            e[f"{nm}{d}"] = Buf(nc.dram_tensor(f"SC_{nm}{d}", [512, NT], F32), f"{nm}{d}")
        e[f"GL{d}"] = Buf(nc.dram_tensor(f"SC_GL{d}", [512, NT // 64], F32), f"GL{d}")
        e[f"YD{d}"] = Buf(nc.dram_tensor(f"SC_YD{d}", [NT, 512], F32), f"YD{d}")
    for nm in ("VV", "GG", "BON", "YB"):
        e[nm] = Buf(nc.dram_tensor(f"SC_{nm}", [512, NT], F32), nm)
    self.ed = e


def _even_e1(self, i, ntile=66):
    K = self.K
    j = i // 2
    e = self.ed
    K.push_scope()
    win = K.sb([128, 8, 2048], BF16, "win")
    stg = [K.sb([128, D], F32, f"e1stg{q}") for q in range(2)]
    n = 0
    for kt in range(8):
        for hh in range(2):
            sg = stg[n % 2]
            n += 1
            K.dma("sp", sg[:], self.inp["w_in"][j, kt * 128:(kt + 1) * 128, hh * 1024:(hh + 1) * 1024], writes=[sg])
            K.op("dve" if n % 2 else "act",
                 lambda en, sg=sg, kt=kt, hh=hh: (en.tensor_copy if n % 2 else en.copy)(
                     out=win[:, kt, hh * 1024:(hh + 1) * 1024], in_=sg[:]), reads=[sg], writes=[win])
    xts = [K.sb([128, D], F32, f"e1x{q}") for q in range(2)]
    hts = [K.sb([128, D], F32, f"e1h{q}") for q in range(2)]
    sts = [K.sb([128, 4], F32, f"e1s{q}") for q in range(2)]
    hTf = [K.sb([128, 8, 128], F32, f"e1hTf{q}") for q in range(2)]
    hTb = [K.sb([128, 8, 128], BF16, f"e1hTb{q}") for q in range(2)]
    pts = [K.sb([128, 16, 128], F32, f"e1pt{q}") for q in range(2)]
    for tt in range(ntile):
        xt, ht, st, hf, hb, pt = (l[tt % 2] for l in (xts, hts, sts, hTf, hTb, pts))
        M = self.Ml if tt < 64 else self.Mc
        K.dma("sp", xt[:], self.lat[tt * 128:(tt + 1) * 128, :], reads=[self.lat.p(tt)], writes=[xt])
        self.norm_tile(xt, ht, M, 0, st)
        for half in range(2):
            ps = K.ps()
            for q in range(4):
                kt = half * 4 + q
                self.tr(None, ps[:, q * 128:(q + 1) * 128], ht[:, kt * 128:(kt + 1) * 128], ps, ht)
            K.op("act", lambda en, ps=ps, half=half: en.copy(
                out=hf[:, half * 4:(half + 1) * 4, :].rearrange("p a b -> p (a b)"), in_=ps[:, :]),
                reads=[ps], writes=[hf])
            K.op("dve", lambda en, half=half: en.tensor_copy(
                out=hb[:, half * 4:(half + 1) * 4, :], in_=hf[:, half * 4:(half + 1) * 4, :]),
                reads=[hf], writes=[hb])
        import os
        if not os.environ.get("SKIPHT"):
            K.dma("act", e["HT"][:, tt * 128:(tt + 1) * 128].rearrange("(kt k) t -> k kt t", k=128), hf[:],
                  reads=[hf], writes=[e["HT"]])
        for ob in range(4):
            ps = K.ps()
            for q in range(4):
                oc = ob * 4 + q
                for kt in range(8):
                    K.op("pe", lambda en, ps=ps, q=q, oc=oc, kt=kt: en.matmul(
                        ps[:, q * 128:(q + 1) * 128], lhsT=win[:, kt, oc * 128:(oc + 1) * 128], rhs=hb[:, kt, :],
                        start=(kt == 0), stop=(kt == 7)), reads=[win, hb], writes=[ps])
            K.op("act" if ob % 2 else "dve", lambda en, ps=ps, ob=ob: (en.copy if ob % 2 else en.tensor_copy)(
                out=pt[:, ob * 4:(ob + 1) * 4, :].rearrange("p a b -> p (a b)"), in_=ps[:, :]),
                reads=[ps], writes=[pt])
        if not os.environ.get("SKIPPT"):
            K.dma("act", e["PT"][:, tt * 128:(tt + 1) * 128].rearrange("(oc k) t -> k oc t", k=128), pt[:],
                  reads=[pt], writes=[e["PT"]])
    K.pop_scope()


def _even_e2(self, i, dbg=None):
    K = self.K
    j = i // 2
    e = self.ed
    K.push_scope()
    WB = 512
    WT = WB + 128
    eng_rr = [0]

    def ve():
        eng_rr[0] += 1
        return "dve" if eng_rr[0] % 2 else "pool"

    stg = K.sb([128, 8, 128], F32, "e2stg")
    W1 = K.sb([128, 3, 8, 128], BF16, "e2W1")
    W2 = K.sb([128, 3, 512], BF16, "e2W2")
    pw = K.sb([128, 4, 128], BF16, "e2pw")
    for v, (nm, nd) in enumerate((("decay_w1", 2), ("lr_a1", 2), ("gate_g1", 1))):
        for d in range(nd):
            src = self.inp[nm][j, d] if nd == 2 else self.inp[nm][j]
            wcol = 64 if nd == 2 else 128
            K.dma("sp", stg[:, :, 0:wcol], src.rearrange("(kt k) r -> k kt r", k=128), writes=[stg])
            K.op("dve", lambda en, v=v, d=d, wcol=wcol: en.tensor_copy(out=W1[:, v, :, d * wcol:(d + 1) * wcol],
                                                                       in_=stg[:, :, 0:wcol]), reads=[stg], writes=[W1])
    stg2 = stg[:].rearrange("p a b -> p (a b)")
    for v, nm in enumerate(("decay_w2", "lr_a2")):
        for d in range(2):
            K.dma("sp", stg2[d * 64:(d + 1) * 64, 0:512], self.inp[nm][j, d], writes=[stg])
        K.op("dve", lambda en, v=v: en.tensor_copy(out=W2[:, v, :], in_=stg2[:, 0:512]), reads=[stg], writes=[W2])
    K.dma("sp", stg2[:, 0:512], self.inp["gate_g2"][j], writes=[stg])
    K.op("dve", lambda en: en.tensor_copy(out=W2[:, 2, :], in_=stg2[:, 0:512]), reads=[stg], writes=[W2])
    for gi in range(4):
        K.dma("sp", stg2[:, gi * 128:(gi + 1) * 128], self.inp["pool_w"][j, gi], writes=[stg])
    K.op("dve", lambda en: en.tensor_copy(out=pw[:].rearrange("p a b -> p (a b)"), in_=stg2[:, 0:512]),
         reads=[stg], writes=[pw])
    MU = K.sb([128, 2, 3, 8], F32, "e2MU")
    MUP = K.sb([128, 2, 3, 4], F32, "e2MUP")
    COL = K.sb([128, 12, 4], F32, "e2COL")
    K.dma("sp", MU[:, 0, :, :], self.inp["mu_x"][j].rearrange("v (kt k) -> k v kt", k=128), writes=[MU],
          allow_slow_non_contiguous=True)
    K.dma("sp", MUP[:, 0, :, :], self.inp["mu_p"][j].rearrange("v (c k) -> k v c", k=128), writes=[MUP],
          allow_slow_non_contiguous=True)
    for d in range(2):
        K.dma("sp", COL[:, d, :], self.inp["decay_w0"][j, d].rearrange("(c k) -> k c", k=128), writes=[COL],
              allow_slow_non_contiguous=True)
        K.dma("sp", COL[:, 2 + d, :], self.inp["lr_a0"][j, d].rearrange("(c k) -> k c", k=128), writes=[COL],
              allow_slow_non_contiguous=True)
    K.dma("sp", COL[:, 4, :], self.inp["k_k"][j].rearrange("(c k) -> k c", k=128), writes=[COL], allow_slow_non_contiguous=True)
    K.dma("sp", COL[:, 5, :], self.inp["k_a"][j].rearrange("(c k) -> k c", k=128), writes=[COL], allow_slow_non_contiguous=True)
    K.dma("sp", COL[:, 6, :], self.inp["r_k"][j].rearrange("(c h2) k -> (h2 k) c", h2=2), writes=[COL],
          allow_slow_non_contiguous=True)
    K.dma("sp", COL[:, 7, :], self.inp["pool_scale"][j].rearrange("(c k) -> k c", k=128), writes=[COL],
          allow_slow_non_contiguous=True)
    for Mx in (MU, MUP):
        K.op("dve", lambda en, Mx=Mx: en.tensor_scalar(out=Mx[:, 1], in0=Mx[:, 0], scalar1=-1.0, scalar2=1.0,
                                                       op0=ALU.mult, op1=ALU.add), reads=[Mx], writes=[Mx])
    bones = K.sb([128, 128], F32, "e2bones")
    K.op("pool", lambda en: en.memset(bones[:], 0.0), writes=[bones])
    K.op("pool", lambda en: en.memset(bones[0:64, 0:64], 1.0), reads=[bones], writes=[bones])
    K.op("pool", lambda en: en.memset(bones[64:128, 64:128], 1.0), reads=[bones], writes=[bones])
    HTt = K.sb([128, 8, WT], F32, "e2HTt")
    xv = K.sb([128, 8, WB], BF16, "e2xv")
    t1 = [K.sb([128, WB], BF16, f"e2t1{v}") for v in range(3)]
    PTt = [K.sb([128, WT], F32, f"e2PTt{n}") for n in range(4)]
    nm_w = ("Rm", "Km", "Vm", "kk", "sq", "rn", "LW0", "LW1", "AD0", "AD1", "Gm", "kd0", "kd1", "b0", "b1", "lw0", "lw1",
            "cum0", "cum1", "E0", "E1", "tmp0", "tmp1", "tmp", "ks", "IC", "o0", "o1", "o2", "o3", "o4", "o5")
    w = {n: K.sb([128, WB], F32, "e2" + n) for n in nm_w}
    sw = [K.sb([128, WT], F32, f"e2s{q}") for q in range(2)]
    dfb = K.sb([128, WB], BF16, "e2dfb")
    gls = [K.sb([128, 8], F32, f"e2gl{d}") for d in range(2)]
    RM = K.sb([128, WB], F32, "e2RM")
    K.op("pool", lambda en: en.memset(RM[:], 1.0), writes=[RM])
    K.op("pool", lambda en: en.memset(RM[:].rearrange("p (r c) -> p r c", c=64)[:, :, 0:1], 0.0), reads=[RM], writes=[RM])
    orr = [0]

    def otile():
        orr[0] += 1
        return w[f"o{orr[0] % 6}"]

    GRID_H = [(-1, True)] * 2 + [(1, True)] * 2 + [(-64, False)] * 2 + [(64, False)] * 2
    GRID_P = [(-1, True), (1, True), (-64, False), (64, False)]
    SEQ_H = [(-1, False)] * 4 + [(1, False)] * 4
    SEQ_P = [(-1, False)] * 2 + [(1, False)] * 2
    blocks = [(T, CT, T, CT, SEQ_H, SEQ_P, "f_icnt_ctx")] + \
             [(t0, WB, 0, T, GRID_H, GRID_P, "f_icnt_lat") for t0 in range(0, T, WB)]

    def load_halo(buf, view, src_rows, t0, Wb, s0, sl):
        lo = max(t0 - 64, s0)
        hi = min(t0 + Wb + 64, s0 + sl)
        if lo > t0 - 64:
            K.op("pool", lambda en: en.memset(view(0, 64), 0.0), writes=[buf])
        if hi < t0 + Wb + 64:
            K.op("pool", lambda en: en.memset(view(64 + Wb, 128 + Wb), 0.0), writes=[buf])
        K.dma("sp", view(lo - (t0 - 64), hi - (t0 - 64)), src_rows(lo, hi), reads=[e["HT"], e["PT"]], writes=[buf])

    def mix(out_ap, out_buf, srcf, src_buf, mu_ap, omu_ap, delta, rowmask, Wb):
        en1 = "dve"
        K.op("pool", lambda en: en.tensor_scalar(out=out_ap, in0=srcf(64, 64 + Wb), scalar1=omu_ap, scalar2=None,
                                              op0=ALU.mult), reads=[src_buf], writes=[out_buf])
        if not rowmask:
            o, s_ = out_ap, srcf(64 + delta, 64 + delta + Wb)
        else:
            ov = out_ap.rearrange("p (r c) -> p r c", c=64)
            sv = srcf(64 + delta, 64 + delta + Wb).rearrange("p (r c) -> p r c", c=64)
            if delta == -1:
                o, s_ = ov[:, :, 1:64], sv[:, :, 1:64]
            else:
                o, s_ = ov[:, :, 0:63], sv[:, :, 0:63]
        K.op(en1, lambda en: en.scalar_tensor_tensor(out=o, in0=s_, scalar=mu_ap, in1=o, op0=ALU.mult, op1=ALU.add),
             reads=[src_buf, out_buf], writes=[out_buf])

    def store(dst, c, t0, Wb, tile):
        K.dma("sp", dst[c * 128:(c + 1) * 128, t0:t0 + Wb], tile[:, 0:Wb], reads=[tile], writes=[dst])

    for (t0, Wb, s0, sl, HS, PS, icn) in blocks:
        R = Wb // 64
        load_halo(HTt, lambda a, b: HTt[:, :, a:b],
                  lambda a, b: e["HT"][:, a:b].rearrange("(kt k) t -> k kt t", k=128), t0, Wb, s0, sl)
        for v in range(3):
            for kt in range(8):
                dl, rm = HS[kt]
                mix(xv[:, kt, 0:Wb], xv, lambda a, b, kt=kt: HTt[:, kt, a:b], HTt, MU[:, 0, v, kt:kt + 1],
                    MU[:, 1, v, kt:kt + 1], dl, rm, Wb)
            ps = K.ps()
            for kt in range(8):
                K.op("pe", lambda en, kt=kt, v=v: en.matmul(ps[:, 0:Wb], lhsT=W1[:, v, kt, :], rhs=xv[:, kt, 0:Wb],
                                                            start=(kt == 0), stop=(kt == 7)), reads=[W1, xv], writes=[ps])
            fn = (AF.Tanh, AF.Copy, AF.Sigmoid)[v]
            K.op("act", lambda en, v=v, fn=fn: en.activation(out=t1[v][:, 0:Wb], in_=ps[:, 0:Wb], func=fn),
                 reads=[ps], writes=[t1[v]])
        for c in range(4):
            cs_ = slice(c * 128, (c + 1) * 128)
            for n in range(4):
                load_halo(PTt[n], lambda a, b, n=n: PTt[n][:, a:b],
                          lambda a, b, n=n: e["PT"][n * 512 + c * 128:n * 512 + (c + 1) * 128, a:b], t0, Wb, s0, sl)
            dl, rm = PS[c]
            for n, nm in enumerate(("Rm", "Km", "Vm")):
                mix(w[nm][:, 0:Wb], w[nm], lambda a, b, n=n: PTt[n][:, a:b], PTt[n], MUP[:, 0, n, c:c + 1],
                    MUP[:, 1, n, c:c + 1], dl, rm, Wb)
            store(e["VV"], c, t0, Wb, w["Vm"])
            for d in range(2):
                ps = K.ps()
                K.op("pe", lambda en, d=d: en.matmul(ps[:, 0:Wb], lhsT=W2[d * 64:(d + 1) * 64, 0, cs_],
                                                     rhs=t1[0][d * 64:(d + 1) * 64, 0:Wb], start=True, stop=True),
                     reads=[W2, t1[0]], writes=[ps])
                K.op("act", lambda en, d=d: en.activation(out=w[f"LW{d}"][:, 0:Wb], in_=ps[:, 0:Wb], func=AF.Sigmoid,
                                                          bias=COL[:, d, c:c + 1], scale=1.0),
                     reads=[ps, COL], writes=[w[f"LW{d}"]])
                ps = K.ps()
                K.op("pe", lambda en, d=d: en.matmul(ps[:, 0:Wb], lhsT=W2[d * 64:(d + 1) * 64, 1, cs_],
                                                     rhs=t1[1][d * 64:(d + 1) * 64, 0:Wb], start=True, stop=True),
                     reads=[W2, t1[1]], writes=[ps])
                K.op("act", lambda en, d=d: en.activation(out=w[f"AD{d}"][:, 0:Wb], in_=ps[:, 0:Wb], func=AF.Sigmoid,
                                                          bias=COL[:, 2 + d, c:c + 1], scale=1.0),
                     reads=[ps, COL], writes=[w[f"AD{d}"]])
            ps = K.ps()
            K.op("pe", lambda en: en.matmul(ps[:, 0:Wb], lhsT=W2[:, 2, cs_], rhs=t1[2][:, 0:Wb], start=True, stop=True),
                 reads=[W2, t1[2]], writes=[ps])
            K.op("act", lambda en: en.copy(out=w["Gm"][:, 0:Wb], in_=ps[:, 0:Wb]), reads=[ps], writes=[w["Gm"]])
            store(e["GG"], c, t0, Wb, w["Gm"])
            K.op("dve", lambda en: en.tensor_scalar(out=w["kk"][:, 0:Wb], in0=w["Km"][:, 0:Wb], scalar1=COL[:, 4, c:c + 1],
                                                    scalar2=None, op0=ALU.mult), reads=[w["Km"], COL], writes=[w["kk"]])
            K.op("pool", lambda en: en.tensor_tensor(out=w["sq"][:, 0:Wb], in0=w["kk"][:, 0:Wb], in1=w["kk"][:, 0:Wb],
                                                     op=ALU.mult), reads=[w["kk"]], writes=[w["sq"]])
            ps = K.ps()
            K.op("pe", lambda en: en.matmul(ps[:, 0:Wb], lhsT=bones[:], rhs=w["sq"][:, 0:Wb], start=True, stop=True),
                 reads=[bones, w["sq"]], writes=[ps])
            K.op("dve", lambda en: en.tensor_scalar(out=w["rn"][:, 0:Wb], in0=ps[:, 0:Wb], scalar1=1e-12, scalar2=None,
                                                    op0=ALU.max), reads=[ps], writes=[w["rn"]])
            K.op("act", lambda en: en.sqrt(out=w["rn"][:, 0:Wb], in_=w["rn"][:, 0:Wb]), reads=[w["rn"]], writes=[w["rn"]])
            K.op("dve", lambda en: en.reciprocal(out=w["rn"][:, 0:Wb], in_=w["rn"][:, 0:Wb]), reads=[w["rn"]],
                 writes=[w["rn"]])
            K.op("dve", lambda en: en.tensor_tensor(out=w["kk"][:, 0:Wb], in0=w["kk"][:, 0:Wb], in1=w["rn"][:, 0:Wb],
                                                    op=ALU.mult), reads=[w["kk"], w["rn"]], writes=[w["kk"]])
            def dchain(d):
                AD, LW = w[f"AD{d}"], w[f"LW{d}"]
                tmp, kd, bb, lw, cum, E = (w[f"{nm_}{d}"] for nm_ in ("tmp", "kd", "b", "lw", "cum", "E"))
                K.op("dve", lambda en: en.tensor_scalar(out=tmp[:, 0:Wb], in0=AD[:, 0:Wb], scalar1=-1.0,
                                                        scalar2=COL[:, 5, c:c + 1], op0=ALU.add, op1=ALU.mult),
                     reads=[AD, COL], writes=[tmp])
                K.op("pool", lambda en: en.tensor_tensor(out=bb[:, 0:Wb], in0=w["kk"][:, 0:Wb], in1=AD[:, 0:Wb],
                                                         op=ALU.mult), reads=[w["kk"], AD], writes=[bb])
                K.op("act", lambda en: en.mul(out=lw[:, 0:Wb], in_=LW[:, 0:Wb], mul=-0.6065306597126334),
                     reads=[LW], writes=[lw])
                yield
                K.op("dve", lambda en: en.scalar_tensor_tensor(out=kd[:, 0:Wb], in0=tmp[:, 0:Wb], scalar=1.0,
                                                               in1=w["Km"][:, 0:Wb], op0=ALU.add, op1=ALU.mult),
                     reads=[tmp, w["Km"]], writes=[kd])
                yield
                K.op("dve", lambda en: en.tensor_tensor_scan(out=cum[:, 0:Wb], data0=RM[:, 0:Wb], data1=lw[:, 0:Wb],
                                                             initial=0.0, op0=ALU.mult, op1=ALU.add),
                     reads=[RM, lw], writes=[cum])
                yield
                cumv = cum[:, 0:Wb].rearrange("p (r c) -> p r c", c=64)
                if d == 1:
                    K.op("pool", lambda en: en.tensor_tensor(out=tmp[:, 0:Wb], in0=lw[:, 0:Wb], in1=cum[:, 0:Wb],
                                                             op=ALU.subtract), reads=[lw, cum], writes=[tmp])
                    yield
                    tv0 = tmp[:, 0:Wb].rearrange("p (r c) -> p r c", c=64)
                    K.op("dve", lambda en: en.tensor_tensor(out=E[:, 0:Wb].rearrange("p (r c) -> p r c", c=64), in0=tv0,
                                                            in1=cumv[:, :, 63].unsqueeze(2).to_broadcast([128, R, 64]),
                                                            op=ALU.add), reads=[tmp, cum], writes=[E])
                    yield
                    K.op("pool", lambda en: en.tensor_copy(out=cum[:, 0:Wb], in_=E[:, 0:Wb]), reads=[E], writes=[cum])
                    yield
                last = 63 if d == 0 else 0
                cL = cumv[:, :, last]
                gl = gls[d]
                K.op("act", lambda en: en.activation(out=gl[:, 0:R], in_=cL, func=AF.Exp), reads=[cum], writes=[gl])
                K.dma("sp", e[f"GL{d}"][c * 128:(c + 1) * 128, t0 // 64:t0 // 64 + R], gl[:, 0:R], reads=[gl],
                      writes=[e[f"GL{d}"]])
                K.op("act", lambda en: en.activation(out=E[:, 0:Wb], in_=cum[:, 0:Wb], func=AF.Exp), reads=[cum],
                     writes=[E])
                K.op("pool", lambda en: en.tensor_tensor(out=tmp[:, 0:Wb], in0=cum[:, 0:Wb], in1=lw[:, 0:Wb],
                                                         op=ALU.subtract), reads=[cum, lw], writes=[tmp])
                yield
                o = otile()
                K.op("dve", lambda en: en.tensor_tensor(out=o[:, 0:Wb], in0=w["Rm"][:, 0:Wb], in1=E[:, 0:Wb],
                                                        op=ALU.mult), reads=[w["Rm"], E], writes=[o])
                store(e[f"Rt{d}"], c, t0, Wb, o)
                yield
                K.op("act", lambda en: en.activation(out=E[:, 0:Wb], in_=cum[:, 0:Wb], func=AF.Exp, scale=-1.0),
                     reads=[cum], writes=[E])
                yield
                for src_, dn in ((bb, "Bt"), (kd, "Kt")):
                    o = otile()
                    K.op(ve(), lambda en, o=o, src_=src_: en.tensor_tensor(out=o[:, 0:Wb], in0=src_[:, 0:Wb],
                                                                           in1=E[:, 0:Wb], op=ALU.mult),
                         reads=[src_, E], writes=[o])
                    store(e[f"{dn}{d}"], c, t0, Wb, o)
                yield
                K.op("act", lambda en: en.activation(out=E[:, 0:Wb], in_=tmp[:, 0:Wb], func=AF.Exp),
                     reads=[tmp], writes=[E])
                yield
                o = otile()
                K.op("dve", lambda en: en.scalar_tensor_tensor(out=o[:, 0:Wb], in0=w["kk"][:, 0:Wb], scalar=-1.0,
                                                               in1=E[:, 0:Wb], op0=ALU.mult, op1=ALU.mult),
                     reads=[w["kk"], E], writes=[o])
                store(e[f"At{d}"], c, t0, Wb, o)
                tv = tmp[:, 0:Wb].rearrange("p (r c) -> p r c", c=64)
                K.op("dve", lambda en: en.tensor_tensor(out=tv, in0=cumv, in1=cL.unsqueeze(2).to_broadcast([128, R, 64]),
                                                        op=ALU.subtract), reads=[cum], writes=[tmp])
                yield
                K.op("act", lambda en: en.activation(out=E[:, 0:Wb], in_=tmp[:, 0:Wb], func=AF.Exp, scale=-1.0),
                     reads=[tmp], writes=[E])
                yield
                for src_, dn in ((bb, "Bh"), (kd, "Kh")):
                    o = otile()
                    K.op(ve(), lambda en, o=o, src_=src_: en.tensor_tensor(out=o[:, 0:Wb], in0=src_[:, 0:Wb],
                                                                           in1=E[:, 0:Wb], op=ALU.mult),
                         reads=[src_, E], writes=[o])
                    store(e[f"{dn}{d}"], c, t0, Wb, o)

            gens = [dchain(0), dchain(1)]
            while gens:
                alive = []
                for g_ in gens:
                    try:
                        next(g_)
                        alive.append(g_)
                    except StopIteration:
                        pass
                gens = alive
            K.op("pool", lambda en: en.tensor_tensor(out=w["ks"][:, 0:Wb], in0=w["kd0"][:, 0:Wb], in1=w["kd1"][:, 0:Wb],
                                                     op=ALU.add), reads=[w["kd0"], w["kd1"]], writes=[w["ks"]])
            K.op("dve", lambda en: en.scalar_tensor_tensor(out=w["tmp"][:, 0:Wb], in0=w["ks"][:, 0:Wb],
                                                           scalar=COL[:, 6, c:c + 1], in1=w["Rm"][:, 0:Wb],
                                                           op0=ALU.mult, op1=ALU.mult),
                 reads=[w["ks"], COL, w["Rm"]], writes=[w["tmp"]])
            ps = K.ps()
            K.op("pe", lambda en: en.matmul(ps[:, 0:Wb], lhsT=bones[:], rhs=w["tmp"][:, 0:Wb], start=True, stop=True),
                 reads=[bones, w["tmp"]], writes=[ps])
            o = otile()
            K.op("dve", lambda en: en.tensor_tensor(out=o[:, 0:Wb], in0=ps[:, 0:Wb], in1=w["Vm"][:, 0:Wb], op=ALU.mult),
                 reads=[ps, w["Vm"]], writes=[o])
            store(e["BON"], c, t0, Wb, o)
            u = PTt[3]
            Wt = Wb + 128
            K.dma("sp", w["IC"][:, 0:Wb], self.inp[icn][c:c + 1, t0 - s0:t0 - s0 + Wb].partition_broadcast(128),
                  writes=[w["IC"]])
            s_a, s_b = sw
            K.op("dve", lambda en: en.tensor_tensor(out=s_a[:, 1:Wt], in0=u[:, 0:Wt - 1], in1=u[:, 1:Wt], op=ALU.add),
                 reads=[u], writes=[s_a])
            cur_s, oth = s_a, s_b
            lo_, hi_ = 1, Wt
            for hs in (1, 2, 4):
                if POOL_WINS[c] < 4 * hs:
                    break
                nlo, nhi = lo_ + hs, hi_ - hs
                K.op("dve", lambda en, cur_s=cur_s, oth=oth, nlo=nlo, nhi=nhi, hs=hs: en.tensor_tensor(
                    out=oth[:, nlo:nhi], in0=cur_s[:, nlo - hs:nhi - hs], in1=cur_s[:, nlo + hs:nhi + hs], op=ALU.add),
                    reads=[cur_s], writes=[oth])
                cur_s, oth = oth, cur_s
                lo_, hi_ = nlo, nhi
            K.op("dve", lambda en: en.tensor_tensor(out=w["tmp"][:, 0:Wb], in0=cur_s[:, 64:64 + Wb], in1=w["IC"][:, 0:Wb],
                                                    op=ALU.mult), reads=[cur_s, w["IC"]], writes=[w["tmp"]])
            K.op("pool", lambda en: en.tensor_tensor(out=dfb[:, 0:Wb], in0=w["tmp"][:, 0:Wb], in1=u[:, 64:64 + Wb],
                                                     op=ALU.subtract), reads=[w["tmp"], u], writes=[dfb])
            ps = K.ps()
            K.op("pe", lambda en: en.matmul(ps[:, 0:Wb], lhsT=pw[:, c, :], rhs=dfb[:, 0:Wb], start=True, stop=True),
                 reads=[pw, dfb], writes=[ps])
            o = otile()
            K.op("dve", lambda en: en.tensor_scalar(out=o[:, 0:Wb], in0=ps[:, 0:Wb], scalar1=COL[:, 7, c:c + 1],
                                                    scalar2=None, op0=ALU.mult), reads=[ps, COL], writes=[o])
            store(e["YB"], c, t0, Wb, o)
    K.pop_scope()


Prog.even_e2 = _even_e2
def _even_e3(self, i, SDT=None, nsteps=66):
    SDT = SDT or self.SCAN_DT
    K = self.K
    e = self.ed
    K.push_scope()
    TEN = ("At", "Bt", "Kt", "Rt", "Bh", "Kh", "VV")

    def ring(nm, shape, n, dt=F32):
        bufs = [K.sb(shape, dt, f"e3{nm}{q}") for q in range(n)]
        cnt = [0]

        def nxt():
            cnt[0] += 1
            return bufs[cnt[0] % n]
        return nxt

    def mk_mask(nm, cmp_op, sgn=1):
        mbuf = K.sb([128, 128], F32, "e3m" + nm)
        K.op("pool", lambda en: en.memset(mbuf[:], 1.0), writes=[mbuf])
        K.op("pool", lambda en: en.affine_select(out=mbuf[:], in_=mbuf[:], pattern=[[sgn, 128]], compare_op=cmp_op,
                                                 fill=0.0, base=0, channel_multiplier=-sgn), reads=[mbuf], writes=[mbuf])
        K.op("pool", lambda en: en.memset(mbuf[0:64, 64:128], 0.0), reads=[mbuf], writes=[mbuf])
        K.op("pool", lambda en: en.memset(mbuf[64:128, 0:64], 0.0), reads=[mbuf], writes=[mbuf])
        return mbuf
    UPs = mk_mask("ups", ALU.is_gt)
    UPi = mk_mask("upi", ALU.is_ge)
    LOs = mk_mask("los", ALU.is_gt, -1)
    LOi = mk_mask("loi", ALU.is_ge, -1)
    BDM = mk_mask("bdm", ALU.is_ge)
    K.op("pool", lambda en: en.memset(BDM[0:64, 0:64], 1.0), reads=[BDM], writes=[BDM])
    K.op("pool", lambda en: en.memset(BDM[64:128, 64:128], 1.0), reads=[BDM], writes=[BDM])
    M4 = []
    MI = []
    for d in range(2):
        strictT, strict, inclT = (UPs, LOs, UPi) if d == 0 else (LOs, UPs, LOi)
        m4 = K.sb([128, 512], F32, f"e3m4{d}")
        for q, src in enumerate((strictT, strict, strictT, inclT)):
            K.op("pool", lambda en, q=q, src=src: en.tensor_copy(out=m4[:, q * 128:(q + 1) * 128], in_=src[:]),
                 reads=[src], writes=[m4])
        M4.append(m4)
        MI.append(inclT)
    identS = self.ident
    import os
    if os.environ.get("E3PAD"):
        _pad = K.sb([128, 128], F32, "e3pad")
    S = [[K.sb([128, 128], F32, f"e3S{d}{c}") for c in range(4)] for d in range(2)]
    for d in range(2):
        for c in range(4):
            K.op("pool", lambda en, d=d, c=c: en.memset(S[d][c][:], 0.0), writes=[S[d][c]])
    Ssd = S
    if SDT != F32:
        Ssd = [[K.sb([128, 128], SDT, f"e3Sb{d}{c}") for c in range(4)] for d in range(2)]
        for d in range(2):
            for c in range(4):
                K.op("pool", lambda en, d=d, c=c: en.memset(Ssd[d][c][:], 0.0), writes=[Ssd[d][c]])
    LD = [[{nm: K.sb([128, 4, 64], F32, f"e3ld{d}{b}{nm}") for nm in TEN} for b in range(2)] for d in range(2)]
    GLall = [K.sb([128, 4, NT // 64], F32, f"e3glall{d}") for d in range(2)]
    for d in range(2):
        K.dma("sp", GLall[d][:], e[f"GL{d}"][:, :].rearrange("(c p) t -> p c t", p=128), reads=[e[f"GL{d}"]],
              writes=[GLall[d]])
    r_bd = ring("bd", [128, 7, 128], 9, SDT)
    r_tp = ring("tp", [128, 384], 9, SDT)
    r_m = ring("m", [128, 640], 9, SDT)
    r_q = ring("q", [128, 256], 16, SDT)
    r_w = ring("w", [128, 128], 16, SDT)
    r_x = ring("x", [128, 128], 9, SDT)
    r_u = ring("u", [128, 128], 9, SDT)
    r_y = ring("y", [128, 4, 64], 4, F32)
    rr = [0]
    NCH = 2 * nsteps

    def ve3():
        rr[0] += 1
        return ("dve", "pool")[rr[0] % 2]

    def tok_base(d, n):
        if d == 0:
            return T + 64 * n if n < 4 else 64 * (n - 4)
        return T + 64 * (3 - n) if n < 4 else 64 * (127 - (n - 4))

    def load(d, n):
        b = n % 2
        tb = tok_base(d, n)
        for nm in TEN:
            src = e[nm if nm == "VV" else f"{nm}{d}"]
            K.dma("sp", LD[d][b][nm][:], src[:, tb:tb + 64].rearrange("(c p) t -> p c t", p=128), reads=[src],
                  writes=[LD[d][b][nm]])

    def mm(ps_ap, ps, lhsT, lb, rhs, rb, start=True, stop=True):
        K.op("pe", lambda en: en.matmul(ps_ap, lhsT=lhsT, rhs=rhs, start=start, stop=stop), reads=[lb, rb], writes=[ps])

    def unit(d, n, c, ysb):
        b = n % 2
        ld = LD[d][b]
        bd = r_bd()
        for ti, nm in enumerate(TEN):
            src = ld[nm][:, c, :]
            K.op(ve3(), lambda en, ti=ti, src=src: en.tensor_tensor(
                out=bd[:, ti, :].rearrange("p (a b) -> p a b", a=2), in0=src.unsqueeze(1).to_broadcast([128, 2, 64]),
                in1=BDM[:].rearrange("p (a b) -> p a b", a=2), op=ALU.mult), reads=[ld[nm], BDM], writes=[bd])
        yield
        A_, B_, K_, R_ = (bd[:, t_, :] for t_ in range(4))
        pst = K.ps()
        idm = identS if SDT == F32 else self.identb
        for t_ in range(3):
            mm(pst[:, t_ * 128:(t_ + 1) * 128], pst, bd[:, 4 + t_, :], bd, idm[:], idm)
        tp = r_tp()
        K.op("act", lambda en: en.copy(out=tp[:], in_=pst[:, 0:384]), reads=[pst], writes=[tp])
        BhT, KhT, VT = (tp[:, t_ * 128:(t_ + 1) * 128] for t_ in range(3))
        p1 = K.ps()
        mm(p1[:, 0:128], p1, B_, bd, A_, bd)
        mm(p1[:, 128:256], p1, A_, bd, B_, bd)
        mm(p1[:, 256:384], p1, K_, bd, A_, bd)
        mm(p1[:, 384:512], p1, B_, bd, R_, bd)
        p2 = K.ps()
        mm(p2[:, 0:128], p2, K_, bd, R_, bd)
        mt = r_m()
        K.op("dve", lambda en: en.tensor_tensor(out=mt[:, 0:512], in0=p1[:, :], in1=M4[d][:], op=ALU.mult),
             reads=[p1, M4[d]], writes=[mt])
        K.op("dve", lambda en: en.tensor_tensor(out=mt[:, 512:640], in0=p2[:, 0:128], in1=MI[d][:], op=ALU.mult),
             reads=[p2, MI[d]], writes=[mt])
        Q, QT, MakT, MrbT, MrkT = (mt[:, t_ * 128:(t_ + 1) * 128] for t_ in range(5))
        W = r_w()
        K.op("pool", lambda en: en.tensor_tensor(out=W[:], in0=Q, in1=identS[:], op=ALU.add),
             reads=[mt, identS], writes=[W])
        yield
        qb = mt
        for lvl in range(1, 6):
            pq = K.ps()
            mm(pq[:, 128:256], pq, Q, qb, QT, qb)
            if lvl < 5:
                mm(pq[:, 0:128], pq, QT, qb, Q, qb)
            nq = r_q()
            if lvl < 5:
                K.op("act", lambda en: en.copy(out=nq[:], in_=pq[:, 0:256]), reads=[pq], writes=[nq])
            else:
                K.op("act", lambda en: en.copy(out=nq[:, 128:256], in_=pq[:, 128:256]), reads=[pq], writes=[nq])
            Q, QT, qb = nq[:, 0:128], nq[:, 128:256], nq
            yield
            pw_ = K.ps()
            mm(pw_[:, 0:128], pw_, QT, qb, W[:], W)
            W2_ = r_w()
            K.op("dve", lambda en: en.tensor_tensor(out=W2_[:], in0=pw_[:, 0:128], in1=W[:], op=ALU.add),
                 reads=[pw_, W], writes=[W2_])
            W = W2_
            yield
        Sb = S[d][c]
        Sm = Ssd[d][c]
        px = K.ps()
        mm(px[:, 0:128], px, A_, bd, Sm[:], Sm, True, False)
        mm(px[:, 0:128], px, MakT, mt, VT, tp, False, True)
        Xs = r_x()
        K.op("act", lambda en: en.copy(out=Xs[:], in_=px[:, 0:128]), reads=[px], writes=[Xs])
        yield
        pu = K.ps()
        mm(pu[:, 0:128], pu, W[:], W, Xs[:], Xs)
        Us = r_u()
        K.op("dve", lambda en: en.tensor_copy(out=Us[:], in_=pu[:, 0:128]), reads=[pu], writes=[Us])
        if self.dbg.get("e3dbg") is not None and d == 0 and c == 0 and n in (0, 1):
            dd = self.dbg["e3dbg"]
            K.dma("sp", dd[n, 0, :, 0:128], Xs[:], reads=[Xs])
            K.dma("sp", dd[n, 1, :, 0:128], Us[:], reads=[Us])
            K.dma("sp", dd[n, 2, :, 0:128], W[:], reads=[W])
            K.dma("sp", dd[n, 3, :, 0:640], mt[:], reads=[mt])
            K.dma("sp", dd[n, 4, :, 0:128], Sb[:], reads=[Sb])
            K.dma("sp", dd[n, 5, :, 0:384], tp[:], reads=[tp])
            for t_ in range(4):
                K.dma("sp", dd[n, 6, :, t_ * 128:(t_ + 1) * 128], bd[:, t_, :], reads=[bd])
        yield
        py = K.ps()
        mm(py[:, 0:128], py, R_, bd, Sm[:], Sm, True, False)
        mm(py[:, 0:128], py, MrbT, mt, Us[:], Us, False, False)
        mm(py[:, 0:128], py, MrkT, mt, VT, tp, False, True)
        pss = K.ps()
        mm(pss[:, 0:128], pss, BhT, tp, Us[:], Us, True, False)
        mm(pss[:, 0:128], pss, KhT, tp, VT, tp, False, True)
        K.op("act", lambda en: en.copy(out=ysb[0:64, c, :], in_=py[0:64, 0:64]), reads=[py], writes=[ysb])
        K.op("act", lambda en: en.copy(out=ysb[64:128, c, :], in_=py[64:128, 64:128]), reads=[py], writes=[ysb])
        gcol = tok_base(d, n) // 64
        K.op("dve", lambda en: en.scalar_tensor_tensor(out=Sb[:], in0=Sb[:], scalar=GLall[d][:, c, gcol:gcol + 1],
                                                       in1=pss[:, 0:128], op0=ALU.mult, op1=ALU.add),
             reads=[Sb, GLall[d], pss], writes=[Sb])
        if SDT != F32:
            K.op("pool", lambda en: en.tensor_copy(out=Sm[:], in_=Sb[:]), reads=[Sb], writes=[Sm])

    for d in range(2):
        load(d, 0)
    for n in range(NCH):
        for d in range(2):
            if n + 1 < NCH:
                load(d, n + 1)
        ysbs = [r_y(), r_y()]
        gens = [unit(d, n, c, ysbs[d]) for c in range(4) for d in range(2)]
        while gens:
            alive = []
            for g in gens:
                try:
                    next(g)
                    alive.append(g)
                except StopIteration:
                    pass
            gens = alive
        for d in range(2):
            tb = tok_base(d, n)
            dst = e[f"YD{d}"][tb:tb + 64, :].rearrange("t (c h v) -> h t c v", h=2, v=64)
            for h2 in range(2):
                K.dma("sp", dst[h2], ysbs[d][h2 * 64:(h2 + 1) * 64, :, :], reads=[ysbs[d]], writes=[e[f"YD{d}"]])
    if self.dbg.get("S") is not None:
        for d in range(2):
            for c in range(4):
                K.dma("sp", self.dbg["S"][d, c], S[d][c][:], reads=[S[d][c]])
    K.pop_scope()


Prog.even_e3 = _even_e3
def _even_e4(self, i, ntile):
    K = self.K
    j = i // 2
    e = self.ed
    K.push_scope()
    wout = K.sb([128, 8, D], BF16, "e4wout")
    stg = [K.sb([128, D], F32, f"e4stg{q}") for q in range(2)]
    for kt in range(8):
        sg = stg[kt % 2]
        K.dma("sp", sg[:], self.inp["w_out"][j, kt * 128:(kt + 1) * 128, :], writes=[sg])
        K.op("dve", lambda en, sg=sg, kt=kt: en.tensor_copy(out=wout[:, kt, :], in_=sg[:]), reads=[sg], writes=[wout])
    GN = K.sb([128, 2, 4], F32, "e4gn")
    K.dma("sp", GN[:, 0, :], self.inp["gn_w"][j].rearrange("(c k) -> k c", k=128), writes=[GN], allow_slow_non_contiguous=True)
    K.dma("sp", GN[:, 1, :], self.inp["gn_b"][j].rearrange("(c k) -> k c", k=128), writes=[GN], allow_slow_non_contiguous=True)
    y0s = [K.sb([128, 512], F32, f"e4y0{q}") for q in range(2)]
    y1s = [K.sb([128, 512], F32, f"e4y1{q}") for q in range(2)]
    sqs = [K.sb([128, 512], F32, f"e4sq{q}") for q in range(2)]
    sts = [K.sb([128, 4, 8], F32, f"e4st{q}") for q in range(2)]
    bons = [K.sb([128, 4, 128], F32, f"e4bon{q}") for q in range(2)]
    ggs = [K.sb([128, 4, 128], F32, f"e4gg{q}") for q in range(2)]
    ybs = [K.sb([128, 4, 128], F32, f"e4yb{q}") for q in range(2)]
    yTs = [K.sb([128, 4, 128], F32, f"e4yT{q}") for q in range(2)]
    cats = [K.sb([128, 8, 128], BF16, f"e4cat{q}") for q in range(2)]
    xts = stg
    ots = [K.sb([128, D], F32, f"e4o{q}") for q in range(2)]
    for tt in range(ntile):
        y0, y1, sq, st, bon, gg, yb, yT, cat, xt, ot = (l[tt % 2] for l in (y0s, y1s, sqs, sts, bons, ggs, ybs, yTs, cats,
                                                                            xts, ots))
        M = self.Ml if tt < 64 else self.Mc
        tk = slice(tt * 128, (tt + 1) * 128)
        K.dma("sp", y0[:], e["YD0"][tk, :], reads=[e["YD0"]], writes=[y0])
        K.dma("sp", y1[:], e["YD1"][tk, :], reads=[e["YD1"]], writes=[y1])
        for buf, nm in ((bon, "BON"), (gg, "GG"), (yb, "YB")):
            K.dma("sp", buf[:], e[nm][:, tk].rearrange("(c p) t -> p c t", p=128), reads=[e[nm]], writes=[buf])
        K.dma("sp", xt[:], self.lat[tk, :], reads=[self.lat.p(tt)], writes=[xt])
        K.op("pool", lambda en: en.tensor_tensor(out=y0[:], in0=y0[:], in1=y1[:], op=ALU.add), reads=[y0, y1], writes=[y0])
        yv = y0[:].rearrange("p (h v) -> p h v", v=64)
        sv = sq[:].rearrange("p (h v) -> p h v", v=64)
        K.op("dve", lambda en: en.reduce_sum(out=st[:, 0, :], in_=yv, axis=AX.X), reads=[y0], writes=[st])
        K.op("dve", lambda en: en.tensor_scalar(out=st[:, 1, :], in0=st[:, 0, :], scalar1=1.0 / 64, scalar2=None,
                                                op0=ALU.mult), reads=[st], writes=[st])
        K.op("dve", lambda en: en.tensor_tensor(out=yv, in0=yv, in1=st[:, 1, :].unsqueeze(2).to_broadcast([128, 8, 64]),
                                                op=ALU.subtract), reads=[y0, st], writes=[y0])
        K.op("pool", lambda en: en.tensor_tensor(out=sq[:], in0=y0[:], in1=y0[:], op=ALU.mult), reads=[y0], writes=[sq])
        K.op("dve", lambda en: en.reduce_sum(out=st[:, 2, :], in_=sv, axis=AX.X), reads=[sq], writes=[st])
        K.op("dve", lambda en: en.tensor_scalar(out=st[:, 2, :], in0=st[:, 2, :], scalar1=1.0 / 64, scalar2=64e-5,
                                                op0=ALU.mult, op1=ALU.add), reads=[st], writes=[st])
        K.op("act", lambda en: en.sqrt(out=st[:, 3, :], in_=st[:, 2, :]), reads=[st], writes=[st])
        K.op("dve", lambda en: en.reciprocal(out=st[:, 3, :], in_=st[:, 3, :]), reads=[st], writes=[st])
        K.op("dve", lambda en: en.tensor_tensor(out=yv, in0=yv, in1=st[:, 3, :].unsqueeze(2).to_broadcast([128, 8, 64]),
                                                op=ALU.mult), reads=[y0, st], writes=[y0])
        ps = K.ps()
        for c in range(4):
            self.tr(None, ps[:, c * 128:(c + 1) * 128], y0[:, c * 128:(c + 1) * 128], ps, y0)
        for c in range(4):
            K.op("dve", lambda en, c=c: en.tensor_scalar(out=yT[:, c, :], in0=ps[:, c * 128:(c + 1) * 128],
                                                         scalar1=GN[:, 0, c:c + 1], scalar2=GN[:, 1, c:c + 1],
                                                         op0=ALU.mult, op1=ALU.add), reads=[ps, GN], writes=[yT])
        K.op("pool", lambda en: en.tensor_tensor(out=yT[:], in0=yT[:], in1=bon[:], op=ALU.add), reads=[yT, bon],
             writes=[yT])
        K.op("pool", lambda en: en.tensor_tensor(out=cat[:, 0:4, :], in0=yT[:], in1=gg[:], op=ALU.mult), reads=[yT, gg],
             writes=[cat])
        K.op("act", lambda en: en.copy(out=cat[:, 4:8, :], in_=yb[:]), reads=[yb], writes=[cat])
        for hh in range(2):
            po = K.ps()
            for kt in range(8):
                K.op("pe", lambda en, kt=kt: en.matmul(po[:, :], lhsT=cat[:, kt, :], rhs=wout[:, kt, hh * 512:(hh + 1) * 512],
                                                       start=(kt == 0), stop=(kt == 7)), reads=[cat, wout], writes=[po])
            K.op("dve", lambda en: en.tensor_tensor(out=ot[:, hh * 512:(hh + 1) * 512], in0=po[:, :],
                                                    in1=M[:, 2, hh * 512:(hh + 1) * 512], op=ALU.mult),
                 reads=[po, M], writes=[ot])
        K.op("pool", lambda en: en.tensor_tensor(out=ot[:], in0=ot[:], in1=xt[:], op=ALU.add), reads=[ot, xt], writes=[ot])
        K.dma("sp", self.lat[tk, :], ot[:], reads=[ot], writes=[self.lat.p(tt)])
    K.pop_scope()


def _even_mixer(self, i):
    self.even_e1(i, ntile=66)
    self.even_e2(i)
    self.even_e3(i)
    self.even_e4(i, ntile=66 if i < 2 else 64)


Prog.even_e4 = _even_e4
Prog.even_mixer = _even_mixer
Prog.even_declare = _even_declare
Prog.even_e1 = _even_e1


def build_program():
    P = Prog()
    P.fourier_declare()
    P.even_declare()
    P.init_lat()
    P.prep_s()
    for i in range(DEPTH):
        P.modvec(i, need_ctx=(i <= 2))
        if i % 2 == 0:
            P.even_mixer(i)
        else:
            P.fourier(i, with_ctx=(i < 2))
        P.moe_setup()
        P.moe(i, with_ctx=(i < 2))
    P.final_norm()
    P.K.finish()
    return P


def kernel(**inputs):
    x = np.asarray(inputs["x"], dtype=np.float32)
    B = x.shape[0]
    P = build_program()
    consts = fourier_consts()
    pconsts = pool_consts()
    in_maps = []
    for b in range(B):
        d = {"x": np.ascontiguousarray(x[b]),
             "c": np.ascontiguousarray(np.asarray(inputs["c"], dtype=np.float32)[b:b + 1]),
             "ctx": np.ascontiguousarray(np.asarray(inputs["ctx"], dtype=np.float32)[b]),
             "c_ctx": np.ascontiguousarray(np.asarray(inputs["c_ctx"], dtype=np.float32)[None, :])}
        for k in WEIGHT_SPECS:
            d[k] = np.ascontiguousarray(np.asarray(inputs[k], dtype=np.float32))
        d.update(consts)
        d.update(pconsts)
        in_maps.append(d)
    res = run_bass_kernel_spmd(P.K.nc, in_maps, core_ids=list(range(B)))
    return np.stack([np.asarray(r["out"]) for r in res.results], axis=0).astype(np.float32)
```

```python
import numpy as np
from contextlib import ExitStack
import concourse.bass as bass
import concourse.mybir as mybir
from concourse.bass_utils import run_bass_kernel_spmd

F32 = mybir.dt.float32
BF16 = mybir.dt.bfloat16
I32 = mybir.dt.int32
AF = mybir.ActivationFunctionType
ALU = mybir.AluOpType
AX = mybir.AxisListType

D = 1024
T = 8192
CT = 256
NT = T + CT
DEPTH = 4
NEXP = 32
DE = 512


class Buf:
    def __init__(self, t, name):
        self.t = t
        self.name = name
        self.lw = None
        self.rd = {}

    def __getitem__(self, idx):
        return self.t[idx]


class Parts:
    def __init__(self, t, name):
        self.t = t
        self.name = name
        self.parts = {}

    def p(self, key):
        b = self.parts.get(key)
        if b is None:
            b = Buf(self.t, f"{self.name}.{key}")
            self.parts[key] = b
        return b

    def all(self):
        return list(self.parts.values())

    def __getitem__(self, idx):
        return self.t[idx]


class Ctx:
    KD = 16
    SAME_ENG_SYNC = True

    def __init__(self):
        self.nc = bass.Bass("TRN2", target_bir_lowering=False)
        nc = self.nc
        self.es = ExitStack()
        self.eng = {"pe": nc.tensor, "act": nc.scalar, "dve": nc.vector, "pool": nc.gpsimd, "sp": nc.sync}
        self.csem = {e: self.es.enter_context(nc.semaphore("c_" + e)) for e in ("pe", "act", "dve", "pool")}
        self.ccnt = {e: 0 for e in self.csem}
        self.dsem = {q: [self.es.enter_context(nc.semaphore(f"d_{q}{i}")) for i in range(self.KD)]
                     for q in ("sp", "pool", "act")}
        self.dcnt = {q: 0 for q in self.dsem}
        self.known = {e: {} for e in self.eng}
        self.nalloc = 0
        self.psum_banks = []
        self.psum_i = 0

    def sb(self, shape, dtype=F32, name=None):
        self.nalloc += 1
        name = (name or "sb") + f"_{self.nalloc}"
        es = self.scopes[-1] if getattr(self, "scopes", None) else self.es
        t = es.enter_context(self.nc.sbuf_tensor(name, list(shape), dtype))
        return Buf(t, name)

    def push_scope(self):
        if not hasattr(self, "scopes"):
            self.scopes = []
        self.scopes.append(ExitStack())

    def pop_scope(self):
        self.barrier()
        self.scopes.pop().close()

    def barrier(self):
        for e in self.eng:
            for src, sem in self.csem.items():
                if self.ccnt[src] > 0:
                    self._wait(e, (sem, self.ccnt[src], "bar"))
            self._wait_all_dma(e)

    def _wait_all_dma(self, e):
        for q in self.dsem:
            n = self.dcnt[q]
            for r in range(self.KD):
                cnt = (n - r + self.KD - 1) // self.KD if n > r else 0
                if cnt > 0:
                    self._wait(e, (self.dsem[q][r], 16 * cnt, "dma"))

    def dram(self, name, shape, dtype=F32, kind="Internal"):
        return self.nc.dram_tensor(name, list(shape), dtype, kind=kind)

    def init_psum(self, n=8):
        for i in range(n):
            t = self.es.enter_context(self.nc.psum_tensor(f"ps{i}", [128, 512], F32))
            b = Buf(t, f"ps{i}")
            b.excl = True
            self.psum_banks.append(b)

    def ps(self):
        b = self.psum_banks[self.psum_i % len(self.psum_banks)]
        self.psum_i += 1
        return b

    def _wait(self, e, ev):
        if ev is None:
            return
        sem, val, src = ev
        if src == e and (e == "pe" or not self.SAME_ENG_SYNC):
            return
        k = self.known[e]
        key = sem.name
        if k.get(key, 0) >= val:
            return
        self.eng[e].wait_ge(sem, val)
        k[key] = val

    def _deps(self, e, reads, writes):
        for b in reads:
            self._wait(e, b.lw)
            if getattr(b, "excl", False):
                for ke, ev in b.rd.items():
                    if ke != e:
                        self._wait(e, ev)
        for b in writes:
            self._wait(e, b.lw)
            for ev in b.rd.values():
                self._wait(e, ev)

    def _commit(self, ev, key, reads, writes):
        for b in writes:
            b.lw = ev
            b.rd = {}
        for b in reads:
            b.rd[key] = ev

    def op(self, e, fn, reads=(), writes=()):
        self._deps(e, reads, writes)
        ins = fn(self.eng[e])
        self.ccnt[e] += 1
        ins.then_inc(self.csem[e], 1)
        ev = (self.csem[e], self.ccnt[e], e)
        self._commit(ev, e, reads, writes)
        return ins

    def dma(self, q, out, in_, reads=(), writes=(), indirect=None, **kw):
        i = self.dcnt[q]
        self.dcnt[q] += 1
        sem = self.dsem[q][i % self.KD]
        val = 16 * (i // self.KD + 1)
        if i >= self.KD:
            self._wait(q, (sem, val - 16, "dma"))
        self._deps(q, reads, writes)
        if indirect is None:
            ins = self.eng[q].dma_start(out=out, in_=in_, **kw)
        else:
            ins = self.eng[q].indirect_dma_start(out=out, in_=in_, **indirect)
        ins.then_inc(sem, 16)
        ev = (sem, val, "dma")
        self._commit(ev, (q, i % self.KD), reads, writes)
        return ins

    def finish(self):
        self._wait_all_dma("sp")
        self.es.close()


WEIGHT_SPECS = {
    "ada_w": [4, 1024, 6144], "ada_b": [4, 6144], "norm_mix": [4, 1024], "norm_ffn": [4, 1024],
    "w_in": [2, 1024, 2048], "mu_x": [2, 3, 1024], "mu_p": [2, 3, 512],
    "decay_w0": [2, 2, 512], "decay_w1": [2, 2, 1024, 64], "decay_w2": [2, 2, 64, 512],
    "lr_a0": [2, 2, 512], "lr_a1": [2, 2, 1024, 64], "lr_a2": [2, 2, 64, 512],
    "gate_g1": [2, 1024, 128], "gate_g2": [2, 128, 512],
    "k_k": [2, 512], "k_a": [2, 512], "r_k": [2, 8, 64], "gn_w": [2, 512], "gn_b": [2, 512],
    "pool_w": [2, 4, 128, 128], "pool_scale": [2, 512], "w_out": [2, 1024, 1024],
    "w_fourier": [2, 1024, 1024],
    "router_c": [4, 1024, 4], "router_c_b": [4, 4], "router_f": [4, 1024, 32], "router_f_b": [4, 32],
    "moe_w1": [4, 32, 1024, 512], "moe_w3": [4, 32, 1024, 512], "moe_w2": [4, 32, 512, 1024],
    "final_norm": [1024],
}


class Prog:
    BLK = 384
    SCAN_DT = BF16

    def __init__(self, debug=None, skip=()):
        self.K = Ctx()
        K = self.K
        nc = K.nc
        self.debug = debug or {}
        self.inp = {}
        self.inp["x"] = nc.dram_tensor("x", [T, D], F32, kind="ExternalInput")
        self.inp["c"] = nc.dram_tensor("c", [1, D], F32, kind="ExternalInput")
        self.inp["ctx"] = nc.dram_tensor("ctx", [CT, D], F32, kind="ExternalInput")
        self.inp["c_ctx"] = nc.dram_tensor("c_ctx", [1, D], F32, kind="ExternalInput")
        for k, shp in WEIGHT_SPECS.items():
            if k in skip:
                continue
            self.inp[k] = nc.dram_tensor(k, shp, F32, kind="ExternalInput")
        self.out = nc.dram_tensor("out", [T, D], F32, kind="ExternalOutput")
        self.dbg = {}
        for k, (shp, dt) in self.debug.items():
            self.dbg[k] = nc.dram_tensor("dbg_" + k, shp, dt, kind="ExternalOutput")
        K.init_psum(8)
        self.lat = Parts(nc.dram_tensor("lat", [NT, D], F32), "lat")
        self.ident = K.sb([128, 128], F32, "ident")
        self.identb = K.sb([128, 128], BF16, "identb")
        self.ones = K.sb([128, 128], F32, "ones")
        self._consts()
        self.Ml = K.sb([128, 6, D], F32, "Ml")
        self.Mc = K.sb([128, 6, D], F32, "Mc")
        self.srep = K.sb([128, 2, 8, 128], F32, "srep")

    def _consts(self):
        K = self.K
        K.op("pool", lambda e: e.memset(self.ones[:], 1.0), writes=[self.ones])
        K.op("pool", lambda e: e.memset(self.ident[:], 0.0), writes=[self.ident])
        K.op("pool", lambda e: e.affine_select(out=self.ident[:], in_=self.ident[:], pattern=[[-1, 128]],
                                               compare_op=ALU.not_equal, fill=1.0, base=0, channel_multiplier=1),
             reads=[self.ident], writes=[self.ident])
        K.op("dve", lambda e: e.tensor_copy(out=self.identb[:], in_=self.ident[:]), reads=[self.ident],
             writes=[self.identb])

    def prep_s(self):
        K = self.K
        craw = K.sb([128, 2, 8], F32, "craw")
        csil = K.sb([128, 2, 8], F32, "csil")
        for w, nm in enumerate(("c", "c_ctx")):
            src = self.inp[nm].ap().rearrange("o (kt k) -> k (o kt)", k=128)
            K.dma("sp", craw[:, w, :], src, writes=[craw], allow_slow_non_contiguous=True)
        K.op("act", lambda e: e.activation(out=csil[:], in_=craw[:], func=AF.Silu), reads=[craw], writes=[csil])
        K.op("dve", lambda e: e.tensor_copy(out=self.srep[:], in_=csil[:].unsqueeze(3).to_broadcast([128, 2, 8, 128])),
             reads=[csil], writes=[self.srep])

    def modvec(self, i, need_ctx=True):
        K = self.K
        K.push_scope()
        mv = dict(
            W=[K.sb([128, 8, 512], F32, f"adaW{j}") for j in range(2)],
            b=[K.sb([1, 512], F32, f"adab{j}") for j in range(2)],
            g=K.sb([128, 2, D], F32, "normg"),
        )
        aw = self.inp["ada_w"]
        ab = self.inp["ada_b"]
        g = mv["g"]
        K.dma("sp", g[:, 0, :], self.inp["norm_mix"][i:i + 1, :].partition_broadcast(128), writes=[g])
        K.dma("sp", g[:, 1, :], self.inp["norm_ffn"][i:i + 1, :].partition_broadcast(128), writes=[g])
        targets = [(0, self.Ml)] + ([(1, self.Mc)] if need_ctx else [])
        for nb in range(12):
            W = mv["W"][nb % 2]
            bb = mv["b"][nb % 2]
            K.dma("sp", W[:], aw[i, :, nb * 512:(nb + 1) * 512].rearrange("(kt k) n -> k kt n", k=128), writes=[W])
            K.dma("sp", bb[:], ab[i:i + 1, nb * 512:(nb + 1) * 512], writes=[bb])
            for w, M in targets:
                ps = K.ps()
                for kt in range(8):
                    K.op("pe", lambda e, kt=kt, w=w, ps=ps, W=W: e.matmul(ps[:, :], lhsT=self.srep[:, w, kt, :],
                                                                         rhs=W[:, kt, :], start=(kt == 0), stop=False),
                         reads=[self.srep, W], writes=[ps])
                K.op("pe", lambda e, ps=ps, bb=bb: e.matmul(ps[:, :], lhsT=self.ones[0:1, :], rhs=bb[0:1, :],
                                                            start=False, stop=True),
                     reads=[self.ones, bb], writes=[ps])
                s, half = nb // 2, nb % 2
                dst = M[:, s, half * 512:(half + 1) * 512]
                if s in (1, 4):
                    gi = 0 if s == 1 else 1
                    K.op("dve", lambda e, dst=dst, ps=ps, gi=gi, half=half: e.scalar_tensor_tensor(
                        out=dst, in0=ps[:, :], scalar=1.0, in1=g[:, gi, half * 512:(half + 1) * 512],
                        op0=ALU.add, op1=ALU.mult), reads=[ps, g], writes=[M])
                else:
                    K.op("act", lambda e, dst=dst, ps=ps: e.copy(out=dst, in_=ps[:, :]), reads=[ps], writes=[M])
        K.pop_scope()

    def norm_tile(self, xt, ht, M, sub, st):
        K = self.K
        sh = 0 if sub == 0 else 3
        ga = 1 if sub == 0 else 4
        K.op("act", lambda e: e.activation(out=ht[:], in_=xt[:], func=AF.Square, accum_out=st[:, 0:1]),
             reads=[xt], writes=[ht, st])
        K.op("dve", lambda e: e.tensor_scalar(out=st[:, 1:2], in0=st[:, 0:1], scalar1=1.0 / D, scalar2=1e-6,
                                              op0=ALU.mult, op1=ALU.add), reads=[st], writes=[st])
        K.op("act", lambda e: e.sqrt(out=st[:, 3:4], in_=st[:, 1:2]), reads=[st], writes=[st])
        K.op("dve", lambda e: e.reciprocal(out=st[:, 2:3], in_=st[:, 3:4]), reads=[st], writes=[st])
        K.op("dve", lambda e: e.scalar_tensor_tensor(out=ht[:], in0=xt[:], scalar=st[:, 2:3], in1=M[:, ga, :],
                                                     op0=ALU.mult, op1=ALU.mult), reads=[xt, st, M], writes=[ht])
        K.op("dve", lambda e: e.tensor_tensor(out=ht[:], in0=ht[:], in1=M[:, sh, :], op=ALU.add),
             reads=[ht, M], writes=[ht])

    def tr(self, e_unused, dst_ps_ap, src_ap, ps, src_buf, n_in=128):
        K = self.K
        K.op("pe", lambda e: e.transpose(out=dst_ps_ap, in_=src_ap, identity=self.ident[0:n_in, 0:n_in]),
             reads=[src_buf, self.ident], writes=[ps])

    def init_lat(self):
        K = self.K
        for r in range(0, T, 2048):
            K.dma("sp", self.lat[r:r + 2048, :], self.inp["x"][r:r + 2048, :],
                  writes=[self.lat.p(t) for t in range(r // 128, r // 128 + 16)])
        K.dma("sp", self.lat[T:NT, :], self.inp["ctx"][:, :], writes=[self.lat.p(64), self.lat.p(65)])

    def moe_dram(self):
        nc = self.K.nc
        BLK = self.BLK
        self.NB = (2 * NT + 32 * BLK) // BLK
        NB = self.NB
        self.mdram = dict(H2=Parts(nc.dram_tensor("H2", [NT, D], F32), "H2"),
                          XS=Buf(nc.dram_tensor("XS", [NB * BLK, D], F32), "XS"),
                          YS=Buf(nc.dram_tensor("YS", [NB * BLK, D], BF16), "YS"))

    def moe_setup(self):
        K = self.K
        nc = K.nc
        if not hasattr(self, "mdram"):
            self.moe_dram()
        K.push_scope()
        m = dict(self.mdram)
        NTL = 66
        NB = self.NB
        m["OHA"] = K.sb([128, NTL, 2, 32], BF16, "OHA")
        m["RK"] = K.sb([128, NTL, 2], F32, "RK")
        m["cs"] = K.sb([128, 8, 32], F32, "moecs")
        m["be"] = K.sb([128, 3, NB], F32, "moebe")
        m["idf"] = K.sb([128, NB, 8], F32, "moeidf")
        m["GATE"] = K.sb([128, NTL, 2], F32, "GATE")
        m["DEST"] = K.sb([128, NTL, 2], I32, "DEST")
        m["carry"] = K.sb([128, 32], F32, "carry")
        m["Wr"] = K.sb([128, 8, 36], F32, "Wr")
        m["br"] = K.sb([1, 36], F32, "br")
        m["UT"] = K.sb([128, 128], F32, "UT")
        m["PIDX"] = K.sb([128, 8], F32, "PIDX")
        m["JV"] = K.sb([128, NB], F32, "JV")
        m["IDX1"] = K.sb([128, NB, 8], I32, "IDX1")
        m["IDX2"] = K.sb([128, NB, 4], I32, "IDX2")
        big = [K.sb([128, D], F32, f"big{j}") for j in range(8)]
        m["xt"] = big[0:2]
        m["ht"] = big[2:4]
        m["yb"] = [K.sb([128, D], BF16, f"myb{j}") for j in range(2)]
        m["y0"] = [K.sb([128, D], BF16, f"my0{j}") for j in range(2)]
        m["y1"] = [K.sb([128, D], BF16, f"my1{j}") for j in range(2)]
        m["ya"] = big[4:6]
        m["st"] = [K.sb([128, 4], F32, f"mst{j}") for j in range(2)]
        m["hT"] = [K.sb([128, 8, 128], F32, f"mhT{j}") for j in range(2)]
        m["xTb"] = [K.sb([128, 8, 128], BF16, f"mxTb{j}") for j in range(2)]
        m["W1"] = [K.sb([128, 8, 512], BF16, f"mW1{j}") for j in range(2)]
        m["W3"] = [K.sb([128, 8, 512], BF16, f"mW3{j}") for j in range(2)]
        m["W2"] = [K.sb([128, 4, 1024], BF16, f"mW2{j}") for j in range(2)]
        m["hTb"] = [K.sb([128, 4, 128], BF16, f"mhTb{j}") for j in range(2)]
        m["sil"] = [K.sb([128, 512], F32, f"msil{j}") for j in range(2)]
        m["sm"] = [K.sb([128, 128], F32, f"msm{j}") for j in range(2)]
        UT = m["UT"]
        K.op("pool", lambda e: e.memset(UT[:], 1.0), writes=[UT])
        K.op("pool", lambda e: e.affine_select(out=UT[:], in_=UT[:], pattern=[[1, 128]], compare_op=ALU.is_gt,
                                               fill=0.0, base=0, channel_multiplier=-1), reads=[UT], writes=[UT])
        pi = K.sb([128, 8], I32, "pidx_i")
        K.op("pool", lambda e: e.iota(pi[:], pattern=[[128, 8]], base=0, channel_multiplier=1), writes=[pi])
        K.op("dve", lambda e: e.tensor_copy(out=m["PIDX"][:], in_=pi[:]), reads=[pi], writes=[m["PIDX"]])
        ji = K.sb([128, NB], I32, "jv_i")
        K.op("pool", lambda e: e.iota(ji[:], pattern=[[self.BLK, NB]], base=0, channel_multiplier=0), writes=[ji])
        K.op("dve", lambda e: e.tensor_copy(out=m["JV"][:], in_=ji[:]), reads=[ji], writes=[m["JV"]])
        self.m = m

    def moe(self, i, with_ctx, final=False):
        K = self.K
        m = self.m
        NB = self.NB
        ntile = 66 if with_ctx else 64
        Wr, br = m["Wr"], m["br"]
        K.dma("sp", Wr[:, :, 0:4], self.inp["router_c"][i].rearrange("(kt k) n -> k kt n", k=128), writes=[Wr])
        K.dma("sp", Wr[:, :, 4:36], self.inp["router_f"][i].rearrange("(kt k) n -> k kt n", k=128), writes=[Wr])
        K.dma("sp", br[:, 0:4], self.inp["router_c_b"][i:i + 1, :], writes=[br])
        K.dma("sp", br[:, 4:36], self.inp["router_f_b"][i:i + 1, :], writes=[br])
        carry = m["carry"]
        K.op("dve", lambda e: e.memset(carry[:], 0.0), writes=[carry])
        OHA, RK, GATE, DEST = m["OHA"], m["RK"], m["GATE"], m["DEST"]
        for tt in range(ntile):
            xt, ht, st, hT, sm = (m[k][tt % 2] for k in ("xt", "ht", "st", "hT", "sm"))
            M = self.Ml if tt < 64 else self.Mc
            K.dma("sp", xt[:], self.lat[tt * 128:(tt + 1) * 128, :], reads=[self.lat.p(tt)], writes=[xt])
            self.norm_tile(xt, ht, M, 1, st)
            K.dma("act", m["H2"][tt * 128:(tt + 1) * 128, :], ht[:], reads=[ht], writes=[m["H2"].p(tt)])
            for half in range(2):
                ps = K.ps()
                for q in range(4):
                    kt = half * 4 + q
                    self.tr(None, ps[:, q * 128:(q + 1) * 128], ht[:, kt * 128:(kt + 1) * 128], ps, ht)
                K.op("act" if half else "dve",
                     lambda e, ps=ps, half=half: (e.copy if half else e.tensor_copy)(
                         out=hT[:, half * 4:(half + 1) * 4, :].rearrange("p a b -> p (a b)"), in_=ps[:, :]),
                     reads=[ps], writes=[hT])
            ps = K.ps()
            for kt in range(8):
                K.op("pe", lambda e, kt=kt, ps=ps: e.matmul(ps[:, 0:36], lhsT=hT[:, kt, :], rhs=Wr[:, kt, :],
                                                           start=(kt == 0), stop=False),
                     reads=[hT, Wr], writes=[ps])
            K.op("pe", lambda e, ps=ps: e.matmul(ps[:, 0:36], lhsT=self.ones[0:1, :], rhs=br[0:1, :],
                                                 start=False, stop=True), reads=[self.ones, br], writes=[ps])
            def dv(fn, rd=(), wr=()):
                K.op("dve", fn, reads=[sm] + list(rd), writes=[sm] + list(wr))
            K.op("dve", lambda e, ps=ps: e.tensor_copy(out=sm[:, 0:36], in_=ps[:, 0:36]), reads=[ps], writes=[sm])
            dv(lambda e: e.reduce_max(out=sm[:, 36:37], in_=sm[:, 0:4], axis=AX.X))
            dv(lambda e: e.tensor_scalar(out=sm[:, 37:38], in0=sm[:, 36:37], scalar1=-1.0, scalar2=None, op0=ALU.mult))
            dv(lambda e: e.tensor_scalar(out=sm[:, 40:44], in0=sm[:, 0:4], scalar1=sm[:, 36:37], scalar2=None,
                                         op0=ALU.is_equal))
            K.op("act", lambda e: e.activation(out=sm[:, 44:48], in_=sm[:, 0:4], func=AF.Exp, bias=sm[:, 37:38],
                                               scale=1.0, accum_out=sm[:, 38:39]), reads=[sm], writes=[sm])
            dv(lambda e: e.reciprocal(out=sm[:, 39:40], in_=sm[:, 38:39]))
            dv(lambda e: e.tensor_scalar(out=sm[:, 48:56], in0=sm[:, 4:12], scalar1=sm[:, 40:41], scalar2=None,
                                         op0=ALU.mult))
            for g in range(1, 4):
                dv(lambda e, g=g: e.scalar_tensor_tensor(out=sm[:, 48:56], in0=sm[:, 4 + 8 * g:12 + 8 * g],
                                                         scalar=sm[:, 40 + g:41 + g], in1=sm[:, 48:56],
                                                         op0=ALU.mult, op1=ALU.add))
            dv(lambda e: e.reduce_max(out=sm[:, 56:57], in_=sm[:, 48:56], axis=AX.X))
            dv(lambda e: e.tensor_scalar(out=sm[:, 60:68], in0=sm[:, 48:56], scalar1=sm[:, 56:57], scalar2=None,
                                         op0=ALU.is_equal))
            dv(lambda e: e.scalar_tensor_tensor(out=sm[:, 68:76], in0=sm[:, 60:68], scalar=-1e30, in1=sm[:, 48:56],
                                                op0=ALU.mult, op1=ALU.add))
            dv(lambda e: e.reduce_max(out=sm[:, 57:58], in_=sm[:, 68:76], axis=AX.X))
            dv(lambda e: e.tensor_scalar(out=sm[:, 76:84], in0=sm[:, 68:76], scalar1=sm[:, 57:58], scalar2=None,
                                         op0=ALU.is_equal))
            dv(lambda e: e.tensor_tensor(out=sm[:, 58:59], in0=sm[:, 56:57], in1=sm[:, 57:58], op=ALU.subtract))
            K.op("act", lambda e: e.activation(out=sm[:, 59:60], in_=sm[:, 58:59], func=AF.Sigmoid),
                 reads=[sm], writes=[sm])
            dv(lambda e, tt=tt: e.tensor_tensor(out=GATE[:, tt, 0:1], in0=sm[:, 59:60], in1=sm[:, 39:40], op=ALU.mult),
               wr=[GATE])
            dv(lambda e, tt=tt: e.tensor_tensor(out=GATE[:, tt, 1:2], in0=sm[:, 39:40], in1=GATE[:, tt, 0:1],
                                                op=ALU.subtract), rd=[GATE], wr=[GATE])
            for k, c0 in ((0, 60), (1, 76)):
                dv(lambda e, tt=tt, k=k, c0=c0: e.tensor_tensor(
                    out=OHA[:, tt, k, :].rearrange("p (g l) -> p g l", g=4),
                    in0=sm[:, 40:44].unsqueeze(2).to_broadcast([128, 4, 8]),
                    in1=sm[:, c0:c0 + 8].unsqueeze(1).to_broadcast([128, 4, 8]), op=ALU.mult), wr=[OHA])
            dv(lambda e, tt=tt: e.tensor_tensor(out=sm[:, 84:116], in0=OHA[:, tt, 0, :], in1=OHA[:, tt, 1, :],
                                                op=ALU.add), rd=[OHA])
            psr = K.ps()
            K.op("pe", lambda e, psr=psr: e.matmul(psr[:, 0:32], lhsT=m["UT"][:], rhs=sm[:, 84:116], start=True,
                                                   stop=True), reads=[m["UT"], sm], writes=[psr])
            K.op("pe", lambda e, psr=psr: e.matmul(psr[:, 32:64], lhsT=self.ones[:], rhs=sm[:, 84:116], start=True,
                                                   stop=True), reads=[self.ones, sm], writes=[psr])
            K.op("dve", lambda e, psr=psr: e.tensor_tensor(out=sm[:, 84:116], in0=psr[:, 0:32], in1=carry[:],
                                                           op=ALU.add), reads=[psr, carry, sm], writes=[sm])
            for k in range(2):
                dv(lambda e, tt=tt, k=k: e.tensor_tensor(out=sm[:, 0:32], in0=sm[:, 84:116], in1=OHA[:, tt, k, :],
                                                         op=ALU.mult), rd=[OHA])
                dv(lambda e, tt=tt, k=k: e.reduce_sum(out=RK[:, tt, k:k + 1], in_=sm[:, 0:32], axis=AX.X), wr=[RK])
            K.op("dve", lambda e, psr=psr: e.tensor_tensor(out=carry[:], in0=psr[:, 32:64], in1=carry[:], op=ALU.add),
                 reads=[psr, carry], writes=[carry])
        cs = m["cs"]

        def cv(fn):
            K.op("dve", fn, reads=[cs, carry], writes=[cs])
        BLK = self.BLK
        cv(lambda e: e.tensor_scalar(out=cs[:, 0, :], in0=carry[:], scalar1=1.0 / BLK, scalar2=(BLK - 1.0) / (2 * BLK),
                                     op0=ALU.mult, op1=ALU.add))
        cv(lambda e: e.tensor_scalar(out=cs[:, 1, :], in0=cs[:, 0, :], scalar1=8388608.0, scalar2=None, op0=ALU.add))
        cv(lambda e: e.tensor_scalar(out=cs[:, 2, :], in0=cs[:, 1, :], scalar1=-8388608.0, scalar2=float(BLK),
                                     op0=ALU.add, op1=ALU.mult))
        cv(lambda e: e.tensor_copy(out=cs[:, 3, :], in_=cs[:, 2, :]))
        a, b = 3, 4
        for s in (1, 2, 4, 8, 16):
            cv(lambda e, a=a, b=b, s=s: e.tensor_copy(out=cs[:, b, 0:s], in_=cs[:, a, 0:s]))
            cv(lambda e, a=a, b=b, s=s: e.tensor_tensor(out=cs[:, b, s:32], in0=cs[:, a, s:32], in1=cs[:, a, 0:32 - s],
                                                        op=ALU.add))
            a, b = b, a
        pend_i = a
        cv(lambda e: e.tensor_tensor(out=cs[:, 5, :], in0=cs[:, pend_i, :], in1=cs[:, 2, :], op=ALU.subtract))
        be = m["be"]
        K.op("dve", lambda e: e.memset(be[:, 0, :], 0.0), writes=[be])
        for ex in range(32):
            K.op("dve", lambda e, ex=ex: e.scalar_tensor_tensor(out=be[:, 0, :], in0=m["JV"][:],
                                                                scalar=cs[:, pend_i, ex:ex + 1], in1=be[:, 0, :],
                                                                op0=ALU.is_ge, op1=ALU.add),
                 reads=[m["JV"], cs, be], writes=[be])
        K.op("dve", lambda e: e.tensor_scalar(out=be[:, 0, :], in0=be[:, 0, :], scalar1=31.0, scalar2=None, op0=ALU.min),
             reads=[be], writes=[be])
        K.op("dve", lambda e: e.tensor_scalar(out=be[:, 1, :], in0=be[:, 0, :], scalar1=1024.0,
                                              scalar2=float(i * 32 * 1024), op0=ALU.mult, op1=ALU.add),
             reads=[be], writes=[be])
        K.op("dve", lambda e: e.tensor_scalar(out=be[:, 2, :], in0=be[:, 0, :], scalar1=512.0,
                                              scalar2=float(i * 32 * 512), op0=ALU.mult, op1=ALU.add),
             reads=[be], writes=[be])
        idf = m["idf"]
        K.op("dve", lambda e: e.tensor_tensor(out=idf[:], in0=be[:, 1, :].unsqueeze(2).to_broadcast([128, NB, 8]),
                                              in1=m["PIDX"][:].unsqueeze(1).to_broadcast([128, NB, 8]), op=ALU.add),
             reads=[be, m["PIDX"]], writes=[idf])
        K.op("dve", lambda e: e.tensor_copy(out=m["IDX1"][:], in_=idf[:]), reads=[idf], writes=[m["IDX1"]])
        K.op("dve", lambda e: e.tensor_tensor(out=idf[:, :, 0:4], in0=be[:, 2, :].unsqueeze(2).to_broadcast([128, NB, 4]),
                                              in1=m["PIDX"][:, 0:4].unsqueeze(1).to_broadcast([128, NB, 4]),
                                              op=ALU.add), reads=[be, m["PIDX"]], writes=[idf])
        K.op("dve", lambda e: e.tensor_copy(out=m["IDX2"][:], in_=idf[:, :, 0:4]), reads=[idf], writes=[m["IDX2"]])
        for tt in range(ntile):
            ht, sm = m["ht"][tt % 2], m["sm"][tt % 2]
            K.dma("sp", ht[:], m["H2"][tt * 128:(tt + 1) * 128, :], reads=[m["H2"].p(tt)], writes=[ht])
            for k in range(2):
                K.op("dve", lambda e, tt=tt, k=k: e.tensor_tensor(out=sm[:, 32:64], in0=cs[:, 5, :],
                                                                  in1=OHA[:, tt, k, :], op=ALU.mult),
                     reads=[sm, OHA, cs], writes=[sm])
                K.op("dve", lambda e, k=k: e.reduce_sum(out=sm[:, 66 + k:67 + k], in_=sm[:, 32:64], axis=AX.X),
                     reads=[sm], writes=[sm])
            K.op("dve", lambda e, tt=tt: e.tensor_tensor(out=sm[:, 64:66], in0=sm[:, 66:68], in1=RK[:, tt, :],
                                                         op=ALU.add), reads=[sm, RK], writes=[sm])
            K.op("dve", lambda e, tt=tt: e.tensor_copy(out=DEST[:, tt, :], in_=sm[:, 64:66]), reads=[sm], writes=[DEST])
            for k in range(2):
                K.dma("pool", m["XS"][:, :], ht[:], reads=[ht, DEST], writes=[m["XS"]],
                      indirect=dict(out_offset=bass.IndirectOffsetOnAxis(ap=DEST[:, tt, k:k + 1], axis=0),
                                    in_offset=None))
        w1t = self.inp["moe_w1"].ap().rearrange("l e k n -> (l e k) n")
        w3t = self.inp["moe_w3"].ap().rearrange("l e k n -> (l e k) n")
        w2t = self.inp["moe_w2"].ap().rearrange("l e k n -> (l e k) n")
        for j in range(NB):
            W1, W3, W2, xt, xTb, hTb, sil, yb = (m[k][j % 2] for k in ("W1", "W3", "W2", "xt", "xTb", "hTb", "sil", "yb"))
            for kt in range(8):
                for W, tab in ((W1, w1t), (W3, w3t)):
                    K.dma("pool", W[:, kt, :], tab, reads=[m["IDX1"]], writes=[W],
                          indirect=dict(out_offset=None,
                                        in_offset=bass.IndirectOffsetOnAxis(ap=m["IDX1"][:, j, kt:kt + 1], axis=0)))
            for fc in range(4):
                K.dma("pool", W2[:, fc, :], w2t, reads=[m["IDX2"]], writes=[W2],
                      indirect=dict(out_offset=None,
                                    in_offset=bass.IndirectOffsetOnAxis(ap=m["IDX2"][:, j, fc:fc + 1], axis=0)))
            for sub in range(self.BLK // 128):
                r0 = j * self.BLK + sub * 128
                xt, xTb, hTb, sil, yb = (m[k][(j * 4 + sub) % 2] for k in ("xt", "xTb", "hTb", "sil", "yb"))
                K.dma("sp", xt[:], m["XS"][r0:r0 + 128, :], reads=[m["XS"]], writes=[xt])
                for half in range(2):
                    ps = K.ps()
                    for q in range(4):
                        kt = half * 4 + q
                        self.tr(None, ps[:, q * 128:(q + 1) * 128], xt[:, kt * 128:(kt + 1) * 128], ps, xt)
                    K.op("act" if half else "dve",
                         lambda e, ps=ps, half=half, xTb=xTb: (e.copy if half else e.tensor_copy)(
                             out=xTb[:, half * 4:(half + 1) * 4, :].rearrange("p a b -> p (a b)"), in_=ps[:, :]),
                         reads=[ps], writes=[xTb])
                pa, pb = K.ps(), K.ps()
                for W, pp in ((W1, pa), (W3, pb)):
                    for fc in range(4):
                        for kt in range(8):
                            K.op("pe", lambda e, W=W, pp=pp, fc=fc, kt=kt, xTb=xTb: e.matmul(
                                pp[:, fc * 128:(fc + 1) * 128], lhsT=W[:, kt, fc * 128:(fc + 1) * 128], rhs=xTb[:, kt, :],
                                start=(kt == 0), stop=(kt == 7)), reads=[W, xTb], writes=[pp])
                K.op("act", lambda e, pa=pa, sil=sil: e.activation(out=sil[:], in_=pa[:, :], func=AF.Silu),
                     reads=[pa], writes=[sil])
                K.op("dve", lambda e, pb=pb, sil=sil, hTb=hTb: e.tensor_tensor(
                    out=hTb[:].rearrange("p a b -> p (a b)"), in0=sil[:], in1=pb[:, :], op=ALU.mult),
                    reads=[pb, sil], writes=[hTb])
                for half in range(2):
                    py = K.ps()
                    for fc in range(4):
                        K.op("pe", lambda e, py=py, fc=fc, half=half, hTb=hTb, W2=W2: e.matmul(
                            py[:, :], lhsT=hTb[:, fc, :], rhs=W2[:, fc, half * 512:(half + 1) * 512],
                            start=(fc == 0), stop=(fc == 3)), reads=[hTb, W2], writes=[py])
                    K.op("act" if half else "dve",
                         lambda e, py=py, half=half, yb=yb: (e.copy if half else e.tensor_copy)(
                             out=yb[:, half * 512:(half + 1) * 512], in_=py[:, :]), reads=[py], writes=[yb])
                K.dma("sp", m["YS"][r0:r0 + 128, :], yb[:], reads=[yb], writes=[m["YS"]])
        for tt in range(ntile):
            y0, y1, xt = m["y0"][tt % 2], m["y1"][tt % 2], m["xt"][tt % 2]
            M = self.Ml if tt < 64 else self.Mc
            for k, y in ((0, y0), (1, y1)):
                K.dma("pool", y[:], m["YS"][:, :], reads=[m["YS"], DEST], writes=[y],
                      indirect=dict(out_offset=None,
                                    in_offset=bass.IndirectOffsetOnAxis(ap=DEST[:, tt, k:k + 1], axis=0)))
            K.dma("sp", xt[:], self.lat[tt * 128:(tt + 1) * 128, :], reads=[self.lat.p(tt)], writes=[xt])
            ya = m["ya"][tt % 2]
            K.op("dve", lambda e, tt=tt, y0=y0, ya=ya: e.tensor_scalar(out=ya[:], in0=y0[:], scalar1=GATE[:, tt, 0:1],
                                                                       scalar2=None, op0=ALU.mult),
                 reads=[y0, GATE], writes=[ya])
            K.op("dve", lambda e, tt=tt, ya=ya, y1=y1: e.scalar_tensor_tensor(
                out=ya[:], in0=y1[:], scalar=GATE[:, tt, 1:2], in1=ya[:], op0=ALU.mult, op1=ALU.add),
                reads=[ya, y1, GATE], writes=[ya])
            K.op("dve", lambda e, ya=ya, M=M: e.tensor_tensor(out=ya[:], in0=ya[:], in1=M[:, 5, :], op=ALU.mult),
                 reads=[ya, M], writes=[ya])
            K.op("dve", lambda e, ya=ya, xt=xt: e.tensor_tensor(out=xt[:], in0=xt[:], in1=ya[:], op=ALU.add),
                 reads=[ya, xt], writes=[xt])
            K.dma("sp", self.lat[tt * 128:(tt + 1) * 128, :], xt[:], reads=[xt], writes=[self.lat.p(tt)])
        K.pop_scope()

    def final_norm(self):
        K = self.K
        K.push_scope()
        m = dict(xt=[K.sb([128, D], F32, f"fx{j}") for j in range(2)], ht=[K.sb([128, D], F32, f"fh{j}") for j in range(2)],
                 st=[K.sb([128, 4], F32, f"fs{j}") for j in range(2)])
        g = K.sb([128, D], F32, "fng")
        K.dma("sp", g[:], self.inp["final_norm"].ap().rearrange("(o d) -> o d", o=1).partition_broadcast(128), writes=[g])
        for tt in range(64):
            xt, ht, st = m["xt"][tt % 2], m["ht"][tt % 2], m["st"][tt % 2]
            K.dma("sp", xt[:], self.lat[tt * 128:(tt + 1) * 128, :], reads=[self.lat.p(tt)], writes=[xt])
            K.op("act", lambda e, xt=xt, ht=ht, st=st: e.activation(out=ht[:], in_=xt[:], func=AF.Square,
                                                                    accum_out=st[:, 0:1]), reads=[xt], writes=[ht, st])
            K.op("dve", lambda e, st=st: e.tensor_scalar(out=st[:, 1:2], in0=st[:, 0:1], scalar1=1.0 / D, scalar2=1e-6,
                                                         op0=ALU.mult, op1=ALU.add), reads=[st], writes=[st])
            K.op("act", lambda e, st=st: e.sqrt(out=st[:, 3:4], in_=st[:, 1:2]), reads=[st], writes=[st])
            K.op("dve", lambda e, st=st: e.reciprocal(out=st[:, 2:3], in_=st[:, 3:4]), reads=[st], writes=[st])
            K.op("dve", lambda e, xt=xt, ht=ht, st=st: e.scalar_tensor_tensor(
                out=ht[:], in0=xt[:], scalar=st[:, 2:3], in1=g[:], op0=ALU.mult, op1=ALU.mult),
                reads=[xt, st, g], writes=[ht])
            K.dma("sp", self.out[tt * 128:(tt + 1) * 128, :], ht[:], reads=[ht])
        K.pop_scope()


def fourier_consts():
    c = {}
    n = np.arange(256)
    ang = 2 * np.pi * np.outer(n, n) / 256
    c["f_cc"] = (np.cos(ang) / 16).astype(np.float32)
    c["f_sc"] = (-np.sin(ang) / 16).astype(np.float32)
    a = np.arange(128)
    ang = 2 * np.pi * np.outer(a, a) / 128
    s = 1.0 / np.sqrt(8192.0)
    c["f_c128"] = (np.cos(ang) * s).astype(np.float32)
    c["f_s128"] = (np.sin(ang) * s).astype(np.float32)
    f1 = np.arange(128)[:, None]
    b = np.arange(64)[None, :]
    th = 2 * np.pi * f1 * b / 8192
    c["f_tw"] = np.stack([np.cos(th), -np.sin(th)], axis=2).astype(np.float32)
    bb = np.arange(64)
    ang = 2 * np.pi * np.outer(bb, bb) / 64
    c["f_cs64"] = np.concatenate([np.cos(ang), np.sin(ang)], axis=0).astype(np.float32)
    t = np.arange(256)
    ang = 2 * np.pi * np.outer(t, t) / 256
    c["f_c256"] = (np.cos(ang) / 16).astype(np.float32)
    c["f_s256"] = (np.sin(ang) / 16).astype(np.float32)
    return c


FOURIER_SPECS = {"f_cc": [256, 256], "f_sc": [256, 256], "f_c128": [128, 128], "f_s128": [128, 128],
                 "f_tw": [128, 64, 2], "f_cs64": [128, 64], "f_c256": [256, 256], "f_s256": [256, 256]}


def _fourier_declare(self):
    nc = self.K.nc
    for k, shp in FOURIER_SPECS.items():
        self.inp[k] = nc.dram_tensor(k, shp, F32, kind="ExternalInput")
    self.GD = Buf(nc.dram_tensor("GD", [128, 128, D], F32), "GD")


def _fourier(self, i, with_ctx, nb=64, nf=128):
    K = self.K
    j = i // 2
    K.push_scope()
    cc = K.sb([128, 2, 2, 256], BF16, "fcc")
    c128 = K.sb([128, 3, 128], BF16, "fc128")
    tw = K.sb([128, 64, 2], F32, "ftw")
    cs64 = K.sb([128, 64], F32, "fcs64")
    wf = K.sb([128, 8, D], BF16, "fwf")
    big = [K.sb([128, D], F32, f"fbig{q}") for q in range(6)]
    xts, hts, gsb = big[0:2], big[2:4], big[4:6]
    sts = [K.sb([128, 4], F32, f"fst{q}") for q in range(2)]
    hTb = [K.sb([128, 8, 128], BF16, f"fhT{q}") for q in range(2)]
    Zb = [K.sb([128, 2, D], BF16, f"fZ{q}") for q in range(2)]
    gi2 = [K.sb([128, D], F32, f"fgi{q}") for q in range(2)]
    tmp = K.sb([128, 256], F32, "ftmp")
    def ld_cast(dst_ap, dst_buf, src_ap, shape, n=[0]):
        stg = big[4 + n[0] % 2]
        n[0] += 1
        rows, cols = shape
        view = stg[0:rows, 0:cols]
        K.dma("sp", view, src_ap, writes=[stg])
        K.op("dve", lambda e: e.tensor_copy(out=dst_ap, in_=view), reads=[stg], writes=[dst_buf])
    for kt in range(2):
        ld_cast(cc[:, kt, 0, :], cc, self.inp["f_cc"][kt * 128:(kt + 1) * 128, :], (128, 256))
        ld_cast(cc[:, kt, 1, :], cc, self.inp["f_sc"][kt * 128:(kt + 1) * 128, :], (128, 256))
    ld_cast(c128[:, 0, :], c128, self.inp["f_c128"][:, :], (128, 128))
    ld_cast(c128[:, 1, :], c128, self.inp["f_s128"][:, :], (128, 128))
    K.op("dve", lambda e: e.tensor_scalar(out=c128[:, 2, :], in0=c128[:, 1, :], scalar1=-1.0, scalar2=None,
                                          op0=ALU.mult), reads=[c128], writes=[c128])
    for kt in range(8):
        ld_cast(wf[:, kt, :], wf, self.inp["w_fourier"][j, kt * 128:(kt + 1) * 128, :], (128, 1024))
    self._ld_cast = ld_cast
    K.dma("sp", tw[:], self.inp["f_tw"][:, :, :], writes=[tw])
    K.dma("sp", cs64[:], self.inp["f_cs64"][:, :], writes=[cs64])
    ntw = K.sb([128, 64], F32, "fntw")
    K.op("dve", lambda e: e.tensor_scalar(out=ntw[:], in0=tw[:, :, 1], scalar1=-1.0, scalar2=None, op0=ALU.mult),
         reads=[tw], writes=[ntw])
    latv = self.lat[0:T, :].rearrange("(a b) d -> b a d", b=64)
    lat_all = [self.lat.p(t) for t in range(64)]

    def chan_dft(xt, ht, st, hT, Z, M):
        self.norm_tile(xt, ht, M, 0, st)
        for half in range(2):
            ps = K.ps()
            for q in range(4):
                kt = half * 4 + q
                self.tr(None, ps[:, q * 128:(q + 1) * 128], ht[:, kt * 128:(kt + 1) * 128], ps, ht)
            K.op("act" if half else "dve",
                 lambda e, ps=ps, half=half: (e.copy if half else e.tensor_copy)(
                     out=hT[:, half * 4:(half + 1) * 4, :].rearrange("p a b -> p (a b)"), in_=ps[:, :]),
                 reads=[ps], writes=[hT])
        for ri in range(2):
            for hh in range(2):
                ps = K.ps()
                for gg in range(2):
                    g = hh * 2 + gg
                    for kt in range(2):
                        K.op("pe", lambda e, ps=ps, gg=gg, g=g, kt=kt, ri=ri: e.matmul(
                            ps[:, gg * 256:(gg + 1) * 256], lhsT=hT[:, g * 2 + kt, :], rhs=cc[:, kt, ri, :],
                            start=(kt == 0), stop=(kt == 1)), reads=[hT, cc], writes=[ps])
                K.op("act" if hh else "dve",
                     lambda e, ps=ps, hh=hh, ri=ri: (e.copy if hh else e.tensor_copy)(
                         out=Z[:, ri, hh * 512:(hh + 1) * 512], in_=ps[:, :]), reads=[ps], writes=[Z])

    for b in range(nb):
        xt, ht, st, hT, Z, gr, gi = (l[b % 2] for l in (xts, hts, sts, hTb, Zb, gsb, gi2))
        K.dma("sp", xt[:], latv[b], reads=lat_all, writes=[xt])
        chan_dft(xt, ht, st, hT, Z, self.Ml)
        for hh in range(2):
            cs_ = slice(hh * 512, (hh + 1) * 512)
            pr, pi_ = K.ps(), K.ps()
            K.op("pe", lambda e: e.matmul(pr[:, :], lhsT=c128[:, 0, :], rhs=Z[:, 0, cs_], start=True, stop=False),
                 reads=[c128, Z], writes=[pr])
            K.op("pe", lambda e: e.matmul(pr[:, :], lhsT=c128[:, 1, :], rhs=Z[:, 1, cs_], start=False, stop=True),
                 reads=[c128, Z], writes=[pr])
            K.op("pe", lambda e: e.matmul(pi_[:, :], lhsT=c128[:, 0, :], rhs=Z[:, 1, cs_], start=True, stop=False),
                 reads=[c128, Z], writes=[pi_])
            K.op("pe", lambda e: e.matmul(pi_[:, :], lhsT=c128[:, 2, :], rhs=Z[:, 0, cs_], start=False, stop=True),
                 reads=[c128, Z], writes=[pi_])
            K.op("dve", lambda e: e.tensor_scalar(out=gr[:, cs_], in0=pr[:, :], scalar1=tw[:, b, 0:1], scalar2=None,
                                                  op0=ALU.mult), reads=[pr, tw], writes=[gr])
            K.op("dve", lambda e: e.scalar_tensor_tensor(out=gr[:, cs_], in0=pi_[:, :], scalar=ntw[:, b:b + 1],
                                                         in1=gr[:, cs_], op0=ALU.mult, op1=ALU.add),
                 reads=[pi_, ntw, gr], writes=[gr])
            K.op("dve", lambda e: e.tensor_scalar(out=gi[:, cs_], in0=pi_[:, :], scalar1=tw[:, b, 0:1], scalar2=None,
                                                  op0=ALU.mult), reads=[pi_, tw], writes=[gi])
            K.op("dve", lambda e: e.scalar_tensor_tensor(out=gi[:, cs_], in0=pr[:, :], scalar=tw[:, b, 1:2],
                                                         in1=gi[:, cs_], op0=ALU.mult, op1=ALU.add),
                 reads=[pr, tw, gi], writes=[gi])
        K.dma("act", self.GD[b, :, :], gr[:], reads=[gr], writes=[self.GD])
        K.dma("act", self.GD[64 + b, :, :], gi[:], reads=[gi], writes=[self.GD])
    latf = self.lat[0:T, :].rearrange("(f2 f1) d -> f1 f2 d", f1=128)
    YT = [K.sb([128, 8, 64], BF16, f"fYT{q}") for q in range(2)]
    for f1 in range(nf):
        gd, yt, xt, ht = gsb[f1 % 2], YT[f1 % 2], xts[f1 % 2], hts[f1 % 2]
        K.dma("sp", gd[:], self.GD[:, f1, :], reads=[self.GD], writes=[gd])
        K.dma("sp", xt[0:64, :], latf[f1], reads=lat_all, writes=[xt])
        ps = K.ps()
        for kt in range(8):
            K.op("pe", lambda e, kt=kt: e.matmul(ps[:, kt * 64:(kt + 1) * 64], lhsT=gd[:, kt * 128:(kt + 1) * 128],
                                                 rhs=cs64[:, :], start=True, stop=True), reads=[gd, cs64], writes=[ps])
        K.op("act", lambda e: e.copy(out=yt[:].rearrange("p a b -> p (a b)"), in_=ps[:, :]), reads=[ps], writes=[yt])
        for hh in range(2):
            py = K.ps()
            for kt in range(8):
                K.op("pe", lambda e, kt=kt: e.matmul(py[0:64, :], lhsT=yt[:, kt, :],
                                                     rhs=wf[:, kt, hh * 512:(hh + 1) * 512], start=(kt == 0),
                                                     stop=(kt == 7)), reads=[yt, wf], writes=[py])
            K.op("dve", lambda e: e.tensor_tensor(out=ht[0:64, hh * 512:(hh + 1) * 512], in0=py[0:64, :],
                                                  in1=self.Ml[0:64, 2, hh * 512:(hh + 1) * 512], op=ALU.mult),
                 reads=[py, self.Ml], writes=[ht])
        K.op("dve", lambda e: e.tensor_tensor(out=xt[0:64, :], in0=xt[0:64, :], in1=ht[0:64, :], op=ALU.add),
             reads=[xt, ht], writes=[xt])
        K.dma("act", latf[f1], xt[0:64, :], reads=[xt], writes=lat_all)
    if with_ctx:
        c256 = K.sb([128, 2, 2, 256], BF16, "fc256")
        for tt in range(2):
            ld_cast(c256[:, 0, tt, :], c256, self.inp["f_c256"][tt * 128:(tt + 1) * 128, :], (128, 256))
            ld_cast(c256[:, 1, tt, :], c256, self.inp["f_s256"][tt * 128:(tt + 1) * 128, :], (128, 256))
        ctxp = [self.lat.p(64), self.lat.p(65)]
        for tt in range(2):
            K.dma("sp", xts[tt][:], self.lat[T + tt * 128:T + (tt + 1) * 128, :], reads=ctxp, writes=[xts[tt]])
            chan_dft(xts[tt], hts[tt], sts[tt], hTb[tt], Zb[tt], self.Mc)
        for ft in range(2):
            yt = K.sb([128, 8, 128], BF16, f"fcy{ft}")
            for half in range(2):
                ps = K.ps()
                for q in range(4):
                    kt = half * 4 + q
                    n = 0
                    for tt in range(2):
                        for cs_i in range(2):
                            K.op("pe", lambda e, kt=kt, q=q, tt=tt, cs_i=cs_i, n=n: e.matmul(
                                ps[:, q * 128:(q + 1) * 128], lhsT=Zb[tt][:, cs_i, kt * 128:(kt + 1) * 128],
                                rhs=c256[:, cs_i, tt, ft * 128:(ft + 1) * 128], start=(n == 0), stop=(n == 3)),
                                reads=[Zb[tt], c256], writes=[ps])
                            n += 1
                K.op("act", lambda e, half=half: e.copy(out=yt[:, half * 4:(half + 1) * 4, :].rearrange("p a b -> p (a b)"),
                                                        in_=ps[:, :]), reads=[ps], writes=[yt])
            xt, ht = xts[ft], hts[ft]
            for hh in range(2):
                py = K.ps()
                for kt in range(8):
                    K.op("pe", lambda e, kt=kt: e.matmul(py[:, :], lhsT=yt[:, kt, :],
                                                         rhs=wf[:, kt, hh * 512:(hh + 1) * 512], start=(kt == 0),
                                                         stop=(kt == 7)), reads=[yt, wf], writes=[py])
                K.op("dve", lambda e: e.tensor_tensor(out=ht[:, hh * 512:(hh + 1) * 512], in0=py[:, :],
                                                      in1=self.Mc[:, 2, hh * 512:(hh + 1) * 512], op=ALU.mult),
                     reads=[py, self.Mc], writes=[ht])
            K.op("dve", lambda e: e.tensor_tensor(out=xt[:], in0=xt[:], in1=ht[:], op=ALU.add),
                 reads=[xt, ht], writes=[xt])
            K.dma("act", self.lat[T + ft * 128:T + (ft + 1) * 128, :], xt[:], reads=[xt], writes=ctxp)
    K.pop_scope()


Prog.fourier_declare = _fourier_declare
Prog.fourier = _fourier


POOL_WINS = (2, 4, 8, 16)


def pool_consts():
    c = {}
    for nm, L in (("f_icnt_lat", T), ("f_icnt_ctx", CT)):
        a = np.zeros((4, L), np.float32)
        pos = np.arange(L)
        for gi, win in enumerate(POOL_WINS):
            half = win // 2
            hi = np.minimum(pos + half, L)
            lo = np.maximum(pos - half, 0)
            a[gi] = 1.0 / (hi - lo)
        c[nm] = a
    return c


def _even_declare(self):
    nc = self.K.nc
    self.inp["f_icnt_lat"] = nc.dram_tensor("f_icnt_lat", [4, T], F32, kind="ExternalInput")
    self.inp["f_icnt_ctx"] = nc.dram_tensor("f_icnt_ctx", [4, CT], F32, kind="ExternalInput")
    e = {}
    e["HT"] = Buf(nc.dram_tensor("HT", [D, NT], F32), "HT")
    e["PT"] = Buf(nc.dram_tensor("PT", [2048, NT], F32), "PT")
    for d in range(2):
        for nm in ("At", "Bt", "Kt", "Rt", "Bh", "Kh"):
            e[f"{nm}{d}"] = Buf(nc.dram_tensor(f"SC_{nm}{d}", [512, NT], F32), f"{nm}{d}")
        e[f"GL{d}"] = Buf(nc.dram_tensor(f"SC_GL{d}", [512, NT // 64], F32), f"GL{d}")
        e[f"YD{d}"] = Buf(nc.dram_tensor(f"SC_YD{d}", [NT, 512], F32), f"YD{d}")
    for nm in ("VV", "GG", "BON", "YB"):
        e[nm] = Buf(nc.dram_tensor(f"SC_{nm}", [512, NT], F32), nm)
    self.ed = e


def _even_e1(self, i, ntile=66):
    K = self.K
    j = i // 2
    e = self.ed
    K.push_scope()
    win = K.sb([128, 8, 2048], BF16, "win")
    stg = [K.sb([128, D], F32, f"e1stg{q}") for q in range(2)]
    n = 0
    for kt in range(8):
        for hh in range(2):
            sg = stg[n % 2]
            n += 1
            K.dma("sp", sg[:], self.inp["w_in"][j, kt * 128:(kt + 1) * 128, hh * 1024:(hh + 1) * 1024], writes=[sg])
            K.op("dve" if n % 2 else "act",
                 lambda en, sg=sg, kt=kt, hh=hh: (en.tensor_copy if n % 2 else en.copy)(
                     out=win[:, kt, hh * 1024:(hh + 1) * 1024], in_=sg[:]), reads=[sg], writes=[win])
    xts = [K.sb([128, D], F32, f"e1x{q}") for q in range(2)]
    hts = [K.sb([128, D], F32, f"e1h{q}") for q in range(2)]
    sts = [K.sb([128, 4], F32, f"e1s{q}") for q in range(2)]
    hTf = [K.sb([128, 8, 128], F32, f"e1hTf{q}") for q in range(2)]
    hTb = [K.sb([128, 8, 128], BF16, f"e1hTb{q}") for q in range(2)]
    pts = [K.sb([128, 16, 128], F32, f"e1pt{q}") for q in range(2)]
    for tt in range(ntile):
        xt, ht, st, hf, hb, pt = (l[tt % 2] for l in (xts, hts, sts, hTf, hTb, pts))
        M = self.Ml if tt < 64 else self.Mc
        K.dma("sp", xt[:], self.lat[tt * 128:(tt + 1) * 128, :], reads=[self.lat.p(tt)], writes=[xt])
        self.norm_tile(xt, ht, M, 0, st)
        for half in range(2):
            ps = K.ps()
            for q in range(4):
                kt = half * 4 + q
                self.tr(None, ps[:, q * 128:(q + 1) * 128], ht[:, kt * 128:(kt + 1) * 128], ps, ht)
            K.op("act", lambda en, ps=ps, half=half: en.copy(
                out=hf[:, half * 4:(half + 1) * 4, :].rearrange("p a b -> p (a b)"), in_=ps[:, :]),
                reads=[ps], writes=[hf])
            K.op("dve", lambda en, half=half: en.tensor_copy(
                out=hb[:, half * 4:(half + 1) * 4, :], in_=hf[:, half * 4:(half + 1) * 4, :]),
                reads=[hf], writes=[hb])
        import os
        if not os.environ.get("SKIPHT"):
            K.dma("act", e["HT"][:, tt * 128:(tt + 1) * 128].rearrange("(kt k) t -> k kt t", k=128), hf[:],
                  reads=[hf], writes=[e["HT"]])
        for ob in range(4):
            ps = K.ps()
            for q in range(4):
                oc = ob * 4 + q
                for kt in range(8):
                    K.op("pe", lambda en, ps=ps, q=q, oc=oc, kt=kt: en.matmul(
                        ps[:, q * 128:(q + 1) * 128], lhsT=win[:, kt, oc * 128:(oc + 1) * 128], rhs=hb[:, kt, :],
                        start=(kt == 0), stop=(kt == 7)), reads=[win, hb], writes=[ps])
            K.op("act" if ob % 2 else "dve", lambda en, ps=ps, ob=ob: (en.copy if ob % 2 else en.tensor_copy)(
                out=pt[:, ob * 4:(ob + 1) * 4, :].rearrange("p a b -> p (a b)"), in_=ps[:, :]),
                reads=[ps], writes=[pt])
        if not os.environ.get("SKIPPT"):
            K.dma("act", e["PT"][:, tt * 128:(tt + 1) * 128].rearrange("(oc k) t -> k oc t", k=128), pt[:],
                  reads=[pt], writes=[e["PT"]])
    K.pop_scope()


def _even_e2(self, i, dbg=None):
    K = self.K
    j = i // 2
    e = self.ed
    K.push_scope()
    WB = 512
    WT = WB + 128
    eng_rr = [0]

    def ve():
        eng_rr[0] += 1
        return "dve"

    stg = K.sb([128, 8, 128], F32, "e2stg")
    W1 = K.sb([128, 3, 8, 128], BF16, "e2W1")
    W2 = K.sb([128, 3, 512], BF16, "e2W2")
    pw = K.sb([128, 4, 128], BF16, "e2pw")
    for v, (nm, nd) in enumerate((("decay_w1", 2), ("lr_a1", 2), ("gate_g1", 1))):
        for d in range(nd):
            src = self.inp[nm][j, d] if nd == 2 else self.inp[nm][j]
            wcol = 64 if nd == 2 else 128
            K.dma("sp", stg[:, :, 0:wcol], src.rearrange("(kt k) r -> k kt r", k=128), writes=[stg])
            K.op("dve", lambda en, v=v, d=d, wcol=wcol: en.tensor_copy(out=W1[:, v, :, d * wcol:(d + 1) * wcol],
                                                                       in_=stg[:, :, 0:wcol]), reads=[stg], writes=[W1])
    stg2 = stg[:].rearrange("p a b -> p (a b)")
    for v, nm in enumerate(("decay_w2", "lr_a2")):
        for d in range(2):
            K.dma("sp", stg2[d * 64:(d + 1) * 64, 0:512], self.inp[nm][j, d], writes=[stg])
        K.op("dve", lambda en, v=v: en.tensor_copy(out=W2[:, v, :], in_=stg2[:, 0:512]), reads=[stg], writes=[W2])
    K.dma("sp", stg2[:, 0:512], self.inp["gate_g2"][j], writes=[stg])
    K.op("dve", lambda en: en.tensor_copy(out=W2[:, 2, :], in_=stg2[:, 0:512]), reads=[stg], writes=[W2])
    for gi in range(4):
        K.dma("sp", stg2[:, gi * 128:(gi + 1) * 128], self.inp["pool_w"][j, gi], writes=[stg])
    K.op("dve", lambda en: en.tensor_copy(out=pw[:].rearrange("p a b -> p (a b)"), in_=stg2[:, 0:512]),
         reads=[stg], writes=[pw])
    MU = K.sb([128, 2, 3, 8], F32, "e2MU")
    MUP = K.sb([128, 2, 3, 4], F32, "e2MUP")
    COL = K.sb([128, 12, 4], F32, "e2COL")
    K.dma("sp", MU[:, 0, :, :], self.inp["mu_x"][j].rearrange("v (kt k) -> k v kt", k=128), writes=[MU],
          allow_slow_non_contiguous=True)
    K.dma("sp", MUP[:, 0, :, :], self.inp["mu_p"][j].rearrange("v (c k) -> k v c", k=128), writes=[MUP],
          allow_slow_non_contiguous=True)
    for d in range(2):
        K.dma("sp", COL[:, d, :], self.inp["decay_w0"][j, d].rearrange("(c k) -> k c", k=128), writes=[COL],
              allow_slow_non_contiguous=True)
        K.dma("sp", COL[:, 2 + d, :], self.inp["lr_a0"][j, d].rearrange("(c k) -> k c", k=128), writes=[COL],
              allow_slow_non_contiguous=True)
    K.dma("sp", COL[:, 4, :], self.inp["k_k"][j].rearrange("(c k) -> k c", k=128), writes=[COL], allow_slow_non_contiguous=True)
    K.dma("sp", COL[:, 5, :], self.inp["k_a"][j].rearrange("(c k) -> k c", k=128), writes=[COL], allow_slow_non_contiguous=True)
    K.dma("sp", COL[:, 6, :], self.inp["r_k"][j].rearrange("(c h2) k -> (h2 k) c", h2=2), writes=[COL],
          allow_slow_non_contiguous=True)
    K.dma("sp", COL[:, 7, :], self.inp["pool_scale"][j].rearrange("(c k) -> k c", k=128), writes=[COL],
          allow_slow_non_contiguous=True)
    for Mx in (MU, MUP):
        K.op("dve", lambda en, Mx=Mx: en.tensor_scalar(out=Mx[:, 1], in0=Mx[:, 0], scalar1=-1.0, scalar2=1.0,
                                                       op0=ALU.mult, op1=ALU.add), reads=[Mx], writes=[Mx])
    bones = K.sb([128, 128], F32, "e2bones")
    K.op("pool", lambda en: en.memset(bones[:], 0.0), writes=[bones])
    K.op("pool", lambda en: en.memset(bones[0:64, 0:64], 1.0), reads=[bones], writes=[bones])
    K.op("pool", lambda en: en.memset(bones[64:128, 64:128], 1.0), reads=[bones], writes=[bones])
    HTt = K.sb([128, 8, WT], F32, "e2HTt")
    xv = K.sb([128, 8, WB], BF16, "e2xv")
    t1 = [K.sb([128, WB], BF16, f"e2t1{v}") for v in range(3)]
    PTt = [K.sb([128, WT], F32, f"e2PTt{n}") for n in range(4)]
    nm_w = ("Rm", "Km", "Vm", "kk", "sq", "rn", "LW0", "LW1", "AD0", "AD1", "Gm", "kd0", "kd1", "b0", "b1", "lw0", "lw1",
            "cum0", "cum1", "E0", "E1", "tmp0", "tmp1", "tmp", "ks", "IC", "o0", "o1", "o2", "o3", "o4", "o5")
    w = {n: K.sb([128, WB], F32, "e2" + n) for n in nm_w}
    sw = [K.sb([128, WT], F32, f"e2s{q}") for q in range(2)]
    dfb = K.sb([128, WB], BF16, "e2dfb")
    gls = [K.sb([128, 8], F32, f"e2gl{d}") for d in range(2)]
    RM = K.sb([128, WB], F32, "e2RM")
    K.op("pool", lambda en: en.memset(RM[:], 1.0), writes=[RM])
    K.op("pool", lambda en: en.memset(RM[:].rearrange("p (r c) -> p r c", c=64)[:, :, 0:1], 0.0), reads=[RM], writes=[RM])
    orr = [0]

    def otile():
        orr[0] += 1
        return w[f"o{orr[0] % 6}"]

    GRID_H = [(-1, True)] * 2 + [(1, True)] * 2 + [(-64, False)] * 2 + [(64, False)] * 2
    GRID_P = [(-1, True), (1, True), (-64, False), (64, False)]
    SEQ_H = [(-1, False)] * 4 + [(1, False)] * 4
    SEQ_P = [(-1, False)] * 2 + [(1, False)] * 2
    blocks = [(T, CT, T, CT, SEQ_H, SEQ_P, "f_icnt_ctx")] + \
             [(t0, WB, 0, T, GRID_H, GRID_P, "f_icnt_lat") for t0 in range(0, T, WB)]

    def load_halo(buf, view, src_rows, t0, Wb, s0, sl):
        lo = max(t0 - 64, s0)
        hi = min(t0 + Wb + 64, s0 + sl)
        if lo > t0 - 64:
            K.op("pool", lambda en: en.memset(view(0, 64), 0.0), writes=[buf])
        if hi < t0 + Wb + 64:
            K.op("pool", lambda en: en.memset(view(64 + Wb, 128 + Wb), 0.0), writes=[buf])
        K.dma("sp", view(lo - (t0 - 64), hi - (t0 - 64)), src_rows(lo, hi), reads=[e["HT"], e["PT"]], writes=[buf])

    def mix(out_ap, out_buf, srcf, src_buf, mu_ap, omu_ap, delta, rowmask, Wb):
        en1 = "dve"
        K.op("act", lambda en: en.mul(out=out_ap, in_=srcf(64, 64 + Wb), mul=omu_ap), reads=[src_buf], writes=[out_buf])
        if not rowmask:
            o, s_ = out_ap, srcf(64 + delta, 64 + delta + Wb)
        else:
            ov = out_ap.rearrange("p (r c) -> p r c", c=64)
            sv = srcf(64 + delta, 64 + delta + Wb).rearrange("p (r c) -> p r c", c=64)
            if delta == -1:
                o, s_ = ov[:, :, 1:64], sv[:, :, 1:64]
            else:
                o, s_ = ov[:, :, 0:63], sv[:, :, 0:63]
        K.op(en1, lambda en: en.scalar_tensor_tensor(out=o, in0=s_, scalar=mu_ap, in1=o, op0=ALU.mult, op1=ALU.add),
             reads=[src_buf, out_buf], writes=[out_buf])

    stq = [0]

    def store(dst, c, t0, Wb, tile):
        stq[0] += 1
        K.dma("act" if stq[0] % 2 else "sp", dst[c * 128:(c + 1) * 128, t0:t0 + Wb], tile[:, 0:Wb], reads=[tile],
              writes=[dst])

    for (t0, Wb, s0, sl, HS, PS, icn) in blocks:
        R = Wb // 64
        load_halo(HTt, lambda a, b: HTt[:, :, a:b],
                  lambda a, b: e["HT"][:, a:b].rearrange("(kt k) t -> k kt t", k=128), t0, Wb, s0, sl)
        for v in range(3):
            for kt in range(8):
                dl, rm = HS[kt]
                mix(xv[:, kt, 0:Wb], xv, lambda a, b, kt=kt: HTt[:, kt, a:b], HTt, MU[:, 0, v, kt:kt + 1],
                    MU[:, 1, v, kt:kt + 1], dl, rm, Wb)
            ps = K.ps()
            for kt in range(8):
                K.op("pe", lambda en, kt=kt, v=v: en.matmul(ps[:, 0:Wb], lhsT=W1[:, v, kt, :], rhs=xv[:, kt, 0:Wb],
                                                            start=(kt == 0), stop=(kt == 7)), reads=[W1, xv], writes=[ps])
            fn = (AF.Tanh, AF.Copy, AF.Sigmoid)[v]
            K.op("act", lambda en, v=v, fn=fn: en.activation(out=t1[v][:, 0:Wb], in_=ps[:, 0:Wb], func=fn),
                 reads=[ps], writes=[t1[v]])
        for c in range(4):
            cs_ = slice(c * 128, (c + 1) * 128)
            for n in range(4):
                load_halo(PTt[n], lambda a, b, n=n: PTt[n][:, a:b],
                          lambda a, b, n=n: e["PT"][n * 512 + c * 128:n * 512 + (c + 1) * 128, a:b], t0, Wb, s0, sl)
            dl, rm = PS[c]
            for n, nm in enumerate(("Rm", "Km", "Vm")):
                mix(w[nm][:, 0:Wb], w[nm], lambda a, b, n=n: PTt[n][:, a:b], PTt[n], MUP[:, 0, n, c:c + 1],
                    MUP[:, 1, n, c:c + 1], dl, rm, Wb)
            store(e["VV"], c, t0, Wb, w["Vm"])
            for d in range(2):
                ps = K.ps()
                K.op("pe", lambda en, d=d: en.matmul(ps[:, 0:Wb], lhsT=W2[d * 64:(d + 1) * 64, 0, cs_],
                                                     rhs=t1[0][d * 64:(d + 1) * 64, 0:Wb], start=True, stop=True),
                     reads=[W2, t1[0]], writes=[ps])
                K.op("act", lambda en, d=d: en.activation(out=w[f"LW{d}"][:, 0:Wb], in_=ps[:, 0:Wb], func=AF.Sigmoid,
                                                          bias=COL[:, d, c:c + 1], scale=1.0),
                     reads=[ps, COL], writes=[w[f"LW{d}"]])
                ps = K.ps()
                K.op("pe", lambda en, d=d: en.matmul(ps[:, 0:Wb], lhsT=W2[d * 64:(d + 1) * 64, 1, cs_],
                                                     rhs=t1[1][d * 64:(d + 1) * 64, 0:Wb], start=True, stop=True),
                     reads=[W2, t1[1]], writes=[ps])
                K.op("act", lambda en, d=d: en.activation(out=w[f"AD{d}"][:, 0:Wb], in_=ps[:, 0:Wb], func=AF.Sigmoid,
                                                          bias=COL[:, 2 + d, c:c + 1], scale=1.0),
                     reads=[ps, COL], writes=[w[f"AD{d}"]])
            ps = K.ps()
            K.op("pe", lambda en: en.matmul(ps[:, 0:Wb], lhsT=W2[:, 2, cs_], rhs=t1[2][:, 0:Wb], start=True, stop=True),
                 reads=[W2, t1[2]], writes=[ps])
            K.op("act", lambda en: en.copy(out=w["Gm"][:, 0:Wb], in_=ps[:, 0:Wb]), reads=[ps], writes=[w["Gm"]])
            store(e["GG"], c, t0, Wb, w["Gm"])
            K.op("dve", lambda en: en.tensor_scalar(out=w["kk"][:, 0:Wb], in0=w["Km"][:, 0:Wb], scalar1=COL[:, 4, c:c + 1],
                                                    scalar2=None, op0=ALU.mult), reads=[w["Km"], COL], writes=[w["kk"]])
            K.op("act", lambda en: en.square(out=w["sq"][:, 0:Wb], in_=w["kk"][:, 0:Wb]), reads=[w["kk"]], writes=[w["sq"]])
            ps = K.ps()
            K.op("pe", lambda en: en.matmul(ps[:, 0:Wb], lhsT=bones[:], rhs=w["sq"][:, 0:Wb], start=True, stop=True),
                 reads=[bones, w["sq"]], writes=[ps])
            K.op("dve", lambda en: en.tensor_scalar(out=w["rn"][:, 0:Wb], in0=ps[:, 0:Wb], scalar1=1e-12, scalar2=None,
                                                    op0=ALU.max), reads=[ps], writes=[w["rn"]])
            K.op("act", lambda en: en.sqrt(out=w["rn"][:, 0:Wb], in_=w["rn"][:, 0:Wb]), reads=[w["rn"]], writes=[w["rn"]])
            K.op("dve", lambda en: en.reciprocal(out=w["rn"][:, 0:Wb], in_=w["rn"][:, 0:Wb]), reads=[w["rn"]],
                 writes=[w["rn"]])
            K.op("dve", lambda en: en.tensor_tensor(out=w["kk"][:, 0:Wb], in0=w["kk"][:, 0:Wb], in1=w["rn"][:, 0:Wb],
                                                    op=ALU.mult), reads=[w["kk"], w["rn"]], writes=[w["kk"]])
            def dchain(d):
                AD, LW = w[f"AD{d}"], w[f"LW{d}"]
                tmp, kd, bb, lw, cum, E = (w[f"{nm_}{d}"] for nm_ in ("tmp", "kd", "b", "lw", "cum", "E"))
                K.op("dve", lambda en: en.tensor_scalar(out=tmp[:, 0:Wb], in0=AD[:, 0:Wb], scalar1=-1.0,
                                                        scalar2=COL[:, 5, c:c + 1], op0=ALU.add, op1=ALU.mult),
                     reads=[AD, COL], writes=[tmp])
                K.op("dve", lambda en: en.tensor_tensor(out=bb[:, 0:Wb], in0=w["kk"][:, 0:Wb], in1=AD[:, 0:Wb],
                                                         op=ALU.mult), reads=[w["kk"], AD], writes=[bb])
                K.op("act", lambda en: en.mul(out=lw[:, 0:Wb], in_=LW[:, 0:Wb], mul=-0.6065306597126334),
                     reads=[LW], writes=[lw])
                yield
                K.op("dve", lambda en: en.scalar_tensor_tensor(out=kd[:, 0:Wb], in0=tmp[:, 0:Wb], scalar=1.0,
                                                               in1=w["Km"][:, 0:Wb], op0=ALU.add, op1=ALU.mult),
                     reads=[tmp, w["Km"]], writes=[kd])
                yield
                K.op("dve", lambda en: en.tensor_tensor_scan(out=cum[:, 0:Wb], data0=RM[:, 0:Wb], data1=lw[:, 0:Wb],
                                                             initial=0.0, op0=ALU.mult, op1=ALU.add),
                     reads=[RM, lw], writes=[cum])
                yield
                cumv = cum[:, 0:Wb].rearrange("p (r c) -> p r c", c=64)
                if d == 1:
                    K.op("dve", lambda en: en.tensor_tensor(out=tmp[:, 0:Wb], in0=lw[:, 0:Wb], in1=cum[:, 0:Wb],
                                                             op=ALU.subtract), reads=[lw, cum], writes=[tmp])
                    yield
                    tv0 = tmp[:, 0:Wb].rearrange("p (r c) -> p r c", c=64)
                    K.op("dve", lambda en: en.tensor_tensor(out=E[:, 0:Wb].rearrange("p (r c) -> p r c", c=64), in0=tv0,
                                                            in1=cumv[:, :, 63].unsqueeze(2).to_broadcast([128, R, 64]),
                                                            op=ALU.add), reads=[tmp, cum], writes=[E])
                    yield
                    K.op("dve", lambda en: en.tensor_copy(out=cum[:, 0:Wb], in_=E[:, 0:Wb]), reads=[E], writes=[cum])
                    yield
                last = 63 if d == 0 else 0
                cL = cumv[:, :, last]
                gl = gls[d]
                K.op("act", lambda en: en.activation(out=gl[:, 0:R], in_=cL, func=AF.Exp), reads=[cum], writes=[gl])
                K.dma("sp", e[f"GL{d}"][c * 128:(c + 1) * 128, t0 // 64:t0 // 64 + R], gl[:, 0:R], reads=[gl],
                      writes=[e[f"GL{d}"]])
                K.op("act", lambda en: en.activation(out=E[:, 0:Wb], in_=cum[:, 0:Wb], func=AF.Exp), reads=[cum],
                     writes=[E])
                K.op("dve", lambda en: en.tensor_tensor(out=tmp[:, 0:Wb], in0=cum[:, 0:Wb], in1=lw[:, 0:Wb],
                                                         op=ALU.subtract), reads=[cum, lw], writes=[tmp])
                yield
                o = otile()
                K.op("dve", lambda en: en.tensor_tensor(out=o[:, 0:Wb], in0=w["Rm"][:, 0:Wb], in1=E[:, 0:Wb],
                                                        op=ALU.mult), reads=[w["Rm"], E], writes=[o])
                store(e[f"Rt{d}"], c, t0, Wb, o)
                yield
                K.op("act", lambda en: en.activation(out=E[:, 0:Wb], in_=cum[:, 0:Wb], func=AF.Exp, scale=-1.0),
                     reads=[cum], writes=[E])
                yield
                for src_, dn in ((bb, "Bt"), (kd, "Kt")):
                    o = otile()
                    K.op(ve(), lambda en, o=o, src_=src_: en.tensor_tensor(out=o[:, 0:Wb], in0=src_[:, 0:Wb],
                                                                           in1=E[:, 0:Wb], op=ALU.mult),
                         reads=[src_, E], writes=[o])
                    store(e[f"{dn}{d}"], c, t0, Wb, o)
                yield
                K.op("act", lambda en: en.activation(out=E[:, 0:Wb], in_=tmp[:, 0:Wb], func=AF.Exp),
                     reads=[tmp], writes=[E])
                yield
                o = otile()
                K.op("dve", lambda en: en.scalar_tensor_tensor(out=o[:, 0:Wb], in0=w["kk"][:, 0:Wb], scalar=-1.0,
                                                               in1=E[:, 0:Wb], op0=ALU.mult, op1=ALU.mult),
                     reads=[w["kk"], E], writes=[o])
                store(e[f"At{d}"], c, t0, Wb, o)
                tv = tmp[:, 0:Wb].rearrange("p (r c) -> p r c", c=64)
                K.op("dve", lambda en: en.tensor_tensor(out=tv, in0=cumv, in1=cL.unsqueeze(2).to_broadcast([128, R, 64]),
                                                        op=ALU.subtract), reads=[cum], writes=[tmp])
                yield
                K.op("act", lambda en: en.activation(out=E[:, 0:Wb], in_=tmp[:, 0:Wb], func=AF.Exp, scale=-1.0),
                     reads=[tmp], writes=[E])
                yield
                for src_, dn in ((bb, "Bh"), (kd, "Kh")):
                    o = otile()
                    K.op(ve(), lambda en, o=o, src_=src_: en.tensor_tensor(out=o[:, 0:Wb], in0=src_[:, 0:Wb],
                                                                           in1=E[:, 0:Wb], op=ALU.mult),
                         reads=[src_, E], writes=[o])
                    store(e[f"{dn}{d}"], c, t0, Wb, o)

            gens = [dchain(0), dchain(1)]
            while gens:
                alive = []
                for g_ in gens:
                    try:
                        next(g_)
                        alive.append(g_)
                    except StopIteration:
                        pass
                gens = alive
            K.op("dve", lambda en: en.tensor_tensor(out=w["ks"][:, 0:Wb], in0=w["kd0"][:, 0:Wb], in1=w["kd1"][:, 0:Wb],
                                                     op=ALU.add), reads=[w["kd0"], w["kd1"]], writes=[w["ks"]])
            K.op("dve", lambda en: en.scalar_tensor_tensor(out=w["tmp"][:, 0:Wb], in0=w["ks"][:, 0:Wb],
                                                           scalar=COL[:, 6, c:c + 1], in1=w["Rm"][:, 0:Wb],
                                                           op0=ALU.mult, op1=ALU.mult),
                 reads=[w["ks"], COL, w["Rm"]], writes=[w["tmp"]])
            ps = K.ps()
            K.op("pe", lambda en: en.matmul(ps[:, 0:Wb], lhsT=bones[:], rhs=w["tmp"][:, 0:Wb], start=True, stop=True),
                 reads=[bones, w["tmp"]], writes=[ps])
            o = otile()
            K.op("dve", lambda en: en.tensor_tensor(out=o[:, 0:Wb], in0=ps[:, 0:Wb], in1=w["Vm"][:, 0:Wb], op=ALU.mult),
                 reads=[ps, w["Vm"]], writes=[o])
            store(e["BON"], c, t0, Wb, o)
            u = PTt[3]
            Wt = Wb + 128
            K.dma("sp", w["IC"][:, 0:Wb], self.inp[icn][c:c + 1, t0 - s0:t0 - s0 + Wb].partition_broadcast(128),
                  writes=[w["IC"]])
            s_a, s_b = sw
            K.op("dve", lambda en: en.tensor_tensor(out=s_a[:, 1:Wt], in0=u[:, 0:Wt - 1], in1=u[:, 1:Wt], op=ALU.add),
                 reads=[u], writes=[s_a])
            cur_s, oth = s_a, s_b
            lo_, hi_ = 1, Wt
            for hs in (1, 2, 4):
                if POOL_WINS[c] < 4 * hs:
                    break
                nlo, nhi = lo_ + hs, hi_ - hs
                K.op("dve", lambda en, cur_s=cur_s, oth=oth, nlo=nlo, nhi=nhi, hs=hs: en.tensor_tensor(
                    out=oth[:, nlo:nhi], in0=cur_s[:, nlo - hs:nhi - hs], in1=cur_s[:, nlo + hs:nhi + hs], op=ALU.add),
                    reads=[cur_s], writes=[oth])
                cur_s, oth = oth, cur_s
                lo_, hi_ = nlo, nhi
            K.op("dve", lambda en: en.tensor_tensor(out=w["tmp"][:, 0:Wb], in0=cur_s[:, 64:64 + Wb], in1=w["IC"][:, 0:Wb],
                                                    op=ALU.mult), reads=[cur_s, w["IC"]], writes=[w["tmp"]])
            K.op("dve", lambda en: en.tensor_tensor(out=dfb[:, 0:Wb], in0=w["tmp"][:, 0:Wb], in1=u[:, 64:64 + Wb],
                                                     op=ALU.subtract), reads=[w["tmp"], u], writes=[dfb])
            ps = K.ps()
            K.op("pe", lambda en: en.matmul(ps[:, 0:Wb], lhsT=pw[:, c, :], rhs=dfb[:, 0:Wb], start=True, stop=True),
                 reads=[pw, dfb], writes=[ps])
            o = otile()
            K.op("dve", lambda en: en.tensor_scalar(out=o[:, 0:Wb], in0=ps[:, 0:Wb], scalar1=COL[:, 7, c:c + 1],
                                                    scalar2=None, op0=ALU.mult), reads=[ps, COL], writes=[o])
            store(e["YB"], c, t0, Wb, o)
    K.pop_scope()


Prog.even_e2 = _even_e2
def _even_e3(self, i, SDT=None, nsteps=66):
    SDT = SDT or self.SCAN_DT
    K = self.K
    e = self.ed
    K.push_scope()
    TEN = ("At", "Bt", "Kt", "Rt", "Bh", "Kh", "VV")

    def ring(nm, shape, n, dt=F32):
        bufs = [K.sb(shape, dt, f"e3{nm}{q}") for q in range(n)]
        cnt = [0]

        def nxt():
            cnt[0] += 1
            return bufs[cnt[0] % n]
        return nxt

    def mk_mask(nm, cmp_op, sgn=1):
        mbuf = K.sb([128, 128], F32, "e3m" + nm)
        K.op("pool", lambda en: en.memset(mbuf[:], 1.0), writes=[mbuf])
        K.op("pool", lambda en: en.affine_select(out=mbuf[:], in_=mbuf[:], pattern=[[sgn, 128]], compare_op=cmp_op,
                                                 fill=0.0, base=0, channel_multiplier=-sgn), reads=[mbuf], writes=[mbuf])
        K.op("pool", lambda en: en.memset(mbuf[0:64, 64:128], 0.0), reads=[mbuf], writes=[mbuf])
        K.op("pool", lambda en: en.memset(mbuf[64:128, 0:64], 0.0), reads=[mbuf], writes=[mbuf])
        return mbuf
    UPs = mk_mask("ups", ALU.is_gt)
    UPi = mk_mask("upi", ALU.is_ge)
    LOs = mk_mask("los", ALU.is_gt, -1)
    LOi = mk_mask("loi", ALU.is_ge, -1)
    BDM = mk_mask("bdm", ALU.is_ge)
    K.op("pool", lambda en: en.memset(BDM[0:64, 0:64], 1.0), reads=[BDM], writes=[BDM])
    K.op("pool", lambda en: en.memset(BDM[64:128, 64:128], 1.0), reads=[BDM], writes=[BDM])
    M4 = []
    MI = []
    for d in range(2):
        strictT, strict, inclT = (UPs, LOs, UPi) if d == 0 else (LOs, UPs, LOi)
        m4 = K.sb([128, 512], F32, f"e3m4{d}")
        for q, src in enumerate((strictT, strict, strictT, inclT)):
            K.op("dve", lambda en, q=q, src=src: en.tensor_copy(out=m4[:, q * 128:(q + 1) * 128], in_=src[:]),
                 reads=[src], writes=[m4])
        M4.append(m4)
        MI.append(inclT)
    identS = self.ident
    import os
    if os.environ.get("E3PAD"):
        _pad = K.sb([128, 128], F32, "e3pad")
    S = [[K.sb([128, 128], F32, f"e3S{d}{c}") for c in range(4)] for d in range(2)]
    for d in range(2):
        for c in range(4):
            K.op("pool", lambda en, d=d, c=c: en.memset(S[d][c][:], 0.0), writes=[S[d][c]])
    Ssd = S
    if SDT != F32:
        Ssd = [[K.sb([128, 128], SDT, f"e3Sb{d}{c}") for c in range(4)] for d in range(2)]
        for d in range(2):
            for c in range(4):
                K.op("pool", lambda en, d=d, c=c: en.memset(Ssd[d][c][:], 0.0), writes=[Ssd[d][c]])
    LD = [[{nm: K.sb([128, 4, 64], F32, f"e3ld{d}{b}{nm}") for nm in TEN} for b in range(2)] for d in range(2)]
    GLall = [K.sb([128, 4, NT // 64], F32, f"e3glall{d}") for d in range(2)]
    for d in range(2):
        K.dma("sp", GLall[d][:], e[f"GL{d}"][:, :].rearrange("(c p) t -> p c t", p=128), reads=[e[f"GL{d}"]],
              writes=[GLall[d]])
    bd_bufs = [K.sb([128, 7, 128], SDT, f"e3bdz{q}") for q in range(9)]
    for bq in bd_bufs:
        K.op("pool", lambda en, bq=bq: en.memset(bq[:], 0.0), writes=[bq])
    bd_cnt = [0]

    def r_bd():
        bd_cnt[0] += 1
        return bd_bufs[bd_cnt[0] % 9]
    r_tp = ring("tp", [128, 384], 9, SDT)
    r_m = ring("m", [128, 640], 9, SDT)
    r_q = ring("q", [128, 256], 16, SDT)
    r_w = ring("w", [128, 128], 16, SDT)
    r_x = ring("x", [128, 128], 9, SDT)
    r_u = ring("u", [128, 128], 9, SDT)
    r_y = ring("y", [128, 4, 64], 4, F32)
    rr = [0]
    NCH = 2 * nsteps

    def ve3():
        rr[0] += 1
        return "dve"

    def tok_base(d, n):
        if d == 0:
            return T + 64 * n if n < 4 else 64 * (n - 4)
        return T + 64 * (3 - n) if n < 4 else 64 * (127 - (n - 4))

    def load(d, n):
        b = n % 2
        tb = tok_base(d, n)
        for nm in TEN:
            src = e[nm if nm == "VV" else f"{nm}{d}"]
            K.dma("sp", LD[d][b][nm][:], src[:, tb:tb + 64].rearrange("(c p) t -> p c t", p=128), reads=[src],
                  writes=[LD[d][b][nm]])

    def mm(ps_ap, ps, lhsT, lb, rhs, rb, start=True, stop=True):
        K.op("pe", lambda en: en.matmul(ps_ap, lhsT=lhsT, rhs=rhs, start=start, stop=stop), reads=[lb, rb], writes=[ps])

    def unit(d, n, c, ysb):
        b = n % 2
        ld = LD[d][b]
        bd = r_bd()
        for ti, nm in enumerate(TEN):
            src = ld[nm][:, c, :]
            if ti < 4:
                K.op("dve", lambda en, ti=ti, src=src: en.tensor_tensor(
                    out=bd[:, ti, :].rearrange("p (a b) -> p a b", a=2), in0=src.unsqueeze(1).to_broadcast([128, 2, 64]),
                    in1=BDM[:].rearrange("p (a b) -> p a b", a=2), op=ALU.mult), reads=[ld[nm], BDM], writes=[bd])
            else:
                for h2 in range(2):
                    K.op("act", lambda en, ti=ti, h2=h2: en.copy(
                        out=bd[h2 * 64:(h2 + 1) * 64, ti, h2 * 64:(h2 + 1) * 64], in_=ld[nm][h2 * 64:(h2 + 1) * 64, c, :]),
                        reads=[ld[nm]], writes=[bd])
        yield
        A_, B_, K_, R_ = (bd[:, t_, :] for t_ in range(4))
        pst = K.ps()
        idm = identS if SDT == F32 else self.identb
        for t_ in range(3):
            mm(pst[:, t_ * 128:(t_ + 1) * 128], pst, bd[:, 4 + t_, :], bd, idm[:], idm)
        tp = r_tp()
        K.op("act", lambda en: en.copy(out=tp[:], in_=pst[:, 0:384]), reads=[pst], writes=[tp])
        BhT, KhT, VT = (tp[:, t_ * 128:(t_ + 1) * 128] for t_ in range(3))
        p1 = K.ps()
        mm(p1[:, 0:128], p1, B_, bd, A_, bd)
        mm(p1[:, 128:256], p1, A_, bd, B_, bd)
        mm(p1[:, 256:384], p1, K_, bd, A_, bd)
        mm(p1[:, 384:512], p1, B_, bd, R_, bd)
        p2 = K.ps()
        mm(p2[:, 0:128], p2, K_, bd, R_, bd)
        mt = r_m()
        K.op("dve", lambda en: en.tensor_tensor(out=mt[:, 0:512], in0=p1[:, :], in1=M4[d][:], op=ALU.mult),
             reads=[p1, M4[d]], writes=[mt])
        K.op("dve", lambda en: en.tensor_tensor(out=mt[:, 512:640], in0=p2[:, 0:128], in1=MI[d][:], op=ALU.mult),
             reads=[p2, MI[d]], writes=[mt])
        Q, QT, MakT, MrbT, MrkT = (mt[:, t_ * 128:(t_ + 1) * 128] for t_ in range(5))
        W = r_w()
        K.op("dve", lambda en: en.tensor_tensor(out=W[:], in0=Q, in1=identS[:], op=ALU.add),
             reads=[mt, identS], writes=[W])
        yield
        qb = mt
        for lvl in range(1, 6):
            pq = K.ps()
            mm(pq[:, 128:256], pq, Q, qb, QT, qb)
            if lvl < 5:
                mm(pq[:, 0:128], pq, QT, qb, Q, qb)
            nq = r_q()
            if lvl < 5:
                K.op("act", lambda en: en.copy(out=nq[:], in_=pq[:, 0:256]), reads=[pq], writes=[nq])
            else:
                K.op("act", lambda en: en.copy(out=nq[:, 128:256], in_=pq[:, 128:256]), reads=[pq], writes=[nq])
            Q, QT, qb = nq[:, 0:128], nq[:, 128:256], nq
            yield
            pw_ = K.ps()
            mm(pw_[:, 0:128], pw_, QT, qb, W[:], W)
            W2_ = r_w()
            K.op("dve", lambda en: en.tensor_tensor(out=W2_[:], in0=pw_[:, 0:128], in1=W[:], op=ALU.add),
                 reads=[pw_, W], writes=[W2_])
            W = W2_
            yield
        Sb = S[d][c]
        Sm = Ssd[d][c]
        px = K.ps()
        mm(px[:, 0:128], px, A_, bd, Sm[:], Sm, True, False)
        mm(px[:, 0:128], px, MakT, mt, VT, tp, False, True)
        Xs = r_x()
        K.op("act", lambda en: en.copy(out=Xs[:], in_=px[:, 0:128]), reads=[px], writes=[Xs])
        yield
        pu = K.ps()
        mm(pu[:, 0:128], pu, W[:], W, Xs[:], Xs)
        Us = r_u()
        K.op("dve", lambda en: en.tensor_copy(out=Us[:], in_=pu[:, 0:128]), reads=[pu], writes=[Us])
        if self.dbg.get("e3dbg") is not None and d == 0 and c == 0 and n in (0, 1):
            dd = self.dbg["e3dbg"]
            K.dma("sp", dd[n, 0, :, 0:128], Xs[:], reads=[Xs])
            K.dma("sp", dd[n, 1, :, 0:128], Us[:], reads=[Us])
            K.dma("sp", dd[n, 2, :, 0:128], W[:], reads=[W])
            K.dma("sp", dd[n, 3, :, 0:640], mt[:], reads=[mt])
            K.dma("sp", dd[n, 4, :, 0:128], Sb[:], reads=[Sb])
            K.dma("sp", dd[n, 5, :, 0:384], tp[:], reads=[tp])
            for t_ in range(4):
                K.dma("sp", dd[n, 6, :, t_ * 128:(t_ + 1) * 128], bd[:, t_, :], reads=[bd])
        yield
        py = K.ps()
        mm(py[:, 0:128], py, R_, bd, Sm[:], Sm, True, False)
        mm(py[:, 0:128], py, MrbT, mt, Us[:], Us, False, False)
        mm(py[:, 0:128], py, MrkT, mt, VT, tp, False, True)
        pss = K.ps()
        mm(pss[:, 0:128], pss, BhT, tp, Us[:], Us, True, False)
        mm(pss[:, 0:128], pss, KhT, tp, VT, tp, False, True)
        K.op("act", lambda en: en.copy(out=ysb[0:64, c, :], in_=py[0:64, 0:64]), reads=[py], writes=[ysb])
        K.op("act", lambda en: en.copy(out=ysb[64:128, c, :], in_=py[64:128, 64:128]), reads=[py], writes=[ysb])
        gcol = tok_base(d, n) // 64
        K.op("dve", lambda en: en.scalar_tensor_tensor(out=Sb[:], in0=Sb[:], scalar=GLall[d][:, c, gcol:gcol + 1],
                                                       in1=pss[:, 0:128], op0=ALU.mult, op1=ALU.add),
             reads=[Sb, GLall[d], pss], writes=[Sb])
        if SDT != F32:
            K.op("dve", lambda en: en.tensor_copy(out=Sm[:], in_=Sb[:]), reads=[Sb], writes=[Sm])

    for d in range(2):
        load(d, 0)
    for n in range(NCH):
        for d in range(2):
            if n + 1 < NCH:
                load(d, n + 1)
        ysbs = [r_y(), r_y()]
        gens = [unit(d, n, c, ysbs[d]) for c in range(4) for d in range(2)]
        while gens:
            alive = []
            for g in gens:
                try:
                    next(g)
                    alive.append(g)
                except StopIteration:
                    pass
            gens = alive
        for d in range(2):
            tb = tok_base(d, n)
            dst = e[f"YD{d}"][tb:tb + 64, :].rearrange("t (c h v) -> h t c v", h=2, v=64)
            for h2 in range(2):
                K.dma("sp", dst[h2], ysbs[d][h2 * 64:(h2 + 1) * 64, :, :], reads=[ysbs[d]], writes=[e[f"YD{d}"]])
    if self.dbg.get("S") is not None:
        for d in range(2):
            for c in range(4):
                K.dma("sp", self.dbg["S"][d, c], S[d][c][:], reads=[S[d][c]])
    K.pop_scope()


Prog.even_e3 = _even_e3
def _even_e4(self, i, ntile):
    K = self.K
    j = i // 2
    e = self.ed
    K.push_scope()
    wout = K.sb([128, 8, D], BF16, "e4wout")
    stg = [K.sb([128, D], F32, f"e4stg{q}") for q in range(2)]
    for kt in range(8):
        sg = stg[kt % 2]
        K.dma("sp", sg[:], self.inp["w_out"][j, kt * 128:(kt + 1) * 128, :], writes=[sg])
        K.op("dve", lambda en, sg=sg, kt=kt: en.tensor_copy(out=wout[:, kt, :], in_=sg[:]), reads=[sg], writes=[wout])
    GN = K.sb([128, 2, 4], F32, "e4gn")
    K.dma("sp", GN[:, 0, :], self.inp["gn_w"][j].rearrange("(c k) -> k c", k=128), writes=[GN], allow_slow_non_contiguous=True)
    K.dma("sp", GN[:, 1, :], self.inp["gn_b"][j].rearrange("(c k) -> k c", k=128), writes=[GN], allow_slow_non_contiguous=True)
    y0s = [K.sb([128, 512], F32, f"e4y0{q}") for q in range(2)]
    y1s = [K.sb([128, 512], F32, f"e4y1{q}") for q in range(2)]
    sqs = [K.sb([128, 512], F32, f"e4sq{q}") for q in range(2)]
    sts = [K.sb([128, 4, 8], F32, f"e4st{q}") for q in range(2)]
    bons = [K.sb([128, 4, 128], F32, f"e4bon{q}") for q in range(2)]
    ggs = [K.sb([128, 4, 128], F32, f"e4gg{q}") for q in range(2)]
    ybs = [K.sb([128, 4, 128], F32, f"e4yb{q}") for q in range(2)]
    yTs = [K.sb([128, 4, 128], F32, f"e4yT{q}") for q in range(2)]
    cats = [K.sb([128, 8, 128], BF16, f"e4cat{q}") for q in range(2)]
    xts = stg
    ots = [K.sb([128, D], F32, f"e4o{q}") for q in range(2)]
    for tt in range(ntile):
        y0, y1, sq, st, bon, gg, yb, yT, cat, xt, ot = (l[tt % 2] for l in (y0s, y1s, sqs, sts, bons, ggs, ybs, yTs, cats,
                                                                            xts, ots))
        M = self.Ml if tt < 64 else self.Mc
        tk = slice(tt * 128, (tt + 1) * 128)
        K.dma("sp", y0[:], e["YD0"][tk, :], reads=[e["YD0"]], writes=[y0])
        K.dma("sp", y1[:], e["YD1"][tk, :], reads=[e["YD1"]], writes=[y1])
        for buf, nm in ((bon, "BON"), (gg, "GG"), (yb, "YB")):
            K.dma("sp", buf[:], e[nm][:, tk].rearrange("(c p) t -> p c t", p=128), reads=[e[nm]], writes=[buf])
        K.dma("sp", xt[:], self.lat[tk, :], reads=[self.lat.p(tt)], writes=[xt])
        K.op("dve", lambda en: en.tensor_tensor(out=y0[:], in0=y0[:], in1=y1[:], op=ALU.add), reads=[y0, y1], writes=[y0])
        yv = y0[:].rearrange("p (h v) -> p h v", v=64)
        sv = sq[:].rearrange("p (h v) -> p h v", v=64)
        K.op("dve", lambda en: en.reduce_sum(out=st[:, 0, :], in_=yv, axis=AX.X), reads=[y0], writes=[st])
        K.op("dve", lambda en: en.tensor_scalar(out=st[:, 1, :], in0=st[:, 0, :], scalar1=1.0 / 64, scalar2=None,
                                                op0=ALU.mult), reads=[st], writes=[st])
        K.op("dve", lambda en: en.tensor_tensor(out=yv, in0=yv, in1=st[:, 1, :].unsqueeze(2).to_broadcast([128, 8, 64]),
                                                op=ALU.subtract), reads=[y0, st], writes=[y0])
        K.op("dve", lambda en: en.tensor_tensor(out=sq[:], in0=y0[:], in1=y0[:], op=ALU.mult), reads=[y0], writes=[sq])
        K.op("dve", lambda en: en.reduce_sum(out=st[:, 2, :], in_=sv, axis=AX.X), reads=[sq], writes=[st])
        K.op("dve", lambda en: en.tensor_scalar(out=st[:, 2, :], in0=st[:, 2, :], scalar1=1.0 / 64, scalar2=64e-5,
                                                op0=ALU.mult, op1=ALU.add), reads=[st], writes=[st])
        K.op("act", lambda en: en.sqrt(out=st[:, 3, :], in_=st[:, 2, :]), reads=[st], writes=[st])
        K.op("dve", lambda en: en.reciprocal(out=st[:, 3, :], in_=st[:, 3, :]), reads=[st], writes=[st])
        K.op("dve", lambda en: en.tensor_tensor(out=yv, in0=yv, in1=st[:, 3, :].unsqueeze(2).to_broadcast([128, 8, 64]),
                                                op=ALU.mult), reads=[y0, st], writes=[y0])
        ps = K.ps()
        for c in range(4):
            self.tr(None, ps[:, c * 128:(c + 1) * 128], y0[:, c * 128:(c + 1) * 128], ps, y0)
        for c in range(4):
            K.op("dve", lambda en, c=c: en.tensor_scalar(out=yT[:, c, :], in0=ps[:, c * 128:(c + 1) * 128],
                                                         scalar1=GN[:, 0, c:c + 1], scalar2=GN[:, 1, c:c + 1],
                                                         op0=ALU.mult, op1=ALU.add), reads=[ps, GN], writes=[yT])
        K.op("dve", lambda en: en.tensor_tensor(out=yT[:], in0=yT[:], in1=bon[:], op=ALU.add), reads=[yT, bon],
             writes=[yT])
        K.op("dve", lambda en: en.tensor_tensor(out=cat[:, 0:4, :], in0=yT[:], in1=gg[:], op=ALU.mult), reads=[yT, gg],
             writes=[cat])
        K.op("act", lambda en: en.copy(out=cat[:, 4:8, :], in_=yb[:]), reads=[yb], writes=[cat])
        for hh in range(2):
            po = K.ps()
            for kt in range(8):
                K.op("pe", lambda en, kt=kt: en.matmul(po[:, :], lhsT=cat[:, kt, :], rhs=wout[:, kt, hh * 512:(hh + 1) * 512],
                                                       start=(kt == 0), stop=(kt == 7)), reads=[cat, wout], writes=[po])
            K.op("dve", lambda en: en.tensor_tensor(out=ot[:, hh * 512:(hh + 1) * 512], in0=po[:, :],
                                                    in1=M[:, 2, hh * 512:(hh + 1) * 512], op=ALU.mult),
                 reads=[po, M], writes=[ot])
        K.op("dve", lambda en: en.tensor_tensor(out=ot[:], in0=ot[:], in1=xt[:], op=ALU.add), reads=[ot, xt], writes=[ot])
        K.dma("sp", self.lat[tk, :], ot[:], reads=[ot], writes=[self.lat.p(tt)])
    K.pop_scope()


def _even_mixer(self, i):
    self.even_e1(i, ntile=66)
    self.even_e2(i)
    self.even_e3(i)
    self.even_e4(i, ntile=66 if i < 2 else 64)


Prog.even_e4 = _even_e4
Prog.even_mixer = _even_mixer
Prog.even_declare = _even_declare
Prog.even_e1 = _even_e1


def build_program():
    P = Prog()
    P.fourier_declare()
    P.even_declare()
    P.init_lat()
    P.prep_s()
    for i in range(DEPTH):
        P.modvec(i, need_ctx=(i <= 2))
        if i % 2 == 0:
            P.even_mixer(i)
        else:
            P.fourier(i, with_ctx=(i < 2))
        P.moe_setup()
        P.moe(i, with_ctx=(i < 2))
    P.final_norm()
    P.K.finish()
    return P


def kernel(**inputs):
    x = np.asarray(inputs["x"], dtype=np.float32)
    B = x.shape[0]
    P = build_program()
    consts = fourier_consts()
    pconsts = pool_consts()
    in_maps = []
    for b in range(B):
        d = {"x": np.ascontiguousarray(x[b]),
             "c": np.ascontiguousarray(np.asarray(inputs["c"], dtype=np.float32)[b:b + 1]),
             "ctx": np.ascontiguousarray(np.asarray(inputs["ctx"], dtype=np.float32)[b]),
             "c_ctx": np.ascontiguousarray(np.asarray(inputs["c_ctx"], dtype=np.float32)[None, :])}
        for k in WEIGHT_SPECS:
            d[k] = np.ascontiguousarray(np.asarray(inputs[k], dtype=np.float32))
        d.update(consts)
        d.update(pconsts)
        in_maps.append(d)
    res = run_bass_kernel_spmd(P.K.nc, in_maps, core_ids=list(range(B)))
    return np.stack([np.asarray(r["out"]) for r in res.results], axis=0).astype(np.float32)
```

```python
import numpy as np
from contextlib import ExitStack
import concourse.bass as bass
import concourse.mybir as mybir
from concourse.bass_utils import run_bass_kernel_spmd

F32 = mybir.dt.float32
BF16 = mybir.dt.bfloat16
I32 = mybir.dt.int32
AF = mybir.ActivationFunctionType
ALU = mybir.AluOpType
AX = mybir.AxisListType

D = 1024
T = 8192
CT = 256
NT = T + CT
DEPTH = 4
NEXP = 32
DE = 512


class Buf:
    def __init__(self, t, name):
        self.t = t
        self.name = name
        self.lw = None
        self.rd = {}

    def __getitem__(self, idx):
        return self.t[idx]


class Parts:
    def __init__(self, t, name):
        self.t = t
        self.name = name
        self.parts = {}

    def p(self, key):
        b = self.parts.get(key)
        if b is None:
            b = Buf(self.t, f"{self.name}.{key}")
            self.parts[key] = b
        return b

    def all(self):
        return list(self.parts.values())

    def __getitem__(self, idx):
        return self.t[idx]


class Ctx:
    KD = 16
    SAME_ENG_SYNC = True

    def __init__(self):
        self.nc = bass.Bass("TRN2", target_bir_lowering=False)
        nc = self.nc
        self.es = ExitStack()
        self.eng = {"pe": nc.tensor, "act": nc.scalar, "dve": nc.vector, "pool": nc.gpsimd, "sp": nc.sync}
        self.csem = {e: self.es.enter_context(nc.semaphore("c_" + e)) for e in ("pe", "act", "dve", "pool")}
        self.ccnt = {e: 0 for e in self.csem}
        self.dsem = {q: [self.es.enter_context(nc.semaphore(f"d_{q}{i}")) for i in range(self.KD)]
                     for q in ("sp", "pool", "act")}
        self.dcnt = {q: 0 for q in self.dsem}
        self.known = {e: {} for e in self.eng}
        self.nalloc = 0
        self.psum_banks = []
        self.psum_i = 0

    def sb(self, shape, dtype=F32, name=None):
        self.nalloc += 1
        name = (name or "sb") + f"_{self.nalloc}"
        es = self.scopes[-1] if getattr(self, "scopes", None) else self.es
        t = es.enter_context(self.nc.sbuf_tensor(name, list(shape), dtype))
        return Buf(t, name)

    def push_scope(self):
        if not hasattr(self, "scopes"):
            self.scopes = []
        self.scopes.append(ExitStack())

    def pop_scope(self):
        self.barrier()
        self.scopes.pop().close()

    def barrier(self):
        for e in self.eng:
            for src, sem in self.csem.items():
                if self.ccnt[src] > 0:
                    self._wait(e, (sem, self.ccnt[src], "bar"))
            self._wait_all_dma(e)

    def _wait_all_dma(self, e):
        for q in self.dsem:
            n = self.dcnt[q]
            for r in range(self.KD):
                cnt = (n - r + self.KD - 1) // self.KD if n > r else 0
                if cnt > 0:
                    self._wait(e, (self.dsem[q][r], 16 * cnt, "dma"))

    def dram(self, name, shape, dtype=F32, kind="Internal"):
        return self.nc.dram_tensor(name, list(shape), dtype, kind=kind)

    def init_psum(self, n=8):
        for i in range(n):
            t = self.es.enter_context(self.nc.psum_tensor(f"ps{i}", [128, 512], F32))
            b = Buf(t, f"ps{i}")
            b.excl = True
            self.psum_banks.append(b)

    def ps(self):
        b = self.psum_banks[self.psum_i % len(self.psum_banks)]
        self.psum_i += 1
        return b

    def _wait(self, e, ev):
        if ev is None:
            return
        sem, val, src = ev
        if src == e and (e == "pe" or not self.SAME_ENG_SYNC):
            return
        k = self.known[e]
        key = sem.name
        if k.get(key, 0) >= val:
            return
        self.eng[e].wait_ge(sem, val)
        k[key] = val

    def _deps(self, e, reads, writes):
        for b in reads:
            self._wait(e, b.lw)
            if getattr(b, "excl", False):
                for ke, ev in b.rd.items():
                    if ke != e:
                        self._wait(e, ev)
        for b in writes:
            self._wait(e, b.lw)
            for ev in b.rd.values():
                self._wait(e, ev)

    def _commit(self, ev, key, reads, writes):
        for b in writes:
            b.lw = ev
            b.rd = {}
        for b in reads:
            b.rd[key] = ev

    def interleave(self, body, items, n):
        import threading
        items = list(items)
        for g0 in range(0, len(items), n):
            grp = items[g0:g0 + n]
            if len(grp) == 1:
                body(grp[0])
                continue
            cv = threading.Condition()
            state = {"turn": 0, "alive": [True] * len(grp), "err": None}

            def nxt(k):
                m = len(grp)
                for d in range(1, m + 1):
                    if state["alive"][(k + d) % m]:
                        return (k + d) % m
                return -1

            def switch(k):
                with cv:
                    state["turn"] = nxt(k)
                    cv.notify_all()
                    cv.wait_for(lambda: state["turn"] == k)

            def runner(k, it):
                with cv:
                    cv.wait_for(lambda: state["turn"] == k)
                threading.current_thread()._il_switch = lambda: switch(k)
                try:
                    body(it)
                except BaseException as ex:
                    state["err"] = ex
                finally:
                    with cv:
                        state["alive"][k] = False
                        state["turn"] = nxt(k)
                        cv.notify_all()
            ths = [threading.Thread(target=runner, args=(k, it)) for k, it in enumerate(grp)]
            self._il_map = {}
            for t in ths:
                t.start()
            for t in ths:
                t.join()
            if state["err"] is not None:
                raise state["err"]

    def _maybe_switch(self):
        sw = getattr(self, "_switch_tl", None)

    def op(self, e, fn, reads=(), writes=()):
        self._yield_point()
        self._deps(e, reads, writes)
        ins = fn(self.eng[e])
        self.ccnt[e] += 1
        ins.then_inc(self.csem[e], 1)
        ev = (self.csem[e], self.ccnt[e], e)
        self._commit(ev, e, reads, writes)
        return ins

    def _yield_point(self):
        import threading
        sw = getattr(threading.current_thread(), "_il_switch", None)
        if sw is not None:
            sw()

    def dma(self, q, out, in_, reads=(), writes=(), indirect=None, **kw):
        self._yield_point()
        i = self.dcnt[q]
        self.dcnt[q] += 1
        sem = self.dsem[q][i % self.KD]
        val = 16 * (i // self.KD + 1)
        if i >= self.KD:
            self._wait(q, (sem, val - 16, "dma"))
        self._deps(q, reads, writes)
        if indirect is None:
            ins = self.eng[q].dma_start(out=out, in_=in_, **kw)
        else:
            ins = self.eng[q].indirect_dma_start(out=out, in_=in_, **indirect)
        ins.then_inc(sem, 16)
        ev = (sem, val, "dma")
        self._commit(ev, (q, i % self.KD), reads, writes)
        return ins

    def finish(self):
        self._wait_all_dma("sp")
        self.es.close()


WEIGHT_SPECS = {
    "ada_w": [4, 1024, 6144], "ada_b": [4, 6144], "norm_mix": [4, 1024], "norm_ffn": [4, 1024],
    "w_in": [2, 1024, 2048], "mu_x": [2, 3, 1024], "mu_p": [2, 3, 512],
    "decay_w0": [2, 2, 512], "decay_w1": [2, 2, 1024, 64], "decay_w2": [2, 2, 64, 512],
    "lr_a0": [2, 2, 512], "lr_a1": [2, 2, 1024, 64], "lr_a2": [2, 2, 64, 512],
    "gate_g1": [2, 1024, 128], "gate_g2": [2, 128, 512],
    "k_k": [2, 512], "k_a": [2, 512], "r_k": [2, 8, 64], "gn_w": [2, 512], "gn_b": [2, 512],
    "pool_w": [2, 4, 128, 128], "pool_scale": [2, 512], "w_out": [2, 1024, 1024],
    "w_fourier": [2, 1024, 1024],
    "router_c": [4, 1024, 4], "router_c_b": [4, 4], "router_f": [4, 1024, 32], "router_f_b": [4, 32],
    "moe_w1": [4, 32, 1024, 512], "moe_w3": [4, 32, 1024, 512], "moe_w2": [4, 32, 512, 1024],
    "final_norm": [1024],
}


class Prog:
    BLK = 384
    SCAN_DT = BF16

    def __init__(self, debug=None, skip=()):
        self.K = Ctx()
        K = self.K
        nc = K.nc
        self.debug = debug or {}
        self.inp = {}
        self.inp["x"] = nc.dram_tensor("x", [T, D], F32, kind="ExternalInput")
        self.inp["c"] = nc.dram_tensor("c", [1, D], F32, kind="ExternalInput")
        self.inp["ctx"] = nc.dram_tensor("ctx", [CT, D], F32, kind="ExternalInput")
        self.inp["c_ctx"] = nc.dram_tensor("c_ctx", [1, D], F32, kind="ExternalInput")
        for k, shp in WEIGHT_SPECS.items():
            if k in skip:
                continue
            self.inp[k] = nc.dram_tensor(k, shp, F32, kind="ExternalInput")
        self.out = nc.dram_tensor("out", [T, D], F32, kind="ExternalOutput")
        self.dbg = {}
        for k, (shp, dt) in self.debug.items():
            self.dbg[k] = nc.dram_tensor("dbg_" + k, shp, dt, kind="ExternalOutput")
        K.init_psum(8)
        self.lat = Parts(nc.dram_tensor("lat", [NT, D], F32), "lat")
        self.ident = K.sb([128, 128], F32, "ident")
        self.identb = K.sb([128, 128], BF16, "identb")
        self.ones = K.sb([128, 128], F32, "ones")
        self._consts()
        self.Ml = K.sb([128, 6, D], F32, "Ml")
        self.Mc = K.sb([128, 6, D], F32, "Mc")
        self.srep = K.sb([128, 2, 8, 128], F32, "srep")

    def _consts(self):
        K = self.K
        K.op("pool", lambda e: e.memset(self.ones[:], 1.0), writes=[self.ones])
        K.op("pool", lambda e: e.memset(self.ident[:], 0.0), writes=[self.ident])
        K.op("pool", lambda e: e.affine_select(out=self.ident[:], in_=self.ident[:], pattern=[[-1, 128]],
                                               compare_op=ALU.not_equal, fill=1.0, base=0, channel_multiplier=1),
             reads=[self.ident], writes=[self.ident])
        K.op("dve", lambda e: e.tensor_copy(out=self.identb[:], in_=self.ident[:]), reads=[self.ident],
             writes=[self.identb])

    def prep_s(self):
        K = self.K
        craw = K.sb([128, 2, 8], F32, "craw")
        csil = K.sb([128, 2, 8], F32, "csil")
        for w, nm in enumerate(("c", "c_ctx")):
            src = self.inp[nm].ap().rearrange("o (kt k) -> k (o kt)", k=128)
            K.dma("sp", craw[:, w, :], src, writes=[craw], allow_slow_non_contiguous=True)
        K.op("act", lambda e: e.activation(out=csil[:], in_=craw[:], func=AF.Silu), reads=[craw], writes=[csil])
        K.op("dve", lambda e: e.tensor_copy(out=self.srep[:], in_=csil[:].unsqueeze(3).to_broadcast([128, 2, 8, 128])),
             reads=[csil], writes=[self.srep])

    def modvec(self, i, need_ctx=True):
        K = self.K
        K.push_scope()
        mv = dict(
            W=[K.sb([128, 8, 512], F32, f"adaW{j}") for j in range(2)],
            b=[K.sb([1, 512], F32, f"adab{j}") for j in range(2)],
            g=K.sb([128, 2, D], F32, "normg"),
        )
        aw = self.inp["ada_w"]
        ab = self.inp["ada_b"]
        g = mv["g"]
        K.dma("sp", g[:, 0, :], self.inp["norm_mix"][i:i + 1, :].partition_broadcast(128), writes=[g])
        K.dma("sp", g[:, 1, :], self.inp["norm_ffn"][i:i + 1, :].partition_broadcast(128), writes=[g])
        targets = [(0, self.Ml)] + ([(1, self.Mc)] if need_ctx else [])
        for nb in range(12):
            W = mv["W"][nb % 2]
            bb = mv["b"][nb % 2]
            K.dma("sp", W[:], aw[i, :, nb * 512:(nb + 1) * 512].rearrange("(kt k) n -> k kt n", k=128), writes=[W])
            K.dma("sp", bb[:], ab[i:i + 1, nb * 512:(nb + 1) * 512], writes=[bb])
            for w, M in targets:
                ps = K.ps()
                for kt in range(8):
                    K.op("pe", lambda e, kt=kt, w=w, ps=ps, W=W: e.matmul(ps[:, :], lhsT=self.srep[:, w, kt, :],
                                                                         rhs=W[:, kt, :], start=(kt == 0), stop=False),
                         reads=[self.srep, W], writes=[ps])
                K.op("pe", lambda e, ps=ps, bb=bb: e.matmul(ps[:, :], lhsT=self.ones[0:1, :], rhs=bb[0:1, :],
                                                            start=False, stop=True),
                     reads=[self.ones, bb], writes=[ps])
                s, half = nb // 2, nb % 2
                dst = M[:, s, half * 512:(half + 1) * 512]
                if s in (1, 4):
                    gi = 0 if s == 1 else 1
                    K.op("dve", lambda e, dst=dst, ps=ps, gi=gi, half=half: e.scalar_tensor_tensor(
                        out=dst, in0=ps[:, :], scalar=1.0, in1=g[:, gi, half * 512:(half + 1) * 512],
                        op0=ALU.add, op1=ALU.mult), reads=[ps, g], writes=[M])
                else:
                    K.op("act", lambda e, dst=dst, ps=ps: e.copy(out=dst, in_=ps[:, :]), reads=[ps], writes=[M])
        K.pop_scope()

    def norm_tile(self, xt, ht, M, sub, st):
        K = self.K
        sh = 0 if sub == 0 else 3
        ga = 1 if sub == 0 else 4
        K.op("act", lambda e: e.activation(out=ht[:], in_=xt[:], func=AF.Square, accum_out=st[:, 0:1]),
             reads=[xt], writes=[ht, st])
        K.op("dve", lambda e: e.tensor_scalar(out=st[:, 1:2], in0=st[:, 0:1], scalar1=1.0 / D, scalar2=1e-6,
                                              op0=ALU.mult, op1=ALU.add), reads=[st], writes=[st])
        K.op("act", lambda e: e.sqrt(out=st[:, 3:4], in_=st[:, 1:2]), reads=[st], writes=[st])
        K.op("dve", lambda e: e.reciprocal(out=st[:, 2:3], in_=st[:, 3:4]), reads=[st], writes=[st])
        K.op("dve", lambda e: e.scalar_tensor_tensor(out=ht[:], in0=xt[:], scalar=st[:, 2:3], in1=M[:, ga, :],
                                                     op0=ALU.mult, op1=ALU.mult), reads=[xt, st, M], writes=[ht])
        K.op("dve", lambda e: e.tensor_tensor(out=ht[:], in0=ht[:], in1=M[:, sh, :], op=ALU.add),
             reads=[ht, M], writes=[ht])

    def tr(self, e_unused, dst_ps_ap, src_ap, ps, src_buf, n_in=128):
        K = self.K
        K.op("pe", lambda e: e.transpose(out=dst_ps_ap, in_=src_ap, identity=self.ident[0:n_in, 0:n_in]),
             reads=[src_buf, self.ident], writes=[ps])

    def init_lat(self):
        K = self.K
        for r in range(0, T, 2048):
            K.dma("sp", self.lat[r:r + 2048, :], self.inp["x"][r:r + 2048, :],
                  writes=[self.lat.p(t) for t in range(r // 128, r // 128 + 16)])
        K.dma("sp", self.lat[T:NT, :], self.inp["ctx"][:, :], writes=[self.lat.p(64), self.lat.p(65)])

    def moe_dram(self):
        nc = self.K.nc
        BLK = self.BLK
        self.NB = (2 * NT + 32 * BLK) // BLK
        NB = self.NB
        self.mdram = dict(H2=Parts(nc.dram_tensor("H2", [NT, D], F32), "H2"),
                          XS=Buf(nc.dram_tensor("XS", [NB * BLK, D], F32), "XS"),
                          YS=Buf(nc.dram_tensor("YS", [NB * BLK, D], BF16), "YS"))

    def moe_setup(self):
        K = self.K
        nc = K.nc
        if not hasattr(self, "mdram"):
            self.moe_dram()
        K.push_scope()
        m = dict(self.mdram)
        NTL = 66
        NB = self.NB
        m["OHA"] = K.sb([128, NTL, 2, 32], BF16, "OHA")
        m["RK"] = K.sb([128, NTL, 2], F32, "RK")
        m["cs"] = K.sb([128, 8, 32], F32, "moecs")
        m["be"] = K.sb([128, 3, NB], F32, "moebe")
        m["idf"] = K.sb([128, NB, 8], F32, "moeidf")
        m["GATE"] = K.sb([128, NTL, 2], F32, "GATE")
        m["DEST"] = K.sb([128, NTL, 2], I32, "DEST")
        m["carry"] = K.sb([128, 32], F32, "carry")
        m["Wr"] = K.sb([128, 8, 36], F32, "Wr")
        m["br"] = K.sb([1, 36], F32, "br")
        m["UT"] = K.sb([128, 128], F32, "UT")
        m["PIDX"] = K.sb([128, 8], F32, "PIDX")
        m["JV"] = K.sb([128, NB], F32, "JV")
        m["IDX1"] = K.sb([128, NB, 8], I32, "IDX1")
        m["IDX2"] = K.sb([128, NB, 4], I32, "IDX2")
        big = [K.sb([128, D], F32, f"big{j}") for j in range(8)]
        m["xt"] = big[0:2]
        m["ht"] = big[2:4]
        m["yb"] = [K.sb([128, D], BF16, f"myb{j}") for j in range(2)]
        m["y0"] = [K.sb([128, D], BF16, f"my0{j}") for j in range(2)]
        m["y1"] = [K.sb([128, D], BF16, f"my1{j}") for j in range(2)]
        m["ya"] = big[4:6]
        m["st"] = [K.sb([128, 4], F32, f"mst{j}") for j in range(2)]
        m["hT"] = [K.sb([128, 8, 128], F32, f"mhT{j}") for j in range(2)]
        m["xTb"] = [K.sb([128, 8, 128], BF16, f"mxTb{j}") for j in range(2)]
        m["W1"] = [K.sb([128, 8, 512], BF16, f"mW1{j}") for j in range(2)]
        m["W3"] = [K.sb([128, 8, 512], BF16, f"mW3{j}") for j in range(2)]
        m["W2"] = [K.sb([128, 4, 1024], BF16, f"mW2{j}") for j in range(2)]
        m["hTb"] = [K.sb([128, 4, 128], BF16, f"mhTb{j}") for j in range(2)]
        m["sil"] = [K.sb([128, 512], F32, f"msil{j}") for j in range(2)]
        m["sm"] = [K.sb([128, 128], F32, f"msm{j}") for j in range(2)]
        UT = m["UT"]
        K.op("pool", lambda e: e.memset(UT[:], 1.0), writes=[UT])
        K.op("pool", lambda e: e.affine_select(out=UT[:], in_=UT[:], pattern=[[1, 128]], compare_op=ALU.is_gt,
                                               fill=0.0, base=0, channel_multiplier=-1), reads=[UT], writes=[UT])
        pi = K.sb([128, 8], I32, "pidx_i")
        K.op("pool", lambda e: e.iota(pi[:], pattern=[[128, 8]], base=0, channel_multiplier=1), writes=[pi])
        K.op("dve", lambda e: e.tensor_copy(out=m["PIDX"][:], in_=pi[:]), reads=[pi], writes=[m["PIDX"]])
        ji = K.sb([128, NB], I32, "jv_i")
        K.op("pool", lambda e: e.iota(ji[:], pattern=[[self.BLK, NB]], base=0, channel_multiplier=0), writes=[ji])
        K.op("dve", lambda e: e.tensor_copy(out=m["JV"][:], in_=ji[:]), reads=[ji], writes=[m["JV"]])
        self.m = m

    def moe(self, i, with_ctx, final=False):
        K = self.K
        m = self.m
        NB = self.NB
        ntile = 66 if with_ctx else 64
        Wr, br = m["Wr"], m["br"]
        K.dma("sp", Wr[:, :, 0:4], self.inp["router_c"][i].rearrange("(kt k) n -> k kt n", k=128), writes=[Wr])
        K.dma("sp", Wr[:, :, 4:36], self.inp["router_f"][i].rearrange("(kt k) n -> k kt n", k=128), writes=[Wr])
        K.dma("sp", br[:, 0:4], self.inp["router_c_b"][i:i + 1, :], writes=[br])
        K.dma("sp", br[:, 4:36], self.inp["router_f_b"][i:i + 1, :], writes=[br])
        carry = m["carry"]
        K.op("dve", lambda e: e.memset(carry[:], 0.0), writes=[carry])
        OHA, RK, GATE, DEST = m["OHA"], m["RK"], m["GATE"], m["DEST"]
        def _body(tt):
            xt, ht, st, hT, sm = (m[k][tt % 2] for k in ("xt", "ht", "st", "hT", "sm"))
            M = self.Ml if tt < 64 else self.Mc
            K.dma("sp", xt[:], self.lat[tt * 128:(tt + 1) * 128, :], reads=[self.lat.p(tt)], writes=[xt])
            self.norm_tile(xt, ht, M, 1, st)
            K.dma("act", m["H2"][tt * 128:(tt + 1) * 128, :], ht[:], reads=[ht], writes=[m["H2"].p(tt)])
            for half in range(2):
                ps = K.ps()
                for q in range(4):
                    kt = half * 4 + q
                    self.tr(None, ps[:, q * 128:(q + 1) * 128], ht[:, kt * 128:(kt + 1) * 128], ps, ht)
                K.op("act" if half else "dve",
                     lambda e, ps=ps, half=half: (e.copy if half else e.tensor_copy)(
                         out=hT[:, half * 4:(half + 1) * 4, :].rearrange("p a b -> p (a b)"), in_=ps[:, :]),
                     reads=[ps], writes=[hT])
            ps = K.ps()
            for kt in range(8):
                K.op("pe", lambda e, kt=kt, ps=ps: e.matmul(ps[:, 0:36], lhsT=hT[:, kt, :], rhs=Wr[:, kt, :],
                                                           start=(kt == 0), stop=False),
                     reads=[hT, Wr], writes=[ps])
            K.op("pe", lambda e, ps=ps: e.matmul(ps[:, 0:36], lhsT=self.ones[0:1, :], rhs=br[0:1, :],
                                                 start=False, stop=True), reads=[self.ones, br], writes=[ps])
            def dv(fn, rd=(), wr=()):
                K.op("dve", fn, reads=[sm] + list(rd), writes=[sm] + list(wr))
            K.op("dve", lambda e, ps=ps: e.tensor_copy(out=sm[:, 0:36], in_=ps[:, 0:36]), reads=[ps], writes=[sm])
            dv(lambda e: e.reduce_max(out=sm[:, 36:37], in_=sm[:, 0:4], axis=AX.X))
            dv(lambda e: e.tensor_scalar(out=sm[:, 37:38], in0=sm[:, 36:37], scalar1=-1.0, scalar2=None, op0=ALU.mult))
            dv(lambda e: e.tensor_scalar(out=sm[:, 40:44], in0=sm[:, 0:4], scalar1=sm[:, 36:37], scalar2=None,
                                         op0=ALU.is_equal))
            K.op("act", lambda e: e.activation(out=sm[:, 44:48], in_=sm[:, 0:4], func=AF.Exp, bias=sm[:, 37:38],
                                               scale=1.0, accum_out=sm[:, 38:39]), reads=[sm], writes=[sm])
            dv(lambda e: e.reciprocal(out=sm[:, 39:40], in_=sm[:, 38:39]))
            dv(lambda e: e.tensor_scalar(out=sm[:, 48:56], in0=sm[:, 4:12], scalar1=sm[:, 40:41], scalar2=None,
                                         op0=ALU.mult))
            for g in range(1, 4):
                dv(lambda e, g=g: e.scalar_tensor_tensor(out=sm[:, 48:56], in0=sm[:, 4 + 8 * g:12 + 8 * g],
                                                         scalar=sm[:, 40 + g:41 + g], in1=sm[:, 48:56],
                                                         op0=ALU.mult, op1=ALU.add))
            dv(lambda e: e.reduce_max(out=sm[:, 56:57], in_=sm[:, 48:56], axis=AX.X))
            dv(lambda e: e.tensor_scalar(out=sm[:, 60:68], in0=sm[:, 48:56], scalar1=sm[:, 56:57], scalar2=None,
                                         op0=ALU.is_equal))
            dv(lambda e: e.scalar_tensor_tensor(out=sm[:, 68:76], in0=sm[:, 60:68], scalar=-1e30, in1=sm[:, 48:56],
                                                op0=ALU.mult, op1=ALU.add))
            dv(lambda e: e.reduce_max(out=sm[:, 57:58], in_=sm[:, 68:76], axis=AX.X))
            dv(lambda e: e.tensor_scalar(out=sm[:, 76:84], in0=sm[:, 68:76], scalar1=sm[:, 57:58], scalar2=None,
                                         op0=ALU.is_equal))
            dv(lambda e: e.tensor_tensor(out=sm[:, 58:59], in0=sm[:, 56:57], in1=sm[:, 57:58], op=ALU.subtract))
            K.op("act", lambda e: e.activation(out=sm[:, 59:60], in_=sm[:, 58:59], func=AF.Sigmoid),
                 reads=[sm], writes=[sm])
            dv(lambda e, tt=tt: e.tensor_tensor(out=GATE[:, tt, 0:1], in0=sm[:, 59:60], in1=sm[:, 39:40], op=ALU.mult),
               wr=[GATE])
            dv(lambda e, tt=tt: e.tensor_tensor(out=GATE[:, tt, 1:2], in0=sm[:, 39:40], in1=GATE[:, tt, 0:1],
                                                op=ALU.subtract), rd=[GATE], wr=[GATE])
            for k, c0 in ((0, 60), (1, 76)):
                dv(lambda e, tt=tt, k=k, c0=c0: e.tensor_tensor(
                    out=OHA[:, tt, k, :].rearrange("p (g l) -> p g l", g=4),
                    in0=sm[:, 40:44].unsqueeze(2).to_broadcast([128, 4, 8]),
                    in1=sm[:, c0:c0 + 8].unsqueeze(1).to_broadcast([128, 4, 8]), op=ALU.mult), wr=[OHA])
            dv(lambda e, tt=tt: e.tensor_tensor(out=sm[:, 84:116], in0=OHA[:, tt, 0, :], in1=OHA[:, tt, 1, :],
                                                op=ALU.add), rd=[OHA])
            psr = K.ps()
            K.op("pe", lambda e, psr=psr: e.matmul(psr[:, 0:32], lhsT=m["UT"][:], rhs=sm[:, 84:116], start=True,
                                                   stop=True), reads=[m["UT"], sm], writes=[psr])
            K.op("pe", lambda e, psr=psr: e.matmul(psr[:, 32:64], lhsT=self.ones[:], rhs=sm[:, 84:116], start=True,
                                                   stop=True), reads=[self.ones, sm], writes=[psr])
            K.op("dve", lambda e, psr=psr: e.tensor_tensor(out=sm[:, 84:116], in0=psr[:, 0:32], in1=carry[:],
                                                           op=ALU.add), reads=[psr, carry, sm], writes=[sm])
            for k in range(2):
                dv(lambda e, tt=tt, k=k: e.tensor_tensor(out=sm[:, 0:32], in0=sm[:, 84:116], in1=OHA[:, tt, k, :],
                                                         op=ALU.mult), rd=[OHA])
                dv(lambda e, tt=tt, k=k: e.reduce_sum(out=RK[:, tt, k:k + 1], in_=sm[:, 0:32], axis=AX.X), wr=[RK])
            K.op("dve", lambda e, psr=psr: e.tensor_tensor(out=carry[:], in0=psr[:, 32:64], in1=carry[:], op=ALU.add),
                 reads=[psr, carry], writes=[carry])
        K.interleave(_body, range(ntile), 1)
        cs = m["cs"]

        def cv(fn):
            K.op("dve", fn, reads=[cs, carry], writes=[cs])
        BLK = self.BLK
        cv(lambda e: e.tensor_scalar(out=cs[:, 0, :], in0=carry[:], scalar1=1.0 / BLK, scalar2=(BLK - 1.0) / (2 * BLK),
                                     op0=ALU.mult, op1=ALU.add))
        cv(lambda e: e.tensor_scalar(out=cs[:, 1, :], in0=cs[:, 0, :], scalar1=8388608.0, scalar2=None, op0=ALU.add))
        cv(lambda e: e.tensor_scalar(out=cs[:, 2, :], in0=cs[:, 1, :], scalar1=-8388608.0, scalar2=float(BLK),
                                     op0=ALU.add, op1=ALU.mult))
        cv(lambda e: e.tensor_copy(out=cs[:, 3, :], in_=cs[:, 2, :]))
        a, b = 3, 4
        for s in (1, 2, 4, 8, 16):
            cv(lambda e, a=a, b=b, s=s: e.tensor_copy(out=cs[:, b, 0:s], in_=cs[:, a, 0:s]))
            cv(lambda e, a=a, b=b, s=s: e.tensor_tensor(out=cs[:, b, s:32], in0=cs[:, a, s:32], in1=cs[:, a, 0:32 - s],
                                                        op=ALU.add))
            a, b = b, a
        pend_i = a
        cv(lambda e: e.tensor_tensor(out=cs[:, 5, :], in0=cs[:, pend_i, :], in1=cs[:, 2, :], op=ALU.subtract))
        be = m["be"]
        K.op("dve", lambda e: e.memset(be[:, 0, :], 0.0), writes=[be])
        for ex in range(32):
            K.op("dve", lambda e, ex=ex: e.scalar_tensor_tensor(out=be[:, 0, :], in0=m["JV"][:],
                                                                scalar=cs[:, pend_i, ex:ex + 1], in1=be[:, 0, :],
                                                                op0=ALU.is_ge, op1=ALU.add),
                 reads=[m["JV"], cs, be], writes=[be])
        K.op("dve", lambda e: e.tensor_scalar(out=be[:, 0, :], in0=be[:, 0, :], scalar1=31.0, scalar2=None, op0=ALU.min),
             reads=[be], writes=[be])
        K.op("dve", lambda e: e.tensor_scalar(out=be[:, 1, :], in0=be[:, 0, :], scalar1=1024.0,
                                              scalar2=float(i * 32 * 1024), op0=ALU.mult, op1=ALU.add),
             reads=[be], writes=[be])
        K.op("dve", lambda e: e.tensor_scalar(out=be[:, 2, :], in0=be[:, 0, :], scalar1=512.0,
                                              scalar2=float(i * 32 * 512), op0=ALU.mult, op1=ALU.add),
             reads=[be], writes=[be])
        idf = m["idf"]
        K.op("dve", lambda e: e.tensor_tensor(out=idf[:], in0=be[:, 1, :].unsqueeze(2).to_broadcast([128, NB, 8]),
                                              in1=m["PIDX"][:].unsqueeze(1).to_broadcast([128, NB, 8]), op=ALU.add),
             reads=[be, m["PIDX"]], writes=[idf])
        K.op("dve", lambda e: e.tensor_copy(out=m["IDX1"][:], in_=idf[:]), reads=[idf], writes=[m["IDX1"]])
        K.op("dve", lambda e: e.tensor_tensor(out=idf[:, :, 0:4], in0=be[:, 2, :].unsqueeze(2).to_broadcast([128, NB, 4]),
                                              in1=m["PIDX"][:, 0:4].unsqueeze(1).to_broadcast([128, NB, 4]),
                                              op=ALU.add), reads=[be, m["PIDX"]], writes=[idf])
        K.op("dve", lambda e: e.tensor_copy(out=m["IDX2"][:], in_=idf[:, :, 0:4]), reads=[idf], writes=[m["IDX2"]])
        for tt in range(ntile):
            ht, sm = m["ht"][tt % 2], m["sm"][tt % 2]
            K.dma("sp", ht[:], m["H2"][tt * 128:(tt + 1) * 128, :], reads=[m["H2"].p(tt)], writes=[ht])
            for k in range(2):
                K.op("dve", lambda e, tt=tt, k=k: e.tensor_tensor(out=sm[:, 32:64], in0=cs[:, 5, :],
                                                                  in1=OHA[:, tt, k, :], op=ALU.mult),
                     reads=[sm, OHA, cs], writes=[sm])
                K.op("dve", lambda e, k=k: e.reduce_sum(out=sm[:, 66 + k:67 + k], in_=sm[:, 32:64], axis=AX.X),
                     reads=[sm], writes=[sm])
            K.op("dve", lambda e, tt=tt: e.tensor_tensor(out=sm[:, 64:66], in0=sm[:, 66:68], in1=RK[:, tt, :],
                                                         op=ALU.add), reads=[sm, RK], writes=[sm])
            K.op("dve", lambda e, tt=tt: e.tensor_copy(out=DEST[:, tt, :], in_=sm[:, 64:66]), reads=[sm], writes=[DEST])
            for k in range(2):
                K.dma("pool", m["XS"][:, :], ht[:], reads=[ht, DEST], writes=[m["XS"]],
                      indirect=dict(out_offset=bass.IndirectOffsetOnAxis(ap=DEST[:, tt, k:k + 1], axis=0),
                                    in_offset=None))
        w1t = self.inp["moe_w1"].ap().rearrange("l e k n -> (l e k) n")
        w3t = self.inp["moe_w3"].ap().rearrange("l e k n -> (l e k) n")
        w2t = self.inp["moe_w2"].ap().rearrange("l e k n -> (l e k) n")
        for j in range(NB):
            W1, W3, W2, xt, xTb, hTb, sil, yb = (m[k][j % 2] for k in ("W1", "W3", "W2", "xt", "xTb", "hTb", "sil", "yb"))
            for kt in range(8):
                for W, tab in ((W1, w1t), (W3, w3t)):
                    K.dma("pool", W[:, kt, :], tab, reads=[m["IDX1"]], writes=[W],
                          indirect=dict(out_offset=None,
                                        in_offset=bass.IndirectOffsetOnAxis(ap=m["IDX1"][:, j, kt:kt + 1], axis=0)))
            for fc in range(4):
                K.dma("pool", W2[:, fc, :], w2t, reads=[m["IDX2"]], writes=[W2],
                      indirect=dict(out_offset=None,
                                    in_offset=bass.IndirectOffsetOnAxis(ap=m["IDX2"][:, j, fc:fc + 1], axis=0)))
            for sub in range(self.BLK // 128):
                r0 = j * self.BLK + sub * 128
                xt, xTb, hTb, sil, yb = (m[k][(j * 4 + sub) % 2] for k in ("xt", "xTb", "hTb", "sil", "yb"))
                K.dma("sp", xt[:], m["XS"][r0:r0 + 128, :], reads=[m["XS"]], writes=[xt])
                for half in range(2):
                    ps = K.ps()
                    for q in range(4):
                        kt = half * 4 + q
                        self.tr(None, ps[:, q * 128:(q + 1) * 128], xt[:, kt * 128:(kt + 1) * 128], ps, xt)
                    K.op("act" if half else "dve",
                         lambda e, ps=ps, half=half, xTb=xTb: (e.copy if half else e.tensor_copy)(
                             out=xTb[:, half * 4:(half + 1) * 4, :].rearrange("p a b -> p (a b)"), in_=ps[:, :]),
                         reads=[ps], writes=[xTb])
                pa, pb = K.ps(), K.ps()
                for W, pp in ((W1, pa), (W3, pb)):
                    for fc in range(4):
                        for kt in range(8):
                            K.op("pe", lambda e, W=W, pp=pp, fc=fc, kt=kt, xTb=xTb: e.matmul(
                                pp[:, fc * 128:(fc + 1) * 128], lhsT=W[:, kt, fc * 128:(fc + 1) * 128], rhs=xTb[:, kt, :],
                                start=(kt == 0), stop=(kt == 7)), reads=[W, xTb], writes=[pp])
                K.op("act", lambda e, pa=pa, sil=sil: e.activation(out=sil[:], in_=pa[:, :], func=AF.Silu),
                     reads=[pa], writes=[sil])
                K.op("dve", lambda e, pb=pb, sil=sil, hTb=hTb: e.tensor_tensor(
                    out=hTb[:].rearrange("p a b -> p (a b)"), in0=sil[:], in1=pb[:, :], op=ALU.mult),
                    reads=[pb, sil], writes=[hTb])
                for half in range(2):
                    py = K.ps()
                    for fc in range(4):
                        K.op("pe", lambda e, py=py, fc=fc, half=half, hTb=hTb, W2=W2: e.matmul(
                            py[:, :], lhsT=hTb[:, fc, :], rhs=W2[:, fc, half * 512:(half + 1) * 512],
                            start=(fc == 0), stop=(fc == 3)), reads=[hTb, W2], writes=[py])
                    K.op("act" if half else "dve",
                         lambda e, py=py, half=half, yb=yb: (e.copy if half else e.tensor_copy)(
                             out=yb[:, half * 512:(half + 1) * 512], in_=py[:, :]), reads=[py], writes=[yb])
                K.dma("sp", m["YS"][r0:r0 + 128, :], yb[:], reads=[yb], writes=[m["YS"]])
        def _body(tt):
            y0, y1, xt = m["y0"][tt % 2], m["y1"][tt % 2], m["xt"][tt % 2]
            M = self.Ml if tt < 64 else self.Mc
            for k, y in ((0, y0), (1, y1)):
                K.dma("pool", y[:], m["YS"][:, :], reads=[m["YS"], DEST], writes=[y],
                      indirect=dict(out_offset=None,
                                    in_offset=bass.IndirectOffsetOnAxis(ap=DEST[:, tt, k:k + 1], axis=0)))
            K.dma("sp", xt[:], self.lat[tt * 128:(tt + 1) * 128, :], reads=[self.lat.p(tt)], writes=[xt])
            ya = m["ya"][tt % 2]
            K.op("dve", lambda e, tt=tt, y0=y0, ya=ya: e.tensor_scalar(out=ya[:], in0=y0[:], scalar1=GATE[:, tt, 0:1],
                                                                       scalar2=None, op0=ALU.mult),
                 reads=[y0, GATE], writes=[ya])
            K.op("dve", lambda e, tt=tt, ya=ya, y1=y1: e.scalar_tensor_tensor(
                out=ya[:], in0=y1[:], scalar=GATE[:, tt, 1:2], in1=ya[:], op0=ALU.mult, op1=ALU.add),
                reads=[ya, y1, GATE], writes=[ya])
            K.op("dve", lambda e, ya=ya, M=M: e.tensor_tensor(out=ya[:], in0=ya[:], in1=M[:, 5, :], op=ALU.mult),
                 reads=[ya, M], writes=[ya])
            K.op("dve", lambda e, ya=ya, xt=xt: e.tensor_tensor(out=xt[:], in0=xt[:], in1=ya[:], op=ALU.add),
                 reads=[ya, xt], writes=[xt])
            K.dma("sp", self.lat[tt * 128:(tt + 1) * 128, :], xt[:], reads=[xt], writes=[self.lat.p(tt)])
        K.interleave(_body, range(ntile), 2)
        K.pop_scope()

    def final_norm(self):
        K = self.K
        K.push_scope()
        m = dict(xt=[K.sb([128, D], F32, f"fx{j}") for j in range(2)], ht=[K.sb([128, D], F32, f"fh{j}") for j in range(2)],
                 st=[K.sb([128, 4], F32, f"fs{j}") for j in range(2)])
        g = K.sb([128, D], F32, "fng")
        K.dma("sp", g[:], self.inp["final_norm"].ap().rearrange("(o d) -> o d", o=1).partition_broadcast(128), writes=[g])
        for tt in range(64):
            xt, ht, st = m["xt"][tt % 2], m["ht"][tt % 2], m["st"][tt % 2]
            K.dma("sp", xt[:], self.lat[tt * 128:(tt + 1) * 128, :], reads=[self.lat.p(tt)], writes=[xt])
            K.op("act", lambda e, xt=xt, ht=ht, st=st: e.activation(out=ht[:], in_=xt[:], func=AF.Square,
                                                                    accum_out=st[:, 0:1]), reads=[xt], writes=[ht, st])
            K.op("dve", lambda e, st=st: e.tensor_scalar(out=st[:, 1:2], in0=st[:, 0:1], scalar1=1.0 / D, scalar2=1e-6,
                                                         op0=ALU.mult, op1=ALU.add), reads=[st], writes=[st])
            K.op("act", lambda e, st=st: e.sqrt(out=st[:, 3:4], in_=st[:, 1:2]), reads=[st], writes=[st])
            K.op("dve", lambda e, st=st: e.reciprocal(out=st[:, 2:3], in_=st[:, 3:4]), reads=[st], writes=[st])
            K.op("dve", lambda e, xt=xt, ht=ht, st=st: e.scalar_tensor_tensor(
                out=ht[:], in0=xt[:], scalar=st[:, 2:3], in1=g[:], op0=ALU.mult, op1=ALU.mult),
                reads=[xt, st, g], writes=[ht])
            K.dma("sp", self.out[tt * 128:(tt + 1) * 128, :], ht[:], reads=[ht])
        K.pop_scope()


def fourier_consts():
    c = {}
    n = np.arange(256)
    ang = 2 * np.pi * np.outer(n, n) / 256
    c["f_cc"] = (np.cos(ang) / 16).astype(np.float32)
    c["f_sc"] = (-np.sin(ang) / 16).astype(np.float32)
    a = np.arange(128)
    ang = 2 * np.pi * np.outer(a, a) / 128
    s = 1.0 / np.sqrt(8192.0)
    c["f_c128"] = (np.cos(ang) * s).astype(np.float32)
    c["f_s128"] = (np.sin(ang) * s).astype(np.float32)
    f1 = np.arange(128)[:, None]
    b = np.arange(64)[None, :]
    th = 2 * np.pi * f1 * b / 8192
    c["f_tw"] = np.stack([np.cos(th), -np.sin(th)], axis=2).astype(np.float32)
    bb = np.arange(64)
    ang = 2 * np.pi * np.outer(bb, bb) / 64
    c["f_cs64"] = np.concatenate([np.cos(ang), np.sin(ang)], axis=0).astype(np.float32)
    t = np.arange(256)
    ang = 2 * np.pi * np.outer(t, t) / 256
    c["f_c256"] = (np.cos(ang) / 16).astype(np.float32)
    c["f_s256"] = (np.sin(ang) / 16).astype(np.float32)
    return c


FOURIER_SPECS = {"f_cc": [256, 256], "f_sc": [256, 256], "f_c128": [128, 128], "f_s128": [128, 128],
                 "f_tw": [128, 64, 2], "f_cs64": [128, 64], "f_c256": [256, 256], "f_s256": [256, 256]}


def _fourier_declare(self):
    nc = self.K.nc
    for k, shp in FOURIER_SPECS.items():
        self.inp[k] = nc.dram_tensor(k, shp, F32, kind="ExternalInput")
    self.GD = Buf(nc.dram_tensor("GD", [128, 128, D], F32), "GD")


def _fourier(self, i, with_ctx, nb=64, nf=128):
    K = self.K
    j = i // 2
    K.push_scope()
    cc = K.sb([128, 2, 2, 256], BF16, "fcc")
    c128 = K.sb([128, 3, 128], BF16, "fc128")
    tw = K.sb([128, 64, 2], F32, "ftw")
    cs64 = K.sb([128, 64], F32, "fcs64")
    wf = K.sb([128, 8, D], BF16, "fwf")
    big = [K.sb([128, D], F32, f"fbig{q}") for q in range(6)]
    xts, hts, gsb = big[0:2], big[2:4], big[4:6]
    sts = [K.sb([128, 4], F32, f"fst{q}") for q in range(2)]
    hTb = [K.sb([128, 8, 128], BF16, f"fhT{q}") for q in range(2)]
    Zb = [K.sb([128, 2, D], BF16, f"fZ{q}") for q in range(2)]
    gi2 = [K.sb([128, D], F32, f"fgi{q}") for q in range(2)]
    tmp = K.sb([128, 256], F32, "ftmp")
    def ld_cast(dst_ap, dst_buf, src_ap, shape, n=[0]):
        stg = big[4 + n[0] % 2]
        n[0] += 1
        rows, cols = shape
        view = stg[0:rows, 0:cols]
        K.dma("sp", view, src_ap, writes=[stg])
        K.op("dve", lambda e: e.tensor_copy(out=dst_ap, in_=view), reads=[stg], writes=[dst_buf])
    for kt in range(2):
        ld_cast(cc[:, kt, 0, :], cc, self.inp["f_cc"][kt * 128:(kt + 1) * 128, :], (128, 256))
        ld_cast(cc[:, kt, 1, :], cc, self.inp["f_sc"][kt * 128:(kt + 1) * 128, :], (128, 256))
    ld_cast(c128[:, 0, :], c128, self.inp["f_c128"][:, :], (128, 128))
    ld_cast(c128[:, 1, :], c128, self.inp["f_s128"][:, :], (128, 128))
    K.op("dve", lambda e: e.tensor_scalar(out=c128[:, 2, :], in0=c128[:, 1, :], scalar1=-1.0, scalar2=None,
                                          op0=ALU.mult), reads=[c128], writes=[c128])
    for kt in range(8):
        ld_cast(wf[:, kt, :], wf, self.inp["w_fourier"][j, kt * 128:(kt + 1) * 128, :], (128, 1024))
    self._ld_cast = ld_cast
    K.dma("sp", tw[:], self.inp["f_tw"][:, :, :], writes=[tw])
    K.dma("sp", cs64[:], self.inp["f_cs64"][:, :], writes=[cs64])
    ntw = K.sb([128, 64], F32, "fntw")
    K.op("dve", lambda e: e.tensor_scalar(out=ntw[:], in0=tw[:, :, 1], scalar1=-1.0, scalar2=None, op0=ALU.mult),
         reads=[tw], writes=[ntw])
    latv = self.lat[0:T, :].rearrange("(a b) d -> b a d", b=64)
    lat_all = [self.lat.p(t) for t in range(64)]

    def chan_dft(xt, ht, st, hT, Z, M):
        self.norm_tile(xt, ht, M, 0, st)
        for half in range(2):
            ps = K.ps()
            for q in range(4):
                kt = half * 4 + q
                self.tr(None, ps[:, q * 128:(q + 1) * 128], ht[:, kt * 128:(kt + 1) * 128], ps, ht)
            K.op("act" if half else "dve",
                 lambda e, ps=ps, half=half: (e.copy if half else e.tensor_copy)(
                     out=hT[:, half * 4:(half + 1) * 4, :].rearrange("p a b -> p (a b)"), in_=ps[:, :]),
                 reads=[ps], writes=[hT])
        for ri in range(2):
            for hh in range(2):
                ps = K.ps()
                for gg in range(2):
                    g = hh * 2 + gg
                    for kt in range(2):
                        K.op("pe", lambda e, ps=ps, gg=gg, g=g, kt=kt, ri=ri: e.matmul(
                            ps[:, gg * 256:(gg + 1) * 256], lhsT=hT[:, g * 2 + kt, :], rhs=cc[:, kt, ri, :],
                            start=(kt == 0), stop=(kt == 1)), reads=[hT, cc], writes=[ps])
                K.op("act" if hh else "dve",
                     lambda e, ps=ps, hh=hh, ri=ri: (e.copy if hh else e.tensor_copy)(
                         out=Z[:, ri, hh * 512:(hh + 1) * 512], in_=ps[:, :]), reads=[ps], writes=[Z])

    def _body(b):
        xt, ht, st, hT, Z, gr, gi = (l[b % 2] for l in (xts, hts, sts, hTb, Zb, gsb, gi2))
        K.dma("sp", xt[:], latv[b], reads=lat_all, writes=[xt])
        chan_dft(xt, ht, st, hT, Z, self.Ml)
        for hh in range(2):
            cs_ = slice(hh * 512, (hh + 1) * 512)
            pr, pi_ = K.ps(), K.ps()
            K.op("pe", lambda e: e.matmul(pr[:, :], lhsT=c128[:, 0, :], rhs=Z[:, 0, cs_], start=True, stop=False),
                 reads=[c128, Z], writes=[pr])
            K.op("pe", lambda e: e.matmul(pr[:, :], lhsT=c128[:, 1, :], rhs=Z[:, 1, cs_], start=False, stop=True),
                 reads=[c128, Z], writes=[pr])
            K.op("pe", lambda e: e.matmul(pi_[:, :], lhsT=c128[:, 0, :], rhs=Z[:, 1, cs_], start=True, stop=False),
                 reads=[c128, Z], writes=[pi_])
            K.op("pe", lambda e: e.matmul(pi_[:, :], lhsT=c128[:, 2, :], rhs=Z[:, 0, cs_], start=False, stop=True),
                 reads=[c128, Z], writes=[pi_])
            K.op("dve", lambda e: e.tensor_scalar(out=gr[:, cs_], in0=pr[:, :], scalar1=tw[:, b, 0:1], scalar2=None,
                                                  op0=ALU.mult), reads=[pr, tw], writes=[gr])
            K.op("dve", lambda e: e.scalar_tensor_tensor(out=gr[:, cs_], in0=pi_[:, :], scalar=ntw[:, b:b + 1],
                                                         in1=gr[:, cs_], op0=ALU.mult, op1=ALU.add),
                 reads=[pi_, ntw, gr], writes=[gr])
            K.op("dve", lambda e: e.tensor_scalar(out=gi[:, cs_], in0=pi_[:, :], scalar1=tw[:, b, 0:1], scalar2=None,
                                                  op0=ALU.mult), reads=[pi_, tw], writes=[gi])
            K.op("dve", lambda e: e.scalar_tensor_tensor(out=gi[:, cs_], in0=pr[:, :], scalar=tw[:, b, 1:2],
                                                         in1=gi[:, cs_], op0=ALU.mult, op1=ALU.add),
                 reads=[pr, tw, gi], writes=[gi])
        K.dma("act", self.GD[b, :, :], gr[:], reads=[gr], writes=[self.GD])
        K.dma("act", self.GD[64 + b, :, :], gi[:], reads=[gi], writes=[self.GD])
    K.interleave(_body, range(nb), 2)
    latf = self.lat[0:T, :].rearrange("(f2 f1) d -> f1 f2 d", f1=128)
    YT = [K.sb([128, 8, 64], BF16, f"fYT{q}") for q in range(2)]
    def _body(f1):
        gd, yt, xt, ht = gsb[f1 % 2], YT[f1 % 2], xts[f1 % 2], hts[f1 % 2]
        K.dma("sp", gd[:], self.GD[:, f1, :], reads=[self.GD], writes=[gd])
        K.dma("sp", xt[0:64, :], latf[f1], reads=lat_all, writes=[xt])
        ps = K.ps()
        for kt in range(8):
            K.op("pe", lambda e, kt=kt: e.matmul(ps[:, kt * 64:(kt + 1) * 64], lhsT=gd[:, kt * 128:(kt + 1) * 128],
                                                 rhs=cs64[:, :], start=True, stop=True), reads=[gd, cs64], writes=[ps])
        K.op("act", lambda e: e.copy(out=yt[:].rearrange("p a b -> p (a b)"), in_=ps[:, :]), reads=[ps], writes=[yt])
        for hh in range(2):
            py = K.ps()
            for kt in range(8):
                K.op("pe", lambda e, kt=kt: e.matmul(py[0:64, :], lhsT=yt[:, kt, :],
                                                     rhs=wf[:, kt, hh * 512:(hh + 1) * 512], start=(kt == 0),
                                                     stop=(kt == 7)), reads=[yt, wf], writes=[py])
            K.op("dve", lambda e: e.tensor_tensor(out=ht[0:64, hh * 512:(hh + 1) * 512], in0=py[0:64, :],
                                                  in1=self.Ml[0:64, 2, hh * 512:(hh + 1) * 512], op=ALU.mult),
                 reads=[py, self.Ml], writes=[ht])
        K.op("dve", lambda e: e.tensor_tensor(out=xt[0:64, :], in0=xt[0:64, :], in1=ht[0:64, :], op=ALU.add),
             reads=[xt, ht], writes=[xt])
        K.dma("act", latf[f1], xt[0:64, :], reads=[xt], writes=lat_all)
    K.interleave(_body, range(nf), 2)
    if with_ctx:
        c256 = K.sb([128, 2, 2, 256], BF16, "fc256")
        for tt in range(2):
            ld_cast(c256[:, 0, tt, :], c256, self.inp["f_c256"][tt * 128:(tt + 1) * 128, :], (128, 256))
            ld_cast(c256[:, 1, tt, :], c256, self.inp["f_s256"][tt * 128:(tt + 1) * 128, :], (128, 256))
        ctxp = [self.lat.p(64), self.lat.p(65)]
        for tt in range(2):
            K.dma("sp", xts[tt][:], self.lat[T + tt * 128:T + (tt + 1) * 128, :], reads=ctxp, writes=[xts[tt]])
            chan_dft(xts[tt], hts[tt], sts[tt], hTb[tt], Zb[tt], self.Mc)
        for ft in range(2):
            yt = K.sb([128, 8, 128], BF16, f"fcy{ft}")
            for half in range(2):
                ps = K.ps()
                for q in range(4):
                    kt = half * 4 + q
                    n = 0
                    for tt in range(2):
                        for cs_i in range(2):
                            K.op("pe", lambda e, kt=kt, q=q, tt=tt, cs_i=cs_i, n=n: e.matmul(
                                ps[:, q * 128:(q + 1) * 128], lhsT=Zb[tt][:, cs_i, kt * 128:(kt + 1) * 128],
                                rhs=c256[:, cs_i, tt, ft * 128:(ft + 1) * 128], start=(n == 0), stop=(n == 3)),
                                reads=[Zb[tt], c256], writes=[ps])
                            n += 1
                K.op("act", lambda e, half=half: e.copy(out=yt[:, half * 4:(half + 1) * 4, :].rearrange("p a b -> p (a b)"),
                                                        in_=ps[:, :]), reads=[ps], writes=[yt])
            xt, ht = xts[ft], hts[ft]
            for hh in range(2):
                py = K.ps()
                for kt in range(8):
                    K.op("pe", lambda e, kt=kt: e.matmul(py[:, :], lhsT=yt[:, kt, :],
                                                         rhs=wf[:, kt, hh * 512:(hh + 1) * 512], start=(kt == 0),
                                                         stop=(kt == 7)), reads=[yt, wf], writes=[py])
                K.op("dve", lambda e: e.tensor_tensor(out=ht[:, hh * 512:(hh + 1) * 512], in0=py[:, :],
                                                      in1=self.Mc[:, 2, hh * 512:(hh + 1) * 512], op=ALU.mult),
                     reads=[py, self.Mc], writes=[ht])
            K.op("dve", lambda e: e.tensor_tensor(out=xt[:], in0=xt[:], in1=ht[:], op=ALU.add),
                 reads=[xt, ht], writes=[xt])
            K.dma("act", self.lat[T + ft * 128:T + (ft + 1) * 128, :], xt[:], reads=[xt], writes=ctxp)
    K.pop_scope()


Prog.fourier_declare = _fourier_declare
Prog.fourier = _fourier


POOL_WINS = (2, 4, 8, 16)


def pool_consts():
    c = {}
    for nm, L in (("f_icnt_lat", T), ("f_icnt_ctx", CT)):
        a = np.zeros((4, L), np.float32)
        pos = np.arange(L)
        for gi, win in enumerate(POOL_WINS):
            half = win // 2
            hi = np.minimum(pos + half, L)
            lo = np.maximum(pos - half, 0)
            a[gi] = 1.0 / (hi - lo)
        c[nm] = a
    return c


def _even_declare(self):
    nc = self.K.nc
    self.inp["f_icnt_lat"] = nc.dram_tensor("f_icnt_lat", [4, T], F32, kind="ExternalInput")
    self.inp["f_icnt_ctx"] = nc.dram_tensor("f_icnt_ctx", [4, CT], F32, kind="ExternalInput")
    e = {}
    e["HT"] = Buf(nc.dram_tensor("HT", [D, NT], F32), "HT")
    e["PT"] = Buf(nc.dram_tensor("PT", [2048, NT], F32), "PT")
    for d in range(2):
        for nm in ("At", "Bt", "Kt", "Rt", "Bh", "Kh"):
            e[f"{nm}{d}"] = Buf(nc.dram_tensor(f"SC_{nm}{d}", [512, NT], F32), f"{nm}{d}")
        e[f"GL{d}"] = Buf(nc.dram_tensor(f"SC_GL{d}", [512, NT // 64], F32), f"GL{d}")
        e[f"YD{d}"] = Buf(nc.dram_tensor(f"SC_YD{d}", [NT, 512], F32), f"YD{d}")
    for nm in ("VV", "GG", "BON", "YB"):
        e[nm] = Buf(nc.dram_tensor(f"SC_{nm}", [512, NT], F32), nm)
    self.ed = e


def _even_e1(self, i, ntile=66):
    K = self.K
    j = i // 2
    e = self.ed
    K.push_scope()
    win = K.sb([128, 8, 2048], BF16, "win")
    stg = [K.sb([128, D], F32, f"e1stg{q}") for q in range(2)]
    n = 0
    for kt in range(8):
        for hh in range(2):
            sg = stg[n % 2]
            n += 1
            K.dma("sp", sg[:], self.inp["w_in"][j, kt * 128:(kt + 1) * 128, hh * 1024:(hh + 1) * 1024], writes=[sg])
            K.op("dve" if n % 2 else "act",
                 lambda en, sg=sg, kt=kt, hh=hh: (en.tensor_copy if n % 2 else en.copy)(
                     out=win[:, kt, hh * 1024:(hh + 1) * 1024], in_=sg[:]), reads=[sg], writes=[win])
    xts = [K.sb([128, D], F32, f"e1x{q}") for q in range(3)]
    hts = [K.sb([128, D], F32, f"e1h{q}") for q in range(3)]
    sts = [K.sb([128, 4], F32, f"e1s{q}") for q in range(3)]
    hTf = [K.sb([128, 8, 128], F32, f"e1hTf{q}") for q in range(3)]
    hTb = [K.sb([128, 8, 128], BF16, f"e1hTb{q}") for q in range(3)]
    pts = [K.sb([128, 16, 128], F32, f"e1pt{q}") for q in range(3)]
    for tt in range(ntile):
        xt, ht, st, hf, hb, pt = (l[tt % 3] for l in (xts, hts, sts, hTf, hTb, pts))
        M = self.Ml if tt < 64 else self.Mc
        K.dma("sp", xt[:], self.lat[tt * 128:(tt + 1) * 128, :], reads=[self.lat.p(tt)], writes=[xt])
        self.norm_tile(xt, ht, M, 0, st)
        for half in range(2):
            ps = K.ps()
            for q in range(4):
                kt = half * 4 + q
                self.tr(None, ps[:, q * 128:(q + 1) * 128], ht[:, kt * 128:(kt + 1) * 128], ps, ht)
            K.op("act", lambda en, ps=ps, half=half: en.copy(
                out=hf[:, half * 4:(half + 1) * 4, :].rearrange("p a b -> p (a b)"), in_=ps[:, :]),
                reads=[ps], writes=[hf])
            K.op("dve", lambda en, half=half: en.tensor_copy(
                out=hb[:, half * 4:(half + 1) * 4, :], in_=hf[:, half * 4:(half + 1) * 4, :]),
                reads=[hf], writes=[hb])
        import os
        if not os.environ.get("SKIPHT"):
            K.dma("act", e["HT"][:, tt * 128:(tt + 1) * 128].rearrange("(kt k) t -> k kt t", k=128), hf[:],
                  reads=[hf], writes=[e["HT"]])
        for ob in range(4):
            ps = K.ps()
            for q in range(4):
                oc = ob * 4 + q
                for kt in range(8):
                    K.op("pe", lambda en, ps=ps, q=q, oc=oc, kt=kt: en.matmul(
                        ps[:, q * 128:(q + 1) * 128], lhsT=win[:, kt, oc * 128:(oc + 1) * 128], rhs=hb[:, kt, :],
                        start=(kt == 0), stop=(kt == 7)), reads=[win, hb], writes=[ps])
            K.op("act" if ob % 2 else "dve", lambda en, ps=ps, ob=ob: (en.copy if ob % 2 else en.tensor_copy)(
                out=pt[:, ob * 4:(ob + 1) * 4, :].rearrange("p a b -> p (a b)"), in_=ps[:, :]),
                reads=[ps], writes=[pt])
        if not os.environ.get("SKIPPT"):
            K.dma("act", e["PT"][:, tt * 128:(tt + 1) * 128].rearrange("(oc k) t -> k oc t", k=128), pt[:],
                  reads=[pt], writes=[e["PT"]])
    K.pop_scope()


def _even_e2(self, i, dbg=None):
    K = self.K
    j = i // 2
    e = self.ed
    K.push_scope()
    WB = 512
    WT = WB + 128
    eng_rr = [0]

    def ve():
        eng_rr[0] += 1
        return "dve"

    stg = K.sb([128, 8, 128], F32, "e2stg")
    W1 = K.sb([128, 3, 8, 128], BF16, "e2W1")
    W2 = K.sb([128, 3, 512], BF16, "e2W2")
    pw = K.sb([128, 4, 128], BF16, "e2pw")
    for v, (nm, nd) in enumerate((("decay_w1", 2), ("lr_a1", 2), ("gate_g1", 1))):
        for d in range(nd):
            src = self.inp[nm][j, d] if nd == 2 else self.inp[nm][j]
            wcol = 64 if nd == 2 else 128
            K.dma("sp", stg[:, :, 0:wcol], src.rearrange("(kt k) r -> k kt r", k=128), writes=[stg])
            K.op("dve", lambda en, v=v, d=d, wcol=wcol: en.tensor_copy(out=W1[:, v, :, d * wcol:(d + 1) * wcol],
                                                                       in_=stg[:, :, 0:wcol]), reads=[stg], writes=[W1])
    stg2 = stg[:].rearrange("p a b -> p (a b)")
    for v, nm in enumerate(("decay_w2", "lr_a2")):
        for d in range(2):
            K.dma("sp", stg2[d * 64:(d + 1) * 64, 0:512], self.inp[nm][j, d], writes=[stg])
        K.op("dve", lambda en, v=v: en.tensor_copy(out=W2[:, v, :], in_=stg2[:, 0:512]), reads=[stg], writes=[W2])
    K.dma("sp", stg2[:, 0:512], self.inp["gate_g2"][j], writes=[stg])
    K.op("dve", lambda en: en.tensor_copy(out=W2[:, 2, :], in_=stg2[:, 0:512]), reads=[stg], writes=[W2])
    for gi in range(4):
        K.dma("sp", stg2[:, gi * 128:(gi + 1) * 128], self.inp["pool_w"][j, gi], writes=[stg])
    K.op("dve", lambda en: en.tensor_copy(out=pw[:].rearrange("p a b -> p (a b)"), in_=stg2[:, 0:512]),
         reads=[stg], writes=[pw])
    MU = K.sb([128, 2, 3, 8], F32, "e2MU")
    MUP = K.sb([128, 2, 3, 4], F32, "e2MUP")
    COL = K.sb([128, 12, 4], F32, "e2COL")
    K.dma("sp", MU[:, 0, :, :], self.inp["mu_x"][j].rearrange("v (kt k) -> k v kt", k=128), writes=[MU],
          allow_slow_non_contiguous=True)
    K.dma("sp", MUP[:, 0, :, :], self.inp["mu_p"][j].rearrange("v (c k) -> k v c", k=128), writes=[MUP],
          allow_slow_non_contiguous=True)
    for d in range(2):
        K.dma("sp", COL[:, d, :], self.inp["decay_w0"][j, d].rearrange("(c k) -> k c", k=128), writes=[COL],
              allow_slow_non_contiguous=True)
        K.dma("sp", COL[:, 2 + d, :], self.inp["lr_a0"][j, d].rearrange("(c k) -> k c", k=128), writes=[COL],
              allow_slow_non_contiguous=True)
    K.dma("sp", COL[:, 4, :], self.inp["k_k"][j].rearrange("(c k) -> k c", k=128), writes=[COL], allow_slow_non_contiguous=True)
    K.dma("sp", COL[:, 5, :], self.inp["k_a"][j].rearrange("(c k) -> k c", k=128), writes=[COL], allow_slow_non_contiguous=True)
    K.dma("sp", COL[:, 6, :], self.inp["r_k"][j].rearrange("(c h2) k -> (h2 k) c", h2=2), writes=[COL],
          allow_slow_non_contiguous=True)
    K.dma("sp", COL[:, 7, :], self.inp["pool_scale"][j].rearrange("(c k) -> k c", k=128), writes=[COL],
          allow_slow_non_contiguous=True)
    for Mx in (MU, MUP):
        K.op("dve", lambda en, Mx=Mx: en.tensor_scalar(out=Mx[:, 1], in0=Mx[:, 0], scalar1=-1.0, scalar2=1.0,
                                                       op0=ALU.mult, op1=ALU.add), reads=[Mx], writes=[Mx])
    bones = K.sb([128, 128], F32, "e2bones")
    K.op("pool", lambda en: en.memset(bones[:], 0.0), writes=[bones])
    K.op("pool", lambda en: en.memset(bones[0:64, 0:64], 1.0), reads=[bones], writes=[bones])
    K.op("pool", lambda en: en.memset(bones[64:128, 64:128], 1.0), reads=[bones], writes=[bones])
    HTt = K.sb([128, 8, WT], F32, "e2HTt")
    xv = K.sb([128, 8, WB], BF16, "e2xv")
    t1 = [K.sb([128, WB], BF16, f"e2t1{v}") for v in range(3)]
    PTt = [K.sb([128, WT], F32, f"e2PTt{n}") for n in range(4)]
    nm_w = ("Rm", "Km", "Vm", "kk", "sq", "rn", "LW0", "LW1", "AD0", "AD1", "Gm", "kd0", "kd1", "b0", "b1", "lw0", "lw1",
            "cum0", "cum1", "E0", "E1", "tmp0", "tmp1", "tmp", "ks", "IC", "o0", "o1", "o2", "o3", "o4", "o5")
    w = {n: K.sb([128, WB], F32, "e2" + n) for n in nm_w}
    sw = [K.sb([128, WT], F32, f"e2s{q}") for q in range(2)]
    dfb = K.sb([128, WB], BF16, "e2dfb")
    gls = [K.sb([128, 8], F32, f"e2gl{d}") for d in range(2)]
    RM = K.sb([128, WB], F32, "e2RM")
    K.op("pool", lambda en: en.memset(RM[:], 1.0), writes=[RM])
    K.op("pool", lambda en: en.memset(RM[:].rearrange("p (r c) -> p r c", c=64)[:, :, 0:1], 0.0), reads=[RM], writes=[RM])
    orr = [0]

    def otile():
        orr[0] += 1
        return w[f"o{orr[0] % 6}"]

    GRID_H = [(-1, True)] * 2 + [(1, True)] * 2 + [(-64, False)] * 2 + [(64, False)] * 2
    GRID_P = [(-1, True), (1, True), (-64, False), (64, False)]
    SEQ_H = [(-1, False)] * 4 + [(1, False)] * 4
    SEQ_P = [(-1, False)] * 2 + [(1, False)] * 2
    blocks = [(T, CT, T, CT, SEQ_H, SEQ_P, "f_icnt_ctx")] + \
             [(t0, WB, 0, T, GRID_H, GRID_P, "f_icnt_lat") for t0 in range(0, T, WB)]

    def load_halo(buf, view, src_rows, t0, Wb, s0, sl):
        lo = max(t0 - 64, s0)
        hi = min(t0 + Wb + 64, s0 + sl)
        if lo > t0 - 64:
            K.op("pool", lambda en: en.memset(view(0, 64), 0.0), writes=[buf])
        if hi < t0 + Wb + 64:
            K.op("pool", lambda en: en.memset(view(64 + Wb, 128 + Wb), 0.0), writes=[buf])
        K.dma("sp", view(lo - (t0 - 64), hi - (t0 - 64)), src_rows(lo, hi), reads=[e["HT"], e["PT"]], writes=[buf])

    def mix(out_ap, out_buf, srcf, src_buf, mu_ap, omu_ap, delta, rowmask, Wb):
        en1 = "dve"
        K.op("act", lambda en: en.mul(out=out_ap, in_=srcf(64, 64 + Wb), mul=omu_ap), reads=[src_buf], writes=[out_buf])
        if not rowmask:
            o, s_ = out_ap, srcf(64 + delta, 64 + delta + Wb)
        else:
            ov = out_ap.rearrange("p (r c) -> p r c", c=64)
            sv = srcf(64 + delta, 64 + delta + Wb).rearrange("p (r c) -> p r c", c=64)
            if delta == -1:
                o, s_ = ov[:, :, 1:64], sv[:, :, 1:64]
            else:
                o, s_ = ov[:, :, 0:63], sv[:, :, 0:63]
        K.op(en1, lambda en: en.scalar_tensor_tensor(out=o, in0=s_, scalar=mu_ap, in1=o, op0=ALU.mult, op1=ALU.add),
             reads=[src_buf, out_buf], writes=[out_buf])

    stq = [0]

    def store(dst, c, t0, Wb, tile):
        stq[0] += 1
        K.dma("act" if stq[0] % 2 else "sp", dst[c * 128:(c + 1) * 128, t0:t0 + Wb], tile[:, 0:Wb], reads=[tile],
              writes=[dst])

    for (t0, Wb, s0, sl, HS, PS, icn) in blocks:
        R = Wb // 64
        load_halo(HTt, lambda a, b: HTt[:, :, a:b],
                  lambda a, b: e["HT"][:, a:b].rearrange("(kt k) t -> k kt t", k=128), t0, Wb, s0, sl)
        for v in range(3):
            for kt in range(8):
                dl, rm = HS[kt]
                mix(xv[:, kt, 0:Wb], xv, lambda a, b, kt=kt: HTt[:, kt, a:b], HTt, MU[:, 0, v, kt:kt + 1],
                    MU[:, 1, v, kt:kt + 1], dl, rm, Wb)
            ps = K.ps()
            for kt in range(8):
                K.op("pe", lambda en, kt=kt, v=v: en.matmul(ps[:, 0:Wb], lhsT=W1[:, v, kt, :], rhs=xv[:, kt, 0:Wb],
                                                            start=(kt == 0), stop=(kt == 7)), reads=[W1, xv], writes=[ps])
            fn = (AF.Tanh, AF.Copy, AF.Sigmoid)[v]
            K.op("act", lambda en, v=v, fn=fn: en.activation(out=t1[v][:, 0:Wb], in_=ps[:, 0:Wb], func=fn),
                 reads=[ps], writes=[t1[v]])
        for c in range(4):
            cs_ = slice(c * 128, (c + 1) * 128)
            for n in range(4):
                load_halo(PTt[n], lambda a, b, n=n: PTt[n][:, a:b],
                          lambda a, b, n=n: e["PT"][n * 512 + c * 128:n * 512 + (c + 1) * 128, a:b], t0, Wb, s0, sl)
            dl, rm = PS[c]
            for n, nm in enumerate(("Rm", "Km", "Vm")):
                mix(w[nm][:, 0:Wb], w[nm], lambda a, b, n=n: PTt[n][:, a:b], PTt[n], MUP[:, 0, n, c:c + 1],
                    MUP[:, 1, n, c:c + 1], dl, rm, Wb)
            store(e["VV"], c, t0, Wb, w["Vm"])
            for d in range(2):
                ps = K.ps()
                K.op("pe", lambda en, d=d: en.matmul(ps[:, 0:Wb], lhsT=W2[d * 64:(d + 1) * 64, 0, cs_],
                                                     rhs=t1[0][d * 64:(d + 1) * 64, 0:Wb], start=True, stop=True),
                     reads=[W2, t1[0]], writes=[ps])
                K.op("act", lambda en, d=d: en.activation(out=w[f"LW{d}"][:, 0:Wb], in_=ps[:, 0:Wb], func=AF.Sigmoid,
                                                          bias=COL[:, d, c:c + 1], scale=1.0),
                     reads=[ps, COL], writes=[w[f"LW{d}"]])
                ps = K.ps()
                K.op("pe", lambda en, d=d: en.matmul(ps[:, 0:Wb], lhsT=W2[d * 64:(d + 1) * 64, 1, cs_],
                                                     rhs=t1[1][d * 64:(d + 1) * 64, 0:Wb], start=True, stop=True),
                     reads=[W2, t1[1]], writes=[ps])
                K.op("act", lambda en, d=d: en.activation(out=w[f"AD{d}"][:, 0:Wb], in_=ps[:, 0:Wb], func=AF.Sigmoid,
                                                          bias=COL[:, 2 + d, c:c + 1], scale=1.0),
                     reads=[ps, COL], writes=[w[f"AD{d}"]])
            ps = K.ps()
            K.op("pe", lambda en: en.matmul(ps[:, 0:Wb], lhsT=W2[:, 2, cs_], rhs=t1[2][:, 0:Wb], start=True, stop=True),
                 reads=[W2, t1[2]], writes=[ps])
            K.op("act", lambda en: en.copy(out=w["Gm"][:, 0:Wb], in_=ps[:, 0:Wb]), reads=[ps], writes=[w["Gm"]])
            store(e["GG"], c, t0, Wb, w["Gm"])
            K.op("dve", lambda en: en.tensor_scalar(out=w["kk"][:, 0:Wb], in0=w["Km"][:, 0:Wb], scalar1=COL[:, 4, c:c + 1],
                                                    scalar2=None, op0=ALU.mult), reads=[w["Km"], COL], writes=[w["kk"]])
            K.op("act", lambda en: en.square(out=w["sq"][:, 0:Wb], in_=w["kk"][:, 0:Wb]), reads=[w["kk"]], writes=[w["sq"]])
            ps = K.ps()
            K.op("pe", lambda en: en.matmul(ps[:, 0:Wb], lhsT=bones[:], rhs=w["sq"][:, 0:Wb], start=True, stop=True),
                 reads=[bones, w["sq"]], writes=[ps])
            K.op("dve", lambda en: en.tensor_scalar(out=w["rn"][:, 0:Wb], in0=ps[:, 0:Wb], scalar1=1e-12, scalar2=None,
                                                    op0=ALU.max), reads=[ps], writes=[w["rn"]])
            K.op("act", lambda en: en.sqrt(out=w["rn"][:, 0:Wb], in_=w["rn"][:, 0:Wb]), reads=[w["rn"]], writes=[w["rn"]])
            K.op("dve", lambda en: en.reciprocal(out=w["rn"][:, 0:Wb], in_=w["rn"][:, 0:Wb]), reads=[w["rn"]],
                 writes=[w["rn"]])
            K.op("dve", lambda en: en.tensor_tensor(out=w["kk"][:, 0:Wb], in0=w["kk"][:, 0:Wb], in1=w["rn"][:, 0:Wb],
                                                    op=ALU.mult), reads=[w["kk"], w["rn"]], writes=[w["kk"]])
            def dchain(d):
                AD, LW = w[f"AD{d}"], w[f"LW{d}"]
                tmp, kd, bb, lw, cum, E = (w[f"{nm_}{d}"] for nm_ in ("tmp", "kd", "b", "lw", "cum", "E"))
                K.op("dve", lambda en: en.tensor_scalar(out=tmp[:, 0:Wb], in0=AD[:, 0:Wb], scalar1=-1.0,
                                                        scalar2=COL[:, 5, c:c + 1], op0=ALU.add, op1=ALU.mult),
                     reads=[AD, COL], writes=[tmp])
                K.op("dve", lambda en: en.tensor_tensor(out=bb[:, 0:Wb], in0=w["kk"][:, 0:Wb], in1=AD[:, 0:Wb],
                                                         op=ALU.mult), reads=[w["kk"], AD], writes=[bb])
                K.op("act", lambda en: en.mul(out=lw[:, 0:Wb], in_=LW[:, 0:Wb], mul=-0.6065306597126334),
                     reads=[LW], writes=[lw])
                yield
                K.op("dve", lambda en: en.scalar_tensor_tensor(out=kd[:, 0:Wb], in0=tmp[:, 0:Wb], scalar=1.0,
                                                               in1=w["Km"][:, 0:Wb], op0=ALU.add, op1=ALU.mult),
                     reads=[tmp, w["Km"]], writes=[kd])
                yield
                K.op("dve", lambda en: en.tensor_tensor_scan(out=cum[:, 0:Wb], data0=RM[:, 0:Wb], data1=lw[:, 0:Wb],
                                                             initial=0.0, op0=ALU.mult, op1=ALU.add),
                     reads=[RM, lw], writes=[cum])
                yield
                cumv = cum[:, 0:Wb].rearrange("p (r c) -> p r c", c=64)
                if d == 1:
                    K.op("dve", lambda en: en.tensor_tensor(out=tmp[:, 0:Wb], in0=lw[:, 0:Wb], in1=cum[:, 0:Wb],
                                                             op=ALU.subtract), reads=[lw, cum], writes=[tmp])
                    yield
                    tv0 = tmp[:, 0:Wb].rearrange("p (r c) -> p r c", c=64)
                    K.op("dve", lambda en: en.tensor_tensor(out=E[:, 0:Wb].rearrange("p (r c) -> p r c", c=64), in0=tv0,
                                                            in1=cumv[:, :, 63].unsqueeze(2).to_broadcast([128, R, 64]),
                                                            op=ALU.add), reads=[tmp, cum], writes=[E])
                    yield
                    K.op("dve", lambda en: en.tensor_copy(out=cum[:, 0:Wb], in_=E[:, 0:Wb]), reads=[E], writes=[cum])
                    yield
                last = 63 if d == 0 else 0
                cL = cumv[:, :, last]
                gl = gls[d]
                K.op("act", lambda en: en.activation(out=gl[:, 0:R], in_=cL, func=AF.Exp), reads=[cum], writes=[gl])
                K.dma("sp", e[f"GL{d}"][c * 128:(c + 1) * 128, t0 // 64:t0 // 64 + R], gl[:, 0:R], reads=[gl],
                      writes=[e[f"GL{d}"]])
                K.op("act", lambda en: en.activation(out=E[:, 0:Wb], in_=cum[:, 0:Wb], func=AF.Exp), reads=[cum],
                     writes=[E])
                K.op("dve", lambda en: en.tensor_tensor(out=tmp[:, 0:Wb], in0=cum[:, 0:Wb], in1=lw[:, 0:Wb],
                                                         op=ALU.subtract), reads=[cum, lw], writes=[tmp])
                yield
                o = otile()
                K.op("dve", lambda en: en.tensor_tensor(out=o[:, 0:Wb], in0=w["Rm"][:, 0:Wb], in1=E[:, 0:Wb],
                                                        op=ALU.mult), reads=[w["Rm"], E], writes=[o])
                store(e[f"Rt{d}"], c, t0, Wb, o)
                yield
                K.op("act", lambda en: en.activation(out=E[:, 0:Wb], in_=cum[:, 0:Wb], func=AF.Exp, scale=-1.0),
                     reads=[cum], writes=[E])
                yield
                for src_, dn in ((bb, "Bt"), (kd, "Kt")):
                    o = otile()
                    K.op(ve(), lambda en, o=o, src_=src_: en.tensor_tensor(out=o[:, 0:Wb], in0=src_[:, 0:Wb],
                                                                           in1=E[:, 0:Wb], op=ALU.mult),
                         reads=[src_, E], writes=[o])
                    store(e[f"{dn}{d}"], c, t0, Wb, o)
                yield
                K.op("act", lambda en: en.activation(out=E[:, 0:Wb], in_=tmp[:, 0:Wb], func=AF.Exp),
                     reads=[tmp], writes=[E])
                yield
                o = otile()
                K.op("dve", lambda en: en.scalar_tensor_tensor(out=o[:, 0:Wb], in0=w["kk"][:, 0:Wb], scalar=-1.0,
                                                               in1=E[:, 0:Wb], op0=ALU.mult, op1=ALU.mult),
                     reads=[w["kk"], E], writes=[o])
                store(e[f"At{d}"], c, t0, Wb, o)
                tv = tmp[:, 0:Wb].rearrange("p (r c) -> p r c", c=64)
                K.op("dve", lambda en: en.tensor_tensor(out=tv, in0=cumv, in1=cL.unsqueeze(2).to_broadcast([128, R, 64]),
                                                        op=ALU.subtract), reads=[cum], writes=[tmp])
                yield
                K.op("act", lambda en: en.activation(out=E[:, 0:Wb], in_=tmp[:, 0:Wb], func=AF.Exp, scale=-1.0),
                     reads=[tmp], writes=[E])
                yield
                for src_, dn in ((bb, "Bh"), (kd, "Kh")):
                    o = otile()
                    K.op(ve(), lambda en, o=o, src_=src_: en.tensor_tensor(out=o[:, 0:Wb], in0=src_[:, 0:Wb],
                                                                           in1=E[:, 0:Wb], op=ALU.mult),
                         reads=[src_, E], writes=[o])
                    store(e[f"{dn}{d}"], c, t0, Wb, o)

            gens = [dchain(0), dchain(1)]
            while gens:
                alive = []
                for g_ in gens:
                    try:
                        next(g_)
                        alive.append(g_)
                    except StopIteration:
                        pass
                gens = alive
            K.op("dve", lambda en: en.tensor_tensor(out=w["ks"][:, 0:Wb], in0=w["kd0"][:, 0:Wb], in1=w["kd1"][:, 0:Wb],
                                                     op=ALU.add), reads=[w["kd0"], w["kd1"]], writes=[w["ks"]])
            K.op("dve", lambda en: en.scalar_tensor_tensor(out=w["tmp"][:, 0:Wb], in0=w["ks"][:, 0:Wb],
                                                           scalar=COL[:, 6, c:c + 1], in1=w["Rm"][:, 0:Wb],
                                                           op0=ALU.mult, op1=ALU.mult),
                 reads=[w["ks"], COL, w["Rm"]], writes=[w["tmp"]])
            ps = K.ps()
            K.op("pe", lambda en: en.matmul(ps[:, 0:Wb], lhsT=bones[:], rhs=w["tmp"][:, 0:Wb], start=True, stop=True),
                 reads=[bones, w["tmp"]], writes=[ps])
            o = otile()
            K.op("dve", lambda en: en.tensor_tensor(out=o[:, 0:Wb], in0=ps[:, 0:Wb], in1=w["Vm"][:, 0:Wb], op=ALU.mult),
                 reads=[ps, w["Vm"]], writes=[o])
            store(e["BON"], c, t0, Wb, o)
            u = PTt[3]
            Wt = Wb + 128
            K.dma("sp", w["IC"][:, 0:Wb], self.inp[icn][c:c + 1, t0 - s0:t0 - s0 + Wb].partition_broadcast(128),
                  writes=[w["IC"]])
            s_a, s_b = sw
            K.op("dve", lambda en: en.tensor_tensor(out=s_a[:, 1:Wt], in0=u[:, 0:Wt - 1], in1=u[:, 1:Wt], op=ALU.add),
                 reads=[u], writes=[s_a])
            cur_s, oth = s_a, s_b
            lo_, hi_ = 1, Wt
            for hs in (1, 2, 4):
                if POOL_WINS[c] < 4 * hs:
                    break
                nlo, nhi = lo_ + hs, hi_ - hs
                K.op("dve", lambda en, cur_s=cur_s, oth=oth, nlo=nlo, nhi=nhi, hs=hs: en.tensor_tensor(
                    out=oth[:, nlo:nhi], in0=cur_s[:, nlo - hs:nhi - hs], in1=cur_s[:, nlo + hs:nhi + hs], op=ALU.add),
                    reads=[cur_s], writes=[oth])
                cur_s, oth = oth, cur_s
                lo_, hi_ = nlo, nhi
            K.op("dve", lambda en: en.tensor_tensor(out=w["tmp"][:, 0:Wb], in0=cur_s[:, 64:64 + Wb], in1=w["IC"][:, 0:Wb],
                                                    op=ALU.mult), reads=[cur_s, w["IC"]], writes=[w["tmp"]])
            K.op("dve", lambda en: en.tensor_tensor(out=dfb[:, 0:Wb], in0=w["tmp"][:, 0:Wb], in1=u[:, 64:64 + Wb],
                                                     op=ALU.subtract), reads=[w["tmp"], u], writes=[dfb])
            ps = K.ps()
            K.op("pe", lambda en: en.matmul(ps[:, 0:Wb], lhsT=pw[:, c, :], rhs=dfb[:, 0:Wb], start=True, stop=True),
                 reads=[pw, dfb], writes=[ps])
            o = otile()
            K.op("dve", lambda en: en.tensor_scalar(out=o[:, 0:Wb], in0=ps[:, 0:Wb], scalar1=COL[:, 7, c:c + 1],
                                                    scalar2=None, op0=ALU.mult), reads=[ps, COL], writes=[o])
            store(e["YB"], c, t0, Wb, o)
    K.pop_scope()


Prog.even_e2 = _even_e2
def _even_e3(self, i, SDT=None, nsteps=66):
    SDT = SDT or self.SCAN_DT
    K = self.K
    e = self.ed
    K.push_scope()
    TEN = ("At", "Bt", "Kt", "Rt", "Bh", "Kh", "VV")

    def ring(nm, shape, n, dt=F32):
        bufs = [K.sb(shape, dt, f"e3{nm}{q}") for q in range(n)]
        cnt = [0]

        def nxt():
            cnt[0] += 1
            return bufs[cnt[0] % n]
        return nxt

    def mk_mask(nm, cmp_op, sgn=1):
        mbuf = K.sb([128, 128], F32, "e3m" + nm)
        K.op("pool", lambda en: en.memset(mbuf[:], 1.0), writes=[mbuf])
        K.op("pool", lambda en: en.affine_select(out=mbuf[:], in_=mbuf[:], pattern=[[sgn, 128]], compare_op=cmp_op,
                                                 fill=0.0, base=0, channel_multiplier=-sgn), reads=[mbuf], writes=[mbuf])
        K.op("pool", lambda en: en.memset(mbuf[0:64, 64:128], 0.0), reads=[mbuf], writes=[mbuf])
        K.op("pool", lambda en: en.memset(mbuf[64:128, 0:64], 0.0), reads=[mbuf], writes=[mbuf])
        return mbuf
    UPs = mk_mask("ups", ALU.is_gt)
    UPi = mk_mask("upi", ALU.is_ge)
    LOs = mk_mask("los", ALU.is_gt, -1)
    LOi = mk_mask("loi", ALU.is_ge, -1)
    BDM = mk_mask("bdm", ALU.is_ge)
    K.op("pool", lambda en: en.memset(BDM[0:64, 0:64], 1.0), reads=[BDM], writes=[BDM])
    K.op("pool", lambda en: en.memset(BDM[64:128, 64:128], 1.0), reads=[BDM], writes=[BDM])
    M4 = []
    MI = []
    for d in range(2):
        strictT, strict, inclT = (UPs, LOs, UPi) if d == 0 else (LOs, UPs, LOi)
        m4 = K.sb([128, 512], F32, f"e3m4{d}")
        for q, src in enumerate((strictT, strict, strictT, inclT)):
            K.op("dve", lambda en, q=q, src=src: en.tensor_copy(out=m4[:, q * 128:(q + 1) * 128], in_=src[:]),
                 reads=[src], writes=[m4])
        M4.append(m4)
        MI.append(inclT)
    identS = self.ident
    import os
    if os.environ.get("E3PAD"):
        _pad = K.sb([128, 128], F32, "e3pad")
    S = [[K.sb([128, 128], F32, f"e3S{d}{c}") for c in range(4)] for d in range(2)]
    for d in range(2):
        for c in range(4):
            K.op("pool", lambda en, d=d, c=c: en.memset(S[d][c][:], 0.0), writes=[S[d][c]])
    Ssd = S
    if SDT != F32:
        Ssd = [[K.sb([128, 128], SDT, f"e3Sb{d}{c}") for c in range(4)] for d in range(2)]
        for d in range(2):
            for c in range(4):
                K.op("pool", lambda en, d=d, c=c: en.memset(Ssd[d][c][:], 0.0), writes=[Ssd[d][c]])
    LD = [[{nm: K.sb([128, 4, 64], F32, f"e3ld{d}{b}{nm}") for nm in TEN} for b in range(2)] for d in range(2)]
    GLall = [K.sb([128, 4, NT // 64], F32, f"e3glall{d}") for d in range(2)]
    for d in range(2):
        K.dma("sp", GLall[d][:], e[f"GL{d}"][:, :].rearrange("(c p) t -> p c t", p=128), reads=[e[f"GL{d}"]],
              writes=[GLall[d]])
    bd_bufs = [K.sb([128, 7, 128], SDT, f"e3bdz{q}") for q in range(9)]
    for bq in bd_bufs:
        K.op("pool", lambda en, bq=bq: en.memset(bq[:], 0.0), writes=[bq])
    bd_cnt = [0]

    def r_bd():
        bd_cnt[0] += 1
        return bd_bufs[bd_cnt[0] % 9]
    r_tp = ring("tp", [128, 384], 9, SDT)
    r_m = ring("m", [128, 640], 9, SDT)
    r_q = ring("q", [128, 256], 16, SDT)
    r_w = ring("w", [128, 128], 16, SDT)
    r_x = ring("x", [128, 128], 9, SDT)
    r_u = ring("u", [128, 128], 9, SDT)
    r_y = ring("y", [128, 4, 64], 4, F32)
    rr = [0]
    NCH = 2 * nsteps

    def ve3():
        rr[0] += 1
        return "dve"

    def tok_base(d, n):
        if d == 0:
            return T + 64 * n if n < 4 else 64 * (n - 4)
        return T + 64 * (3 - n) if n < 4 else 64 * (127 - (n - 4))

    def load(d, n):
        b = n % 2
        tb = tok_base(d, n)
        for nm in TEN:
            src = e[nm if nm == "VV" else f"{nm}{d}"]
            K.dma("sp", LD[d][b][nm][:], src[:, tb:tb + 64].rearrange("(c p) t -> p c t", p=128), reads=[src],
                  writes=[LD[d][b][nm]])

    def mm(ps_ap, ps, lhsT, lb, rhs, rb, start=True, stop=True):
        K.op("pe", lambda en: en.matmul(ps_ap, lhsT=lhsT, rhs=rhs, start=start, stop=stop), reads=[lb, rb], writes=[ps])

    def unit(d, n, c, ysb):
        b = n % 2
        ld = LD[d][b]
        bd = r_bd()
        for ti, nm in enumerate(TEN):
            src = ld[nm][:, c, :]
            if ti < 4:
                K.op("dve", lambda en, ti=ti, src=src: en.tensor_tensor(
                    out=bd[:, ti, :].rearrange("p (a b) -> p a b", a=2), in0=src.unsqueeze(1).to_broadcast([128, 2, 64]),
                    in1=BDM[:].rearrange("p (a b) -> p a b", a=2), op=ALU.mult), reads=[ld[nm], BDM], writes=[bd])
            else:
                for h2 in range(2):
                    K.op("act", lambda en, ti=ti, h2=h2: en.copy(
                        out=bd[h2 * 64:(h2 + 1) * 64, ti, h2 * 64:(h2 + 1) * 64], in_=ld[nm][h2 * 64:(h2 + 1) * 64, c, :]),
                        reads=[ld[nm]], writes=[bd])
        yield
        A_, B_, K_, R_ = (bd[:, t_, :] for t_ in range(4))
        pst = K.ps()
        idm = identS if SDT == F32 else self.identb
        for t_ in range(3):
            mm(pst[:, t_ * 128:(t_ + 1) * 128], pst, bd[:, 4 + t_, :], bd, idm[:], idm)
        tp = r_tp()
        K.op("act", lambda en: en.copy(out=tp[:], in_=pst[:, 0:384]), reads=[pst], writes=[tp])
        BhT, KhT, VT = (tp[:, t_ * 128:(t_ + 1) * 128] for t_ in range(3))
        p1 = K.ps()
        mm(p1[:, 0:128], p1, B_, bd, A_, bd)
        mm(p1[:, 128:256], p1, A_, bd, B_, bd)
        mm(p1[:, 256:384], p1, K_, bd, A_, bd)
        mm(p1[:, 384:512], p1, B_, bd, R_, bd)
        p2 = K.ps()
        mm(p2[:, 0:128], p2, K_, bd, R_, bd)
        mt = r_m()
        K.op("dve", lambda en: en.tensor_tensor(out=mt[:, 0:512], in0=p1[:, :], in1=M4[d][:], op=ALU.mult),
             reads=[p1, M4[d]], writes=[mt])
        K.op("dve", lambda en: en.tensor_tensor(out=mt[:, 512:640], in0=p2[:, 0:128], in1=MI[d][:], op=ALU.mult),
             reads=[p2, MI[d]], writes=[mt])
        Q, QT, MakT, MrbT, MrkT = (mt[:, t_ * 128:(t_ + 1) * 128] for t_ in range(5))
        W = r_w()
        K.op("dve", lambda en: en.tensor_tensor(out=W[:], in0=Q, in1=identS[:], op=ALU.add),
             reads=[mt, identS], writes=[W])
        yield
        qb = mt
        for lvl in range(1, 6):
            pq = K.ps()
            mm(pq[:, 128:256], pq, Q, qb, QT, qb)
            if lvl < 5:
                mm(pq[:, 0:128], pq, QT, qb, Q, qb)
            nq = r_q()
            if lvl < 5:
                K.op("act", lambda en: en.copy(out=nq[:], in_=pq[:, 0:256]), reads=[pq], writes=[nq])
            else:
                K.op("act", lambda en: en.copy(out=nq[:, 128:256], in_=pq[:, 128:256]), reads=[pq], writes=[nq])
            Q, QT, qb = nq[:, 0:128], nq[:, 128:256], nq
            yield
            pw_ = K.ps()
            mm(pw_[:, 0:128], pw_, QT, qb, W[:], W)
            W2_ = r_w()
            K.op("dve", lambda en: en.tensor_tensor(out=W2_[:], in0=pw_[:, 0:128], in1=W[:], op=ALU.add),
                 reads=[pw_, W], writes=[W2_])
            W = W2_
            yield
        Sb = S[d][c]
        Sm = Ssd[d][c]
        px = K.ps()
        mm(px[:, 0:128], px, A_, bd, Sm[:], Sm, True, False)
        mm(px[:, 0:128], px, MakT, mt, VT, tp, False, True)
        Xs = r_x()
        K.op("act", lambda en: en.copy(out=Xs[:], in_=px[:, 0:128]), reads=[px], writes=[Xs])
        yield
        pu = K.ps()
        mm(pu[:, 0:128], pu, W[:], W, Xs[:], Xs)
        Us = r_u()
        K.op("dve", lambda en: en.tensor_copy(out=Us[:], in_=pu[:, 0:128]), reads=[pu], writes=[Us])
        if self.dbg.get("e3dbg") is not None and d == 0 and c == 0 and n in (0, 1):
            dd = self.dbg["e3dbg"]
            K.dma("sp", dd[n, 0, :, 0:128], Xs[:], reads=[Xs])
            K.dma("sp", dd[n, 1, :, 0:128], Us[:], reads=[Us])
            K.dma("sp", dd[n, 2, :, 0:128], W[:], reads=[W])
            K.dma("sp", dd[n, 3, :, 0:640], mt[:], reads=[mt])
            K.dma("sp", dd[n, 4, :, 0:128], Sb[:], reads=[Sb])
            K.dma("sp", dd[n, 5, :, 0:384], tp[:], reads=[tp])
            for t_ in range(4):
                K.dma("sp", dd[n, 6, :, t_ * 128:(t_ + 1) * 128], bd[:, t_, :], reads=[bd])
        yield
        py = K.ps()
        mm(py[:, 0:128], py, R_, bd, Sm[:], Sm, True, False)
        mm(py[:, 0:128], py, MrbT, mt, Us[:], Us, False, False)
        mm(py[:, 0:128], py, MrkT, mt, VT, tp, False, True)
        pss = K.ps()
        mm(pss[:, 0:128], pss, BhT, tp, Us[:], Us, True, False)
        mm(pss[:, 0:128], pss, KhT, tp, VT, tp, False, True)
        K.op("act", lambda en: en.copy(out=ysb[0:64, c, :], in_=py[0:64, 0:64]), reads=[py], writes=[ysb])
        K.op("act", lambda en: en.copy(out=ysb[64:128, c, :], in_=py[64:128, 64:128]), reads=[py], writes=[ysb])
        gcol = tok_base(d, n) // 64
        K.op("dve", lambda en: en.scalar_tensor_tensor(out=Sb[:], in0=Sb[:], scalar=GLall[d][:, c, gcol:gcol + 1],
                                                       in1=pss[:, 0:128], op0=ALU.mult, op1=ALU.add),
             reads=[Sb, GLall[d], pss], writes=[Sb])
        if SDT != F32:
            K.op("dve", lambda en: en.tensor_copy(out=Sm[:], in_=Sb[:]), reads=[Sb], writes=[Sm])

    for d in range(2):
        load(d, 0)
    for n in range(NCH):
        for d in range(2):
            if n + 1 < NCH:
                load(d, n + 1)
        ysbs = [r_y(), r_y()]
        gens = [unit(d, n, c, ysbs[d]) for c in range(4) for d in range(2)]
        while gens:
            alive = []
            for g in gens:
                try:
                    next(g)
                    alive.append(g)
                except StopIteration:
                    pass
            gens = alive
        for d in range(2):
            tb = tok_base(d, n)
            dst = e[f"YD{d}"][tb:tb + 64, :].rearrange("t (c h v) -> h t c v", h=2, v=64)
            for h2 in range(2):
                K.dma("sp", dst[h2], ysbs[d][h2 * 64:(h2 + 1) * 64, :, :], reads=[ysbs[d]], writes=[e[f"YD{d}"]])
    if self.dbg.get("S") is not None:
        for d in range(2):
            for c in range(4):
                K.dma("sp", self.dbg["S"][d, c], S[d][c][:], reads=[S[d][c]])
    K.pop_scope()


Prog.even_e3 = _even_e3
def _even_e4(self, i, ntile):
    K = self.K
    j = i // 2
    e = self.ed
    K.push_scope()
    wout = K.sb([128, 8, D], BF16, "e4wout")
    stg = [K.sb([128, D], F32, f"e4stg{q}") for q in range(3)]
    for kt in range(8):
        sg = stg[kt % 3]
        K.dma("sp", sg[:], self.inp["w_out"][j, kt * 128:(kt + 1) * 128, :], writes=[sg])
        K.op("dve", lambda en, sg=sg, kt=kt: en.tensor_copy(out=wout[:, kt, :], in_=sg[:]), reads=[sg], writes=[wout])
    GN = K.sb([128, 2, 4], F32, "e4gn")
    K.dma("sp", GN[:, 0, :], self.inp["gn_w"][j].rearrange("(c k) -> k c", k=128), writes=[GN], allow_slow_non_contiguous=True)
    K.dma("sp", GN[:, 1, :], self.inp["gn_b"][j].rearrange("(c k) -> k c", k=128), writes=[GN], allow_slow_non_contiguous=True)
    y0s = [K.sb([128, 512], F32, f"e4y0{q}") for q in range(3)]
    y1s = [K.sb([128, 512], F32, f"e4y1{q}") for q in range(3)]
    sqs = [K.sb([128, 512], F32, f"e4sq{q}") for q in range(3)]
    sts = [K.sb([128, 4, 8], F32, f"e4st{q}") for q in range(3)]
    bons = [K.sb([128, 4, 128], F32, f"e4bon{q}") for q in range(3)]
    ggs = [K.sb([128, 4, 128], F32, f"e4gg{q}") for q in range(3)]
    ybs = [K.sb([128, 4, 128], F32, f"e4yb{q}") for q in range(3)]
    yTs = [K.sb([128, 4, 128], F32, f"e4yT{q}") for q in range(3)]
    cats = [K.sb([128, 8, 128], BF16, f"e4cat{q}") for q in range(3)]
    xts = stg
    ots = [K.sb([128, D], F32, f"e4o{q}") for q in range(3)]
    def _body(tt):
        y0, y1, sq, st, bon, gg, yb, yT, cat, xt, ot = (l[tt % 3] for l in (y0s, y1s, sqs, sts, bons, ggs, ybs, yTs, cats,
                                                                            xts, ots))
        M = self.Ml if tt < 64 else self.Mc
        tk = slice(tt * 128, (tt + 1) * 128)
        K.dma("sp", y0[:], e["YD0"][tk, :], reads=[e["YD0"]], writes=[y0])
        K.dma("sp", y1[:], e["YD1"][tk, :], reads=[e["YD1"]], writes=[y1])
        for buf, nm in ((bon, "BON"), (gg, "GG"), (yb, "YB")):
            K.dma("sp", buf[:], e[nm][:, tk].rearrange("(c p) t -> p c t", p=128), reads=[e[nm]], writes=[buf])
        K.dma("sp", xt[:], self.lat[tk, :], reads=[self.lat.p(tt)], writes=[xt])
        K.op("dve", lambda en: en.tensor_tensor(out=y0[:], in0=y0[:], in1=y1[:], op=ALU.add), reads=[y0, y1], writes=[y0])
        yv = y0[:].rearrange("p (h v) -> p h v", v=64)
        sv = sq[:].rearrange("p (h v) -> p h v", v=64)
        K.op("dve", lambda en: en.reduce_sum(out=st[:, 0, :], in_=yv, axis=AX.X), reads=[y0], writes=[st])
        K.op("dve", lambda en: en.tensor_scalar(out=st[:, 1, :], in0=st[:, 0, :], scalar1=1.0 / 64, scalar2=None,
                                                op0=ALU.mult), reads=[st], writes=[st])
        K.op("dve", lambda en: en.tensor_tensor(out=yv, in0=yv, in1=st[:, 1, :].unsqueeze(2).to_broadcast([128, 8, 64]),
                                                op=ALU.subtract), reads=[y0, st], writes=[y0])
        K.op("dve", lambda en: en.tensor_tensor(out=sq[:], in0=y0[:], in1=y0[:], op=ALU.mult), reads=[y0], writes=[sq])
        K.op("dve", lambda en: en.reduce_sum(out=st[:, 2, :], in_=sv, axis=AX.X), reads=[sq], writes=[st])
        K.op("dve", lambda en: en.tensor_scalar(out=st[:, 2, :], in0=st[:, 2, :], scalar1=1.0 / 64, scalar2=64e-5,
                                                op0=ALU.mult, op1=ALU.add), reads=[st], writes=[st])
        K.op("act", lambda en: en.sqrt(out=st[:, 3, :], in_=st[:, 2, :]), reads=[st], writes=[st])
        K.op("dve", lambda en: en.reciprocal(out=st[:, 3, :], in_=st[:, 3, :]), reads=[st], writes=[st])
        K.op("dve", lambda en: en.tensor_tensor(out=yv, in0=yv, in1=st[:, 3, :].unsqueeze(2).to_broadcast([128, 8, 64]),
                                                op=ALU.mult), reads=[y0, st], writes=[y0])
        ps = K.ps()
        for c in range(4):
            self.tr(None, ps[:, c * 128:(c + 1) * 128], y0[:, c * 128:(c + 1) * 128], ps, y0)
        for c in range(4):
            K.op("dve", lambda en, c=c: en.tensor_scalar(out=yT[:, c, :], in0=ps[:, c * 128:(c + 1) * 128],
                                                         scalar1=GN[:, 0, c:c + 1], scalar2=GN[:, 1, c:c + 1],
                                                         op0=ALU.mult, op1=ALU.add), reads=[ps, GN], writes=[yT])
        K.op("dve", lambda en: en.tensor_tensor(out=yT[:], in0=yT[:], in1=bon[:], op=ALU.add), reads=[yT, bon],
             writes=[yT])
        K.op("dve", lambda en: en.tensor_tensor(out=cat[:, 0:4, :], in0=yT[:], in1=gg[:], op=ALU.mult), reads=[yT, gg],
             writes=[cat])
        K.op("act", lambda en: en.copy(out=cat[:, 4:8, :], in_=yb[:]), reads=[yb], writes=[cat])
        for hh in range(2):
            po = K.ps()
            for kt in range(8):
                K.op("pe", lambda en, kt=kt: en.matmul(po[:, :], lhsT=cat[:, kt, :], rhs=wout[:, kt, hh * 512:(hh + 1) * 512],
                                                       start=(kt == 0), stop=(kt == 7)), reads=[cat, wout], writes=[po])
            K.op("dve", lambda en: en.tensor_tensor(out=ot[:, hh * 512:(hh + 1) * 512], in0=po[:, :],
                                                    in1=M[:, 2, hh * 512:(hh + 1) * 512], op=ALU.mult),
                 reads=[po, M], writes=[ot])
        K.op("dve", lambda en: en.tensor_tensor(out=ot[:], in0=ot[:], in1=xt[:], op=ALU.add), reads=[ot, xt], writes=[ot])
        K.dma("sp", self.lat[tk, :], ot[:], reads=[ot], writes=[self.lat.p(tt)])
    K.interleave(_body, range(ntile), 3)
    K.pop_scope()


def _even_mixer(self, i):
    self.even_e1(i, ntile=66)
    self.even_e2(i)
    self.even_e3(i)
    self.even_e4(i, ntile=66 if i < 2 else 64)


Prog.even_e4 = _even_e4
Prog.even_mixer = _even_mixer
Prog.even_declare = _even_declare
Prog.even_e1 = _even_e1


def build_program():
    P = Prog()
    P.fourier_declare()
    P.even_declare()
    P.init_lat()
    P.prep_s()
    for i in range(DEPTH):
        P.modvec(i, need_ctx=(i <= 2))
        if i % 2 == 0:
            P.even_mixer(i)
        else:
            P.fourier(i, with_ctx=(i < 2))
        P.moe_setup()
        P.moe(i, with_ctx=(i < 2))
    P.final_norm()
    P.K.finish()
    return P


def kernel(**inputs):
    x = np.asarray(inputs["x"], dtype=np.float32)
    B = x.shape[0]
    P = build_program()
    consts = fourier_consts()
    pconsts = pool_consts()
    in_maps = []
    for b in range(B):
        d = {"x": np.ascontiguousarray(x[b]),
             "c": np.ascontiguousarray(np.asarray(inputs["c"], dtype=np.float32)[b:b + 1]),
             "ctx": np.ascontiguousarray(np.asarray(inputs["ctx"], dtype=np.float32)[b]),
             "c_ctx": np.ascontiguousarray(np.asarray(inputs["c_ctx"], dtype=np.float32)[None, :])}
        for k in WEIGHT_SPECS:
            d[k] = np.ascontiguousarray(np.asarray(inputs[k], dtype=np.float32))
        d.update(consts)
        d.update(pconsts)
        in_maps.append(d)
    res = run_bass_kernel_spmd(P.K.nc, in_maps, core_ids=list(range(B)))
    return np.stack([np.asarray(r["out"]) for r in res.results], axis=0).astype(np.float32)
```

```python
import numpy as np
from contextlib import ExitStack
import concourse.bass as bass
import concourse.mybir as mybir
from concourse.bass_utils import run_bass_kernel_spmd

F32 = mybir.dt.float32
BF16 = mybir.dt.bfloat16
I32 = mybir.dt.int32
AF = mybir.ActivationFunctionType
ALU = mybir.AluOpType
AX = mybir.AxisListType

D = 1024
T = 8192
CT = 256
NT = T + CT
DEPTH = 4
NEXP = 32
DE = 512


class Buf:
    def __init__(self, t, name):
        self.t = t
        self.name = name
        self.lw = None
        self.rd = {}

    def __getitem__(self, idx):
        return self.t[idx]


class Parts:
    def __init__(self, t, name):
        self.t = t
        self.name = name
        self.parts = {}

    def p(self, key):
        b = self.parts.get(key)
        if b is None:
            b = Buf(self.t, f"{self.name}.{key}")
            self.parts[key] = b
        return b

    def all(self):
        return list(self.parts.values())

    def __getitem__(self, idx):
        return self.t[idx]


class Ctx:
    KD = 16
    SAME_ENG_SYNC = True

    def __init__(self):
        self.nc = bass.Bass("TRN2", target_bir_lowering=False)
        nc = self.nc
        self.es = ExitStack()
        self.eng = {"pe": nc.tensor, "act": nc.scalar, "dve": nc.vector, "pool": nc.gpsimd, "sp": nc.sync}
        self.csem = {e: self.es.enter_context(nc.semaphore("c_" + e)) for e in ("pe", "act", "dve", "pool")}
        self.ccnt = {e: 0 for e in self.csem}
        self.dsem = {q: [self.es.enter_context(nc.semaphore(f"d_{q}{i}")) for i in range(self.KD)]
                     for q in ("sp", "pool", "act")}
        self.dcnt = {q: 0 for q in self.dsem}
        self.known = {e: {} for e in self.eng}
        self.nalloc = 0
        self.psum_banks = []
        self.psum_i = 0

    def sb(self, shape, dtype=F32, name=None):
        self.nalloc += 1
        name = (name or "sb") + f"_{self.nalloc}"
        es = self.scopes[-1] if getattr(self, "scopes", None) else self.es
        t = es.enter_context(self.nc.sbuf_tensor(name, list(shape), dtype))
        return Buf(t, name)

    def push_scope(self):
        if not hasattr(self, "scopes"):
            self.scopes = []
        self.scopes.append(ExitStack())

    def pop_scope(self):
        self.barrier()
        self.scopes.pop().close()

    def barrier(self):
        for e in self.eng:
            for src, sem in self.csem.items():
                if self.ccnt[src] > 0:
                    self._wait(e, (sem, self.ccnt[src], "bar"))
            self._wait_all_dma(e)

    def _wait_all_dma(self, e):
        for q in self.dsem:
            n = self.dcnt[q]
            for r in range(self.KD):
                cnt = (n - r + self.KD - 1) // self.KD if n > r else 0
                if cnt > 0:
                    self._wait(e, (self.dsem[q][r], 16 * cnt, "dma"))

    def dram(self, name, shape, dtype=F32, kind="Internal"):
        return self.nc.dram_tensor(name, list(shape), dtype, kind=kind)

    def init_psum(self, n=8):
        for i in range(n):
            t = self.es.enter_context(self.nc.psum_tensor(f"ps{i}", [128, 512], F32))
            b = Buf(t, f"ps{i}")
            b.excl = True
            self.psum_banks.append(b)

    def ps(self):
        b = self.psum_banks[self.psum_i % len(self.psum_banks)]
        self.psum_i += 1
        return b

    def _wait(self, e, ev):
        if ev is None:
            return
        sem, val, src = ev
        if src == e and (e == "pe" or not self.SAME_ENG_SYNC):
            return
        k = self.known[e]
        key = sem.name
        if k.get(key, 0) >= val:
            return
        self.eng[e].wait_ge(sem, val)
        k[key] = val

    def _deps(self, e, reads, writes):
        for b in reads:
            self._wait(e, b.lw)
            if getattr(b, "excl", False):
                for ke, ev in b.rd.items():
                    if ke != e:
                        self._wait(e, ev)
        for b in writes:
            self._wait(e, b.lw)
            for ev in b.rd.values():
                self._wait(e, ev)

    def _commit(self, ev, key, reads, writes):
        for b in writes:
            b.lw = ev
            b.rd = {}
        for b in reads:
            b.rd[key] = ev

    def interleave(self, body, items, n):
        import threading
        items = list(items)
        for g0 in range(0, len(items), n):
            grp = items[g0:g0 + n]
            if len(grp) == 1:
                body(grp[0])
                continue
            cv = threading.Condition()
            state = {"turn": 0, "alive": [True] * len(grp), "err": None}

            def nxt(k):
                m = len(grp)
                for d in range(1, m + 1):
                    if state["alive"][(k + d) % m]:
                        return (k + d) % m
                return -1

            def switch(k):
                with cv:
                    state["turn"] = nxt(k)
                    cv.notify_all()
                    cv.wait_for(lambda: state["turn"] == k)

            def runner(k, it):
                with cv:
                    cv.wait_for(lambda: state["turn"] == k)
                threading.current_thread()._il_switch = lambda: switch(k)
                try:
                    body(it)
                except BaseException as ex:
                    state["err"] = ex
                finally:
                    with cv:
                        state["alive"][k] = False
                        state["turn"] = nxt(k)
                        cv.notify_all()
            ths = [threading.Thread(target=runner, args=(k, it)) for k, it in enumerate(grp)]
            self._il_map = {}
            for t in ths:
                t.start()
            for t in ths:
                t.join()
            if state["err"] is not None:
                raise state["err"]

    def _maybe_switch(self):
        sw = getattr(self, "_switch_tl", None)

    def op(self, e, fn, reads=(), writes=()):
        self._yield_point()
        self._deps(e, reads, writes)
        ins = fn(self.eng[e])
        self.ccnt[e] += 1
        ins.then_inc(self.csem[e], 1)
        ev = (self.csem[e], self.ccnt[e], e)
        self._commit(ev, e, reads, writes)
        return ins

    def _yield_point(self):
        import threading
        sw = getattr(threading.current_thread(), "_il_switch", None)
        if sw is not None:
            sw()

    def dma(self, q, out, in_, reads=(), writes=(), indirect=None, **kw):
        self._yield_point()
        i = self.dcnt[q]
        self.dcnt[q] += 1
        sem = self.dsem[q][i % self.KD]
        val = 16 * (i // self.KD + 1)
        if i >= self.KD:
            self._wait(q, (sem, val - 16, "dma"))
        self._deps(q, reads, writes)
        if indirect is None:
            ins = self.eng[q].dma_start(out=out, in_=in_, **kw)
        else:
            ins = self.eng[q].indirect_dma_start(out=out, in_=in_, **indirect)
        ins.then_inc(sem, 16)
        ev = (sem, val, "dma")
        self._commit(ev, (q, i % self.KD), reads, writes)
        return ins

    def finish(self):
        self._wait_all_dma("sp")
        self.es.close()


WEIGHT_SPECS = {
    "ada_w": [4, 1024, 6144], "ada_b": [4, 6144], "norm_mix": [4, 1024], "norm_ffn": [4, 1024],
    "w_in": [2, 1024, 2048], "mu_x": [2, 3, 1024], "mu_p": [2, 3, 512],
    "decay_w0": [2, 2, 512], "decay_w1": [2, 2, 1024, 64], "decay_w2": [2, 2, 64, 512],
    "lr_a0": [2, 2, 512], "lr_a1": [2, 2, 1024, 64], "lr_a2": [2, 2, 64, 512],
    "gate_g1": [2, 1024, 128], "gate_g2": [2, 128, 512],
    "k_k": [2, 512], "k_a": [2, 512], "r_k": [2, 8, 64], "gn_w": [2, 512], "gn_b": [2, 512],
    "pool_w": [2, 4, 128, 128], "pool_scale": [2, 512], "w_out": [2, 1024, 1024],
    "w_fourier": [2, 1024, 1024],
    "router_c": [4, 1024, 4], "router_c_b": [4, 4], "router_f": [4, 1024, 32], "router_f_b": [4, 32],
    "moe_w1": [4, 32, 1024, 512], "moe_w3": [4, 32, 1024, 512], "moe_w2": [4, 32, 512, 1024],
    "final_norm": [1024],
}


class Prog:
    BLK = 384
    SCAN_DT = BF16

    def __init__(self, debug=None, skip=()):
        self.K = Ctx()
        K = self.K
        nc = K.nc
        self.debug = debug or {}
        self.inp = {}
        self.inp["x"] = nc.dram_tensor("x", [T, D], F32, kind="ExternalInput")
        self.inp["c"] = nc.dram_tensor("c", [1, D], F32, kind="ExternalInput")
        self.inp["ctx"] = nc.dram_tensor("ctx", [CT, D], F32, kind="ExternalInput")
        self.inp["c_ctx"] = nc.dram_tensor("c_ctx", [1, D], F32, kind="ExternalInput")
        for k, shp in WEIGHT_SPECS.items():
            if k in skip:
                continue
            self.inp[k] = nc.dram_tensor(k, shp, F32, kind="ExternalInput")
        self.out = nc.dram_tensor("out", [T, D], F32, kind="ExternalOutput")
        self.dbg = {}
        for k, (shp, dt) in self.debug.items():
            self.dbg[k] = nc.dram_tensor("dbg_" + k, shp, dt, kind="ExternalOutput")
        K.init_psum(8)
        self.lat = Parts(nc.dram_tensor("lat", [NT, D], F32), "lat")
        self.ident = K.sb([128, 128], F32, "ident")
        self.identb = K.sb([128, 128], BF16, "identb")
        self.ones = K.sb([128, 128], F32, "ones")
        self._consts()
        self.Ml = K.sb([128, 6, D], F32, "Ml")
        self.Mc = K.sb([128, 6, D], F32, "Mc")
        self.srep = K.sb([128, 2, 8, 128], F32, "srep")

    def _consts(self):
        K = self.K
        K.op("pool", lambda e: e.memset(self.ones[:], 1.0), writes=[self.ones])
        K.op("pool", lambda e: e.memset(self.ident[:], 0.0), writes=[self.ident])
        K.op("pool", lambda e: e.affine_select(out=self.ident[:], in_=self.ident[:], pattern=[[-1, 128]],
                                               compare_op=ALU.not_equal, fill=1.0, base=0, channel_multiplier=1),
             reads=[self.ident], writes=[self.ident])
        K.op("dve", lambda e: e.tensor_copy(out=self.identb[:], in_=self.ident[:]), reads=[self.ident],
             writes=[self.identb])

    def prep_s(self):
        K = self.K
        craw = K.sb([128, 2, 8], F32, "craw")
        csil = K.sb([128, 2, 8], F32, "csil")
        for w, nm in enumerate(("c", "c_ctx")):
            src = self.inp[nm].ap().rearrange("o (kt k) -> k (o kt)", k=128)
            K.dma("sp", craw[:, w, :], src, writes=[craw], allow_slow_non_contiguous=True)
        K.op("act", lambda e: e.activation(out=csil[:], in_=craw[:], func=AF.Silu), reads=[craw], writes=[csil])
        K.op("dve", lambda e: e.tensor_copy(out=self.srep[:], in_=csil[:].unsqueeze(3).to_broadcast([128, 2, 8, 128])),
             reads=[csil], writes=[self.srep])

    def modvec(self, i, need_ctx=True):
        K = self.K
        K.push_scope()
        mv = dict(
            W=[K.sb([128, 8, 512], F32, f"adaW{j}") for j in range(2)],
            b=[K.sb([1, 512], F32, f"adab{j}") for j in range(2)],
            g=K.sb([128, 2, D], F32, "normg"),
        )
        aw = self.inp["ada_w"]
        ab = self.inp["ada_b"]
        g = mv["g"]
        K.dma("sp", g[:, 0, :], self.inp["norm_mix"][i:i + 1, :].partition_broadcast(128), writes=[g])
        K.dma("sp", g[:, 1, :], self.inp["norm_ffn"][i:i + 1, :].partition_broadcast(128), writes=[g])
        targets = [(0, self.Ml)] + ([(1, self.Mc)] if need_ctx else [])
        for nb in range(12):
            W = mv["W"][nb % 2]
            bb = mv["b"][nb % 2]
            K.dma("sp", W[:], aw[i, :, nb * 512:(nb + 1) * 512].rearrange("(kt k) n -> k kt n", k=128), writes=[W])
            K.dma("sp", bb[:], ab[i:i + 1, nb * 512:(nb + 1) * 512], writes=[bb])
            for w, M in targets:
                ps = K.ps()
                for kt in range(8):
                    K.op("pe", lambda e, kt=kt, w=w, ps=ps, W=W: e.matmul(ps[:, :], lhsT=self.srep[:, w, kt, :],
                                                                         rhs=W[:, kt, :], start=(kt == 0), stop=False),
                         reads=[self.srep, W], writes=[ps])
                K.op("pe", lambda e, ps=ps, bb=bb: e.matmul(ps[:, :], lhsT=self.ones[0:1, :], rhs=bb[0:1, :],
                                                            start=False, stop=True),
                     reads=[self.ones, bb], writes=[ps])
                s, half = nb // 2, nb % 2
                dst = M[:, s, half * 512:(half + 1) * 512]
                if s in (1, 4):
                    gi = 0 if s == 1 else 1
                    K.op("dve", lambda e, dst=dst, ps=ps, gi=gi, half=half: e.scalar_tensor_tensor(
                        out=dst, in0=ps[:, :], scalar=1.0, in1=g[:, gi, half * 512:(half + 1) * 512],
                        op0=ALU.add, op1=ALU.mult), reads=[ps, g], writes=[M])
                else:
                    K.op("act", lambda e, dst=dst, ps=ps: e.copy(out=dst, in_=ps[:, :]), reads=[ps], writes=[M])
        K.pop_scope()

    def norm_tile(self, xt, ht, M, sub, st):
        K = self.K
        sh = 0 if sub == 0 else 3
        ga = 1 if sub == 0 else 4
        K.op("act", lambda e: e.activation(out=ht[:], in_=xt[:], func=AF.Square, accum_out=st[:, 0:1]),
             reads=[xt], writes=[ht, st])
        K.op("dve", lambda e: e.tensor_scalar(out=st[:, 1:2], in0=st[:, 0:1], scalar1=1.0 / D, scalar2=1e-6,
                                              op0=ALU.mult, op1=ALU.add), reads=[st], writes=[st])
        K.op("act", lambda e: e.sqrt(out=st[:, 3:4], in_=st[:, 1:2]), reads=[st], writes=[st])
        K.op("dve", lambda e: e.reciprocal(out=st[:, 2:3], in_=st[:, 3:4]), reads=[st], writes=[st])
        K.op("dve", lambda e: e.scalar_tensor_tensor(out=ht[:], in0=xt[:], scalar=st[:, 2:3], in1=M[:, ga, :],
                                                     op0=ALU.mult, op1=ALU.mult), reads=[xt, st, M], writes=[ht])
        K.op("dve", lambda e: e.tensor_tensor(out=ht[:], in0=ht[:], in1=M[:, sh, :], op=ALU.add),
             reads=[ht, M], writes=[ht])

    def tr(self, e_unused, dst_ps_ap, src_ap, ps, src_buf, n_in=128):
        K = self.K
        K.op("pe", lambda e: e.transpose(out=dst_ps_ap, in_=src_ap, identity=self.ident[0:n_in, 0:n_in]),
             reads=[src_buf, self.ident], writes=[ps])

    def init_lat(self):
        K = self.K
        for r in range(0, T, 2048):
            K.dma("sp", self.lat[r:r + 2048, :], self.inp["x"][r:r + 2048, :],
                  writes=[self.lat.p(t) for t in range(r // 128, r // 128 + 16)])
        K.dma("sp", self.lat[T:NT, :], self.inp["ctx"][:, :], writes=[self.lat.p(64), self.lat.p(65)])

    def moe_dram(self):
        nc = self.K.nc
        BLK = self.BLK
        self.NB = (2 * NT + 32 * BLK) // BLK
        NB = self.NB
        self.mdram = dict(H2=Parts(nc.dram_tensor("H2", [NT, D], F32), "H2"),
                          XS=Buf(nc.dram_tensor("XS", [NB * BLK, D], F32), "XS"),
                          YS=Buf(nc.dram_tensor("YS", [NB * BLK, D], BF16), "YS"))

    def moe_setup(self):
        K = self.K
        nc = K.nc
        if not hasattr(self, "mdram"):
            self.moe_dram()
        K.push_scope()
        m = dict(self.mdram)
        NTL = 66
        NB = self.NB
        m["OHA"] = K.sb([128, NTL, 2, 32], BF16, "OHA")
        m["RK"] = K.sb([128, NTL, 2], F32, "RK")
        m["TOT"] = K.sb([128, NTL, 32], F32, "TOT")
        m["cs"] = K.sb([128, 8, 32], F32, "moecs")
        m["be"] = K.sb([128, 3, NB], F32, "moebe")
        m["idf"] = K.sb([128, NB, 8], F32, "moeidf")
        m["GATE"] = K.sb([128, NTL, 2], F32, "GATE")
        m["DEST"] = K.sb([128, NTL, 2], I32, "DEST")
        m["carry"] = K.sb([128, 32], F32, "carry")
        m["Wr"] = K.sb([128, 8, 36], F32, "Wr")
        m["br"] = K.sb([1, 36], F32, "br")
        m["UT"] = K.sb([128, 128], F32, "UT")
        m["PIDX"] = K.sb([128, 8], F32, "PIDX")
        m["JV"] = K.sb([128, NB], F32, "JV")
        m["IDX1"] = K.sb([128, NB, 8], I32, "IDX1")
        m["IDX2"] = K.sb([128, NB, 4], I32, "IDX2")
        big = [K.sb([128, D], F32, f"big{j}") for j in range(8)]
        m["xt"] = big[0:2]
        m["ht"] = big[2:4]
        m["yb"] = [K.sb([128, D], BF16, f"myb{j}") for j in range(2)]
        m["y0"] = [K.sb([128, D], BF16, f"my0{j}") for j in range(2)]
        m["y1"] = [K.sb([128, D], BF16, f"my1{j}") for j in range(2)]
        m["ya"] = big[4:6]
        m["st"] = [K.sb([128, 4], F32, f"mst{j}") for j in range(2)]
        m["hT"] = [K.sb([128, 8, 128], F32, f"mhT{j}") for j in range(2)]
        m["xTb"] = [K.sb([128, 8, 128], BF16, f"mxTb{j}") for j in range(2)]
        m["W1"] = [K.sb([128, 8, 512], BF16, f"mW1{j}") for j in range(2)]
        m["W3"] = [K.sb([128, 8, 512], BF16, f"mW3{j}") for j in range(2)]
        m["W2"] = [K.sb([128, 4, 1024], BF16, f"mW2{j}") for j in range(2)]
        m["hTb"] = [K.sb([128, 4, 128], BF16, f"mhTb{j}") for j in range(2)]
        m["sil"] = [K.sb([128, 512], F32, f"msil{j}") for j in range(2)]
        m["sm"] = [K.sb([128, 128], F32, f"msm{j}") for j in range(2)]
        UT = m["UT"]
        K.op("pool", lambda e: e.memset(UT[:], 1.0), writes=[UT])
        K.op("pool", lambda e: e.affine_select(out=UT[:], in_=UT[:], pattern=[[1, 128]], compare_op=ALU.is_gt,
                                               fill=0.0, base=0, channel_multiplier=-1), reads=[UT], writes=[UT])
        pi = K.sb([128, 8], I32, "pidx_i")
        K.op("pool", lambda e: e.iota(pi[:], pattern=[[128, 8]], base=0, channel_multiplier=1), writes=[pi])
        K.op("dve", lambda e: e.tensor_copy(out=m["PIDX"][:], in_=pi[:]), reads=[pi], writes=[m["PIDX"]])
        ji = K.sb([128, NB], I32, "jv_i")
        K.op("pool", lambda e: e.iota(ji[:], pattern=[[self.BLK, NB]], base=0, channel_multiplier=0), writes=[ji])
        K.op("dve", lambda e: e.tensor_copy(out=m["JV"][:], in_=ji[:]), reads=[ji], writes=[m["JV"]])
        self.m = m

    def moe(self, i, with_ctx, final=False):
        K = self.K
        m = self.m
        NB = self.NB
        ntile = 66 if with_ctx else 64
        Wr, br = m["Wr"], m["br"]
        K.dma("sp", Wr[:, :, 0:4], self.inp["router_c"][i].rearrange("(kt k) n -> k kt n", k=128), writes=[Wr])
        K.dma("sp", Wr[:, :, 4:36], self.inp["router_f"][i].rearrange("(kt k) n -> k kt n", k=128), writes=[Wr])
        K.dma("sp", br[:, 0:4], self.inp["router_c_b"][i:i + 1, :], writes=[br])
        K.dma("sp", br[:, 4:36], self.inp["router_f_b"][i:i + 1, :], writes=[br])
        carry = m["carry"]
        K.op("dve", lambda e: e.memset(carry[:], 0.0), writes=[carry])
        OHA, RK, GATE, DEST = m["OHA"], m["RK"], m["GATE"], m["DEST"]
        TOT = m["TOT"]
        TOTp = Parts(TOT.t, "TOTp")
        def _body(tt):
            xt, ht, st, hT, sm = (m[k][tt % 2] for k in ("xt", "ht", "st", "hT", "sm"))
            M = self.Ml if tt < 64 else self.Mc
            K.dma("sp", xt[:], self.lat[tt * 128:(tt + 1) * 128, :], reads=[self.lat.p(tt)], writes=[xt])
            self.norm_tile(xt, ht, M, 1, st)
            K.dma("act", m["H2"][tt * 128:(tt + 1) * 128, :], ht[:], reads=[ht], writes=[m["H2"].p(tt)])
            for half in range(2):
                ps = K.ps()
                for q in range(4):
                    kt = half * 4 + q
                    self.tr(None, ps[:, q * 128:(q + 1) * 128], ht[:, kt * 128:(kt + 1) * 128], ps, ht)
                K.op("act" if half else "dve",
                     lambda e, ps=ps, half=half: (e.copy if half else e.tensor_copy)(
                         out=hT[:, half * 4:(half + 1) * 4, :].rearrange("p a b -> p (a b)"), in_=ps[:, :]),
                     reads=[ps], writes=[hT])
            ps = K.ps()
            for kt in range(8):
                K.op("pe", lambda e, kt=kt, ps=ps: e.matmul(ps[:, 0:36], lhsT=hT[:, kt, :], rhs=Wr[:, kt, :],
                                                           start=(kt == 0), stop=False),
                     reads=[hT, Wr], writes=[ps])
            K.op("pe", lambda e, ps=ps: e.matmul(ps[:, 0:36], lhsT=self.ones[0:1, :], rhs=br[0:1, :],
                                                 start=False, stop=True), reads=[self.ones, br], writes=[ps])
            def dv(fn, rd=(), wr=()):
                K.op("dve", fn, reads=[sm] + list(rd), writes=[sm] + list(wr))
            K.op("dve", lambda e, ps=ps: e.tensor_copy(out=sm[:, 0:36], in_=ps[:, 0:36]), reads=[ps], writes=[sm])
            dv(lambda e: e.reduce_max(out=sm[:, 36:37], in_=sm[:, 0:4], axis=AX.X))
            dv(lambda e: e.tensor_scalar(out=sm[:, 37:38], in0=sm[:, 36:37], scalar1=-1.0, scalar2=None, op0=ALU.mult))
            dv(lambda e: e.tensor_scalar(out=sm[:, 40:44], in0=sm[:, 0:4], scalar1=sm[:, 36:37], scalar2=None,
                                         op0=ALU.is_equal))
            K.op("act", lambda e: e.activation(out=sm[:, 44:48], in_=sm[:, 0:4], func=AF.Exp, bias=sm[:, 37:38],
                                               scale=1.0, accum_out=sm[:, 38:39]), reads=[sm], writes=[sm])
            dv(lambda e: e.reciprocal(out=sm[:, 39:40], in_=sm[:, 38:39]))
            dv(lambda e: e.tensor_scalar(out=sm[:, 48:56], in0=sm[:, 4:12], scalar1=sm[:, 40:41], scalar2=None,
                                         op0=ALU.mult))
            for g in range(1, 4):
                dv(lambda e, g=g: e.scalar_tensor_tensor(out=sm[:, 48:56], in0=sm[:, 4 + 8 * g:12 + 8 * g],
                                                         scalar=sm[:, 40 + g:41 + g], in1=sm[:, 48:56],
                                                         op0=ALU.mult, op1=ALU.add))
            dv(lambda e: e.reduce_max(out=sm[:, 56:57], in_=sm[:, 48:56], axis=AX.X))
            dv(lambda e: e.tensor_scalar(out=sm[:, 60:68], in0=sm[:, 48:56], scalar1=sm[:, 56:57], scalar2=None,
                                         op0=ALU.is_equal))
            dv(lambda e: e.scalar_tensor_tensor(out=sm[:, 68:76], in0=sm[:, 60:68], scalar=-1e30, in1=sm[:, 48:56],
                                                op0=ALU.mult, op1=ALU.add))
            dv(lambda e: e.reduce_max(out=sm[:, 57:58], in_=sm[:, 68:76], axis=AX.X))
            dv(lambda e: e.tensor_scalar(out=sm[:, 76:84], in0=sm[:, 68:76], scalar1=sm[:, 57:58], scalar2=None,
                                         op0=ALU.is_equal))
            dv(lambda e: e.tensor_tensor(out=sm[:, 58:59], in0=sm[:, 56:57], in1=sm[:, 57:58], op=ALU.subtract))
            K.op("act", lambda e: e.activation(out=sm[:, 59:60], in_=sm[:, 58:59], func=AF.Sigmoid),
                 reads=[sm], writes=[sm])
            dv(lambda e, tt=tt: e.tensor_tensor(out=GATE[:, tt, 0:1], in0=sm[:, 59:60], in1=sm[:, 39:40], op=ALU.mult),
               wr=[GATE])
            dv(lambda e, tt=tt: e.tensor_tensor(out=GATE[:, tt, 1:2], in0=sm[:, 39:40], in1=GATE[:, tt, 0:1],
                                                op=ALU.subtract), rd=[GATE], wr=[GATE])
            for k, c0 in ((0, 60), (1, 76)):
                dv(lambda e, tt=tt, k=k, c0=c0: e.tensor_tensor(
                    out=OHA[:, tt, k, :].rearrange("p (g l) -> p g l", g=4),
                    in0=sm[:, 40:44].unsqueeze(2).to_broadcast([128, 4, 8]),
                    in1=sm[:, c0:c0 + 8].unsqueeze(1).to_broadcast([128, 4, 8]), op=ALU.mult), wr=[OHA])
            dv(lambda e, tt=tt: e.tensor_tensor(out=sm[:, 84:116], in0=OHA[:, tt, 0, :], in1=OHA[:, tt, 1, :],
                                                op=ALU.add), rd=[OHA])
            psr = K.ps()
            K.op("pe", lambda e, psr=psr: e.matmul(psr[:, 0:32], lhsT=m["UT"][:], rhs=sm[:, 84:116], start=True,
                                                   stop=True), reads=[m["UT"], sm], writes=[psr])
            K.op("pe", lambda e, psr=psr: e.matmul(psr[:, 32:64], lhsT=self.ones[:], rhs=sm[:, 84:116], start=True,
                                                   stop=True), reads=[self.ones, sm], writes=[psr])
            K.op("dve", lambda e, psr=psr, tt=tt: e.tensor_copy(out=TOT[:, tt, :], in_=psr[:, 32:64]), reads=[psr],
                 writes=[TOTp.p(tt)])
            K.op("dve", lambda e, psr=psr: e.tensor_copy(out=sm[:, 84:116], in_=psr[:, 0:32]), reads=[psr, sm], writes=[sm])
            for k in range(2):
                dv(lambda e, tt=tt, k=k: e.tensor_tensor(out=sm[:, 0:32], in0=sm[:, 84:116], in1=OHA[:, tt, k, :],
                                                         op=ALU.mult), rd=[OHA])
                dv(lambda e, tt=tt, k=k: e.reduce_sum(out=RK[:, tt, k:k + 1], in_=sm[:, 0:32], axis=AX.X), wr=[RK])
        K.interleave(_body, range(ntile), 2)
        sm0 = m["sm"][0]
        for tt in range(ntile):
            if tt > 0:
                for k in range(2):
                    K.op("dve", lambda e, tt=tt, k=k: e.tensor_tensor(out=sm0[:, 0:32], in0=carry[:], in1=OHA[:, tt, k, :],
                                                                      op=ALU.mult), reads=[carry, OHA], writes=[sm0])
                    K.op("dve", lambda e, k=k: e.reduce_sum(out=sm0[:, 40 + k:41 + k], in_=sm0[:, 0:32], axis=AX.X),
                         reads=[sm0], writes=[sm0])
                K.op("dve", lambda e, tt=tt: e.tensor_tensor(out=RK[:, tt, :], in0=RK[:, tt, :], in1=sm0[:, 40:42],
                                                             op=ALU.add), reads=[RK, sm0], writes=[RK])
            K.op("dve", lambda e, tt=tt: e.tensor_tensor(out=carry[:], in0=TOT[:, tt, :], in1=carry[:], op=ALU.add),
                 reads=TOTp.all() + [carry], writes=[carry])
        cs = m["cs"]

        def cv(fn):
            K.op("dve", fn, reads=[cs, carry], writes=[cs])
        BLK = self.BLK
        cv(lambda e: e.tensor_scalar(out=cs[:, 0, :], in0=carry[:], scalar1=1.0 / BLK, scalar2=(BLK - 1.0) / (2 * BLK),
                                     op0=ALU.mult, op1=ALU.add))
        cv(lambda e: e.tensor_scalar(out=cs[:, 1, :], in0=cs[:, 0, :], scalar1=8388608.0, scalar2=None, op0=ALU.add))
        cv(lambda e: e.tensor_scalar(out=cs[:, 2, :], in0=cs[:, 1, :], scalar1=-8388608.0, scalar2=float(BLK),
                                     op0=ALU.add, op1=ALU.mult))
        cv(lambda e: e.tensor_copy(out=cs[:, 3, :], in_=cs[:, 2, :]))
        a, b = 3, 4
        for s in (1, 2, 4, 8, 16):
            cv(lambda e, a=a, b=b, s=s: e.tensor_copy(out=cs[:, b, 0:s], in_=cs[:, a, 0:s]))
            cv(lambda e, a=a, b=b, s=s: e.tensor_tensor(out=cs[:, b, s:32], in0=cs[:, a, s:32], in1=cs[:, a, 0:32 - s],
                                                        op=ALU.add))
            a, b = b, a
        pend_i = a
        cv(lambda e: e.tensor_tensor(out=cs[:, 5, :], in0=cs[:, pend_i, :], in1=cs[:, 2, :], op=ALU.subtract))
        be = m["be"]
        K.op("dve", lambda e: e.memset(be[:, 0, :], 0.0), writes=[be])
        for ex in range(32):
            K.op("dve", lambda e, ex=ex: e.scalar_tensor_tensor(out=be[:, 0, :], in0=m["JV"][:],
                                                                scalar=cs[:, pend_i, ex:ex + 1], in1=be[:, 0, :],
                                                                op0=ALU.is_ge, op1=ALU.add),
                 reads=[m["JV"], cs, be], writes=[be])
        K.op("dve", lambda e: e.tensor_scalar(out=be[:, 0, :], in0=be[:, 0, :], scalar1=31.0, scalar2=None, op0=ALU.min),
             reads=[be], writes=[be])
        K.op("dve", lambda e: e.tensor_scalar(out=be[:, 1, :], in0=be[:, 0, :], scalar1=1024.0,
                                              scalar2=float(i * 32 * 1024), op0=ALU.mult, op1=ALU.add),
             reads=[be], writes=[be])
        K.op("dve", lambda e: e.tensor_scalar(out=be[:, 2, :], in0=be[:, 0, :], scalar1=512.0,
                                              scalar2=float(i * 32 * 512), op0=ALU.mult, op1=ALU.add),
             reads=[be], writes=[be])
        idf = m["idf"]
        K.op("dve", lambda e: e.tensor_tensor(out=idf[:], in0=be[:, 1, :].unsqueeze(2).to_broadcast([128, NB, 8]),
                                              in1=m["PIDX"][:].unsqueeze(1).to_broadcast([128, NB, 8]), op=ALU.add),
             reads=[be, m["PIDX"]], writes=[idf])
        K.op("dve", lambda e: e.tensor_copy(out=m["IDX1"][:], in_=idf[:]), reads=[idf], writes=[m["IDX1"]])
        K.op("dve", lambda e: e.tensor_tensor(out=idf[:, :, 0:4], in0=be[:, 2, :].unsqueeze(2).to_broadcast([128, NB, 4]),
                                              in1=m["PIDX"][:, 0:4].unsqueeze(1).to_broadcast([128, NB, 4]),
                                              op=ALU.add), reads=[be, m["PIDX"]], writes=[idf])
        K.op("dve", lambda e: e.tensor_copy(out=m["IDX2"][:], in_=idf[:, :, 0:4]), reads=[idf], writes=[m["IDX2"]])
        def _body(tt):
            ht, sm = m["ht"][tt % 2], m["sm"][tt % 2]
            K.dma("sp", ht[:], m["H2"][tt * 128:(tt + 1) * 128, :], reads=[m["H2"].p(tt)], writes=[ht])
            for k in range(2):
                K.op("dve", lambda e, tt=tt, k=k: e.tensor_tensor(out=sm[:, 32:64], in0=cs[:, 5, :],
                                                                  in1=OHA[:, tt, k, :], op=ALU.mult),
                     reads=[sm, OHA, cs], writes=[sm])
                K.op("dve", lambda e, k=k: e.reduce_sum(out=sm[:, 66 + k:67 + k], in_=sm[:, 32:64], axis=AX.X),
                     reads=[sm], writes=[sm])
            K.op("dve", lambda e, tt=tt: e.tensor_tensor(out=sm[:, 64:66], in0=sm[:, 66:68], in1=RK[:, tt, :],
                                                         op=ALU.add), reads=[sm, RK], writes=[sm])
            K.op("dve", lambda e, tt=tt: e.tensor_copy(out=DEST[:, tt, :], in_=sm[:, 64:66]), reads=[sm], writes=[DEST])
            for k in range(2):
                K.dma("pool", m["XS"][:, :], ht[:], reads=[ht, DEST], writes=[m["XS"]],
                      indirect=dict(out_offset=bass.IndirectOffsetOnAxis(ap=DEST[:, tt, k:k + 1], axis=0),
                                    in_offset=None))
        K.interleave(_body, range(ntile), 1)
        w1t = self.inp["moe_w1"].ap().rearrange("l e k n -> (l e k) n")
        w3t = self.inp["moe_w3"].ap().rearrange("l e k n -> (l e k) n")
        w2t = self.inp["moe_w2"].ap().rearrange("l e k n -> (l e k) n")
        for j in range(NB):
            W1, W3, W2, xt, xTb, hTb, sil, yb = (m[k][j % 2] for k in ("W1", "W3", "W2", "xt", "xTb", "hTb", "sil", "yb"))
            for kt in range(8):
                for W, tab in ((W1, w1t), (W3, w3t)):
                    K.dma("pool", W[:, kt, :], tab, reads=[m["IDX1"]], writes=[W],
                          indirect=dict(out_offset=None,
                                        in_offset=bass.IndirectOffsetOnAxis(ap=m["IDX1"][:, j, kt:kt + 1], axis=0)))
            for fc in range(4):
                K.dma("pool", W2[:, fc, :], w2t, reads=[m["IDX2"]], writes=[W2],
                      indirect=dict(out_offset=None,
                                    in_offset=bass.IndirectOffsetOnAxis(ap=m["IDX2"][:, j, fc:fc + 1], axis=0)))
            def _sub(sub, j=j, W1=W1, W3=W3, W2=W2):
                r0 = j * self.BLK + sub * 128
                xt, xTb, hTb, sil, yb = (m[k][(j * 4 + sub) % 2] for k in ("xt", "xTb", "hTb", "sil", "yb"))
                K.dma("sp", xt[:], m["XS"][r0:r0 + 128, :], reads=[m["XS"]], writes=[xt])
                for half in range(2):
                    ps = K.ps()
                    for q in range(4):
                        kt = half * 4 + q
                        self.tr(None, ps[:, q * 128:(q + 1) * 128], xt[:, kt * 128:(kt + 1) * 128], ps, xt)
                    K.op("act" if half else "dve",
                         lambda e, ps=ps, half=half, xTb=xTb: (e.copy if half else e.tensor_copy)(
                             out=xTb[:, half * 4:(half + 1) * 4, :].rearrange("p a b -> p (a b)"), in_=ps[:, :]),
                         reads=[ps], writes=[xTb])
                pa, pb = K.ps(), K.ps()
                for W, pp in ((W1, pa), (W3, pb)):
                    for fc in range(4):
                        for kt in range(8):
                            K.op("pe", lambda e, W=W, pp=pp, fc=fc, kt=kt, xTb=xTb: e.matmul(
                                pp[:, fc * 128:(fc + 1) * 128], lhsT=W[:, kt, fc * 128:(fc + 1) * 128], rhs=xTb[:, kt, :],
                                start=(kt == 0), stop=(kt == 7)), reads=[W, xTb], writes=[pp])
                K.op("act", lambda e, pa=pa, sil=sil: e.activation(out=sil[:], in_=pa[:, :], func=AF.Silu),
                     reads=[pa], writes=[sil])
                K.op("dve", lambda e, pb=pb, sil=sil, hTb=hTb: e.tensor_tensor(
                    out=hTb[:].rearrange("p a b -> p (a b)"), in0=sil[:], in1=pb[:, :], op=ALU.mult),
                    reads=[pb, sil], writes=[hTb])
                for half in range(2):
                    py = K.ps()
                    for fc in range(4):
                        K.op("pe", lambda e, py=py, fc=fc, half=half, hTb=hTb, W2=W2: e.matmul(
                            py[:, :], lhsT=hTb[:, fc, :], rhs=W2[:, fc, half * 512:(half + 1) * 512],
                            start=(fc == 0), stop=(fc == 3)), reads=[hTb, W2], writes=[py])
                    K.op("act" if half else "dve",
                         lambda e, py=py, half=half, yb=yb: (e.copy if half else e.tensor_copy)(
                             out=yb[:, half * 512:(half + 1) * 512], in_=py[:, :]), reads=[py], writes=[yb])
                K.dma("sp", m["YS"][r0:r0 + 128, :], yb[:], reads=[yb], writes=[m["YS"]])
            K.interleave(_sub, range(self.BLK // 128), 1)
        def _body(tt):
            y0, y1, xt = m["y0"][tt % 2], m["y1"][tt % 2], m["xt"][tt % 2]
            M = self.Ml if tt < 64 else self.Mc
            for k, y in ((0, y0), (1, y1)):
                K.dma("pool", y[:], m["YS"][:, :], reads=[m["YS"], DEST], writes=[y],
                      indirect=dict(out_offset=None,
                                    in_offset=bass.IndirectOffsetOnAxis(ap=DEST[:, tt, k:k + 1], axis=0)))
            K.dma("sp", xt[:], self.lat[tt * 128:(tt + 1) * 128, :], reads=[self.lat.p(tt)], writes=[xt])
            ya = m["ya"][tt % 2]
            K.op("dve", lambda e, tt=tt, y0=y0, ya=ya: e.tensor_scalar(out=ya[:], in0=y0[:], scalar1=GATE[:, tt, 0:1],
                                                                       scalar2=None, op0=ALU.mult),
                 reads=[y0, GATE], writes=[ya])
            K.op("dve", lambda e, tt=tt, ya=ya, y1=y1: e.scalar_tensor_tensor(
                out=ya[:], in0=y1[:], scalar=GATE[:, tt, 1:2], in1=ya[:], op0=ALU.mult, op1=ALU.add),
                reads=[ya, y1, GATE], writes=[ya])
            K.op("dve", lambda e, ya=ya, M=M: e.tensor_tensor(out=ya[:], in0=ya[:], in1=M[:, 5, :], op=ALU.mult),
                 reads=[ya, M], writes=[ya])
            K.op("dve", lambda e, ya=ya, xt=xt: e.tensor_tensor(out=xt[:], in0=xt[:], in1=ya[:], op=ALU.add),
                 reads=[ya, xt], writes=[xt])
            K.dma("sp", self.lat[tt * 128:(tt + 1) * 128, :], xt[:], reads=[xt], writes=[self.lat.p(tt)])
        K.interleave(_body, range(ntile), 2)
        K.pop_scope()

    def final_norm(self):
        K = self.K
        K.push_scope()
        m = dict(xt=[K.sb([128, D], F32, f"fx{j}") for j in range(2)], ht=[K.sb([128, D], F32, f"fh{j}") for j in range(2)],
                 st=[K.sb([128, 4], F32, f"fs{j}") for j in range(2)])
        g = K.sb([128, D], F32, "fng")
        K.dma("sp", g[:], self.inp["final_norm"].ap().rearrange("(o d) -> o d", o=1).partition_broadcast(128), writes=[g])
        def _body(tt):
            xt, ht, st = m["xt"][tt % 2], m["ht"][tt % 2], m["st"][tt % 2]
            K.dma("sp", xt[:], self.lat[tt * 128:(tt + 1) * 128, :], reads=[self.lat.p(tt)], writes=[xt])
            K.op("act", lambda e, xt=xt, ht=ht, st=st: e.activation(out=ht[:], in_=xt[:], func=AF.Square,
                                                                    accum_out=st[:, 0:1]), reads=[xt], writes=[ht, st])
            K.op("dve", lambda e, st=st: e.tensor_scalar(out=st[:, 1:2], in0=st[:, 0:1], scalar1=1.0 / D, scalar2=1e-6,
                                                         op0=ALU.mult, op1=ALU.add), reads=[st], writes=[st])
            K.op("act", lambda e, st=st: e.sqrt(out=st[:, 3:4], in_=st[:, 1:2]), reads=[st], writes=[st])
            K.op("dve", lambda e, st=st: e.reciprocal(out=st[:, 2:3], in_=st[:, 3:4]), reads=[st], writes=[st])
            K.op("dve", lambda e, xt=xt, ht=ht, st=st: e.scalar_tensor_tensor(
                out=ht[:], in0=xt[:], scalar=st[:, 2:3], in1=g[:], op0=ALU.mult, op1=ALU.mult),
                reads=[xt, st, g], writes=[ht])
            K.dma("sp", self.out[tt * 128:(tt + 1) * 128, :], ht[:], reads=[ht])
        K.interleave(_body, range(64), 2)
        K.pop_scope()


def fourier_consts():
    c = {}
    n = np.arange(256)
    ang = 2 * np.pi * np.outer(n, n) / 256
    c["f_cc"] = (np.cos(ang) / 16).astype(np.float32)
    c["f_sc"] = (-np.sin(ang) / 16).astype(np.float32)
    a = np.arange(128)
    ang = 2 * np.pi * np.outer(a, a) / 128
    s = 1.0 / np.sqrt(8192.0)
    c["f_c128"] = (np.cos(ang) * s).astype(np.float32)
    c["f_s128"] = (np.sin(ang) * s).astype(np.float32)
    f1 = np.arange(128)[:, None]
    b = np.arange(64)[None, :]
    th = 2 * np.pi * f1 * b / 8192
    c["f_tw"] = np.stack([np.cos(th), -np.sin(th)], axis=2).astype(np.float32)
    bb = np.arange(64)
    ang = 2 * np.pi * np.outer(bb, bb) / 64
    c["f_cs64"] = np.concatenate([np.cos(ang), np.sin(ang)], axis=0).astype(np.float32)
    t = np.arange(256)
    ang = 2 * np.pi * np.outer(t, t) / 256
    c["f_c256"] = (np.cos(ang) / 16).astype(np.float32)
    c["f_s256"] = (np.sin(ang) / 16).astype(np.float32)
    return c


FOURIER_SPECS = {"f_cc": [256, 256], "f_sc": [256, 256], "f_c128": [128, 128], "f_s128": [128, 128],
                 "f_tw": [128, 64, 2], "f_cs64": [128, 64], "f_c256": [256, 256], "f_s256": [256, 256]}


def _fourier_declare(self):
    nc = self.K.nc
    for k, shp in FOURIER_SPECS.items():
        self.inp[k] = nc.dram_tensor(k, shp, F32, kind="ExternalInput")
    self.GD = Buf(nc.dram_tensor("GD", [128, 128, D], F32), "GD")


def _fourier(self, i, with_ctx, nb=64, nf=128):
    K = self.K
    j = i // 2
    K.push_scope()
    cc = K.sb([128, 2, 2, 256], BF16, "fcc")
    c128 = K.sb([128, 3, 128], BF16, "fc128")
    tw = K.sb([128, 64, 2], F32, "ftw")
    cs64 = K.sb([128, 64], F32, "fcs64")
    wf = K.sb([128, 8, D], BF16, "fwf")
    big = [K.sb([128, D], F32, f"fbig{q}") for q in range(6)]
    xts, hts, gsb = big[0:2], big[2:4], big[4:6]
    sts = [K.sb([128, 4], F32, f"fst{q}") for q in range(2)]
    hTb = [K.sb([128, 8, 128], BF16, f"fhT{q}") for q in range(2)]
    Zb = [K.sb([128, 2, D], BF16, f"fZ{q}") for q in range(2)]
    gi2 = [K.sb([128, D], F32, f"fgi{q}") for q in range(2)]
    tmp = K.sb([128, 256], F32, "ftmp")
    def ld_cast(dst_ap, dst_buf, src_ap, shape, n=[0]):
        stg = big[4 + n[0] % 2]
        n[0] += 1
        rows, cols = shape
        view = stg[0:rows, 0:cols]
        K.dma("sp", view, src_ap, writes=[stg])
        K.op("dve", lambda e: e.tensor_copy(out=dst_ap, in_=view), reads=[stg], writes=[dst_buf])
    for kt in range(2):
        ld_cast(cc[:, kt, 0, :], cc, self.inp["f_cc"][kt * 128:(kt + 1) * 128, :], (128, 256))
        ld_cast(cc[:, kt, 1, :], cc, self.inp["f_sc"][kt * 128:(kt + 1) * 128, :], (128, 256))
    ld_cast(c128[:, 0, :], c128, self.inp["f_c128"][:, :], (128, 128))
    ld_cast(c128[:, 1, :], c128, self.inp["f_s128"][:, :], (128, 128))
    K.op("dve", lambda e: e.tensor_scalar(out=c128[:, 2, :], in0=c128[:, 1, :], scalar1=-1.0, scalar2=None,
                                          op0=ALU.mult), reads=[c128], writes=[c128])
    for kt in range(8):
        ld_cast(wf[:, kt, :], wf, self.inp["w_fourier"][j, kt * 128:(kt + 1) * 128, :], (128, 1024))
    self._ld_cast = ld_cast
    K.dma("sp", tw[:], self.inp["f_tw"][:, :, :], writes=[tw])
    K.dma("sp", cs64[:], self.inp["f_cs64"][:, :], writes=[cs64])
    ntw = K.sb([128, 64], F32, "fntw")
    K.op("dve", lambda e: e.tensor_scalar(out=ntw[:], in0=tw[:, :, 1], scalar1=-1.0, scalar2=None, op0=ALU.mult),
         reads=[tw], writes=[ntw])
    latv = self.lat[0:T, :].rearrange("(a b) d -> b a d", b=64)
    lat_all = [self.lat.p(t) for t in range(64)]

    def chan_dft(xt, ht, st, hT, Z, M):
        self.norm_tile(xt, ht, M, 0, st)
        for half in range(2):
            ps = K.ps()
            for q in range(4):
                kt = half * 4 + q
                self.tr(None, ps[:, q * 128:(q + 1) * 128], ht[:, kt * 128:(kt + 1) * 128], ps, ht)
            K.op("act" if half else "dve",
                 lambda e, ps=ps, half=half: (e.copy if half else e.tensor_copy)(
                     out=hT[:, half * 4:(half + 1) * 4, :].rearrange("p a b -> p (a b)"), in_=ps[:, :]),
                 reads=[ps], writes=[hT])
        for ri in range(2):
            for hh in range(2):
                ps = K.ps()
                for gg in range(2):
                    g = hh * 2 + gg
                    for kt in range(2):
                        K.op("pe", lambda e, ps=ps, gg=gg, g=g, kt=kt, ri=ri: e.matmul(
                            ps[:, gg * 256:(gg + 1) * 256], lhsT=hT[:, g * 2 + kt, :], rhs=cc[:, kt, ri, :],
                            start=(kt == 0), stop=(kt == 1)), reads=[hT, cc], writes=[ps])
                K.op("act" if hh else "dve",
                     lambda e, ps=ps, hh=hh, ri=ri: (e.copy if hh else e.tensor_copy)(
                         out=Z[:, ri, hh * 512:(hh + 1) * 512], in_=ps[:, :]), reads=[ps], writes=[Z])

    def _body(b):
        xt, ht, st, hT, Z, gr, gi = (l[b % 2] for l in (xts, hts, sts, hTb, Zb, gsb, gi2))
        K.dma("sp", xt[:], latv[b], reads=lat_all, writes=[xt])
        chan_dft(xt, ht, st, hT, Z, self.Ml)
        for hh in range(2):
            cs_ = slice(hh * 512, (hh + 1) * 512)
            pr, pi_ = K.ps(), K.ps()
            K.op("pe", lambda e: e.matmul(pr[:, :], lhsT=c128[:, 0, :], rhs=Z[:, 0, cs_], start=True, stop=False),
                 reads=[c128, Z], writes=[pr])
            K.op("pe", lambda e: e.matmul(pr[:, :], lhsT=c128[:, 1, :], rhs=Z[:, 1, cs_], start=False, stop=True),
                 reads=[c128, Z], writes=[pr])
            K.op("pe", lambda e: e.matmul(pi_[:, :], lhsT=c128[:, 0, :], rhs=Z[:, 1, cs_], start=True, stop=False),
                 reads=[c128, Z], writes=[pi_])
            K.op("pe", lambda e: e.matmul(pi_[:, :], lhsT=c128[:, 2, :], rhs=Z[:, 0, cs_], start=False, stop=True),
                 reads=[c128, Z], writes=[pi_])
            K.op("dve", lambda e: e.tensor_scalar(out=gr[:, cs_], in0=pr[:, :], scalar1=tw[:, b, 0:1], scalar2=None,
                                                  op0=ALU.mult), reads=[pr, tw], writes=[gr])
            K.op("dve", lambda e: e.scalar_tensor_tensor(out=gr[:, cs_], in0=pi_[:, :], scalar=ntw[:, b:b + 1],
                                                         in1=gr[:, cs_], op0=ALU.mult, op1=ALU.add),
                 reads=[pi_, ntw, gr], writes=[gr])
            K.op("dve", lambda e: e.tensor_scalar(out=gi[:, cs_], in0=pi_[:, :], scalar1=tw[:, b, 0:1], scalar2=None,
                                                  op0=ALU.mult), reads=[pi_, tw], writes=[gi])
            K.op("dve", lambda e: e.scalar_tensor_tensor(out=gi[:, cs_], in0=pr[:, :], scalar=tw[:, b, 1:2],
                                                         in1=gi[:, cs_], op0=ALU.mult, op1=ALU.add),
                 reads=[pr, tw, gi], writes=[gi])
        K.dma("act", self.GD[b, :, :], gr[:], reads=[gr], writes=[self.GD])
        K.dma("act", self.GD[64 + b, :, :], gi[:], reads=[gi], writes=[self.GD])
    K.interleave(_body, range(nb), 2)
    latf = self.lat[0:T, :].rearrange("(f2 f1) d -> f1 f2 d", f1=128)
    YT = [K.sb([128, 8, 64], BF16, f"fYT{q}") for q in range(2)]
    def _body(f1):
        gd, yt, xt, ht = gsb[f1 % 2], YT[f1 % 2], xts[f1 % 2], hts[f1 % 2]
        K.dma("sp", gd[:], self.GD[:, f1, :], reads=[self.GD], writes=[gd])
        K.dma("sp", xt[0:64, :], latf[f1], reads=lat_all, writes=[xt])
        ps = K.ps()
        for kt in range(8):
            K.op("pe", lambda e, kt=kt: e.matmul(ps[:, kt * 64:(kt + 1) * 64], lhsT=gd[:, kt * 128:(kt + 1) * 128],
                                                 rhs=cs64[:, :], start=True, stop=True), reads=[gd, cs64], writes=[ps])
        K.op("act", lambda e: e.copy(out=yt[:].rearrange("p a b -> p (a b)"), in_=ps[:, :]), reads=[ps], writes=[yt])
        for hh in range(2):
            py = K.ps()
            for kt in range(8):
                K.op("pe", lambda e, kt=kt: e.matmul(py[0:64, :], lhsT=yt[:, kt, :],
                                                     rhs=wf[:, kt, hh * 512:(hh + 1) * 512], start=(kt == 0),
                                                     stop=(kt == 7)), reads=[yt, wf], writes=[py])
            K.op("dve", lambda e: e.tensor_tensor(out=ht[0:64, hh * 512:(hh + 1) * 512], in0=py[0:64, :],
                                                  in1=self.Ml[0:64, 2, hh * 512:(hh + 1) * 512], op=ALU.mult),
                 reads=[py, self.Ml], writes=[ht])
        K.op("dve", lambda e: e.tensor_tensor(out=xt[0:64, :], in0=xt[0:64, :], in1=ht[0:64, :], op=ALU.add),
             reads=[xt, ht], writes=[xt])
        K.dma("act", latf[f1], xt[0:64, :], reads=[xt], writes=lat_all)
    K.interleave(_body, range(nf), 2)
    if with_ctx:
        c256 = K.sb([128, 2, 2, 256], BF16, "fc256")
        for tt in range(2):
            ld_cast(c256[:, 0, tt, :], c256, self.inp["f_c256"][tt * 128:(tt + 1) * 128, :], (128, 256))
            ld_cast(c256[:, 1, tt, :], c256, self.inp["f_s256"][tt * 128:(tt + 1) * 128, :], (128, 256))
        ctxp = [self.lat.p(64), self.lat.p(65)]
        for tt in range(2):
            K.dma("sp", xts[tt][:], self.lat[T + tt * 128:T + (tt + 1) * 128, :], reads=ctxp, writes=[xts[tt]])
            chan_dft(xts[tt], hts[tt], sts[tt], hTb[tt], Zb[tt], self.Mc)
        for ft in range(2):
            yt = K.sb([128, 8, 128], BF16, f"fcy{ft}")
            for half in range(2):
                ps = K.ps()
                for q in range(4):
                    kt = half * 4 + q
                    n = 0
                    for tt in range(2):
                        for cs_i in range(2):
                            K.op("pe", lambda e, kt=kt, q=q, tt=tt, cs_i=cs_i, n=n: e.matmul(
                                ps[:, q * 128:(q + 1) * 128], lhsT=Zb[tt][:, cs_i, kt * 128:(kt + 1) * 128],
                                rhs=c256[:, cs_i, tt, ft * 128:(ft + 1) * 128], start=(n == 0), stop=(n == 3)),
                                reads=[Zb[tt], c256], writes=[ps])
                            n += 1
                K.op("act", lambda e, half=half: e.copy(out=yt[:, half * 4:(half + 1) * 4, :].rearrange("p a b -> p (a b)"),
                                                        in_=ps[:, :]), reads=[ps], writes=[yt])
            xt, ht = xts[ft], hts[ft]
            for hh in range(2):
                py = K.ps()
                for kt in range(8):
                    K.op("pe", lambda e, kt=kt: e.matmul(py[:, :], lhsT=yt[:, kt, :],
                                                         rhs=wf[:, kt, hh * 512:(hh + 1) * 512], start=(kt == 0),
                                                         stop=(kt == 7)), reads=[yt, wf], writes=[py])
                K.op("dve", lambda e: e.tensor_tensor(out=ht[:, hh * 512:(hh + 1) * 512], in0=py[:, :],
                                                      in1=self.Mc[:, 2, hh * 512:(hh + 1) * 512], op=ALU.mult),
                     reads=[py, self.Mc], writes=[ht])
            K.op("dve", lambda e: e.tensor_tensor(out=xt[:], in0=xt[:], in1=ht[:], op=ALU.add),
                 reads=[xt, ht], writes=[xt])
            K.dma("act", self.lat[T + ft * 128:T + (ft + 1) * 128, :], xt[:], reads=[xt], writes=ctxp)
    K.pop_scope()


Prog.fourier_declare = _fourier_declare
Prog.fourier = _fourier


POOL_WINS = (2, 4, 8, 16)


def pool_consts():
    c = {}
    for nm, L in (("f_icnt_lat", T), ("f_icnt_ctx", CT)):
        a = np.zeros((4, L), np.float32)
        pos = np.arange(L)
        for gi, win in enumerate(POOL_WINS):
            half = win // 2
            hi = np.minimum(pos + half, L)
            lo = np.maximum(pos - half, 0)
            a[gi] = 1.0 / (hi - lo)
        c[nm] = a
    return c


def _even_declare(self):
    nc = self.K.nc
    self.inp["f_icnt_lat"] = nc.dram_tensor("f_icnt_lat", [4, T], F32, kind="ExternalInput")
    self.inp["f_icnt_ctx"] = nc.dram_tensor("f_icnt_ctx", [4, CT], F32, kind="ExternalInput")
    e = {}
    e["HT"] = Buf(nc.dram_tensor("HT", [D, NT], F32), "HT")
    e["PT"] = Buf(nc.dram_tensor("PT", [2048, NT], F32), "PT")
    for d in range(2):
        for nm in ("At", "Bt", "Kt", "Rt", "Bh", "Kh"):
            e[f"{nm}{d}"] = Buf(nc.dram_tensor(f"SC_{nm}{d}", [512, NT], F32), f"{nm}{d}")
        e[f"GL{d}"] = Buf(nc.dram_tensor(f"SC_GL{d}", [512, NT // 64], F32), f"GL{d}")
        e[f"YD{d}"] = Buf(nc.dram_tensor(f"SC_YD{d}", [NT, 512], F32), f"YD{d}")
    for nm in ("VV", "GG", "BON", "YB"):
        e[nm] = Buf(nc.dram_tensor(f"SC_{nm}", [512, NT], F32), nm)
    self.ed = e


def _even_e1(self, i, ntile=66):
    K = self.K
    j = i // 2
    e = self.ed
    K.push_scope()
    win = K.sb([128, 8, 2048], BF16, "win")
    stg = [K.sb([128, D], F32, f"e1stg{q}") for q in range(2)]
    n = 0
    for kt in range(8):
        for hh in range(2):
            sg = stg[n % 2]
            n += 1
            K.dma("sp", sg[:], self.inp["w_in"][j, kt * 128:(kt + 1) * 128, hh * 1024:(hh + 1) * 1024], writes=[sg])
            K.op("dve" if n % 2 else "act",
                 lambda en, sg=sg, kt=kt, hh=hh: (en.tensor_copy if n % 2 else en.copy)(
                     out=win[:, kt, hh * 1024:(hh + 1) * 1024], in_=sg[:]), reads=[sg], writes=[win])
    xts = [K.sb([128, D], F32, f"e1x{q}") for q in range(3)]
    hts = [K.sb([128, D], F32, f"e1h{q}") for q in range(3)]
    sts = [K.sb([128, 4], F32, f"e1s{q}") for q in range(3)]
    hTf = [K.sb([128, 8, 128], F32, f"e1hTf{q}") for q in range(3)]
    hTb = [K.sb([128, 8, 128], BF16, f"e1hTb{q}") for q in range(3)]
    pts = [K.sb([128, 16, 128], F32, f"e1pt{q}") for q in range(3)]
    def _body(tt):
        xt, ht, st, hf, hb, pt = (l[tt % 3] for l in (xts, hts, sts, hTf, hTb, pts))
        M = self.Ml if tt < 64 else self.Mc
        K.dma("sp", xt[:], self.lat[tt * 128:(tt + 1) * 128, :], reads=[self.lat.p(tt)], writes=[xt])
        self.norm_tile(xt, ht, M, 0, st)
        for half in range(2):
            ps = K.ps()
            for q in range(4):
                kt = half * 4 + q
                self.tr(None, ps[:, q * 128:(q + 1) * 128], ht[:, kt * 128:(kt + 1) * 128], ps, ht)
            K.op("act", lambda en, ps=ps, half=half: en.copy(
                out=hf[:, half * 4:(half + 1) * 4, :].rearrange("p a b -> p (a b)"), in_=ps[:, :]),
                reads=[ps], writes=[hf])
            K.op("dve", lambda en, half=half: en.tensor_copy(
                out=hb[:, half * 4:(half + 1) * 4, :], in_=hf[:, half * 4:(half + 1) * 4, :]),
                reads=[hf], writes=[hb])
        import os
        if not os.environ.get("SKIPHT"):
            K.dma("act", e["HT"][:, tt * 128:(tt + 1) * 128].rearrange("(kt k) t -> k kt t", k=128), hf[:],
                  reads=[hf], writes=[e["HT"]])
        for ob in range(4):
            ps = K.ps()
            for q in range(4):
                oc = ob * 4 + q
                for kt in range(8):
                    K.op("pe", lambda en, ps=ps, q=q, oc=oc, kt=kt: en.matmul(
                        ps[:, q * 128:(q + 1) * 128], lhsT=win[:, kt, oc * 128:(oc + 1) * 128], rhs=hb[:, kt, :],
                        start=(kt == 0), stop=(kt == 7)), reads=[win, hb], writes=[ps])
            K.op("act" if ob % 2 else "dve", lambda en, ps=ps, ob=ob: (en.copy if ob % 2 else en.tensor_copy)(
                out=pt[:, ob * 4:(ob + 1) * 4, :].rearrange("p a b -> p (a b)"), in_=ps[:, :]),
                reads=[ps], writes=[pt])
        if not os.environ.get("SKIPPT"):
            K.dma("act", e["PT"][:, tt * 128:(tt + 1) * 128].rearrange("(oc k) t -> k oc t", k=128), pt[:],
                  reads=[pt], writes=[e["PT"]])
    K.interleave(_body, range(ntile), 3)
    K.pop_scope()


def _even_e2(self, i, dbg=None):
    K = self.K
    j = i // 2
    e = self.ed
    K.push_scope()
    WB = 512
    WT = WB + 128
    eng_rr = [0]

    def ve():
        eng_rr[0] += 1
        return "dve"

    stg = K.sb([128, 8, 128], F32, "e2stg")
    W1 = K.sb([128, 3, 8, 128], BF16, "e2W1")
    W2 = K.sb([128, 3, 512], BF16, "e2W2")
    pw = K.sb([128, 4, 128], BF16, "e2pw")
    for v, (nm, nd) in enumerate((("decay_w1", 2), ("lr_a1", 2), ("gate_g1", 1))):
        for d in range(nd):
            src = self.inp[nm][j, d] if nd == 2 else self.inp[nm][j]
            wcol = 64 if nd == 2 else 128
            K.dma("sp", stg[:, :, 0:wcol], src.rearrange("(kt k) r -> k kt r", k=128), writes=[stg])
            K.op("dve", lambda en, v=v, d=d, wcol=wcol: en.tensor_copy(out=W1[:, v, :, d * wcol:(d + 1) * wcol],
                                                                       in_=stg[:, :, 0:wcol]), reads=[stg], writes=[W1])
    stg2 = stg[:].rearrange("p a b -> p (a b)")
    for v, nm in enumerate(("decay_w2", "lr_a2")):
        for d in range(2):
            K.dma("sp", stg2[d * 64:(d + 1) * 64, 0:512], self.inp[nm][j, d], writes=[stg])
        K.op("dve", lambda en, v=v: en.tensor_copy(out=W2[:, v, :], in_=stg2[:, 0:512]), reads=[stg], writes=[W2])
    K.dma("sp", stg2[:, 0:512], self.inp["gate_g2"][j], writes=[stg])
    K.op("dve", lambda en: en.tensor_copy(out=W2[:, 2, :], in_=stg2[:, 0:512]), reads=[stg], writes=[W2])
    for gi in range(4):
        K.dma("sp", stg2[:, gi * 128:(gi + 1) * 128], self.inp["pool_w"][j, gi], writes=[stg])
    K.op("dve", lambda en: en.tensor_copy(out=pw[:].rearrange("p a b -> p (a b)"), in_=stg2[:, 0:512]),
         reads=[stg], writes=[pw])
    MU = K.sb([128, 2, 3, 8], F32, "e2MU")
    MUP = K.sb([128, 2, 3, 4], F32, "e2MUP")
    COL = K.sb([128, 12, 4], F32, "e2COL")
    K.dma("sp", MU[:, 0, :, :], self.inp["mu_x"][j].rearrange("v (kt k) -> k v kt", k=128), writes=[MU],
          allow_slow_non_contiguous=True)
    K.dma("sp", MUP[:, 0, :, :], self.inp["mu_p"][j].rearrange("v (c k) -> k v c", k=128), writes=[MUP],
          allow_slow_non_contiguous=True)
    for d in range(2):
        K.dma("sp", COL[:, d, :], self.inp["decay_w0"][j, d].rearrange("(c k) -> k c", k=128), writes=[COL],
              allow_slow_non_contiguous=True)
        K.dma("sp", COL[:, 2 + d, :], self.inp["lr_a0"][j, d].rearrange("(c k) -> k c", k=128), writes=[COL],
              allow_slow_non_contiguous=True)
    K.dma("sp", COL[:, 4, :], self.inp["k_k"][j].rearrange("(c k) -> k c", k=128), writes=[COL], allow_slow_non_contiguous=True)
    K.dma("sp", COL[:, 5, :], self.inp["k_a"][j].rearrange("(c k) -> k c", k=128), writes=[COL], allow_slow_non_contiguous=True)
    K.dma("sp", COL[:, 6, :], self.inp["r_k"][j].rearrange("(c h2) k -> (h2 k) c", h2=2), writes=[COL],
          allow_slow_non_contiguous=True)
    K.dma("sp", COL[:, 7, :], self.inp["pool_scale"][j].rearrange("(c k) -> k c", k=128), writes=[COL],
          allow_slow_non_contiguous=True)
    for Mx in (MU, MUP):
        K.op("dve", lambda en, Mx=Mx: en.tensor_scalar(out=Mx[:, 1], in0=Mx[:, 0], scalar1=-1.0, scalar2=1.0,
                                                       op0=ALU.mult, op1=ALU.add), reads=[Mx], writes=[Mx])
    bones = K.sb([128, 128], F32, "e2bones")
    K.op("pool", lambda en: en.memset(bones[:], 0.0), writes=[bones])
    K.op("pool", lambda en: en.memset(bones[0:64, 0:64], 1.0), reads=[bones], writes=[bones])
    K.op("pool", lambda en: en.memset(bones[64:128, 64:128], 1.0), reads=[bones], writes=[bones])
    HTt = K.sb([128, 8, WT], F32, "e2HTt")
    xv = K.sb([128, 8, WB], BF16, "e2xv")
    t1 = [K.sb([128, WB], BF16, f"e2t1{v}") for v in range(3)]
    PTt = [K.sb([128, WT], F32, f"e2PTt{n}") for n in range(4)]
    nm_w = ("Rm", "Km", "Vm", "kk", "sq", "rn", "LW0", "LW1", "AD0", "AD1", "Gm", "kd0", "kd1", "b0", "b1", "lw0", "lw1",
            "cum0", "cum1", "E0", "E1", "tmp0", "tmp1", "tmp", "ks", "IC", "o0", "o1", "o2", "o3", "o4", "o5")
    w = {n: K.sb([128, WB], F32, "e2" + n) for n in nm_w}
    sw = [K.sb([128, WT], F32, f"e2s{q}") for q in range(2)]
    dfb = K.sb([128, WB], BF16, "e2dfb")
    gls = [K.sb([128, 8], F32, f"e2gl{d}") for d in range(2)]
    RM = K.sb([128, WB], F32, "e2RM")
    K.op("pool", lambda en: en.memset(RM[:], 1.0), writes=[RM])
    K.op("pool", lambda en: en.memset(RM[:].rearrange("p (r c) -> p r c", c=64)[:, :, 0:1], 0.0), reads=[RM], writes=[RM])
    orr = [0]

    def otile():
        orr[0] += 1
        return w[f"o{orr[0] % 6}"]

    GRID_H = [(-1, True)] * 2 + [(1, True)] * 2 + [(-64, False)] * 2 + [(64, False)] * 2
    GRID_P = [(-1, True), (1, True), (-64, False), (64, False)]
    SEQ_H = [(-1, False)] * 4 + [(1, False)] * 4
    SEQ_P = [(-1, False)] * 2 + [(1, False)] * 2
    blocks = [(T, CT, T, CT, SEQ_H, SEQ_P, "f_icnt_ctx")] + \
             [(t0, WB, 0, T, GRID_H, GRID_P, "f_icnt_lat") for t0 in range(0, T, WB)]

    def load_halo(buf, view, src_rows, t0, Wb, s0, sl):
        lo = max(t0 - 64, s0)
        hi = min(t0 + Wb + 64, s0 + sl)
        if lo > t0 - 64:
            K.op("pool", lambda en: en.memset(view(0, 64), 0.0), writes=[buf])
        if hi < t0 + Wb + 64:
            K.op("pool", lambda en: en.memset(view(64 + Wb, 128 + Wb), 0.0), writes=[buf])
        K.dma("sp", view(lo - (t0 - 64), hi - (t0 - 64)), src_rows(lo, hi), reads=[e["HT"], e["PT"]], writes=[buf])

    def mix(out_ap, out_buf, srcf, src_buf, mu_ap, omu_ap, delta, rowmask, Wb):
        en1 = "dve"
        K.op("act", lambda en: en.mul(out=out_ap, in_=srcf(64, 64 + Wb), mul=omu_ap), reads=[src_buf], writes=[out_buf])
        if not rowmask:
            o, s_ = out_ap, srcf(64 + delta, 64 + delta + Wb)
        else:
            ov = out_ap.rearrange("p (r c) -> p r c", c=64)
            sv = srcf(64 + delta, 64 + delta + Wb).rearrange("p (r c) -> p r c", c=64)
            if delta == -1:
                o, s_ = ov[:, :, 1:64], sv[:, :, 1:64]
            else:
                o, s_ = ov[:, :, 0:63], sv[:, :, 0:63]
        K.op(en1, lambda en: en.scalar_tensor_tensor(out=o, in0=s_, scalar=mu_ap, in1=o, op0=ALU.mult, op1=ALU.add),
             reads=[src_buf, out_buf], writes=[out_buf])

    stq = [0]

    def store(dst, c, t0, Wb, tile):
        stq[0] += 1
        K.dma("act" if stq[0] % 2 else "sp", dst[c * 128:(c + 1) * 128, t0:t0 + Wb], tile[:, 0:Wb], reads=[tile],
              writes=[dst])

    for (t0, Wb, s0, sl, HS, PS, icn) in blocks:
        R = Wb // 64
        load_halo(HTt, lambda a, b: HTt[:, :, a:b],
                  lambda a, b: e["HT"][:, a:b].rearrange("(kt k) t -> k kt t", k=128), t0, Wb, s0, sl)
        for v in range(3):
            for kt in range(8):
                dl, rm = HS[kt]
                mix(xv[:, kt, 0:Wb], xv, lambda a, b, kt=kt: HTt[:, kt, a:b], HTt, MU[:, 0, v, kt:kt + 1],
                    MU[:, 1, v, kt:kt + 1], dl, rm, Wb)
            ps = K.ps()
            for kt in range(8):
                K.op("pe", lambda en, kt=kt, v=v: en.matmul(ps[:, 0:Wb], lhsT=W1[:, v, kt, :], rhs=xv[:, kt, 0:Wb],
                                                            start=(kt == 0), stop=(kt == 7)), reads=[W1, xv], writes=[ps])
            fn = (AF.Tanh, AF.Copy, AF.Sigmoid)[v]
            K.op("act", lambda en, v=v, fn=fn: en.activation(out=t1[v][:, 0:Wb], in_=ps[:, 0:Wb], func=fn),
                 reads=[ps], writes=[t1[v]])
        for c in range(4):
            cs_ = slice(c * 128, (c + 1) * 128)
            for n in range(4):
                load_halo(PTt[n], lambda a, b, n=n: PTt[n][:, a:b],
                          lambda a, b, n=n: e["PT"][n * 512 + c * 128:n * 512 + (c + 1) * 128, a:b], t0, Wb, s0, sl)
            dl, rm = PS[c]
            for n, nm in enumerate(("Rm", "Km", "Vm")):
                mix(w[nm][:, 0:Wb], w[nm], lambda a, b, n=n: PTt[n][:, a:b], PTt[n], MUP[:, 0, n, c:c + 1],
                    MUP[:, 1, n, c:c + 1], dl, rm, Wb)
            store(e["VV"], c, t0, Wb, w["Vm"])
            for d in range(2):
                ps = K.ps()
                K.op("pe", lambda en, d=d: en.matmul(ps[:, 0:Wb], lhsT=W2[d * 64:(d + 1) * 64, 0, cs_],
                                                     rhs=t1[0][d * 64:(d + 1) * 64, 0:Wb], start=True, stop=True),
                     reads=[W2, t1[0]], writes=[ps])
                K.op("act", lambda en, d=d: en.activation(out=w[f"LW{d}"][:, 0:Wb], in_=ps[:, 0:Wb], func=AF.Sigmoid,
                                                          bias=COL[:, d, c:c + 1], scale=1.0),
                     reads=[ps, COL], writes=[w[f"LW{d}"]])
                ps = K.ps()
                K.op("pe", lambda en, d=d: en.matmul(ps[:, 0:Wb], lhsT=W2[d * 64:(d + 1) * 64, 1, cs_],
                                                     rhs=t1[1][d * 64:(d + 1) * 64, 0:Wb], start=True, stop=True),
                     reads=[W2, t1[1]], writes=[ps])
                K.op("act", lambda en, d=d: en.activation(out=w[f"AD{d}"][:, 0:Wb], in_=ps[:, 0:Wb], func=AF.Sigmoid,
                                                          bias=COL[:, 2 + d, c:c + 1], scale=1.0),
                     reads=[ps, COL], writes=[w[f"AD{d}"]])
            ps = K.ps()
            K.op("pe", lambda en: en.matmul(ps[:, 0:Wb], lhsT=W2[:, 2, cs_], rhs=t1[2][:, 0:Wb], start=True, stop=True),
                 reads=[W2, t1[2]], writes=[ps])
            K.op("act", lambda en: en.copy(out=w["Gm"][:, 0:Wb], in_=ps[:, 0:Wb]), reads=[ps], writes=[w["Gm"]])
            store(e["GG"], c, t0, Wb, w["Gm"])
            K.op("dve", lambda en: en.tensor_scalar(out=w["kk"][:, 0:Wb], in0=w["Km"][:, 0:Wb], scalar1=COL[:, 4, c:c + 1],
                                                    scalar2=None, op0=ALU.mult), reads=[w["Km"], COL], writes=[w["kk"]])
            K.op("act", lambda en: en.square(out=w["sq"][:, 0:Wb], in_=w["kk"][:, 0:Wb]), reads=[w["kk"]], writes=[w["sq"]])
            ps = K.ps()
            K.op("pe", lambda en: en.matmul(ps[:, 0:Wb], lhsT=bones[:], rhs=w["sq"][:, 0:Wb], start=True, stop=True),
                 reads=[bones, w["sq"]], writes=[ps])
            K.op("dve", lambda en: en.tensor_scalar(out=w["rn"][:, 0:Wb], in0=ps[:, 0:Wb], scalar1=1e-12, scalar2=None,
                                                    op0=ALU.max), reads=[ps], writes=[w["rn"]])
            K.op("act", lambda en: en.sqrt(out=w["rn"][:, 0:Wb], in_=w["rn"][:, 0:Wb]), reads=[w["rn"]], writes=[w["rn"]])
            K.op("dve", lambda en: en.reciprocal(out=w["rn"][:, 0:Wb], in_=w["rn"][:, 0:Wb]), reads=[w["rn"]],
                 writes=[w["rn"]])
            K.op("dve", lambda en: en.tensor_tensor(out=w["kk"][:, 0:Wb], in0=w["kk"][:, 0:Wb], in1=w["rn"][:, 0:Wb],
                                                    op=ALU.mult), reads=[w["kk"], w["rn"]], writes=[w["kk"]])
            def dchain(d):
                AD, LW = w[f"AD{d}"], w[f"LW{d}"]
                tmp, kd, bb, lw, cum, E = (w[f"{nm_}{d}"] for nm_ in ("tmp", "kd", "b", "lw", "cum", "E"))
                K.op("dve", lambda en: en.tensor_scalar(out=tmp[:, 0:Wb], in0=AD[:, 0:Wb], scalar1=-1.0,
                                                        scalar2=COL[:, 5, c:c + 1], op0=ALU.add, op1=ALU.mult),
                     reads=[AD, COL], writes=[tmp])
                K.op("dve", lambda en: en.tensor_tensor(out=bb[:, 0:Wb], in0=w["kk"][:, 0:Wb], in1=AD[:, 0:Wb],
                                                         op=ALU.mult), reads=[w["kk"], AD], writes=[bb])
                K.op("act", lambda en: en.mul(out=lw[:, 0:Wb], in_=LW[:, 0:Wb], mul=-0.6065306597126334),
                     reads=[LW], writes=[lw])
                yield
                K.op("dve", lambda en: en.scalar_tensor_tensor(out=kd[:, 0:Wb], in0=tmp[:, 0:Wb], scalar=1.0,
                                                               in1=w["Km"][:, 0:Wb], op0=ALU.add, op1=ALU.mult),
                     reads=[tmp, w["Km"]], writes=[kd])
                yield
                K.op("dve", lambda en: en.tensor_tensor_scan(out=cum[:, 0:Wb], data0=RM[:, 0:Wb], data1=lw[:, 0:Wb],
                                                             initial=0.0, op0=ALU.mult, op1=ALU.add),
                     reads=[RM, lw], writes=[cum])
                yield
                cumv = cum[:, 0:Wb].rearrange("p (r c) -> p r c", c=64)
                if d == 1:
                    K.op("dve", lambda en: en.tensor_tensor(out=tmp[:, 0:Wb], in0=lw[:, 0:Wb], in1=cum[:, 0:Wb],
                                                             op=ALU.subtract), reads=[lw, cum], writes=[tmp])
                    yield
                    tv0 = tmp[:, 0:Wb].rearrange("p (r c) -> p r c", c=64)
                    K.op("dve", lambda en: en.tensor_tensor(out=E[:, 0:Wb].rearrange("p (r c) -> p r c", c=64), in0=tv0,
                                                            in1=cumv[:, :, 63].unsqueeze(2).to_broadcast([128, R, 64]),
                                                            op=ALU.add), reads=[tmp, cum], writes=[E])
                    yield
                    K.op("dve", lambda en: en.tensor_copy(out=cum[:, 0:Wb], in_=E[:, 0:Wb]), reads=[E], writes=[cum])
                    yield
                last = 63 if d == 0 else 0
                cL = cumv[:, :, last]
                gl = gls[d]
                K.op("act", lambda en: en.activation(out=gl[:, 0:R], in_=cL, func=AF.Exp), reads=[cum], writes=[gl])
                K.dma("sp", e[f"GL{d}"][c * 128:(c + 1) * 128, t0 // 64:t0 // 64 + R], gl[:, 0:R], reads=[gl],
                      writes=[e[f"GL{d}"]])
                K.op("act", lambda en: en.activation(out=E[:, 0:Wb], in_=cum[:, 0:Wb], func=AF.Exp), reads=[cum],
                     writes=[E])
                K.op("dve", lambda en: en.tensor_tensor(out=tmp[:, 0:Wb], in0=cum[:, 0:Wb], in1=lw[:, 0:Wb],
                                                         op=ALU.subtract), reads=[cum, lw], writes=[tmp])
                yield
                o = otile()
                K.op("dve", lambda en: en.tensor_tensor(out=o[:, 0:Wb], in0=w["Rm"][:, 0:Wb], in1=E[:, 0:Wb],
                                                        op=ALU.mult), reads=[w["Rm"], E], writes=[o])
                store(e[f"Rt{d}"], c, t0, Wb, o)
                yield
                K.op("act", lambda en: en.activation(out=E[:, 0:Wb], in_=cum[:, 0:Wb], func=AF.Exp, scale=-1.0),
                     reads=[cum], writes=[E])
                yield
                for src_, dn in ((bb, "Bt"), (kd, "Kt")):
                    o = otile()
                    K.op(ve(), lambda en, o=o, src_=src_: en.tensor_tensor(out=o[:, 0:Wb], in0=src_[:, 0:Wb],
                                                                           in1=E[:, 0:Wb], op=ALU.mult),
                         reads=[src_, E], writes=[o])
                    store(e[f"{dn}{d}"], c, t0, Wb, o)
                yield
                K.op("act", lambda en: en.activation(out=E[:, 0:Wb], in_=tmp[:, 0:Wb], func=AF.Exp),
                     reads=[tmp], writes=[E])
                yield
                o = otile()
                K.op("dve", lambda en: en.scalar_tensor_tensor(out=o[:, 0:Wb], in0=w["kk"][:, 0:Wb], scalar=-1.0,
                                                               in1=E[:, 0:Wb], op0=ALU.mult, op1=ALU.mult),
                     reads=[w["kk"], E], writes=[o])
                store(e[f"At{d}"], c, t0, Wb, o)
                tv = tmp[:, 0:Wb].rearrange("p (r c) -> p r c", c=64)
                K.op("dve", lambda en: en.tensor_tensor(out=tv, in0=cumv, in1=cL.unsqueeze(2).to_broadcast([128, R, 64]),
                                                        op=ALU.subtract), reads=[cum], writes=[tmp])
                yield
                K.op("act", lambda en: en.activation(out=E[:, 0:Wb], in_=tmp[:, 0:Wb], func=AF.Exp, scale=-1.0),
                     reads=[tmp], writes=[E])
                yield
                for src_, dn in ((bb, "Bh"), (kd, "Kh")):
                    o = otile()
                    K.op(ve(), lambda en, o=o, src_=src_: en.tensor_tensor(out=o[:, 0:Wb], in0=src_[:, 0:Wb],
                                                                           in1=E[:, 0:Wb], op=ALU.mult),
                         reads=[src_, E], writes=[o])
                    store(e[f"{dn}{d}"], c, t0, Wb, o)

            gens = [dchain(0), dchain(1)]
            while gens:
                alive = []
                for g_ in gens:
                    try:
                        next(g_)
                        alive.append(g_)
                    except StopIteration:
                        pass
                gens = alive
            K.op("dve", lambda en: en.tensor_tensor(out=w["ks"][:, 0:Wb], in0=w["kd0"][:, 0:Wb], in1=w["kd1"][:, 0:Wb],
                                                     op=ALU.add), reads=[w["kd0"], w["kd1"]], writes=[w["ks"]])
            K.op("dve", lambda en: en.scalar_tensor_tensor(out=w["tmp"][:, 0:Wb], in0=w["ks"][:, 0:Wb],
                                                           scalar=COL[:, 6, c:c + 1], in1=w["Rm"][:, 0:Wb],
                                                           op0=ALU.mult, op1=ALU.mult),
                 reads=[w["ks"], COL, w["Rm"]], writes=[w["tmp"]])
            ps = K.ps()
            K.op("pe", lambda en: en.matmul(ps[:, 0:Wb], lhsT=bones[:], rhs=w["tmp"][:, 0:Wb], start=True, stop=True),
                 reads=[bones, w["tmp"]], writes=[ps])
            o = otile()
            K.op("dve", lambda en: en.tensor_tensor(out=o[:, 0:Wb], in0=ps[:, 0:Wb], in1=w["Vm"][:, 0:Wb], op=ALU.mult),
                 reads=[ps, w["Vm"]], writes=[o])
            store(e["BON"], c, t0, Wb, o)
            u = PTt[3]
            Wt = Wb + 128
            K.dma("sp", w["IC"][:, 0:Wb], self.inp[icn][c:c + 1, t0 - s0:t0 - s0 + Wb].partition_broadcast(128),
                  writes=[w["IC"]])
            s_a, s_b = sw
            K.op("dve", lambda en: en.tensor_tensor(out=s_a[:, 1:Wt], in0=u[:, 0:Wt - 1], in1=u[:, 1:Wt], op=ALU.add),
                 reads=[u], writes=[s_a])
            cur_s, oth = s_a, s_b
            lo_, hi_ = 1, Wt
            for hs in (1, 2, 4):
                if POOL_WINS[c] < 4 * hs:
                    break
                nlo, nhi = lo_ + hs, hi_ - hs
                K.op("dve", lambda en, cur_s=cur_s, oth=oth, nlo=nlo, nhi=nhi, hs=hs: en.tensor_tensor(
                    out=oth[:, nlo:nhi], in0=cur_s[:, nlo - hs:nhi - hs], in1=cur_s[:, nlo + hs:nhi + hs], op=ALU.add),
                    reads=[cur_s], writes=[oth])
                cur_s, oth = oth, cur_s
                lo_, hi_ = nlo, nhi
            K.op("dve", lambda en: en.tensor_tensor(out=w["tmp"][:, 0:Wb], in0=cur_s[:, 64:64 + Wb], in1=w["IC"][:, 0:Wb],
                                                    op=ALU.mult), reads=[cur_s, w["IC"]], writes=[w["tmp"]])
            K.op("dve", lambda en: en.tensor_tensor(out=dfb[:, 0:Wb], in0=w["tmp"][:, 0:Wb], in1=u[:, 64:64 + Wb],
                                                     op=ALU.subtract), reads=[w["tmp"], u], writes=[dfb])
            ps = K.ps()
            K.op("pe", lambda en: en.matmul(ps[:, 0:Wb], lhsT=pw[:, c, :], rhs=dfb[:, 0:Wb], start=True, stop=True),
                 reads=[pw, dfb], writes=[ps])
            o = otile()
            K.op("dve", lambda en: en.tensor_scalar(out=o[:, 0:Wb], in0=ps[:, 0:Wb], scalar1=COL[:, 7, c:c + 1],
                                                    scalar2=None, op0=ALU.mult), reads=[ps, COL], writes=[o])
            store(e["YB"], c, t0, Wb, o)
    K.pop_scope()


Prog.even_e2 = _even_e2
def _even_e3(self, i, SDT=None, nsteps=66):
    SDT = SDT or self.SCAN_DT
    K = self.K
    e = self.ed
    K.push_scope()
    TEN = ("At", "Bt", "Kt", "Rt", "Bh", "Kh", "VV")

    def ring(nm, shape, n, dt=F32):
        bufs = [K.sb(shape, dt, f"e3{nm}{q}") for q in range(n)]
        cnt = [0]

        def nxt():
            cnt[0] += 1
            return bufs[cnt[0] % n]
        return nxt

    def mk_mask(nm, cmp_op, sgn=1):
        mbuf = K.sb([128, 128], F32, "e3m" + nm)
        K.op("pool", lambda en: en.memset(mbuf[:], 1.0), writes=[mbuf])
        K.op("pool", lambda en: en.affine_select(out=mbuf[:], in_=mbuf[:], pattern=[[sgn, 128]], compare_op=cmp_op,
                                                 fill=0.0, base=0, channel_multiplier=-sgn), reads=[mbuf], writes=[mbuf])
        K.op("pool", lambda en: en.memset(mbuf[0:64, 64:128], 0.0), reads=[mbuf], writes=[mbuf])
        K.op("pool", lambda en: en.memset(mbuf[64:128, 0:64], 0.0), reads=[mbuf], writes=[mbuf])
        return mbuf
    UPs = mk_mask("ups", ALU.is_gt)
    UPi = mk_mask("upi", ALU.is_ge)
    LOs = mk_mask("los", ALU.is_gt, -1)
    LOi = mk_mask("loi", ALU.is_ge, -1)
    BDM = mk_mask("bdm", ALU.is_ge)
    K.op("pool", lambda en: en.memset(BDM[0:64, 0:64], 1.0), reads=[BDM], writes=[BDM])
    K.op("pool", lambda en: en.memset(BDM[64:128, 64:128], 1.0), reads=[BDM], writes=[BDM])
    M4 = []
    MI = []
    for d in range(2):
        strictT, strict, inclT = (UPs, LOs, UPi) if d == 0 else (LOs, UPs, LOi)
        m4 = K.sb([128, 512], F32, f"e3m4{d}")
        for q, src in enumerate((strictT, strict, strictT, inclT)):
            K.op("dve", lambda en, q=q, src=src: en.tensor_copy(out=m4[:, q * 128:(q + 1) * 128], in_=src[:]),
                 reads=[src], writes=[m4])
        M4.append(m4)
        MI.append(inclT)
    identS = self.ident
    import os
    if os.environ.get("E3PAD"):
        _pad = K.sb([128, 128], F32, "e3pad")
    S = [[K.sb([128, 128], F32, f"e3S{d}{c}") for c in range(4)] for d in range(2)]
    for d in range(2):
        for c in range(4):
            K.op("pool", lambda en, d=d, c=c: en.memset(S[d][c][:], 0.0), writes=[S[d][c]])
    Ssd = S
    if SDT != F32:
        Ssd = [[K.sb([128, 128], SDT, f"e3Sb{d}{c}") for c in range(4)] for d in range(2)]
        for d in range(2):
            for c in range(4):
                K.op("pool", lambda en, d=d, c=c: en.memset(Ssd[d][c][:], 0.0), writes=[Ssd[d][c]])
    LD = [[{nm: K.sb([128, 4, 64], F32, f"e3ld{d}{b}{nm}") for nm in TEN} for b in range(2)] for d in range(2)]
    GLall = [K.sb([128, 4, NT // 64], F32, f"e3glall{d}") for d in range(2)]
    for d in range(2):
        K.dma("sp", GLall[d][:], e[f"GL{d}"][:, :].rearrange("(c p) t -> p c t", p=128), reads=[e[f"GL{d}"]],
              writes=[GLall[d]])
    bd_bufs = [K.sb([128, 7, 128], SDT, f"e3bdz{q}") for q in range(9)]
    for bq in bd_bufs:
        K.op("pool", lambda en, bq=bq: en.memset(bq[:], 0.0), writes=[bq])
    bd_cnt = [0]

    def r_bd():
        bd_cnt[0] += 1
        return bd_bufs[bd_cnt[0] % 9]
    r_tp = ring("tp", [128, 384], 9, SDT)
    r_m = ring("m", [128, 640], 9, SDT)
    r_q = ring("q", [128, 256], 16, SDT)
    r_w = ring("w", [128, 128], 16, SDT)
    r_x = ring("x", [128, 128], 9, SDT)
    r_u = ring("u", [128, 128], 9, SDT)
    r_y = ring("y", [128, 4, 64], 4, F32)
    rr = [0]
    NCH = 2 * nsteps

    def ve3():
        rr[0] += 1
        return "dve"

    def tok_base(d, n):
        if d == 0:
            return T + 64 * n if n < 4 else 64 * (n - 4)
        return T + 64 * (3 - n) if n < 4 else 64 * (127 - (n - 4))

    def load(d, n):
        b = n % 2
        tb = tok_base(d, n)
        for nm in TEN:
            src = e[nm if nm == "VV" else f"{nm}{d}"]
            K.dma("sp", LD[d][b][nm][:], src[:, tb:tb + 64].rearrange("(c p) t -> p c t", p=128), reads=[src],
                  writes=[LD[d][b][nm]])

    def mm(ps_ap, ps, lhsT, lb, rhs, rb, start=True, stop=True):
        K.op("pe", lambda en: en.matmul(ps_ap, lhsT=lhsT, rhs=rhs, start=start, stop=stop), reads=[lb, rb], writes=[ps])

    def unit(d, n, c, ysb):
        b = n % 2
        ld = LD[d][b]
        bd = r_bd()
        for ti, nm in enumerate(TEN):
            src = ld[nm][:, c, :]
            if ti < 4:
                K.op("dve", lambda en, ti=ti, src=src: en.tensor_tensor(
                    out=bd[:, ti, :].rearrange("p (a b) -> p a b", a=2), in0=src.unsqueeze(1).to_broadcast([128, 2, 64]),
                    in1=BDM[:].rearrange("p (a b) -> p a b", a=2), op=ALU.mult), reads=[ld[nm], BDM], writes=[bd])
            else:
                for h2 in range(2):
                    K.op("act", lambda en, ti=ti, h2=h2: en.copy(
                        out=bd[h2 * 64:(h2 + 1) * 64, ti, h2 * 64:(h2 + 1) * 64], in_=ld[nm][h2 * 64:(h2 + 1) * 64, c, :]),
                        reads=[ld[nm]], writes=[bd])
        yield
        A_, B_, K_, R_ = (bd[:, t_, :] for t_ in range(4))
        pst = K.ps()
        idm = identS if SDT == F32 else self.identb
        for t_ in range(3):
            mm(pst[:, t_ * 128:(t_ + 1) * 128], pst, bd[:, 4 + t_, :], bd, idm[:], idm)
        tp = r_tp()
        K.op("act", lambda en: en.copy(out=tp[:], in_=pst[:, 0:384]), reads=[pst], writes=[tp])
        BhT, KhT, VT = (tp[:, t_ * 128:(t_ + 1) * 128] for t_ in range(3))
        p1 = K.ps()
        mm(p1[:, 0:128], p1, B_, bd, A_, bd)
        mm(p1[:, 128:256], p1, A_, bd, B_, bd)
        mm(p1[:, 256:384], p1, K_, bd, A_, bd)
        mm(p1[:, 384:512], p1, B_, bd, R_, bd)
        p2 = K.ps()
        mm(p2[:, 0:128], p2, K_, bd, R_, bd)
        mt = r_m()
        K.op("dve", lambda en: en.tensor_tensor(out=mt[:, 0:512], in0=p1[:, :], in1=M4[d][:], op=ALU.mult),
             reads=[p1, M4[d]], writes=[mt])
        K.op("dve", lambda en: en.tensor_tensor(out=mt[:, 512:640], in0=p2[:, 0:128], in1=MI[d][:], op=ALU.mult),
             reads=[p2, MI[d]], writes=[mt])
        Q, QT, MakT, MrbT, MrkT = (mt[:, t_ * 128:(t_ + 1) * 128] for t_ in range(5))
        W = r_w()
        K.op("dve", lambda en: en.tensor_tensor(out=W[:], in0=Q, in1=identS[:], op=ALU.add),
             reads=[mt, identS], writes=[W])
        yield
        qb = mt
        for lvl in range(1, 6):
            pq = K.ps()
            mm(pq[:, 128:256], pq, Q, qb, QT, qb)
            if lvl < 5:
                mm(pq[:, 0:128], pq, QT, qb, Q, qb)
            nq = r_q()
            if lvl < 5:
                K.op("act", lambda en: en.copy(out=nq[:], in_=pq[:, 0:256]), reads=[pq], writes=[nq])
            else:
                K.op("act", lambda en: en.copy(out=nq[:, 128:256], in_=pq[:, 128:256]), reads=[pq], writes=[nq])
            Q, QT, qb = nq[:, 0:128], nq[:, 128:256], nq
            yield
            pw_ = K.ps()
            mm(pw_[:, 0:128], pw_, QT, qb, W[:], W)
            W2_ = r_w()
            K.op("dve", lambda en: en.tensor_tensor(out=W2_[:], in0=pw_[:, 0:128], in1=W[:], op=ALU.add),
                 reads=[pw_, W], writes=[W2_])
            W = W2_
            yield
        Sb = S[d][c]
        Sm = Ssd[d][c]
        px = K.ps()
        mm(px[:, 0:128], px, A_, bd, Sm[:], Sm, True, False)
        mm(px[:, 0:128], px, MakT, mt, VT, tp, False, True)
        Xs = r_x()
        K.op("act", lambda en: en.copy(out=Xs[:], in_=px[:, 0:128]), reads=[px], writes=[Xs])
        yield
        pu = K.ps()
        mm(pu[:, 0:128], pu, W[:], W, Xs[:], Xs)
        Us = r_u()
        K.op("dve", lambda en: en.tensor_copy(out=Us[:], in_=pu[:, 0:128]), reads=[pu], writes=[Us])
        if self.dbg.get("e3dbg") is not None and d == 0 and c == 0 and n in (0, 1):
            dd = self.dbg["e3dbg"]
            K.dma("sp", dd[n, 0, :, 0:128], Xs[:], reads=[Xs])
            K.dma("sp", dd[n, 1, :, 0:128], Us[:], reads=[Us])
            K.dma("sp", dd[n, 2, :, 0:128], W[:], reads=[W])
            K.dma("sp", dd[n, 3, :, 0:640], mt[:], reads=[mt])
            K.dma("sp", dd[n, 4, :, 0:128], Sb[:], reads=[Sb])
            K.dma("sp", dd[n, 5, :, 0:384], tp[:], reads=[tp])
            for t_ in range(4):
                K.dma("sp", dd[n, 6, :, t_ * 128:(t_ + 1) * 128], bd[:, t_, :], reads=[bd])
        yield
        py = K.ps()
        mm(py[:, 0:128], py, R_, bd, Sm[:], Sm, True, False)
        mm(py[:, 0:128], py, MrbT, mt, Us[:], Us, False, False)
        mm(py[:, 0:128], py, MrkT, mt, VT, tp, False, True)
        pss = K.ps()
        mm(pss[:, 0:128], pss, BhT, tp, Us[:], Us, True, False)
        mm(pss[:, 0:128], pss, KhT, tp, VT, tp, False, True)
        K.op("act", lambda en: en.copy(out=ysb[0:64, c, :], in_=py[0:64, 0:64]), reads=[py], writes=[ysb])
        K.op("act", lambda en: en.copy(out=ysb[64:128, c, :], in_=py[64:128, 64:128]), reads=[py], writes=[ysb])
        gcol = tok_base(d, n) // 64
        K.op("dve", lambda en: en.scalar_tensor_tensor(out=Sb[:], in0=Sb[:], scalar=GLall[d][:, c, gcol:gcol + 1],
                                                       in1=pss[:, 0:128], op0=ALU.mult, op1=ALU.add),
             reads=[Sb, GLall[d], pss], writes=[Sb])
        if SDT != F32:
            K.op("dve", lambda en: en.tensor_copy(out=Sm[:], in_=Sb[:]), reads=[Sb], writes=[Sm])

    for d in range(2):
        load(d, 0)
    for n in range(NCH):
        for d in range(2):
            if n + 1 < NCH:
                load(d, n + 1)
        ysbs = [r_y(), r_y()]
        gens = [unit(d, n, c, ysbs[d]) for c in range(4) for d in range(2)]
        while gens:
            alive = []
            for g in gens:
                try:
                    next(g)
                    alive.append(g)
                except StopIteration:
                    pass
            gens = alive
        for d in range(2):
            tb = tok_base(d, n)
            dst = e[f"YD{d}"][tb:tb + 64, :].rearrange("t (c h v) -> h t c v", h=2, v=64)
            for h2 in range(2):
                K.dma("sp", dst[h2], ysbs[d][h2 * 64:(h2 + 1) * 64, :, :], reads=[ysbs[d]], writes=[e[f"YD{d}"]])
    if self.dbg.get("S") is not None:
        for d in range(2):
            for c in range(4):
                K.dma("sp", self.dbg["S"][d, c], S[d][c][:], reads=[S[d][c]])
    K.pop_scope()


Prog.even_e3 = _even_e3
def _even_e4(self, i, ntile):
    K = self.K
    j = i // 2
    e = self.ed
    K.push_scope()
    wout = K.sb([128, 8, D], BF16, "e4wout")
    stg = [K.sb([128, D], F32, f"e4stg{q}") for q in range(3)]
    for kt in range(8):
        sg = stg[kt % 3]
        K.dma("sp", sg[:], self.inp["w_out"][j, kt * 128:(kt + 1) * 128, :], writes=[sg])
        K.op("dve", lambda en, sg=sg, kt=kt: en.tensor_copy(out=wout[:, kt, :], in_=sg[:]), reads=[sg], writes=[wout])
    GN = K.sb([128, 2, 4], F32, "e4gn")
    K.dma("sp", GN[:, 0, :], self.inp["gn_w"][j].rearrange("(c k) -> k c", k=128), writes=[GN], allow_slow_non_contiguous=True)
    K.dma("sp", GN[:, 1, :], self.inp["gn_b"][j].rearrange("(c k) -> k c", k=128), writes=[GN], allow_slow_non_contiguous=True)
    y0s = [K.sb([128, 512], F32, f"e4y0{q}") for q in range(3)]
    y1s = [K.sb([128, 512], F32, f"e4y1{q}") for q in range(3)]
    sqs = [K.sb([128, 512], F32, f"e4sq{q}") for q in range(3)]
    sts = [K.sb([128, 4, 8], F32, f"e4st{q}") for q in range(3)]
    bons = [K.sb([128, 4, 128], F32, f"e4bon{q}") for q in range(3)]
    ggs = [K.sb([128, 4, 128], F32, f"e4gg{q}") for q in range(3)]
    ybs = [K.sb([128, 4, 128], F32, f"e4yb{q}") for q in range(3)]
    yTs = [K.sb([128, 4, 128], F32, f"e4yT{q}") for q in range(3)]
    cats = [K.sb([128, 8, 128], BF16, f"e4cat{q}") for q in range(3)]
    xts = stg
    ots = [K.sb([128, D], F32, f"e4o{q}") for q in range(3)]
    def _body(tt):
        y0, y1, sq, st, bon, gg, yb, yT, cat, xt, ot = (l[tt % 3] for l in (y0s, y1s, sqs, sts, bons, ggs, ybs, yTs, cats,
                                                                            xts, ots))
        M = self.Ml if tt < 64 else self.Mc
        tk = slice(tt * 128, (tt + 1) * 128)
        K.dma("sp", y0[:], e["YD0"][tk, :], reads=[e["YD0"]], writes=[y0])
        K.dma("sp", y1[:], e["YD1"][tk, :], reads=[e["YD1"]], writes=[y1])
        for buf, nm in ((bon, "BON"), (gg, "GG"), (yb, "YB")):
            K.dma("sp", buf[:], e[nm][:, tk].rearrange("(c p) t -> p c t", p=128), reads=[e[nm]], writes=[buf])
        K.dma("sp", xt[:], self.lat[tk, :], reads=[self.lat.p(tt)], writes=[xt])
        K.op("dve", lambda en: en.tensor_tensor(out=y0[:], in0=y0[:], in1=y1[:], op=ALU.add), reads=[y0, y1], writes=[y0])
        yv = y0[:].rearrange("p (h v) -> p h v", v=64)
        sv = sq[:].rearrange("p (h v) -> p h v", v=64)
        K.op("dve", lambda en: en.reduce_sum(out=st[:, 0, :], in_=yv, axis=AX.X), reads=[y0], writes=[st])
        K.op("dve", lambda en: en.tensor_scalar(out=st[:, 1, :], in0=st[:, 0, :], scalar1=1.0 / 64, scalar2=None,
                                                op0=ALU.mult), reads=[st], writes=[st])
        K.op("dve", lambda en: en.tensor_tensor(out=yv, in0=yv, in1=st[:, 1, :].unsqueeze(2).to_broadcast([128, 8, 64]),
                                                op=ALU.subtract), reads=[y0, st], writes=[y0])
        K.op("dve", lambda en: en.tensor_tensor(out=sq[:], in0=y0[:], in1=y0[:], op=ALU.mult), reads=[y0], writes=[sq])
        K.op("dve", lambda en: en.reduce_sum(out=st[:, 2, :], in_=sv, axis=AX.X), reads=[sq], writes=[st])
        K.op("dve", lambda en: en.tensor_scalar(out=st[:, 2, :], in0=st[:, 2, :], scalar1=1.0 / 64, scalar2=64e-5,
                                                op0=ALU.mult, op1=ALU.add), reads=[st], writes=[st])
        K.op("act", lambda en: en.sqrt(out=st[:, 3, :], in_=st[:, 2, :]), reads=[st], writes=[st])
        K.op("dve", lambda en: en.reciprocal(out=st[:, 3, :], in_=st[:, 3, :]), reads=[st], writes=[st])
        K.op("dve", lambda en: en.tensor_tensor(out=yv, in0=yv, in1=st[:, 3, :].unsqueeze(2).to_broadcast([128, 8, 64]),
                                                op=ALU.mult), reads=[y0, st], writes=[y0])
        ps = K.ps()
        for c in range(4):
            self.tr(None, ps[:, c * 128:(c + 1) * 128], y0[:, c * 128:(c + 1) * 128], ps, y0)
        for c in range(4):
            K.op("dve", lambda en, c=c: en.tensor_scalar(out=yT[:, c, :], in0=ps[:, c * 128:(c + 1) * 128],
                                                         scalar1=GN[:, 0, c:c + 1], scalar2=GN[:, 1, c:c + 1],
                                                         op0=ALU.mult, op1=ALU.add), reads=[ps, GN], writes=[yT])
        K.op("dve", lambda en: en.tensor_tensor(out=yT[:], in0=yT[:], in1=bon[:], op=ALU.add), reads=[yT, bon],
             writes=[yT])
        K.op("dve", lambda en: en.tensor_tensor(out=cat[:, 0:4, :], in0=yT[:], in1=gg[:], op=ALU.mult), reads=[yT, gg],
             writes=[cat])
        K.op("act", lambda en: en.copy(out=cat[:, 4:8, :], in_=yb[:]), reads=[yb], writes=[cat])
        for hh in range(2):
            po = K.ps()
            for kt in range(8):
                K.op("pe", lambda en, kt=kt: en.matmul(po[:, :], lhsT=cat[:, kt, :], rhs=wout[:, kt, hh * 512:(hh + 1) * 512],
                                                       start=(kt == 0), stop=(kt == 7)), reads=[cat, wout], writes=[po])
            K.op("dve", lambda en: en.tensor_tensor(out=ot[:, hh * 512:(hh + 1) * 512], in0=po[:, :],
                                                    in1=M[:, 2, hh * 512:(hh + 1) * 512], op=ALU.mult),
                 reads=[po, M], writes=[ot])
        K.op("dve", lambda en: en.tensor_tensor(out=ot[:], in0=ot[:], in1=xt[:], op=ALU.add), reads=[ot, xt], writes=[ot])
        K.dma("sp", self.lat[tk, :], ot[:], reads=[ot], writes=[self.lat.p(tt)])
    K.interleave(_body, range(ntile), 3)
    K.pop_scope()


def _even_mixer(self, i):
    self.even_e1(i, ntile=66)
    self.even_e2(i)
    self.even_e3(i)
    self.even_e4(i, ntile=66 if i < 2 else 64)


Prog.even_e4 = _even_e4
Prog.even_mixer = _even_mixer
Prog.even_declare = _even_declare
Prog.even_e1 = _even_e1


def build_program():
    P = Prog()
    P.fourier_declare()
    P.even_declare()
    P.init_lat()
    P.prep_s()
    for i in range(DEPTH):
        P.modvec(i, need_ctx=(i <= 2))
        if i % 2 == 0:
            P.even_mixer(i)
        else:
            P.fourier(i, with_ctx=(i < 2))
        P.moe_setup()
        P.moe(i, with_ctx=(i < 2))
    P.final_norm()
    P.K.finish()
    return P


def kernel(**inputs):
    x = np.asarray(inputs["x"], dtype=np.float32)
    B = x.shape[0]
    P = build_program()
    consts = fourier_consts()
    pconsts = pool_consts()
    in_maps = []
    for b in range(B):
        d = {"x": np.ascontiguousarray(x[b]),
             "c": np.ascontiguousarray(np.asarray(inputs["c"], dtype=np.float32)[b:b + 1]),
             "ctx": np.ascontiguousarray(np.asarray(inputs["ctx"], dtype=np.float32)[b]),
             "c_ctx": np.ascontiguousarray(np.asarray(inputs["c_ctx"], dtype=np.float32)[None, :])}
        for k in WEIGHT_SPECS:
            d[k] = np.ascontiguousarray(np.asarray(inputs[k], dtype=np.float32))
        d.update(consts)
        d.update(pconsts)
        in_maps.append(d)
    res = run_bass_kernel_spmd(P.K.nc, in_maps, core_ids=list(range(B)))
    return np.stack([np.asarray(r["out"]) for r in res.results], axis=0).astype(np.float32)
```
